# Optimizing a Trainium2 kernel written in Bass

```python
import math
import jax, jax.numpy as jnp
from jax import lax
import numpy as np

D_MODEL = 2048
BATCH = 2
SEQ = 4096
DEPTH = 1

HEAD_DIM = 128
N_HEADS_A = 8
WIDTH_A = N_HEADS_A * HEAD_DIM
DILATED_PATTERNS = ((128, 1), (512, 4), (2048, 16))
N_HEADS_B = 4
DIFF_HEAD_DIM = 128
WIDTH_B = N_HEADS_B * 2 * DIFF_HEAD_DIM
IN_SPLITS = (WIDTH_A, WIDTH_A, WIDTH_A, WIDTH_B, WIDTH_B, WIDTH_B, D_MODEL, D_MODEL)
IN_WIDTH = sum(IN_SPLITS)
N_EXPERTS = 32
TOP_K = 4
D_FF = D_MODEL
SWIGLU_LIMIT = 7.0
SWIGLU_ALPHA = 1.702
MOE_BLOCK = 128
Q_BLOCK = 128
N_MOD = 6
EPS = 1e-5
NEG = -1e30

kernel_name = "hybrid_dilated_diffattn_moe_encoder"


def _alibi_slopes(n):
    return np.array([2.0 ** (-8.0 * (i + 1) / n) for i in range(n)], dtype=np.float32)


def rmsnorm(x, g):
    xf = x.astype(jnp.float32)
    y = xf * lax.rsqrt(jnp.mean(xf * xf, axis=-1, keepdims=True) + EPS)
    return (y * g.astype(jnp.float32)).astype(x.dtype)


def _dilated_pattern(q, k, v, window, dilation, slopes):
    B, S, H, Dh = q.shape
    d = dilation
    R = window // (2 * d)
    L = S // d
    nb = -(-L // R)
    Lp = nb * R

    def to_sub(t):
        t = t.reshape(B, L, d, H, Dh).transpose(0, 2, 1, 3, 4)
        t = jnp.pad(t, ((0, 0), (0, 0), (0, Lp - L), (0, 0), (0, 0)))
        return t.reshape(B, d, nb, R, H, Dh)

    def band(t):
        tp = jnp.pad(t, ((0, 0), (0, 0), (1, 1), (0, 0), (0, 0), (0, 0)))
        return jnp.concatenate([tp[:, :, :-2], tp[:, :, 1:-1], tp[:, :, 2:]], axis=3)

    qs = to_sub(q)
    kw = band(to_sub(k))
    vw = band(to_sub(v))
    qi = np.arange(nb)[:, None, None] * R + np.arange(R)[None, :, None]
    kj = (np.arange(nb)[:, None, None] - 1) * R + np.arange(3 * R)[None, None, :]
    rel = kj - qi
    valid = (np.abs(rel) <= R) & (kj >= 0) & (kj < L)
    dist = (np.abs(rel) * d).astype(np.float32)
    bias = -slopes[None, :, None, None] * dist[:, None]
    s = jnp.einsum('brnqhe,brnkhe->brnhqk', qs, kw).astype(jnp.float32) * (Dh ** -0.5) + bias
    s = jnp.where(valid[:, None], s, NEG)
    lse = jax.nn.logsumexp(s, axis=-1)
    p = jnp.exp(s - lse[..., None])
    o = jnp.einsum('brnhqk,brnkhe->brnqhe', p.astype(v.dtype), vw)
    o = o.reshape(B, d, Lp, H, Dh)[:, :, :L].transpose(0, 2, 1, 3, 4).reshape(B, S, H, Dh)
    lse = lse.transpose(0, 1, 2, 4, 3).reshape(B, d, Lp, H)[:, :, :L]
    lse = lse.transpose(0, 2, 1, 3).reshape(B, S, H)
    return o, lse


def dilated_attention(q, k, v, slopes):
    outs, lses = [], []
    for window, dilation in DILATED_PATTERNS:
        o, l = _dilated_pattern(q, k, v, window, dilation, slopes)
        outs.append(o)
        lses.append(l)
    o = jnp.stack(outs, axis=0)
    w = jax.nn.softmax(jnp.stack(lses, axis=0), axis=0)
    return jnp.einsum('pbsh,pbshe->bshe', w.astype(o.dtype), o)


def diff_attention(q, k, v, lam_q1, lam_k1, lam_q2, lam_k2, subln_g, slopes, lambda_init):
    B, S, H, _, Dh = q.shape
    f32 = jnp.float32
    lam = (jnp.exp(jnp.sum(lam_q1.astype(f32) * lam_k1.astype(f32)))
           - jnp.exp(jnp.sum(lam_q2.astype(f32) * lam_k2.astype(f32))) + lambda_init)
    nblk = S // Q_BLOCK
    kpos = jnp.arange(S)
    qb = q.reshape(B, nblk, Q_BLOCK, H, 2, Dh).transpose(1, 0, 2, 3, 4, 5)

    def block(args):
        i, qblk = args
        qpos = i * Q_BLOCK + jnp.arange(Q_BLOCK)
        dist = jnp.abs(qpos[:, None] - kpos[None, :]).astype(f32)
        bias = -slopes[:, None, None] * dist[None]
        s = jnp.einsum('bqhcd,bkhcd->bchqk', qblk, k).astype(f32) * (Dh ** -0.5) + bias[None, None]
        p = jax.nn.softmax(s, axis=-1)
        a = p[:, 0] - lam * p[:, 1]
        return jnp.einsum('bhqk,bkhe->bqhe', a.astype(v.dtype), v)

    o = lax.map(block, (jnp.arange(nblk), qb))
    o = o.transpose(1, 0, 2, 3, 4).reshape(B, S, H, 2 * Dh)
    return rmsnorm(o, subln_g) * (1.0 - lambda_init)


def moe(h, w_router, b_router, w_gate, b_gate, w_up, b_up, w_down, b_down):
    B, S, D = h.shape
    T = B * S
    TK = T * TOP_K
    xt = h.reshape(T, D)
    logits = (xt @ w_router + b_router).astype(jnp.float32)
    top_vals, top_idx = lax.top_k(logits, TOP_K)
    probs = jax.nn.softmax(top_vals, axis=-1)
    flat_e = top_idx.reshape(-1)
    flat_tok = jnp.repeat(jnp.arange(T, dtype=jnp.int32), TOP_K)
    order = jnp.argsort(flat_e)
    se = flat_e[order]
    stok = flat_tok[order]
    counts = jnp.bincount(flat_e, length=N_EXPERTS)
    starts = jnp.cumsum(counts) - counts
    padded = (counts + MOE_BLOCK - 1) // MOE_BLOCK * MOE_BLOCK
    pends = jnp.cumsum(padded)
    pstarts = pends - padded
    dest = pstarts[se] + (jnp.arange(TK) - starts[se])
    nblk = -(-TK // MOE_BLOCK) + N_EXPERTS
    buf_tok = jnp.full((nblk * MOE_BLOCK,), T, dtype=jnp.int32).at[dest].set(stok)
    block_expert = jnp.clip(jnp.searchsorted(pends, jnp.arange(nblk) * MOE_BLOCK, side='right'),
                            0, N_EXPERTS - 1)
    x_pad = jnp.concatenate([xt, jnp.zeros((1, D), xt.dtype)], axis=0)

    def block(args):
        e, toks = args
        xb = x_pad[toks]
        g = jnp.minimum(xb @ w_gate[e] + b_gate[e], SWIGLU_LIMIT)
        u = jnp.clip(xb @ w_up[e] + b_up[e], -SWIGLU_LIMIT, SWIGLU_LIMIT)
        y = (u + 1.0) * (g * jax.nn.sigmoid(SWIGLU_ALPHA * g))
        return y @ w_down[e] + b_down[e]

    ybuf = lax.map(block, (block_expert, buf_tok.reshape(nblk, MOE_BLOCK))).reshape(-1, D)
    y_sorted = ybuf[dest]
    y_slots = jnp.zeros_like(y_sorted).at[order].set(y_sorted).reshape(T, TOP_K, D)
    out = jnp.einsum('tk,tkd->td', probs.astype(y_slots.dtype), y_slots)
    return out.reshape(B, S, D).astype(h.dtype)


def setup_inputs(seed: int = 0) -> dict:
    key = jax.random.key(seed)
    ks = jax.random.split(key, 24)

    def nrm(k, shape, scale):
        return jax.random.normal(k, shape, jnp.float32) * scale

    L = DEPTH
    return {
        "x": nrm(ks[0], (BATCH, SEQ, D_MODEL), 1.0),
        "c": nrm(ks[1], (BATCH, D_MODEL), 1.0),
        "w_ada": nrm(ks[2], (L, D_MODEL, N_MOD * D_MODEL), 0.5 * D_MODEL ** -0.5),
        "b_ada": nrm(ks[3], (L, N_MOD * D_MODEL), 0.02),
        "norm1_g": 1.0 + nrm(ks[4], (L, D_MODEL), 0.02),
        "w_in": nrm(ks[5], (L, D_MODEL, IN_WIDTH), D_MODEL ** -0.5),
        "lam_q1": nrm(ks[6], (L, DIFF_HEAD_DIM), 0.1),
        "lam_k1": nrm(ks[7], (L, DIFF_HEAD_DIM), 0.1),
        "lam_q2": nrm(ks[8], (L, DIFF_HEAD_DIM), 0.1),
        "lam_k2": nrm(ks[9], (L, DIFF_HEAD_DIM), 0.1),
        "subln_g": 1.0 + nrm(ks[10], (L, 2 * DIFF_HEAD_DIM), 0.02),
        "w_out_a": nrm(ks[11], (L, WIDTH_A, D_MODEL), WIDTH_A ** -0.5),
        "w_out_b": nrm(ks[12], (L, WIDTH_B, D_MODEL), WIDTH_B ** -0.5),
        "w_o": nrm(ks[13], (L, D_MODEL, D_MODEL), D_MODEL ** -0.5),
        "norm2_g": 1.0 + nrm(ks[14], (L, D_MODEL), 0.02),
        "w_router": nrm(ks[15], (L, D_MODEL, N_EXPERTS), D_MODEL ** -0.5),
        "b_router": nrm(ks[16], (L, N_EXPERTS), 0.01),
        "w_gate": nrm(ks[17], (L, N_EXPERTS, D_MODEL, D_FF), D_MODEL ** -0.5),
        "b_gate": nrm(ks[18], (L, N_EXPERTS, D_FF), 0.01),
        "w_up": nrm(ks[19], (L, N_EXPERTS, D_MODEL, D_FF), D_MODEL ** -0.5),
        "b_up": nrm(ks[20], (L, N_EXPERTS, D_FF), 0.01),
        "w_down": nrm(ks[21], (L, N_EXPERTS, D_FF, D_MODEL), D_FF ** -0.5),
        "b_down": nrm(ks[22], (L, N_EXPERTS, D_MODEL), 0.01),
        "final_g": 1.0 + nrm(ks[23], (D_MODEL,), 0.02),
    }


def reference(x, c, w_ada, b_ada, norm1_g, w_in, lam_q1, lam_k1, lam_q2, lam_k2, subln_g,
              w_out_a, w_out_b, w_o, norm2_g, w_router, b_router, w_gate, b_gate, w_up, b_up,
              w_down, b_down, final_g):
    B, S, D = x.shape
    slopes = _alibi_slopes(N_HEADS_A + N_HEADS_B)
    slopes_a = jnp.asarray(slopes[:N_HEADS_A])
    slopes_b = jnp.asarray(slopes[N_HEADS_A:])
    split_points = np.cumsum(IN_SPLITS)[:-1].tolist()
    h = x
    for l in range(DEPTH):
        lambda_init = 0.8 - 0.6 * math.exp(-0.3 * l)
        mod = (jax.nn.silu(c) @ w_ada[l] + b_ada[l]).reshape(B, N_MOD, 1, D)
        sh1, sc1, g1, sh2, sc2, g2 = (mod[:, i] for i in range(N_MOD))

        u = rmsnorm(h, norm1_g[l]) * (1.0 + sc1) + sh1
        q_a, k_a, v_a, q_b, k_b, v_b, gate_a, gate_b = jnp.split(u @ w_in[l], split_points, axis=-1)
        y_a = dilated_attention(q_a.reshape(B, S, N_HEADS_A, HEAD_DIM),
                                k_a.reshape(B, S, N_HEADS_A, HEAD_DIM),
                                v_a.reshape(B, S, N_HEADS_A, HEAD_DIM), slopes_a)
        y_b = diff_attention(q_b.reshape(B, S, N_HEADS_B, 2, DIFF_HEAD_DIM),
                             k_b.reshape(B, S, N_HEADS_B, 2, DIFF_HEAD_DIM),
                             v_b.reshape(B, S, N_HEADS_B, 2 * DIFF_HEAD_DIM),
                             lam_q1[l], lam_k1[l], lam_q2[l], lam_k2[l], subln_g[l],
                             slopes_b, lambda_init)
        merged = (jax.nn.sigmoid(gate_a) * (y_a.reshape(B, S, WIDTH_A) @ w_out_a[l])
                  + jax.nn.sigmoid(gate_b) * (y_b.reshape(B, S, WIDTH_B) @ w_out_b[l]))
        h = h + g1 * (merged @ w_o[l])

        u2 = rmsnorm(h, norm2_g[l]) * (1.0 + sc2) + sh2
        h = h + g2 * moe(u2, w_router[l], b_router[l], w_gate[l], b_gate[l], w_up[l], b_up[l],
                         w_down[l], b_down[l])
    return rmsnorm(h, final_g)
```

```python
import contextlib
import math
import numpy as np
import concourse.bass as bass
import concourse.mybir as mybir
from concourse.bass_utils import run_bass_kernel_spmd

F32 = mybir.dt.float32
BF16 = mybir.dt.bfloat16
AF = mybir.ActivationFunctionType
ALU = mybir.AluOpType
AX = mybir.AxisListType

ENGS = ("pe", "act", "dve", "pool", "sp")

D = 2048
SEQ = 4096
OWN = 1024
KC = 16
N_EXP = 32
TOPK = 4
CAP = 512
EPS = 1e-5
SCALE = 128 ** -0.5
LAMBDA_INIT = 0.8 - 0.6 * math.exp(-0.3 * 0)
SLOPES = [2.0 ** (-8.0 * (i + 1) / 12) for i in range(12)]
SLOPES_A = SLOPES[:8]
SLOPES_B = SLOPES[8:]
BIG = 1.0e9
PATTERNS = (1, 4, 16)


class Op:
    __slots__ = ("eng", "fn", "deps", "dma", "idx", "count", "milestone", "slot", "slot_total", "name")

    def __init__(self, eng, fn, name=""):
        self.eng = eng
        self.fn = fn
        self.deps = []
        self.dma = False
        self.milestone = False
        self.count = None
        self.slot = None
        self.slot_total = None
        self.name = name


class Slot:
    def __init__(self, name):
        self.name = name
        self.total = 0
        self.sem = None


class Sched:
    def __init__(self, nc):
        self.nc = nc
        self.ops = []
        self.writers = {}
        self.readers = {}
        self.old_readers = {}
        self.slots = []
        self.nbar = 0

    def slot(self, name):
        s = Slot(name + str(len(self.slots)))
        self.slots.append(s)
        return s

    def _add(self, op, reads, writes):
        deps = {}

        def add_dep(o):
            if o is op:
                return
            if o.dma:
                if op.dma and op.slot is o.slot:
                    return
                deps[("slot", id(o.slot))] = ("slot", o.slot, o.slot.total)
            else:
                k = ("eng", o.eng)
                prev = deps.get(k)
                if prev is None or o.idx > prev[1].idx:
                    deps[k] = ("eng", o, None)

        for k in reads:
            for o in self.writers.get(k, {}).values():
                add_dep(o)
        for k in writes:
            for o in self.writers.get(k, {}).values():
                if (not o.dma) and (not op.dma) and o.eng == op.eng:
                    continue
                add_dep(o)
            for o in list(self.readers.get(k, {}).values()) + list(self.old_readers.get(k, {}).values()):
                if (not o.dma) and (not op.dma) and o.eng == op.eng:
                    continue
                add_dep(o)
        op.deps = list(deps.values())
        for d in op.deps:
            if d[0] == "eng":
                d[1].milestone = True
        op.idx = len(self.ops)
        self.ops.append(op)
        wkey = ("slot", id(op.slot)) if op.dma else op.eng
        for k in writes:
            self.writers[k] = {wkey: op}
            if self.readers.get(k):
                self.old_readers[k] = self.readers[k]
            self.readers[k] = {}
        for k in reads:
            self.readers.setdefault(k, {})[wkey] = op
        return op

    def op(self, eng, fn, reads=(), writes=(), name=""):
        o = Op(eng, fn, name)
        return self._add(o, list(reads), list(writes))

    def dma(self, eng, slot, out, in_, reads=(), writes=(), name="", **kw):
        def fn(e, out=out, in_=in_, kw=kw):
            return e.dma_start(out=out, in_=in_, **kw)
        o = Op(eng, fn, name)
        o.dma = True
        o.slot = slot
        o.slot_total = slot.total + 16
        r = self._add(o, list(reads), list(writes))
        slot.total += 16
        return r

    def barrier(self):
        n = self.nbar
        self.nbar += 1
        sc = self.bar_scratch
        comp = ("pe", "act", "dve", "pool")
        for e in comp:
            if e == "pe":
                self.op("pe", lambda en: en.matmul(self.bar_ps[0:1, 0:1], sc["b"][0:1, 0:1], sc["b"][0:1, 0:1],
                                                    start=True, stop=True),
                        writes=[("bar", n, e), ("bank", 7)])
            elif e == "act":
                self.op("act", lambda en: en.activation(out=sc["act"][0:1, 0:1], in_=sc["one"][0:1, 0:1], func=AF.Copy),
                        writes=[("bar", n, e)])
            elif e == "dve":
                self.op("dve", lambda en: en.tensor_copy(out=sc["dve"][0:1, 0:1], in_=sc["one"][0:1, 0:1]),
                        writes=[("bar", n, e)])
            else:
                self.op("pool", lambda en: en.tensor_copy(out=sc["pool"][0:1, 0:1], in_=sc["one"][0:1, 0:1]),
                        writes=[("bar", n, e)])
        rk = [("bar", n, e) for e in comp]
        for e in ENGS:
            o = Op(e, None, "barwait")
            self._add(o, rk, [])
            for s in self.slots:
                if s.total > 0:
                    o.deps.append(("slot", s, s.total))
        self.writers = {}
        self.readers = {}
        self.old_readers = {}

    def emit(self, final_waits=()):
        nc = self.nc
        by_eng = {e: [] for e in ENGS}
        for o in self.ops:
            by_eng[o.eng].append(o)
        for e in ENGS:
            c = 0
            for o in by_eng[e]:
                if o.milestone and not o.dma:
                    c += 1
                    o.count = c
        with contextlib.ExitStack() as st:
            esem = {e: st.enter_context(nc.semaphore("s_" + e)) for e in ENGS}
            for s in self.slots:
                s.sem = st.enter_context(nc.semaphore("d_" + s.name))
            block = st.enter_context(nc.Block())

            def run(e, eng):
                waited = {}
                for o in by_eng[e]:
                    for d in o.deps:
                        if d[0] == "slot":
                            sem, val, key = d[1].sem, d[2], ("slot", id(d[1]))
                        else:
                            dop = d[1]
                            if dop.eng == e and e == "pe":
                                continue
                            sem, val, key = esem[dop.eng], dop.count, ("eng", dop.eng)
                        if waited.get(key, 0) >= val:
                            continue
                        waited[key] = val
                        eng.wait_ge(sem, val)
                    if o.fn is None:
                        continue
                    ins = o.fn(eng)
                    if o.dma:
                        ins.then_inc(o.slot.sem, 16)
                    elif o.milestone:
                        ins.then_inc(esem[e], 1)
                if e == "sp":
                    for s in final_waits:
                        eng.wait_ge(s.sem, s.total)

            @block.tensor
            def _(eng):
                run("pe", eng)

            @block.scalar
            def _(eng):
                run("act", eng)

            @block.vector
            def _(eng):
                run("dve", eng)

            @block.gpsimd
            def _(eng):
                run("pool", eng)

            @block.sync
            def _(eng):
                run("sp", eng)


class Ctx:
    pass


def a_tiles():
    out = []
    for d in PATTERNS:
        nt = {1: 9, 4: 3, 16: 2}[d]
        for c in range(d):
            for m in range(nt):
                out.append((d, c, m))
    return out


A_TILES = a_tiles()
A_TILE_IDX = {t: i for i, t in enumerate(A_TILES)}


def build_program(debug=(), stages="ABCDE"):
    nc = bass.Bass("TRN2", target_bir_lowering=False)
    cx = Ctx()
    cx.stages = stages
    cx.nc = nc
    cx.debug = set(debug)
    S = Sched(nc)
    cx.S = S

    def din(name, shape, dt=F32):
        return nc.dram_tensor(name, list(shape), dt, kind="ExternalInput").ap()

    def dscr(name, shape, dt=BF16):
        return nc.dram_tensor(name, list(shape), dt, kind="Internal").ap()

    def dout(name, shape, dt=F32):
        return nc.dram_tensor(name, list(shape), dt, kind="ExternalOutput").ap()

    class Lazy(dict):
        def __init__(self):
            super().__init__()
            self.specs = {}

        def __setitem__(self, k, v):
            self.specs[k] = v

        def __missing__(self, k):
            ap = self.specs[k]()
            dict.__setitem__(self, k, ap)
            return ap
    _din = din
    din = lambda name, shape, dt=F32: (lambda: _din(name, shape, dt))
    I = Lazy()
    I["xr"] = din("xr", [SEQ, D])
    I["cT"] = din("cT", [128, KC])
    I["w_ada"] = din("w_ada", [D, 6 * D])
    I["b_ada"] = din("b_ada", [1, 6 * D])
    I["n1"] = din("n1", [1, D])
    I["n2"] = din("n2", [1, D])
    I["nf"] = din("nf", [1, D])
    I["w_in"] = din("w_in", [D, 10240])
    I["lamT"] = din("lamT", [128, 4])
    I["sublnT"] = din("sublnT", [128, 2])
    I["w_out_a"] = din("w_out_a", [1024, D])
    I["w_out_b"] = din("w_out_b", [1024, D])
    I["w_o"] = din("w_o", [D, D])
    I["w_router"] = din("w_router", [D, N_EXP])
    I["b_routerB"] = din("b_routerB", [128, N_EXP])
    I["w_gate"] = din("w_gate", [N_EXP, D, D])
    I["w_up"] = din("w_up", [N_EXP, D, D])
    I["w_down"] = din("w_down", [N_EXP, D, D])
    I["b_gateT"] = din("b_gateT", [128, N_EXP * KC])
    I["b_upT"] = din("b_upT", [128, N_EXP * KC])
    I["b_down"] = din("b_down", [N_EXP, D])
    I["ident"] = din("ident", [128, 128])
    I["distB"] = din("distB", [128, 4 * 1920])
    I["stripAh"] = din("stripAh", [8, 128, 3968])
    I["kbiasA"] = din("kbiasA", [128, 24])
    I["iota_row"] = din("iota_row", [128, CAP])
    I["triu"] = din("triu", [128, 128])
    cx.I = I
    O = {}
    O["out"] = dout("out", [OWN, D])
    for name, shape, dt in DEBUG_SHAPES:
        if name in cx.debug:
            O[name] = dout(name, shape, dt)
    cx.O = O
    W = {}
    W["QaT"] = dscr("QaT", [1024, OWN])
    W["KaT"] = dscr("KaT", [1024, 3072])
    W["Va"] = dscr("Va", [SEQ, 1024])
    W["QbT"] = dscr("QbT", [1024, OWN])
    W["KbT"] = dscr("KbT", [1024, SEQ])
    W["Vb"] = dscr("Vb", [SEQ, 1024])
    W["GT"] = dscr("GT", [4096, OWN])
    W["rows"] = dscr("rows", [4, D], F32)
    W["YaT"] = dscr("YaT", [1024, OWN])
    W["YbT"] = dscr("YbT", [1024, OWN])
    W["U2"] = dscr("U2", [OWN, D])
    W["MT"] = dscr("MT", [D, OWN])
    W["hs"] = dscr("hs", [OWN, D], F32)
    W["Y"] = dscr("Y", [N_EXP * CAP, D])
    cx.W = W

    with contextlib.ExitStack() as st:
        cx.st = st
        sb = lambda name, shape, dt: st.enter_context(nc.sbuf_tensor(name, list(shape), dt))
        cx.banks = [st.enter_context(nc.psum_tensor(f"bank{i}", [128, 512], F32)) for i in range(8)]
        cx.ident_f = sb("ident_f", [128, 128], F32)
        cx.ident_b = sb("ident_b", [128, 128], BF16)
        cx.ones_f = sb("ones_f", [128, 128], F32)
        cx.ones_b = sb("ones_b", [128, 128], BF16)
        cx.colT = sb("colT", [128, 32], F32)
        cx.neglam = sb("neglam", [128, 1], F32)
        cx.sg = sb("sg", [128, 2], F32)
        cx.barsc = sb("barsc", [128, 8], F32)
        cx.barb = sb("barb", [128, 2], BF16)
        cx.epsc = sb("epsc", [128, 2], F32)
        S.bar_scratch = {"b": cx.barb, "one": cx.barsc[:, 0:1], "act": cx.barsc[:, 1:2],
                         "dve": cx.barsc[:, 2:3], "pool": cx.barsc[:, 3:4]}
        S.bar_ps = cx.banks[7]
        cx.slot_const = S.slot("const")
        cx.slot_out = S.slot("out")
        cx.dbg_slot = S.slot("dbg")

        cx.posm = sb("posm", [128, 8, N_EXP], F32)
        cx.prob = sb("prob", [128, 8, N_EXP], F32)
        cx.prob_hi = sb("prob_hi", [128, 8, N_EXP], BF16)
        cx.prob_lo = sb("prob_lo", [128, 8, N_EXP], BF16)
        cx.iota = sb("iota", [128, CAP], F32)
        S.dma("sp", cx.slot_const, cx.iota[:], I["iota_row"][:, :], writes=["iota"])
        stages = cx.stages
        phase0(cx)
        if "A" in stages:
            phaseA(cx)
            S.barrier()
        if "B" in stages:
            attention(cx, "B")
        if "C" in stages:
            attention(cx, "A")
        if "D" in stages:
            phaseD(cx)
        if "E" in stages:
            phaseE1(cx)
            phaseE2(cx)
        S.emit(final_waits=[cx.slot_out, cx.dbg_slot])
    global _LAST_DECLARED
    _LAST_DECLARED = list(dict.keys(I))
    return nc


DEBUG_SHAPES = [
    ("d_yA", [1024, OWN], BF16),
    ("d_mT", [D, OWN], BF16),
    ("d_ya", [1024, OWN], BF16),
    ("d_yb", [1024, OWN], BF16),
    ("d_gat", [128, 1024], BF16),
    ("d_a1", [128, 1024], F32),
    ("d_rowB", [128, D], F32),
    ("d_yB", [1024, OWN], BF16),
    ("d_h1", [OWN, D], F32),
    ("d_lg", [128, 8 * N_EXP], F32),
    ("d_posm", [128, 8 * N_EXP], F32),
    ("d_prob", [128, 8 * N_EXP], F32),
    ("d_colT", [128, 32], F32),
    ("d_mod", [1, 6 * D], F32),
    ("d_rows", [4, D], F32),
    ("d_misc", [128, 4], F32),
    ("d_uT", [128, KC * 512], BF16),
]


def phase0(cx):
    nc, S, I = cx.nc, cx.S, cx.I
    with contextlib.ExitStack() as st:
        sb = lambda name, shape, dt: st.enter_context(nc.sbuf_tensor(name, list(shape), dt))
        cT = sb("p0_cT", [128, KC], F32)
        sc = sb("p0_sc", [128, KC], F32)
        brow = sb("p0_brow", [1, 6 * D], F32)
        modrow = sb("p0_modrow", [1, 6 * D], F32)
        nrow = sb("p0_nrow", [1, 2 * D], F32)
        grow = sb("p0_grow", [1, 2 * D], F32)
        lamT = sb("p0_lamT", [128, 4], F32)
        sublnT = sb("p0_subln", [128, 2], F32)
        prod = sb("p0_prod", [128, 2], F32)
        ex = sb("p0_ex", [128, 2], F32)
        pan = [sb(f"p0_pan{i}", [128, KC, 512], F32) for i in range(2)]
        pslot = [S.slot("pan") for _ in range(2)]
        sc_slot = cx.slot_const
        bk = cx.banks

        S.dma("sp", sc_slot, cx.ident_f[:], I["ident"][:, :], writes=["ident_f"])
        S.dma("sp", sc_slot, cT[:], I["cT"][:, :], writes=["cT"])
        S.dma("sp", sc_slot, brow[:], I["b_ada"][:, :], writes=["brow"])
        S.dma("sp", sc_slot, nrow[0:1, 0:D], I["n1"][:, :], writes=["nrow"])
        S.dma("sp", sc_slot, nrow[0:1, D:2 * D], I["n2"][:, :], writes=["nrow"])
        S.dma("sp", sc_slot, lamT[:], I["lamT"][:, :], writes=["lamT"])
        S.dma("sp", sc_slot, sublnT[:], I["sublnT"][:, :], writes=["sublnT"])

        S.op("dve", lambda e: e.memset(cx.ones_f[:], 1.0), writes=["ones_f"])
        S.op("dve", lambda e: e.memset(cx.ones_b[:], 1.0), writes=["ones_b"])
        S.op("dve", lambda e: e.memset(cx.barsc[:], 1.0), writes=["barsc"])
        S.op("dve", lambda e: e.memset(cx.barb[:], 1.0), writes=["barb"])
        S.op("dve", lambda e: e.memset(cx.epsc[:], EPS), writes=["epsc"])
        S.op("dve", lambda e: e.tensor_copy(out=cx.ident_b[:], in_=cx.ident_f[:]), reads=["ident_f"], writes=["ident_b"])
        S.op("act", lambda e: e.activation(out=sc[:], in_=cT[:], func=AF.Silu), reads=["cT"], writes=["sc"])

        wv = I["w_ada"].rearrange("(k p) n -> p k n", p=128)
        NP = 6 * D // 512
        for pn in range(NP):
            pb = pan[pn % 2]
            for j in range(4):
                q = "sp" if j % 2 == 0 else "act"
                S.dma(q, pslot[pn % 2], pb[:, 4 * j:4 * j + 4, :], wv[:, 4 * j:4 * j + 4, pn * 512:(pn + 1) * 512],
                      writes=[("pan", pn % 2)])
            ps = bk[pn % 2]
            for k in range(KC):
                S.op("pe", lambda e, ps=ps, pb=pb, k=k: e.matmul(ps[0:1, :], sc[:, k:k + 1], pb[:, k, :],
                                                                 start=(k == 0), stop=(k == KC - 1)),
                     reads=["sc", ("pan", pn % 2)], writes=[("bank", pn % 2)])
            S.op("dve", lambda e, ps=ps, pn=pn: e.tensor_tensor(out=modrow[0:1, pn * 512:(pn + 1) * 512], in0=ps[0:1, :],
                                                                 in1=brow[0:1, pn * 512:(pn + 1) * 512], op=ALU.add),
                 reads=[("bank", pn % 2), "brow"], writes=["modrow"])
        mr = lambda m: modrow[0:1, m * D:(m + 1) * D]
        S.op("dve", lambda e: e.scalar_tensor_tensor(out=grow[0:1, 0:D], in0=mr(1), scalar=1.0, in1=nrow[0:1, 0:D],
                                                     op0=ALU.add, op1=ALU.mult),
             reads=["modrow", "nrow"], writes=["grow"])
        S.op("dve", lambda e: e.scalar_tensor_tensor(out=grow[0:1, D:2 * D], in0=mr(4), scalar=1.0, in1=nrow[0:1, D:2 * D],
                                                     op0=ALU.add, op1=ALU.mult),
             reads=["modrow", "nrow"], writes=["grow"])
        psc = bk[2]
        for k in range(KC):
            S.op("pe", lambda e, k=k: e.matmul(psc[:, k:k + 1], grow[0:1, k * 128:(k + 1) * 128], cx.ones_f[0:1, 0:1],
                                               start=True, stop=True),
                 reads=["grow", "ones_f"], writes=[("bank", 2)])
        for k in range(KC):
            S.op("pe", lambda e, k=k: e.matmul(psc[:, KC + k:KC + k + 1], modrow[0:1, k * 128:(k + 1) * 128],
                                               cx.ones_f[0:1, 0:1], start=True, stop=True),
                 reads=["modrow", "ones_f"], writes=[("bank", 2)])
        S.op("dve", lambda e: e.tensor_copy(out=cx.colT[:], in_=psc[:, 0:32]), reads=[("bank", 2)], writes=["colT"])
        rs = cx.slot_const
        S.dma("sp", rs, cx.W["rows"][0:1, :], mr(2), reads=["modrow"], writes=["rows_d"])
        S.dma("sp", rs, cx.W["rows"][1:2, :], grow[0:1, D:2 * D], reads=["grow"], writes=["rows_d"])
        S.dma("sp", rs, cx.W["rows"][2:3, :], mr(3), reads=["modrow"], writes=["rows_d"])
        S.dma("sp", rs, cx.W["rows"][3:4, :], mr(5), reads=["modrow"], writes=["rows_d"])
        S.op("dve", lambda e: e.tensor_tensor(out=prod[:, 0:1], in0=lamT[:, 0:1], in1=lamT[:, 1:2], op=ALU.mult),
             reads=["lamT"], writes=["prod"])
        S.op("dve", lambda e: e.tensor_tensor(out=prod[:, 1:2], in0=lamT[:, 2:3], in1=lamT[:, 3:4], op=ALU.mult),
             reads=["lamT", "prod"], writes=["prod"])
        S.op("pe", lambda e: e.matmul(bk[3][:, 0:2], cx.ones_f[:], prod[:], start=True, stop=True),
             reads=["ones_f", "prod"], writes=[("bank", 3)])
        S.op("act", lambda e: e.activation(out=ex[:], in_=bk[3][:, 0:2], func=AF.Exp), reads=[("bank", 3)], writes=["ex"])
        S.op("dve", lambda e: e.tensor_tensor(out=cx.neglam[:], in0=ex[:, 1:2], in1=ex[:, 0:1], op=ALU.subtract),
             reads=["ex"], writes=["neglam"])
        S.op("dve", lambda e: e.tensor_scalar(out=cx.neglam[:], in0=cx.neglam[:], scalar1=-LAMBDA_INIT, scalar2=None,
                                              op0=ALU.add),
             reads=["neglam"], writes=["neglam"])
        S.op("dve", lambda e: e.tensor_scalar(out=cx.sg[:], in0=sublnT[:], scalar1=(1.0 - LAMBDA_INIT), scalar2=None,
                                              op0=ALU.mult),
             reads=["sublnT"], writes=["sg"])
        if "d_mod" in cx.debug:
            S.dma("sp", cx.dbg_slot, cx.O["d_mod"][:, :], modrow[:], reads=["modrow"])
        if "d_colT" in cx.debug:
            S.dma("sp", cx.dbg_slot, cx.O["d_colT"][:, :], cx.colT[:], reads=["colT"])
            S.dma("sp", cx.dbg_slot, cx.O["d_misc"][:, 0:1], cx.neglam[:], reads=["neglam"], allow_slow_non_contiguous=True)
            S.dma("sp", cx.dbg_slot, cx.O["d_misc"][:, 1:3], cx.sg[:], reads=["sg"], allow_slow_non_contiguous=True)
        S.barrier()


def evac_copy(S, i, out, in_, reads, writes, func=None):
    if func is not None or i % 2 == 0:
        f = func if func is not None else AF.Copy
        return S.op("act", lambda e: e.activation(out=out, in_=in_, func=f), reads=reads, writes=writes)
    return S.op("dve", lambda e: e.tensor_copy(out=out, in_=in_), reads=reads, writes=writes)


def phaseA(cx):
    nc, S, I, W = cx.nc, cx.S, cx.I, cx.W
    bk = cx.banks
    with contextlib.ExitStack() as st:
        sb = lambda name, shape, dt: st.enter_context(nc.sbuf_tensor(name, list(shape), dt))
        uT = sb("A_uT", [128, KC, SEQ], BF16)
        with contextlib.ExitStack() as st1:
            sb1 = lambda name, shape, dt: st1.enter_context(nc.sbuf_tensor(name, list(shape), dt))
            xt = [sb1(f"A_x{i}", [128, D], F32) for i in range(3)]
            xn = [sb1(f"A_xn{i}", [128, D], BF16) for i in range(2)]
            junk = sb1("A_junk", [128, D], BF16)
            ss = sb1("A_ss", [128, 4], F32)
            rt = sb1("A_rt", [128, 4], F32)
            rs = sb1("A_rs", [128, 4], F32)
            xslot = [S.slot("x") for _ in range(3)]
            xv = I["xr"].rearrange("(t p) d -> t p d", p=128)
            NTT = SEQ // 128
            for tt in range(NTT):
                xb = xt[tt % 3]
                S.dma("sp", xslot[tt % 3], xb[:, 0:1024], xv[tt, :, 0:1024], writes=[("x", tt % 3)])
                S.dma("act", xslot[tt % 3], xb[:, 1024:2048], xv[tt, :, 1024:2048], writes=[("x", tt % 3)])
                j = tt % 4
                S.op("act", lambda e, xb=xb, j=j: e.activation(out=junk[:], in_=xb[:], func=AF.Square,
                                                              accum_out=ss[:, j:j + 1]),
                     reads=[("x", tt % 3)], writes=["junk", ("ss", j)])
                S.op("act", lambda e, j=j: e.activation(out=rt[:, j:j + 1], in_=ss[:, j:j + 1], func=AF.Sqrt,
                                                        scale=1.0 / D, bias=cx.epsc[:, 0:1]),
                     reads=[("ss", j)], writes=[("rt", j)])
                S.op("dve", lambda e, j=j: e.reciprocal(out=rs[:, j:j + 1], in_=rt[:, j:j + 1]),
                     reads=[("rt", j)], writes=[("rs", j)])
                xnb = xn[tt % 2]
                S.op("act", lambda e, xb=xb, xnb=xnb, j=j: e.activation(out=xnb[:], in_=xb[:], func=AF.Copy,
                                                                       scale=rs[:, j:j + 1]),
                     reads=[("x", tt % 3), ("rs", j)], writes=[("xn", tt % 2)])
                for half in range(2):
                    bi = (2 * tt + half) % 4
                    pst = bk[bi][:].bitcast(BF16)
                    for q in range(8):
                        k = half * 8 + q
                        S.op("pe", lambda e, pst=pst, q=q, k=k, xnb=xnb: e.transpose(
                            pst[:, q * 128:(q + 1) * 128], xnb[:, k * 128:(k + 1) * 128], cx.ident_b[:]),
                            reads=[("xn", tt % 2), "ident_b"], writes=[("bank", bi)])
                    for q in range(8):
                        k = half * 8 + q
                        S.op("dve", lambda e, pst=pst, q=q, k=k, tt=tt: e.tensor_scalar(
                            out=uT[:, k, tt * 128:(tt + 1) * 128], in0=pst[:, q * 128:(q + 1) * 128],
                            scalar1=cx.colT[:, k:k + 1], scalar2=cx.colT[:, KC + k:KC + k + 1],
                            op0=ALU.mult, op1=ALU.add),
                            reads=[("bank", bi), "colT"], writes=[("uT", tt // 4)])
        if "d_uT" in cx.debug:
            for k in range(KC):
                S.dma("sp", cx.dbg_slot, cx.O["d_uT"][:, k * 512:(k + 1) * 512], uT[:, k, 1024:1536],
                      reads=[("uT", c) for c in range(8)])
        S.barrier()
        wch = [sb(f"A_w{i}", [128, KC, 512], BF16) for i in range(2)]
        stg = [sb(f"A_stg{i}", [128, 512], BF16) for i in range(4)]
        wslot = [S.slot("w") for _ in range(2)]
        sslot = [S.slot("stg") for _ in range(4)]
        wv = I["w_in"].rearrange("(k p) n -> p k n", p=128)
        groups = []
        for g in range(2):
            groups.append(("qa", "f", [2, 3], W["QaT"], g, 1024))
        for g in range(2):
            groups.append(("ka", "f", list(range(6)), W["KaT"], g, 0))
        for g in range(2):
            groups.append(("va", "t", list(range(6)), W["Va"], g, 0))
        for g in range(2):
            groups.append(("qb", "f", [2, 3], W["QbT"], g, 1024))
        for g in range(2):
            groups.append(("kb", "f", list(range(8)), W["KbT"], g, 0))
        for g in range(2):
            groups.append(("vb", "t", list(range(8)), W["Vb"], g, 0))
        for g in range(8):
            groups.append(("gt", "f", [2, 3], W["GT"], g, 1024))
        ev = 0
        for cg, (name, kind, tcs, dst, g, tok0) in enumerate(groups):
            wb = wch[cg % 2]
            for j in range(4):
                S.dma("pool", wslot[cg % 2], wb[:, 4 * j:4 * j + 4, :], wv[:, 4 * j:4 * j + 4, cg * 512:(cg + 1) * 512],
                      writes=[("w", cg % 2)])
            func = AF.Sigmoid if name == "gt" else None
            if kind == "f":
                for s4 in range(4):
                    for tc in tcs:
                        bi = ev % 4
                        ps = bk[bi]
                        for k in range(KC):
                            S.op("pe", lambda e, ps=ps, wb=wb, k=k, s4=s4, tc=tc: e.matmul(
                                ps[:, :], wb[:, k, s4 * 128:(s4 + 1) * 128], uT[:, k, tc * 512:(tc + 1) * 512],
                                start=(k == 0), stop=(k == KC - 1)),
                                reads=[("w", cg % 2), ("uT", tc)], writes=[("bank", bi)])
                        sg = stg[ev % 4]
                        evac_copy(S, ev, sg[:], ps[:, :], [("bank", bi)], [("stg", ev % 4)], func=func)
                        row0 = g * 512 + s4 * 128
                        col0 = tc * 512 - tok0
                        S.dma("sp", sslot[ev % 4], dst[row0:row0 + 128, col0:col0 + 512], sg[:],
                              reads=[("stg", ev % 4)])
                        ev += 1
            else:
                for tc in tcs:
                    for t4 in range(4):
                        tt = tc * 4 + t4
                        bi = ev % 4
                        ps = bk[bi]
                        for k in range(KC):
                            S.op("pe", lambda e, ps=ps, wb=wb, k=k, tt=tt: e.matmul(
                                ps[:, :], uT[:, k, tt * 128:(tt + 1) * 128], wb[:, k, :],
                                start=(k == 0), stop=(k == KC - 1)),
                                reads=[("w", cg % 2), ("uT", tc)], writes=[("bank", bi)])
                        sg = stg[ev % 4]
                        evac_copy(S, ev, sg[:], ps[:, :], [("bank", bi)], [("stg", ev % 4)])
                        S.dma("sp", sslot[ev % 4], dst[tt * 128:(tt + 1) * 128, g * 512:(g + 1) * 512], sg[:],
                              reads=[("stg", ev % 4)])
                        ev += 1


def attention(cx, mixer):
    nc, S, I, W = cx.nc, cx.S, cx.I, cx.W
    bk = cx.banks
    isB = mixer == "B"
    nheads = 4 if isB else 8
    nkt = 32 if isB else 24
    ncomp = 2 if isB else 1
    nv = 2 if isB else 1
    vw = 1024
    with contextlib.ExitStack() as st:
        sb = lambda name, shape, dt: st.enter_context(nc.sbuf_tensor(name, list(shape), dt))
        if isB:
            vbuf = [sb(f"B_v{i}", [128, nkt, 256], BF16) for i in range(2)]
        else:
            v_all = sb("A_v", [128, nkt, vw], BF16)
        yT = sb(mixer + "_yT", [128, 8, OWN], BF16)
        ktb = [sb(f"{mixer}_kt{i}", [128, ncomp, nkt * 128], BF16) for i in range(2)]
        qtb = [sb(f"{mixer}_qt{i}", [128, ncomp, OWN], BF16) for i in range(2)]
        if isB:
            strip = sb("B_strip", [128, 4 * 1920], F32)
            stripb = [strip, strip]
        else:
            stripb = [sb(f"A_strip{i}", [128, 3968], F32) for i in range(2)]
            kbias = sb("A_kbias", [128, 24], F32)
        NS = 5 if isB else 6
        LA = 3
        NB = 6
        tmpb = [sb(f"{mixer}_tmp{i}", [128, 512], F32) for i in range(NB)]
        eb = [sb(f"{mixer}_e{i}", [128, 512], BF16) for i in range(NB)]
        rcp = [sb(f"{mixer}_rcp{i}", [128, 512], F32) for i in range(2)]
        if isB:
            onb = [sb(f"B_on{i}", [128, 2, 2, 512], F32) for i in range(2)]
            diff = sb("B_diff", [128, 2, 512], F32)
            sq = sb("B_sq", [128, 2, 512], BF16)
            rt = sb("B_rt", [128, 512], F32)
            rr = sb("B_rr", [128, 512], F32)
        vslot = S.slot("v")
        hslot = [S.slot("hd") for _ in range(2)]
        vsrc = (W["Vb"] if isB else W["Va"])[0:nkt * 128, :].rearrange("(t p) c -> p t c", p=128)
        if not isB:
            for j in range(0, nkt, 4):
                S.dma("sp" if (j // 4) % 2 == 0 else "act", vslot, v_all[:, j:j + 4, :], vsrc[:, j:j + 4, :], writes=["v_all"])
        if isB:
            for j in range(4):
                S.dma("sp", vslot, strip[:, j * 1920:(j + 1) * 1920], I["distB"][:, j * 1920:(j + 1) * 1920], writes=["strip"])
        else:
            S.dma("sp", vslot, kbias[:], I["kbiasA"][:, :], writes=["kbias"])
        KT = W["KbT"] if isB else W["KaT"]
        QT = W["QbT"] if isB else W["QaT"]
        it = 0
        accset = 0
        for h in range(nheads):
            par = h % 2
            for c in range(ncomp):
                row = (h * ncomp + c) * 128
                for j in range(0, nkt * 128, 1024):
                    S.dma("sp" if c == 0 else "act", hslot[par], ktb[par][:, c, j:j + 1024], KT[row:row + 128, j:j + 1024],
                          writes=[("hd", par)])
                S.dma("act", hslot[par], qtb[par][:, c, :], QT[row:row + 128, :], writes=[("hd", par)])
            if not isB:
                S.dma("sp", hslot[par], stripb[par][:], I["stripAh"][h], writes=[("hd", par)])
            else:
                for j in range(0, nkt, 8):
                    S.dma("sp", hslot[par], vbuf[par][:, j:j + 8, :], vsrc[:, j:j + 8, h * 256:(h + 1) * 256],
                          writes=[("hd", par)])
            hk = [("hd", par), "v_all", "strip", "kbias"]
            slope = SLOPES_B[h] if isB else SLOPES_A[h]
            for qc in range(2):
                for c in range(ncomp):
                    abase = NS
                    O = [bk[abase + v] for v in range(nv)]
                    Dn = bk[abase + nv]
                    akeys = [("bank", abase + v) for v in range(nv)] + [("bank", abase + nv)]
                    if isB:
                        kts = list(range(nkt))
                    else:
                        kts = [kt for kt in range(nkt)
                               if not (128 * kt - 1024 - 512 * qc - 511 > 1024 or 128 * kt + 127 - 1024 - 512 * qc < -1024)]
                    pend = []

                    def emit_pv(first, last, kt, ebb, ei, O=O, Dn=Dn, akeys=akeys, par=par, h=h):
                        for v in range(nv):
                            col = (v * 128) if isB else (h * 128)
                            vt = vbuf[par] if isB else v_all
                            S.op("pe", lambda e, v=v, kt=kt, col=col, ebb=ebb, first=first, last=last, vt=vt: e.matmul(
                                O[v][:, :], vt[:, kt, col:col + 128], ebb[:], start=first, stop=last),
                                reads=[("e", ei), ("hd", par), "v_all"], writes=[akeys[v]])
                        S.op("pe", lambda e, ebb=ebb, first=first, last=last: e.matmul(
                            Dn[:, :], cx.ones_b[:], ebb[:], start=first, stop=last),
                            reads=[("e", ei), "ones_b"], writes=[akeys[-1]])

                    for ki, kt in enumerate(kts):
                        sbk = it % NS
                        ps = bk[sbk]
                        S.op("pe", lambda e, ps=ps, par=par, c=c, kt=kt, qc=qc: e.matmul(
                            ps[:, :], ktb[par][:, c, kt * 128:(kt + 1) * 128], qtb[par][:, c, qc * 512:(qc + 1) * 512],
                            start=True, stop=True), reads=hk, writes=[("bank", sbk)])
                        tb = tmpb[it % NB]
                        if isB:
                            g, ktp = kt // 8, kt % 8
                            x0 = g * 1920 + 512 * qc - 128 * ktp + 896
                            c1 = -SCALE / slope
                        else:
                            x0 = 512 * qc - 128 * kt + 2944
                            c1 = SCALE
                        sp_ = stripb[par]
                        S.op("dve", lambda e, tb=tb, ps=ps, sp_=sp_, x0=x0, c1=c1: e.scalar_tensor_tensor(
                            out=tb[:], in0=ps[:, :], scalar=c1, in1=sp_[:, x0:x0 + 512], op0=ALU.mult, op1=ALU.add),
                            reads=[("bank", sbk)] + hk, writes=[("tmp", it % NB)])
                        ebb = eb[it % NB]
                        if isB:
                            S.op("act", lambda e, ebb=ebb, tb=tb, slope=slope: e.activation(
                                out=ebb[:], in_=tb[:], func=AF.Exp, scale=-slope),
                                reads=[("tmp", it % NB)], writes=[("e", it % NB)])
                        else:
                            S.op("act", lambda e, ebb=ebb, tb=tb, kt=kt: e.activation(
                                out=ebb[:], in_=tb[:], func=AF.Exp, bias=kbias[:, kt:kt + 1]),
                                reads=[("tmp", it % NB)] + hk, writes=[("e", it % NB)])
                        first, last = (ki == 0), (ki == len(kts) - 1)
                        pend.append((first, last, kt, ebb, it % NB))
                        it += 1
                        if len(pend) > LA:
                            emit_pv(*pend.pop(0))
                    while pend:
                        emit_pv(*pend.pop(0))
                    rc = rcp[accset]
                    S.op("dve", lambda e, rc=rc, Dn=Dn: e.reciprocal(out=rc[:], in_=Dn[:, :]),
                         reads=[akeys[-1]], writes=[("rcp", accset)])
                    for v in range(nv):
                        if isB:
                            dst = onb[qc % 2][:, c, v, :]
                            wk = ("on", qc % 2, c)
                        else:
                            dst = yT[:, h, qc * 512:(qc + 1) * 512]
                            wk = ("yT", mixer)
                        S.op("dve", lambda e, dst=dst, O=O, v=v, rc=rc: e.tensor_tensor(
                            out=dst, in0=O[v][:, :], in1=rc[:], op=ALU.mult),
                            reads=[akeys[v], ("rcp", accset)], writes=[wk])
                    accset ^= 1
                if isB:
                    on = onb[qc % 2]
                    ok = [("on", qc % 2, 0), ("on", qc % 2, 1)]
                    for v in range(2):
                        S.op("dve", lambda e, on=on, v=v: e.scalar_tensor_tensor(
                            out=diff[:, v, :], in0=on[:, 1, v, :], scalar=cx.neglam[:, 0:1], in1=on[:, 0, v, :],
                            op0=ALU.mult, op1=ALU.add), reads=ok + ["neglam"], writes=["diff"])
                    S.op("act", lambda e: e.activation(out=sq[:], in_=diff[:], func=AF.Square), reads=["diff"], writes=["sq"])
                    sbk = it % NS
                    it += 1
                    ps = bk[sbk]
                    for v in range(2):
                        S.op("pe", lambda e, ps=ps, v=v: e.matmul(ps[:, :], cx.ones_b[:], sq[:, v, :], start=(v == 0), stop=(v == 1)),
                             reads=["sq", "ones_b"], writes=[("bank", sbk)])
                    S.op("act", lambda e, ps=ps: e.activation(out=rt[:], in_=ps[:, :], func=AF.Sqrt, scale=1.0 / 256,
                                                             bias=cx.epsc[:, 0:1]),
                         reads=[("bank", sbk), "epsc"], writes=["rt"])
                    S.op("dve", lambda e: e.reciprocal(out=rr[:], in_=rt[:]), reads=["rt"], writes=["rr"])
                    for v in range(2):
                        S.op("dve", lambda e, v=v, h=h, qc=qc: e.scalar_tensor_tensor(
                            out=yT[:, 2 * h + v, qc * 512:(qc + 1) * 512], in0=diff[:, v, :], scalar=cx.sg[:, v:v + 1],
                            in1=rr[:], op0=ALU.mult, op1=ALU.mult), reads=["diff", "rr", "sg"], writes=[("yT", mixer)])
        ydst = W["YbT"] if isB else W["YaT"]
        for j in range(8):
            S.dma("sp", vslot, ydst[j * 128:(j + 1) * 128, :], yT[:, j, :], reads=[("yT", mixer)])
        if ("d_y" + mixer) in cx.debug:
            for j in range(8):
                S.dma("sp", cx.dbg_slot, cx.O["d_y" + mixer][j * 128:(j + 1) * 128, :], yT[:, j, :], reads=[("yT", mixer)])
    S.barrier()


def phaseD(cx):
    nc, S, I, W = cx.nc, cx.S, cx.I, cx.W
    bk = cx.banks
    with contextlib.ExitStack() as st:
        sb = lambda name, shape, dt: st.enter_context(nc.sbuf_tensor(name, list(shape), dt))
        with contextlib.ExitStack() as st1:
            sb1 = lambda name, shape, dt: st1.enter_context(nc.sbuf_tensor(name, list(shape), dt))
            mT = sb1("D_mT", [128, KC, OWN], BF16)
            ya = sb1("D_ya", [128, 8, OWN], BF16)
            yb = sb1("D_yb", [128, 8, OWN], BF16)
            woa = sb1("D_woa", [128, 8, D], BF16)
            wob = sb1("D_wob", [128, 8, D], BF16)
            gat = [sb1(f"D_g{i}", [128, 2, 512], BF16) for i in range(2)]
            t1 = [sb1(f"D_t1{i}", [128, 512], F32) for i in range(2)]
            t2 = [sb1(f"D_t2{i}", [128, 512], F32) for i in range(2)]
            ls = S.slot("D1")
            gs = [S.slot("D1g") for _ in range(2)]
            for j in range(8):
                S.dma("sp", ls, ya[:, j, :], W["YaT"][j * 128:(j + 1) * 128, :], writes=["ya"])
                S.dma("act", ls, yb[:, j, :], W["YbT"][j * 128:(j + 1) * 128, :], writes=["yb"])
            wav = I["w_out_a"].rearrange("(k p) n -> p k n", p=128)
            wbv = I["w_out_b"].rearrange("(k p) n -> p k n", p=128)
            for j in range(0, 8, 2):
                S.dma("pool", ls, woa[:, j:j + 2, :], wav[:, j:j + 2, :], writes=["woa"])
                S.dma("pool", ls, wob[:, j:j + 2, :], wbv[:, j:j + 2, :], writes=["wob"])
            if "d_ya" in cx.debug:
                for j in range(8):
                    S.dma("sp", cx.dbg_slot, cx.O["d_ya"][j * 128:(j + 1) * 128, :], ya[:, j, :], reads=["ya"])
                    S.dma("sp", cx.dbg_slot, cx.O["d_yb"][j * 128:(j + 1) * 128, :], yb[:, j, :], reads=["yb"])
            it = 0
            for dc in range(KC):
                for qc in range(2):
                    gb_ = gat[it % 2]
                    S.dma("sp", gs[it % 2], gb_[:, 0, :], W["GT"][dc * 128:(dc + 1) * 128, qc * 512:(qc + 1) * 512],
                          writes=[("gat", it % 2)])
                    S.dma("sp", gs[it % 2], gb_[:, 1, :], W["GT"][2048 + dc * 128:2048 + (dc + 1) * 128, qc * 512:(qc + 1) * 512],
                          writes=[("gat", it % 2)])
                    pa, pb = bk[(2 * it) % 4], bk[(2 * it + 1) % 4]
                    for f in range(8):
                        S.op("pe", lambda e, pa=pa, f=f, dc=dc, qc=qc: e.matmul(
                            pa[:, :], woa[:, f, dc * 128:(dc + 1) * 128], ya[:, f, qc * 512:(qc + 1) * 512],
                            start=(f == 0), stop=(f == 7)), reads=["woa", "ya"], writes=[("bank", (2 * it) % 4)])
                    for f in range(8):
                        S.op("pe", lambda e, pb=pb, f=f, dc=dc, qc=qc: e.matmul(
                            pb[:, :], wob[:, f, dc * 128:(dc + 1) * 128], yb[:, f, qc * 512:(qc + 1) * 512],
                            start=(f == 0), stop=(f == 7)), reads=["wob", "yb"], writes=[("bank", (2 * it + 1) % 4)])
                    a1, a2 = t1[it % 2], t2[it % 2]
                    S.op("dve", lambda e, a1=a1, pa=pa, gb_=gb_: e.tensor_tensor(out=a1[:], in0=pa[:, :], in1=gb_[:, 0, :], op=ALU.mult),
                         reads=[("bank", (2 * it) % 4), ("gat", it % 2)], writes=[("t1", it % 2)])
                    S.op("dve", lambda e, a2=a2, pb=pb, gb_=gb_: e.tensor_tensor(out=a2[:], in0=pb[:, :], in1=gb_[:, 1, :], op=ALU.mult),
                         reads=[("bank", (2 * it + 1) % 4), ("gat", it % 2)], writes=[("t2", it % 2)])
                    if "d_gat" in cx.debug and it == 0:
                        S.dma("sp", cx.dbg_slot, cx.O["d_gat"][:, :], gb_[:].rearrange("p a b -> p (a b)"), reads=[("gat", 0)])
                        S.dma("sp", cx.dbg_slot, cx.O["d_a1"][:, 0:512], a1[:], reads=[("t1", 0)])
                        S.dma("sp", cx.dbg_slot, cx.O["d_a1"][:, 512:1024], a2[:], reads=[("t2", 0)])
                    S.op("dve", lambda e, a1=a1, a2=a2, dc=dc, qc=qc: e.tensor_tensor(
                        out=mT[:, dc, qc * 512:(qc + 1) * 512], in0=a1[:], in1=a2[:], op=ALU.add),
                        reads=[("t1", it % 2), ("t2", it % 2)], writes=["mT"])
                    it += 1
            for k in range(KC):
                S.dma("sp", ls, W["MT"][k * 128:(k + 1) * 128, :], mT[:, k, :], reads=["mT"])
                if "d_mT" in cx.debug:
                    S.dma("sp", cx.dbg_slot, cx.O["d_mT"][k * 128:(k + 1) * 128, :], mT[:, k, :], reads=["mT"])
        S.barrier()
        h = sb("D_h", [128, 8, D], F32)
        rowB = sb("D_rowB", [128, D], F32)
        rowC = sb("D_rowC", [128, D], F32)
        st2 = contextlib.ExitStack()
        sb2 = lambda name, shape, dt: st2.enter_context(nc.sbuf_tensor(name, list(shape), dt))
        mT2 = sb2("D_mT2", [128, KC, OWN], BF16)
        wo = sb2("D_wo", [128, KC, D], BF16)
        tq = [sb2(f"D_tq{i}", [128, 512], F32) for i in range(2)]
        ls = S.slot("D2")
        for k in range(KC):
            S.dma("act", ls, mT2[:, k, :], W["MT"][k * 128:(k + 1) * 128, :], writes=["mT2"])
        wov = I["w_o"].rearrange("(k p) n -> p k n", p=128)
        for j in range(0, KC, 2):
            S.dma("pool", ls, wo[:, j:j + 2, :], wov[:, j:j + 2, :], writes=["wo"])
        xo = I["xr"][1024:2048, :].rearrange("(t p) d -> p t d", p=128)
        for tt in range(8):
            S.dma("sp" if tt % 2 == 0 else "act", ls, h[:, tt, :], xo[:, tt, :], writes=[("h", tt)])
        S.dma("sp", ls, rowB[:], W["rows"][0:1, :].partition_broadcast(128), writes=["rowB"])
        S.dma("sp", ls, rowC[:], W["rows"][2:3, :].partition_broadcast(128), writes=["rowC"])
        it = 0
        for tt in range(8):
            for ch in range(4):
                ps = bk[it % 4]
                for k in range(KC):
                    S.op("pe", lambda e, ps=ps, k=k, tt=tt, ch=ch: e.matmul(
                        ps[:, :], mT2[:, k, tt * 128:(tt + 1) * 128], wo[:, k, ch * 512:(ch + 1) * 512],
                        start=(k == 0), stop=(k == KC - 1)), reads=["wo", "mT2"], writes=[("bank", it % 4)])
                tb = tq[it % 2]
                S.op("dve", lambda e, tb=tb, ps=ps, ch=ch: e.tensor_tensor(
                    out=tb[:], in0=ps[:, :], in1=rowB[:, ch * 512:(ch + 1) * 512], op=ALU.mult),
                    reads=[("bank", it % 4), "rowB"], writes=[("tq", it % 2)])
                S.op("pool", lambda e, tb=tb, tt=tt, ch=ch: e.tensor_tensor(
                    out=h[:, tt, ch * 512:(ch + 1) * 512], in0=h[:, tt, ch * 512:(ch + 1) * 512], in1=tb[:], op=ALU.add),
                    reads=[("tq", it % 2), ("h", tt)], writes=[("h", tt)])
                it += 1
        if "d_rowB" in cx.debug:
            S.dma("sp", cx.dbg_slot, cx.O["d_rowB"][:, :], rowB[:], reads=["rowB"])
        hs = S.slot("hs")
        for tt in range(8):
            S.dma("sp", hs, W["hs"][tt * 128:(tt + 1) * 128, :], h[:, tt, :], reads=[("h", tt)])
            if "d_h1" in cx.debug:
                S.dma("sp", cx.dbg_slot, cx.O["d_h1"][tt * 128:(tt + 1) * 128, :], h[:, tt, :], reads=[("h", tt)])
        S.barrier()
        st2.close()
        S.dma("sp", ls, rowB[:], W["rows"][1:2, :].partition_broadcast(128), reads=[], writes=["rowB"])
        wr = sb("D_wr", [128, KC, N_EXP], F32)
        brB = sb("D_brB", [128, N_EXP], F32)
        triu = sb("D_triu", [128, 128], BF16)
        triu_f = sb("D_triuf", [128, 128], F32)
        S.dma("sp", ls, wr[:], I["w_router"].rearrange("(k p) n -> p k n", p=128), writes=["wr"])
        S.dma("sp", ls, brB[:], I["b_routerB"][:, :], writes=["brB"])
        S.dma("sp", ls, triu_f[:], I["triu"][:, :], writes=["triu_f"])
        S.op("dve", lambda e: e.tensor_copy(out=triu[:], in_=triu_f[:]), reads=["triu_f"], writes=["triu"])
        u2f = [sb(f"D_u2f{i}", [128, D], F32) for i in range(2)]
        u2b = [sb(f"D_u2b{i}", [128, D], BF16) for i in range(2)]
        u2T = [sb(f"D_u2T{i}", [128, KC, 128], F32) for i in range(2)]
        ss = sb("D_ss", [128, 8], F32)
        rt = sb("D_rt", [128, 8], F32)
        rs = sb("D_rs", [128, 8], F32)
        lg = sb("D_lg", [128, 8, N_EXP], F32)
        mx8 = sb("D_mx8", [128, 8, 8], F32)
        negm = sb("D_negm", [128, 8], F32)
        mask = sb("D_mask", [128, 8, N_EXP], F32)
        mask_b = sb("D_maskb", [128, 8, N_EXP], BF16)
        ex = sb("D_ex", [128, 8, N_EXP], F32)
        sm = sb("D_sm", [128, 8], F32)
        rsm = sb("D_rsm", [128, 8], F32)
        us = [S.slot("u2") for _ in range(2)]
        pit = 0
        for tt in range(8):
            uf, ub, uT_ = u2f[tt % 2], u2b[tt % 2], u2T[tt % 2]
            S.op("act", lambda e, ub=ub, tt=tt: e.activation(out=ub[:], in_=h[:, tt, :], func=AF.Square, accum_out=ss[:, tt:tt + 1]),
                 reads=[("h", tt)], writes=[("u2b", tt % 2), ("ss", tt)])
            S.op("act", lambda e, tt=tt: e.activation(out=rt[:, tt:tt + 1], in_=ss[:, tt:tt + 1], func=AF.Sqrt, scale=1.0 / D,
                                                      bias=cx.epsc[:, 0:1]), reads=[("ss", tt), "epsc"], writes=[("rt", tt)])
            S.op("dve", lambda e, tt=tt: e.reciprocal(out=rs[:, tt:tt + 1], in_=rt[:, tt:tt + 1]), reads=[("rt", tt)], writes=[("rs", tt)])
            S.op("dve", lambda e, uf=uf, tt=tt: e.scalar_tensor_tensor(out=uf[:], in0=h[:, tt, :], scalar=rs[:, tt:tt + 1], in1=rowB[:],
                                                                       op0=ALU.mult, op1=ALU.mult),
                 reads=[("h", tt), ("rs", tt), "rowB"], writes=[("u2f", tt % 2)])
            S.op("pool", lambda e, uf=uf: e.tensor_tensor(out=uf[:], in0=uf[:], in1=rowC[:], op=ALU.add),
                 reads=[("u2f", tt % 2), "rowC"], writes=[("u2f", tt % 2)])
            S.op("act", lambda e, ub=ub, uf=uf: e.activation(out=ub[:], in_=uf[:], func=AF.Copy),
                 reads=[("u2f", tt % 2)], writes=[("u2b", tt % 2)])
            S.dma("sp", us[tt % 2], W["U2"][tt * 128:(tt + 1) * 128, :], ub[:], reads=[("u2b", tt % 2)])
            for q4 in range(4):
                bi = pit % 4
                pit += 1
                ps = bk[bi]
                for q in range(4):
                    k = q4 * 4 + q
                    S.op("pe", lambda e, ps=ps, q=q, k=k, uf=uf: e.transpose(ps[:, q * 128:(q + 1) * 128], uf[:, k * 128:(k + 1) * 128],
                                                                              cx.ident_f[:]),
                         reads=[("u2f", tt % 2), "ident_f"], writes=[("bank", bi)])
                evac_copy(S, q4, uT_[:, q4 * 4:(q4 + 1) * 4, :], ps[:, :].rearrange("p (a b) -> p a b", a=4), [("bank", bi)], [("u2T", tt % 2)])
            psl = bk[4 + tt % 2]
            for k in range(KC):
                S.op("pe", lambda e, psl=psl, k=k, uT_=uT_: e.matmul(psl[:, 0:N_EXP], uT_[:, k, :], wr[:, k, :],
                                                                     start=(k == 0), stop=(k == KC - 1)),
                     reads=[("u2T", tt % 2), "wr"], writes=[("bank", 4 + tt % 2)])
            S.op("dve", lambda e, psl=psl, tt=tt: e.tensor_tensor(out=lg[:, tt, :], in0=psl[:, 0:N_EXP], in1=brB[:], op=ALU.add),
                 reads=[("bank", 4 + tt % 2), "brB"], writes=[("lg", tt)])
            S.op("dve", lambda e, tt=tt: e.max(out=mx8[:, tt, :], in_=lg[:, tt, :]), reads=[("lg", tt)], writes=[("mx8", tt)])
            S.op("dve", lambda e, tt=tt: e.tensor_scalar(out=mask[:, tt, :], in0=lg[:, tt, :], scalar1=mx8[:, tt, 3:4], scalar2=None,
                                                         op0=ALU.is_ge), reads=[("lg", tt), ("mx8", tt)], writes=[("mask", tt)])
            S.op("dve", lambda e, tt=tt: e.tensor_scalar(out=negm[:, tt:tt + 1], in0=mx8[:, tt, 0:1], scalar1=-1.0, scalar2=None,
                                                         op0=ALU.mult), reads=[("mx8", tt)], writes=[("negm", tt)])
            S.op("act", lambda e, tt=tt: e.activation(out=ex[:, tt, :], in_=lg[:, tt, :], func=AF.Exp, bias=negm[:, tt:tt + 1]),
                 reads=[("lg", tt), ("negm", tt)], writes=[("ex", tt)])
            S.op("dve", lambda e, tt=tt: e.tensor_tensor(out=ex[:, tt, :], in0=ex[:, tt, :], in1=mask[:, tt, :], op=ALU.mult),
                 reads=[("ex", tt), ("mask", tt)], writes=[("ex", tt)])
            S.op("dve", lambda e, tt=tt: e.reduce_sum(out=sm[:, tt:tt + 1], in_=ex[:, tt, :], axis=AX.X),
                 reads=[("ex", tt)], writes=[("sm", tt)])
            S.op("dve", lambda e, tt=tt: e.reciprocal(out=rsm[:, tt:tt + 1], in_=sm[:, tt:tt + 1]), reads=[("sm", tt)], writes=[("rsm", tt)])
            S.op("dve", lambda e, tt=tt: e.tensor_scalar(out=cx.prob[:, tt, :], in0=ex[:, tt, :], scalar1=rsm[:, tt:tt + 1], scalar2=None,
                                                         op0=ALU.mult), reads=[("ex", tt), ("rsm", tt)], writes=[("prob", tt)])
            S.op("dve", lambda e, tt=tt: e.tensor_copy(out=cx.prob_hi[:, tt, :], in_=cx.prob[:, tt, :]), reads=[("prob", tt)], writes=[("phi", tt)])
            S.op("dve", lambda e, tt=tt: e.tensor_tensor(out=cx.prob_lo[:, tt, :], in0=cx.prob[:, tt, :], in1=cx.prob_hi[:, tt, :],
                                                         op=ALU.subtract), reads=[("prob", tt), ("phi", tt)], writes=[("plo", tt)])
            S.op("dve", lambda e, tt=tt: e.tensor_copy(out=mask_b[:, tt, :], in_=mask[:, tt, :]), reads=[("mask", tt)], writes=[("maskb", tt)])
            pp = bk[6 + tt % 2]
            S.op("pe", lambda e, pp=pp, tt=tt: e.matmul(pp[:, 0:N_EXP], triu[:], mask_b[:, tt, :], start=True, stop=(tt == 0)),
                 reads=["triu", ("maskb", tt)], writes=[("bank", 6 + tt % 2)])
            for t2_ in range(tt):
                S.op("pe", lambda e, pp=pp, t2_=t2_, tt=tt: e.matmul(pp[:, 0:N_EXP], cx.ones_b[:], mask_b[:, t2_, :], start=False,
                                                                    stop=(t2_ == tt - 1)),
                     reads=["ones_b", ("maskb", t2_)], writes=[("bank", 6 + tt % 2)])
            S.op("dve", lambda e, pp=pp, tt=tt: e.tensor_tensor(out=cx.posm[:, tt, :], in0=pp[:, 0:N_EXP], in1=mask[:, tt, :], op=ALU.mult),
                 reads=[("bank", 6 + tt % 2), ("mask", tt)], writes=[("posm", tt)])
        if "d_lg" in cx.debug:
            S.dma("sp", cx.dbg_slot, cx.O["d_lg"][:, :], lg[:].rearrange("p a b -> p (a b)"), reads=[("lg", t) for t in range(8)])
            S.dma("sp", cx.dbg_slot, cx.O["d_posm"][:, :], cx.posm[:].rearrange("p a b -> p (a b)"), reads=[("posm", t) for t in range(8)])
            S.dma("sp", cx.dbg_slot, cx.O["d_prob"][:, :], cx.prob[:].rearrange("p a b -> p (a b)"), reads=[("prob", t) for t in range(8)])
    S.barrier()


def phaseE1(cx):
    nc, S, I, W = cx.nc, cx.S, cx.I, cx.W
    bk = cx.banks
    NR = 5
    NST = CAP // 128
    with contextlib.ExitStack() as st:
        sb = lambda name, shape, dt: st.enter_context(nc.sbuf_tensor(name, list(shape), dt))
        u2b = sb("E_u2b", [128, 8, D], BF16)
        ring = [sb(f"E_w{i}", [128, KC, 512], BF16) for i in range(NR)]
        rslot = [S.slot("ew") for _ in range(NR)]
        sl = sb("E_sel", [128, 8, CAP], BF16)
        x_ = sb("E_xe", [128, KC, CAP], BF16)
        a_ = sb("E_act", [128, KC, CAP], BF16)
        ye = sb("E_ye", [128, NST, D], BF16)
        yslot = S.slot("ye")
        p_ = sb("E_psl", [128, NST], F32)
        bgT = sb("E_bgT", [128, N_EXP * KC], F32)
        buT = sb("E_buT", [128, N_EXP * KC], F32)
        gsb = [sb(f"E_g{i}", [128, CAP], F32) for i in range(2)]
        sig = [sb(f"E_s{i}", [128, CAP], F32) for i in range(2)]
        usb = [sb(f"E_u{i}", [128, CAP], F32) for i in range(2)]
        ls = S.slot("E1")
        for tt in range(8):
            S.dma("sp" if tt % 2 == 0 else "act", ls, u2b[:, tt, :], W["U2"][tt * 128:(tt + 1) * 128, :], writes=["u2b"])
        S.dma("sp", ls, bgT[:], I["b_gateT"][:, :], writes=["bgT"])
        S.dma("sp", ls, buT[:], I["b_upT"][:, :], writes=["buT"])
        wg = I["w_gate"].rearrange("e (k p) n -> e p k n", p=128)
        wu = I["w_up"].rearrange("e (k p) n -> e p k n", p=128)
        wd = I["w_down"].rearrange("e (k p) n -> e p k n", p=128)
        cnt = {"piece": 0}

        def load_piece(src, e, g):
            ri = cnt["piece"] % NR
            cnt["piece"] += 1
            for j in range(4):
                S.dma("pool", rslot[ri], ring[ri][:, 4 * j:4 * j + 4, :], src[e, :, 4 * j:4 * j + 4, g * 512:(g + 1) * 512],
                      writes=[("ring", ri)])
            return ri

        gi = 0
        fi = 0
        di = 0
        yi = 0
        for e in range(N_EXP):
            for tt in range(8):
                S.op("dve", lambda en, tt=tt, e=e: en.tensor_scalar(
                    out=sl[:, tt, :], in0=cx.iota[:], scalar1=cx.posm[:, tt, e:e + 1], scalar2=None, op0=ALU.is_equal),
                    reads=["iota", ("posm", tt)], writes=["sel"])
            for k in range(KC):
                bi = gi % 2
                gi += 1
                ps = bk[bi]
                for tt in range(8):
                    S.op("pe", lambda en, ps=ps, k=k, tt=tt: en.matmul(
                        ps[:, 0:CAP], u2b[:, tt, k * 128:(k + 1) * 128], sl[:, tt, :],
                        start=(tt == 0), stop=(tt == 7)), reads=["u2b", "sel"], writes=[("bank", bi)])
                evac_copy(S, k, x_[:, k, :], ps[:, 0:CAP], [("bank", bi)], ["xe"])
            bi = gi % 2
            gi += 1
            ps = bk[bi]
            for st_ in range(NST):
                n = 0
                for tt in range(8):
                    for pr in (cx.prob_hi, cx.prob_lo):
                        S.op("pe", lambda en, ps=ps, st_=st_, tt=tt, pr=pr, e=e, n=n: en.matmul(
                            ps[:, st_:st_ + 1], sl[:, tt, st_ * 128:(st_ + 1) * 128], pr[:, tt, e:e + 1],
                            start=(n == 0), stop=(n == 15)),
                            reads=["sel", ("phi", tt), ("plo", tt)], writes=[("bank", bi)])
                        n += 1
            S.op("dve", lambda en, ps=ps: en.tensor_copy(out=p_[:], in_=ps[:, 0:NST]), reads=[("bank", bi)], writes=["psl"])
            for g in range(4):
                rg = load_piece(wg, e, g)
                ru = load_piece(wu, e, g)
                for f4 in range(4):
                    fc = 4 * g + f4
                    pg, pu = bk[2 + fi % 2], bk[4 + fi % 2]
                    kg, ku = ("bank", 2 + fi % 2), ("bank", 4 + fi % 2)
                    for k in range(KC):
                        S.op("pe", lambda en, pg=pg, rg=rg, k=k, f4=f4: en.matmul(
                            pg[:, 0:CAP], ring[rg][:, k, f4 * 128:(f4 + 1) * 128], x_[:, k, :], start=(k == 0), stop=(k == KC - 1)),
                            reads=[("ring", rg), "xe"], writes=[kg])
                    for k in range(KC):
                        S.op("pe", lambda en, pu=pu, ru=ru, k=k, f4=f4: en.matmul(
                            pu[:, 0:CAP], ring[ru][:, k, f4 * 128:(f4 + 1) * 128], x_[:, k, :], start=(k == 0), stop=(k == KC - 1)),
                            reads=[("ring", ru), "xe"], writes=[ku])
                    w = fi % 2
                    fi += 1
                    gb_, sg_, ub_ = gsb[w], sig[w], usb[w]
                    col = e * KC + fc
                    S.op("dve", lambda en, gb_=gb_, pg=pg, col=col: en.tensor_scalar(
                        out=gb_[:], in0=pg[:, 0:CAP], scalar1=bgT[:, col:col + 1], scalar2=7.0, op0=ALU.add, op1=ALU.min),
                        reads=[kg, "bgT"], writes=[("gsb", w)])
                    S.op("act", lambda en, sg_=sg_, gb_=gb_: en.activation(out=sg_[:], in_=gb_[:], func=AF.Sigmoid, scale=1.702),
                         reads=[("gsb", w)], writes=[("sig", w)])
                    S.op("act", lambda en, ub_=ub_, pu=pu, col=col: en.activation(
                        out=ub_[:], in_=pu[:, 0:CAP], func=AF.Identity, bias=buT[:, col:col + 1]),
                        reads=[ku, "buT"], writes=[("usb", w)])
                    S.op("dve", lambda en, ub_=ub_: en.tensor_scalar(
                        out=ub_[:], in0=ub_[:], scalar1=7.0, scalar2=-7.0, op0=ALU.min, op1=ALU.max),
                        reads=[("usb", w)], writes=[("usb", w)])
                    S.op("dve", lambda en, gb_=gb_, sg_=sg_: en.tensor_tensor(out=gb_[:], in0=gb_[:], in1=sg_[:], op=ALU.mult),
                         reads=[("gsb", w), ("sig", w)], writes=[("gsb", w)])
                    S.op("dve", lambda en, fc=fc, gb_=gb_, ub_=ub_: en.scalar_tensor_tensor(
                        out=a_[:, fc, :], in0=ub_[:], scalar=1.0, in1=gb_[:], op0=ALU.add, op1=ALU.mult),
                        reads=[("gsb", w), ("usb", w)], writes=["act"])
            for ch in range(4):
                rd = load_piece(wd, e, ch)
                for st_ in range(NST):
                    pd = bk[6 + di % 2]
                    kd = ("bank", 6 + di % 2)
                    for f in range(KC):
                        S.op("pe", lambda en, pd=pd, rd=rd, f=f, st_=st_: en.matmul(
                            pd[:, :], a_[:, f, st_ * 128:(st_ + 1) * 128], ring[rd][:, f, :], start=(f == 0), stop=(f == KC - 1)),
                            reads=[("ring", rd), "act"], writes=[kd])
                    if di % 2 == 0:
                        S.op("act", lambda en, st_=st_, ch=ch, pd=pd: en.activation(
                            out=ye[:, st_, ch * 512:(ch + 1) * 512], in_=pd[:, :], func=AF.Copy, scale=p_[:, st_:st_ + 1]),
                            reads=[kd, "psl"], writes=["ye"])
                    else:
                        S.op("dve", lambda en, st_=st_, ch=ch, pd=pd: en.tensor_scalar(
                            out=ye[:, st_, ch * 512:(ch + 1) * 512], in0=pd[:, :], scalar1=p_[:, st_:st_ + 1], scalar2=None, op0=ALU.mult),
                            reads=[kd, "psl"], writes=["ye"])
                    di += 1
            for st_ in range(NST):
                S.dma("sp", yslot, W["Y"][e * CAP + st_ * 128:e * CAP + (st_ + 1) * 128, :], ye[:, st_, :], reads=["ye"])
    S.barrier()


def cx_rd(cnt, rds, ch):
    return rds[ch]


def phaseE2(cx):
    nc, S, I, W = cx.nc, cx.S, cx.I, cx.W
    bk = cx.banks
    NST = CAP // 128
    NJ = N_EXP * NST
    with contextlib.ExitStack() as st:
        sb = lambda name, shape, dt: st.enter_context(nc.sbuf_tensor(name, list(shape), dt))
        selT = sb("F_selT", [128, NJ, 128], BF16)
        yr = [sb(f"F_y{i}", [128, NST, D], BF16) for i in range(3)]
        yslot = [S.slot("yr") for _ in range(3)]
        selw = [sb(f"F_selw{i}", [128, CAP], BF16) for i in range(2)]
        ht = [sb(f"F_h{i}", [128, D], F32) for i in range(2)]
        hslot = [S.slot("hh") for _ in range(2)]
        g2B = sb("F_g2B", [128, D], F32)
        gfB = sb("F_gfB", [128, D], F32)
        ot = [sb(f"F_ot{i}", [128, D], F32) for i in range(2)]
        tq = [sb(f"F_tq{i}", [128, 512], F32) for i in range(2)]
        bdf = sb("F_bdf", [N_EXP, D], F32)
        bdb = sb("F_bdb", [N_EXP, D], BF16)
        pT = sb("F_pT", [N_EXP, 8, 2, 128], BF16)
        ss = sb("F_ss", [128, 8], F32)
        rt = sb("F_rt", [128, 8], F32)
        rs = sb("F_rs", [128, 8], F32)
        ls = S.slot("E2")
        S.dma("sp", ls, g2B[:], W["rows"][3:4, :].partition_broadcast(128), writes=["g2B"])
        S.dma("sp", ls, gfB[:], I["nf"][0:1, :].partition_broadcast(128), writes=["gfB"])
        S.dma("sp", ls, bdf[:], I["b_down"][:, :], writes=["bdf"])
        S.op("dve", lambda e: e.tensor_copy(out=bdb[:], in_=bdf[:]), reads=["bdf"], writes=["bdb"])
        pTb = bk[6][:].bitcast(BF16)
        for tt in range(8):
            for hl, pr in enumerate((cx.prob_hi, cx.prob_lo)):
                j = (tt * 2 + hl) % 8
                S.op("pe", lambda e, j=j, pr=pr, tt=tt: e.transpose(pTb[0:N_EXP, j * 128:(j + 1) * 128], pr[:, tt, :], cx.ident_b[:]),
                     reads=[("phi", tt), ("plo", tt), "ident_b"], writes=[("bank", 6)])
                S.op("dve", lambda e, j=j, tt=tt, hl=hl: e.tensor_copy(out=pT[:, tt, hl, :], in_=pTb[0:N_EXP, j * 128:(j + 1) * 128]),
                     reads=[("bank", 6)], writes=["pT"])
        yv = W["Y"].rearrange("(e s p) c -> e p s c", p=128, s=NST)
        ti = 0
        yi = 0
        for tt in range(8):
            hb = ht[tt % 2]
            S.dma("act", hslot[tt % 2], hb[:], W["hs"][tt * 128:(tt + 1) * 128, :], writes=[("ht", tt % 2)])
            for e_ in range(N_EXP):
                sw = selw[e_ % 2]
                S.op("dve", lambda en, sw=sw, tt=tt, e_=e_: en.tensor_scalar(
                    out=sw[:], in0=cx.iota[:], scalar1=cx.posm[:, tt, e_:e_ + 1], scalar2=None, op0=ALU.is_equal),
                    reads=["iota", ("posm", tt)], writes=[("selw", e_ % 2)])
                if e_ % 2 == 0:
                    bi = 4 + ti % 2
                    ti += 1
                    pst = bk[bi][:].bitcast(BF16)
                for st_ in range(NST):
                    q = (e_ % 2) * NST + st_
                    S.op("pe", lambda en, pst=pst, q=q, st_=st_, sw=sw: en.transpose(
                        pst[:, q * 128:(q + 1) * 128], sw[:, st_ * 128:(st_ + 1) * 128], cx.ident_b[:]),
                        reads=[("selw", e_ % 2), "ident_b"], writes=[("bank", bi)])
                if e_ % 2 == 1:
                    j0 = (e_ - 1) * NST
                    S.op("act", lambda en, pst=pst, j0=j0: en.activation(
                        out=selT[:, j0:j0 + 2 * NST, :], in_=pst[:, :].rearrange("p (a b) -> p a b", a=2 * NST), func=AF.Copy),
                        reads=[("bank", bi)], writes=["selT"])
            for e_ in range(N_EXP):
                yb_ = yr[yi % 3]
                S.dma("sp", yslot[yi % 3], yb_[:], yv[e_], writes=[("yr", yi % 3)])
                for st_ in range(NST):
                    j = e_ * NST + st_
                    for ch in range(4):
                        S.op("pe", lambda en, ch=ch, j=j, st_=st_, yb_=yb_, e_=e_: en.matmul(
                            bk[ch][:, :], selT[:, j, :], yb_[:, st_, ch * 512:(ch + 1) * 512], start=(j == 0), stop=False),
                            reads=["selT", ("yr", yi % 3)], writes=[("bank", ch)])
                yi += 1
            for ch in range(4):
                for hl in range(2):
                    S.op("pe", lambda en, tt=tt, hl=hl, ch=ch: en.matmul(
                        bk[ch][:, :], pT[:, tt, hl, :], bdb[:, ch * 512:(ch + 1) * 512], start=False, stop=(hl == 1)),
                        reads=["pT", "bdb"], writes=[("bank", ch)])
            for ch in range(4):
                tb = tq[ch % 2]
                S.op("dve", lambda en, tb=tb, ch=ch: en.tensor_tensor(out=tb[:], in0=bk[ch][:, :], in1=g2B[:, ch * 512:(ch + 1) * 512],
                                                                     op=ALU.mult),
                     reads=[("bank", ch), "g2B"], writes=[("tq", ch % 2)])
                S.op("pool", lambda en, tb=tb, hb=hb, ch=ch: en.tensor_tensor(
                    out=hb[:, ch * 512:(ch + 1) * 512], in0=hb[:, ch * 512:(ch + 1) * 512], in1=tb[:], op=ALU.add),
                    reads=[("tq", ch % 2), ("ht", tt % 2)], writes=[("ht", tt % 2)])
            o_ = ot[tt % 2]
            S.op("act", lambda en, o_=o_, hb=hb, tt=tt: en.activation(out=o_[:], in_=hb[:], func=AF.Square, accum_out=ss[:, tt:tt + 1]),
                 reads=[("ht", tt % 2)], writes=[("ot", tt % 2), ("ss", tt)])
            S.op("act", lambda en, tt=tt: en.activation(out=rt[:, tt:tt + 1], in_=ss[:, tt:tt + 1], func=AF.Sqrt, scale=1.0 / D,
                                                        bias=cx.epsc[:, 0:1]), reads=[("ss", tt), "epsc"], writes=[("rt", tt)])
            S.op("dve", lambda en, tt=tt: en.reciprocal(out=rs[:, tt:tt + 1], in_=rt[:, tt:tt + 1]), reads=[("rt", tt)], writes=[("rs", tt)])
            S.op("dve", lambda en, o_=o_, hb=hb, tt=tt: en.scalar_tensor_tensor(out=o_[:], in0=hb[:], scalar=rs[:, tt:tt + 1], in1=gfB[:],
                                                                                op0=ALU.mult, op1=ALU.mult),
                 reads=[("ht", tt % 2), ("rs", tt), "gfB", ("ot", tt % 2)], writes=[("ot", tt % 2)])
            S.dma("sp", cx.slot_out, cx.O["out"][tt * 128:(tt + 1) * 128, :], o_[:], reads=[("ot", tt % 2)])
    S.barrier()


def _colT(v, nchunk):
    return np.ascontiguousarray(v.reshape(nchunk, 128).T)


def prep_inputs(inp):
    f32 = np.float32
    x = np.asarray(inp["x"], f32)
    c = np.asarray(inp["c"], f32)
    shared = {
        "w_ada": np.ascontiguousarray(inp["w_ada"][0], f32),
        "b_ada": np.ascontiguousarray(inp["b_ada"][0][None, :], f32),
        "n1": np.ascontiguousarray(inp["norm1_g"][0][None, :], f32),
        "n2": np.ascontiguousarray(inp["norm2_g"][0][None, :], f32),
        "nf": np.ascontiguousarray(np.asarray(inp["final_g"])[None, :], f32),
        "w_in": np.ascontiguousarray(inp["w_in"][0], f32),
        "lamT": np.ascontiguousarray(np.stack([inp["lam_q1"][0], inp["lam_k1"][0], inp["lam_q2"][0], inp["lam_k2"][0]], axis=1), f32),
        "sublnT": _colT(np.asarray(inp["subln_g"][0], f32), 2),
        "w_out_a": np.ascontiguousarray(inp["w_out_a"][0], f32),
        "w_out_b": np.ascontiguousarray(inp["w_out_b"][0], f32),
        "w_o": np.ascontiguousarray(inp["w_o"][0], f32),
        "w_router": np.ascontiguousarray(inp["w_router"][0], f32),
        "b_routerB": np.ascontiguousarray(np.broadcast_to(np.asarray(inp["b_router"][0], f32)[None, :], (128, N_EXP))),
        "w_gate": np.ascontiguousarray(inp["w_gate"][0], f32),
        "w_up": np.ascontiguousarray(inp["w_up"][0], f32),
        "w_down": np.ascontiguousarray(inp["w_down"][0], f32),
        "b_gateT": np.ascontiguousarray(np.asarray(inp["b_gate"][0], f32).reshape(N_EXP, KC, 128).transpose(2, 0, 1).reshape(128, N_EXP * KC)),
        "b_upT": np.ascontiguousarray(np.asarray(inp["b_up"][0], f32).reshape(N_EXP, KC, 128).transpose(2, 0, 1).reshape(128, N_EXP * KC)),
        "b_down": np.ascontiguousarray(inp["b_down"][0], f32),
        "ident": np.eye(128, dtype=f32),
        "iota_row": np.ascontiguousarray(np.broadcast_to(np.arange(1, CAP + 1, dtype=f32)[None, :], (128, CAP))),
        "triu": np.triu(np.ones((128, 128), f32)),
    }
    p = np.arange(128)[:, None]
    xx = np.arange(3968)[None, :]
    dl = p + 1920 - xx
    ad = np.abs(dl)
    n_ = ((ad <= 64).astype(np.float64) + ((dl % 4 == 0) & (ad <= 256)) + ((dl % 16 == 0) & (ad <= 1024)))
    lnn = np.where(n_ > 0, np.log(np.maximum(n_, 1.0)), 0.0)
    shared["stripAh"] = np.ascontiguousarray(np.stack(
        [np.where(n_ > 0, -SLOPES_A[h] * ad + lnn, -BIG) for h in range(8)], axis=0).astype(f32))
    maps = []
    for core in range(8):
        b, r = core // 4, core % 4
        idx = (np.arange(SEQ) - 1024 + 1024 * r) % SEQ
        m = dict(shared)
        m["xr"] = np.ascontiguousarray(x[b][idx])
        m["cT"] = _colT(c[b], KC)
        dist = np.zeros((128, 4, 1920), f32)
        for g in range(4):
            s_g = ((1024 * g - 1024 + 1024 * r) % SEQ) - 1024 * g
            q_off = -1024 + 1024 * r
            xv = np.arange(1920)[None, :]
            dist[:, g, :] = np.abs(1024 * (1 - g) + (xv - 896) - p + q_off - s_g)
        m["distB"] = np.ascontiguousarray(dist.reshape(128, 4 * 1920))
        kb = np.zeros((128, 24), f32)
        for kt in range(24):
            tok = 128 * kt + np.arange(128) - 1024 + 1024 * r
            kb[:, kt] = np.where((tok >= 0) & (tok < SEQ), 0.0, -BIG)
        m["kbiasA"] = kb
        maps.append(m)
    return maps


_NC_CACHE = {}
_LAST_DECLARED = []


def kernel(**inputs):
    maps = prep_inputs(inputs)
    if "nc" not in _NC_CACHE:
        _NC_CACHE["nc"] = build_program()
    nc = _NC_CACHE["nc"]
    names = set(_LAST_DECLARED)
    maps = [{k: v for k, v in m.items() if k in names} for m in maps]
    res = run_bass_kernel_spmd(nc, maps, core_ids=list(range(8)))
    out = np.zeros((2, SEQ, D), np.float32)
    for core in range(8):
        b, r = core // 4, core % 4
        out[b, 1024 * r:1024 * (r + 1)] = np.asarray(res.results[core]["out"], np.float32)
    return out
```

```python
import contextlib
import math
import numpy as np
import concourse.bass as bass
import concourse.mybir as mybir
from concourse.bass_utils import run_bass_kernel_spmd

F32 = mybir.dt.float32
BF16 = mybir.dt.bfloat16
AF = mybir.ActivationFunctionType
ALU = mybir.AluOpType
AX = mybir.AxisListType

ENGS = ("pe", "act", "dve", "pool", "sp")

D = 2048
SEQ = 4096
OWN = 1024
KC = 16
N_EXP = 32
TOPK = 4
CAP = 512
EPS = 1e-5
SCALE = 128 ** -0.5
LAMBDA_INIT = 0.8 - 0.6 * math.exp(-0.3 * 0)
SLOPES = [2.0 ** (-8.0 * (i + 1) / 12) for i in range(12)]
SLOPES_A = SLOPES[:8]
SLOPES_B = SLOPES[8:]
BIG = 1.0e9
PATTERNS = (1, 4, 16)


class Op:
    __slots__ = ("eng", "fn", "deps", "dma", "idx", "count", "milestone", "slot", "slot_total", "name")

    def __init__(self, eng, fn, name=""):
        self.eng = eng
        self.fn = fn
        self.deps = []
        self.dma = False
        self.milestone = False
        self.count = None
        self.slot = None
        self.slot_total = None
        self.name = name


class Slot:
    def __init__(self, name):
        self.name = name
        self.total = 0
        self.sem = None


class Sched:
    def __init__(self, nc):
        self.nc = nc
        self.ops = []
        self.writers = {}
        self.readers = {}
        self.old_readers = {}
        self.slots = []
        self.nbar = 0

    def slot(self, name):
        s = Slot(name + str(len(self.slots)))
        self.slots.append(s)
        return s

    def _add(self, op, reads, writes):
        deps = {}

        def add_dep(o):
            if o is op:
                return
            if o.dma:
                if op.dma and op.slot is o.slot:
                    return
                deps[("slot", id(o.slot))] = ("slot", o.slot, o.slot.total)
            else:
                k = ("eng", o.eng)
                prev = deps.get(k)
                if prev is None or o.idx > prev[1].idx:
                    deps[k] = ("eng", o, None)

        for k in reads:
            for o in self.writers.get(k, {}).values():
                add_dep(o)
        for k in writes:
            for o in self.writers.get(k, {}).values():
                if (not o.dma) and (not op.dma) and o.eng == op.eng:
                    continue
                add_dep(o)
            for o in list(self.readers.get(k, {}).values()) + list(self.old_readers.get(k, {}).values()):
                if (not o.dma) and (not op.dma) and o.eng == op.eng:
                    continue
                add_dep(o)
        op.deps = list(deps.values())
        for d in op.deps:
            if d[0] == "eng":
                d[1].milestone = True
        op.idx = len(self.ops)
        self.ops.append(op)
        wkey = ("slot", id(op.slot)) if op.dma else op.eng
        for k in writes:
            self.writers[k] = {wkey: op}
            if self.readers.get(k):
                self.old_readers[k] = self.readers[k]
            self.readers[k] = {}
        for k in reads:
            self.readers.setdefault(k, {})[wkey] = op
        return op

    def op(self, eng, fn, reads=(), writes=(), name=""):
        o = Op(eng, fn, name)
        return self._add(o, list(reads), list(writes))

    def dma(self, eng, slot, out, in_, reads=(), writes=(), name="", **kw):
        def fn(e, out=out, in_=in_, kw=kw):
            return e.dma_start(out=out, in_=in_, **kw)
        o = Op(eng, fn, name)
        o.dma = True
        o.slot = slot
        o.slot_total = slot.total + 16
        r = self._add(o, list(reads), list(writes))
        slot.total += 16
        return r

    def barrier(self):
        n = self.nbar
        self.nbar += 1
        sc = self.bar_scratch
        comp = ("pe", "act", "dve", "pool")
        for e in comp:
            if e == "pe":
                self.op("pe", lambda en: en.matmul(self.bar_ps[0:1, 0:1], sc["b"][0:1, 0:1], sc["b"][0:1, 0:1],
                                                    start=True, stop=True),
                        writes=[("bar", n, e), ("bank", 7)])
            elif e == "act":
                self.op("act", lambda en: en.activation(out=sc["act"][0:1, 0:1], in_=sc["one"][0:1, 0:1], func=AF.Copy),
                        writes=[("bar", n, e)])
            elif e == "dve":
                self.op("dve", lambda en: en.tensor_copy(out=sc["dve"][0:1, 0:1], in_=sc["one"][0:1, 0:1]),
                        writes=[("bar", n, e)])
            else:
                self.op("pool", lambda en: en.tensor_copy(out=sc["pool"][0:1, 0:1], in_=sc["one"][0:1, 0:1]),
                        writes=[("bar", n, e)])
        rk = [("bar", n, e) for e in comp]
        for e in ENGS:
            o = Op(e, None, "barwait")
            self._add(o, rk, [])
            for s in self.slots:
                if s.total > 0:
                    o.deps.append(("slot", s, s.total))
        self.writers = {}
        self.readers = {}
        self.old_readers = {}

    def emit(self, final_waits=()):
        nc = self.nc
        by_eng = {e: [] for e in ENGS}
        for o in self.ops:
            by_eng[o.eng].append(o)
        for e in ENGS:
            c = 0
            for o in by_eng[e]:
                if o.milestone and not o.dma:
                    c += 1
                    o.count = c
        with contextlib.ExitStack() as st:
            esem = {e: st.enter_context(nc.semaphore("s_" + e)) for e in ENGS}
            for s in self.slots:
                s.sem = st.enter_context(nc.semaphore("d_" + s.name))
            block = st.enter_context(nc.Block())

            def run(e, eng):
                waited = {}
                for o in by_eng[e]:
                    for d in o.deps:
                        if d[0] == "slot":
                            sem, val, key = d[1].sem, d[2], ("slot", id(d[1]))
                        else:
                            dop = d[1]
                            if dop.eng == e and e == "pe":
                                continue
                            sem, val, key = esem[dop.eng], dop.count, ("eng", dop.eng)
                        if waited.get(key, 0) >= val:
                            continue
                        waited[key] = val
                        eng.wait_ge(sem, val)
                    if o.fn is None:
                        continue
                    ins = o.fn(eng)
                    if o.dma:
                        ins.then_inc(o.slot.sem, 16)
                    elif o.milestone:
                        ins.then_inc(esem[e], 1)
                if e == "sp":
                    for s in final_waits:
                        eng.wait_ge(s.sem, s.total)

            @block.tensor
            def _(eng):
                run("pe", eng)

            @block.scalar
            def _(eng):
                run("act", eng)

            @block.vector
            def _(eng):
                run("dve", eng)

            @block.gpsimd
            def _(eng):
                run("pool", eng)

            @block.sync
            def _(eng):
                run("sp", eng)


class Ctx:
    pass


def a_tiles():
    out = []
    for d in PATTERNS:
        nt = {1: 9, 4: 3, 16: 2}[d]
        for c in range(d):
            for m in range(nt):
                out.append((d, c, m))
    return out


A_TILES = a_tiles()
A_TILE_IDX = {t: i for i, t in enumerate(A_TILES)}


def build_program(debug=(), stages="ABCDE"):
    nc = bass.Bass("TRN2", target_bir_lowering=False)
    cx = Ctx()
    cx.stages = stages
    cx.nc = nc
    cx.debug = set(debug)
    S = Sched(nc)
    cx.S = S

    def din(name, shape, dt=F32):
        return nc.dram_tensor(name, list(shape), dt, kind="ExternalInput").ap()

    def dscr(name, shape, dt=BF16):
        return nc.dram_tensor(name, list(shape), dt, kind="Internal").ap()

    def dout(name, shape, dt=F32):
        return nc.dram_tensor(name, list(shape), dt, kind="ExternalOutput").ap()

    class Lazy(dict):
        def __init__(self):
            super().__init__()
            self.specs = {}

        def __setitem__(self, k, v):
            self.specs[k] = v

        def __missing__(self, k):
            ap = self.specs[k]()
            dict.__setitem__(self, k, ap)
            return ap
    _din = din
    din = lambda name, shape, dt=F32: (lambda: _din(name, shape, dt))
    I = Lazy()
    I["xr"] = din("xr", [SEQ, D])
    I["cT"] = din("cT", [128, KC])
    I["w_ada"] = din("w_ada", [D, 6 * D])
    I["b_ada"] = din("b_ada", [1, 6 * D])
    I["n1"] = din("n1", [1, D])
    I["n2"] = din("n2", [1, D])
    I["nf"] = din("nf", [1, D])
    I["w_in"] = din("w_in", [D, 10240])
    I["lamT"] = din("lamT", [128, 4])
    I["sublnT"] = din("sublnT", [128, 2])
    I["w_out_a"] = din("w_out_a", [1024, D])
    I["w_out_b"] = din("w_out_b", [1024, D])
    I["w_o"] = din("w_o", [D, D])
    I["w_router"] = din("w_router", [D, N_EXP])
    I["b_routerB"] = din("b_routerB", [128, N_EXP])
    I["w_gate"] = din("w_gate", [N_EXP, D, D])
    I["w_up"] = din("w_up", [N_EXP, D, D])
    I["w_down"] = din("w_down", [N_EXP, D, D])
    I["b_gateT"] = din("b_gateT", [128, N_EXP * KC])
    I["b_upT"] = din("b_upT", [128, N_EXP * KC])
    I["b_down"] = din("b_down", [N_EXP, D])
    I["ident"] = din("ident", [128, 128])
    I["distB"] = din("distB", [128, 4 * 1920])
    I["stripAh"] = din("stripAh", [8, 128, 3968])
    I["kbiasA"] = din("kbiasA", [128, 24])
    I["iota_row"] = din("iota_row", [128, CAP])
    I["triu"] = din("triu", [128, 128])
    I["ecapB"] = din("ecapB", [128, 8 * N_EXP])
    cx.I = I
    O = {}
    O["out"] = dout("out", [OWN, D])
    for name, shape, dt in DEBUG_SHAPES:
        if name in cx.debug:
            O[name] = dout(name, shape, dt)
    cx.O = O
    W = {}
    W["QaT"] = dscr("QaT", [1024, OWN])
    W["KaT"] = dscr("KaT", [1024, 3072])
    W["Va"] = dscr("Va", [SEQ, 1024])
    W["QbT"] = dscr("QbT", [1024, OWN])
    W["KbT"] = dscr("KbT", [1024, SEQ])
    W["Vb"] = dscr("Vb", [SEQ, 1024])
    W["GT"] = dscr("GT", [4096, OWN])
    W["rows"] = dscr("rows", [4, D], F32)
    W["YaT"] = dscr("YaT", [1024, OWN])
    W["YbT"] = dscr("YbT", [1024, OWN])
    W["U2"] = dscr("U2", [OWN, D])
    W["MT"] = dscr("MT", [D, OWN])
    W["hs"] = dscr("hs", [OWN, D], F32)
    W["Y"] = dscr("Y", [N_EXP * CAP + 128, D])
    cx.W = W

    with contextlib.ExitStack() as st:
        cx.st = st
        sb = lambda name, shape, dt: st.enter_context(nc.sbuf_tensor(name, list(shape), dt))
        cx.banks = [st.enter_context(nc.psum_tensor(f"bank{i}", [128, 512], F32)) for i in range(8)]
        cx.ident_f = sb("ident_f", [128, 128], F32)
        cx.ident_b = sb("ident_b", [128, 128], BF16)
        cx.ones_f = sb("ones_f", [128, 128], F32)
        cx.ones_b = sb("ones_b", [128, 128], BF16)
        cx.colT = sb("colT", [128, 32], F32)
        cx.neglam = sb("neglam", [128, 1], F32)
        cx.sg = sb("sg", [128, 2], F32)
        cx.barsc = sb("barsc", [128, 8], F32)
        cx.barb = sb("barb", [128, 2], BF16)
        cx.epsc = sb("epsc", [128, 2], F32)
        S.bar_scratch = {"b": cx.barb, "one": cx.barsc[:, 0:1], "act": cx.barsc[:, 1:2],
                         "dve": cx.barsc[:, 2:3], "pool": cx.barsc[:, 3:4]}
        S.bar_ps = cx.banks[7]
        cx.slot_const = S.slot("const")
        cx.slot_out = S.slot("out")
        cx.dbg_slot = S.slot("dbg")

        cx.posm = sb("posm", [128, 8, N_EXP], F32)
        cx.prob = sb("prob", [128, 8, N_EXP], F32)
        cx.prob_hi = sb("prob_hi", [128, 8, N_EXP], BF16)
        cx.prob_lo = sb("prob_lo", [128, 8, N_EXP], BF16)
        cx.iota = sb("iota", [128, CAP], F32)
        S.dma("sp", cx.slot_const, cx.iota[:], I["iota_row"][:, :], writes=["iota"])
        stages = cx.stages
        phase0(cx)
        if "A" in stages:
            phaseA(cx)
            S.barrier()
        if "B" in stages:
            attention(cx, "B")
        if "C" in stages:
            attention(cx, "A")
        if "D" in stages:
            phaseD(cx)
        if "E" in stages:
            phaseE1(cx)
            phaseE2(cx)
        S.emit(final_waits=[cx.slot_out, cx.dbg_slot])
    global _LAST_DECLARED
    _LAST_DECLARED = list(dict.keys(I))
    return nc


DEBUG_SHAPES = [
    ("d_yA", [1024, OWN], BF16),
    ("d_mT", [D, OWN], BF16),
    ("d_ya", [1024, OWN], BF16),
    ("d_yb", [1024, OWN], BF16),
    ("d_gat", [128, 1024], BF16),
    ("d_a1", [128, 1024], F32),
    ("d_rowB", [128, D], F32),
    ("d_yB", [1024, OWN], BF16),
    ("d_h1", [OWN, D], F32),
    ("d_lg", [128, 8 * N_EXP], F32),
    ("d_posm", [128, 8 * N_EXP], F32),
    ("d_prob", [128, 8 * N_EXP], F32),
    ("d_colT", [128, 32], F32),
    ("d_mod", [1, 6 * D], F32),
    ("d_rows", [4, D], F32),
    ("d_misc", [128, 4], F32),
    ("d_uT", [128, KC * 512], BF16),
]


def phase0(cx):
    nc, S, I = cx.nc, cx.S, cx.I
    with contextlib.ExitStack() as st:
        sb = lambda name, shape, dt: st.enter_context(nc.sbuf_tensor(name, list(shape), dt))
        cT = sb("p0_cT", [128, KC], F32)
        sc = sb("p0_sc", [128, KC], F32)
        brow = sb("p0_brow", [1, 6 * D], F32)
        modrow = sb("p0_modrow", [1, 6 * D], F32)
        nrow = sb("p0_nrow", [1, 2 * D], F32)
        grow = sb("p0_grow", [1, 2 * D], F32)
        lamT = sb("p0_lamT", [128, 4], F32)
        sublnT = sb("p0_subln", [128, 2], F32)
        prod = sb("p0_prod", [128, 2], F32)
        ex = sb("p0_ex", [128, 2], F32)
        pan = [sb(f"p0_pan{i}", [128, KC, 512], F32) for i in range(2)]
        pslot = [S.slot("pan") for _ in range(2)]
        sc_slot = cx.slot_const
        bk = cx.banks

        S.dma("sp", sc_slot, cx.ident_f[:], I["ident"][:, :], writes=["ident_f"])
        S.dma("sp", sc_slot, cT[:], I["cT"][:, :], writes=["cT"])
        S.dma("sp", sc_slot, brow[:], I["b_ada"][:, :], writes=["brow"])
        S.dma("sp", sc_slot, nrow[0:1, 0:D], I["n1"][:, :], writes=["nrow"])
        S.dma("sp", sc_slot, nrow[0:1, D:2 * D], I["n2"][:, :], writes=["nrow"])
        S.dma("sp", sc_slot, lamT[:], I["lamT"][:, :], writes=["lamT"])
        S.dma("sp", sc_slot, sublnT[:], I["sublnT"][:, :], writes=["sublnT"])

        S.op("dve", lambda e: e.memset(cx.ones_f[:], 1.0), writes=["ones_f"])
        S.op("dve", lambda e: e.memset(cx.ones_b[:], 1.0), writes=["ones_b"])
        S.op("dve", lambda e: e.memset(cx.barsc[:], 1.0), writes=["barsc"])
        S.op("dve", lambda e: e.memset(cx.barb[:], 1.0), writes=["barb"])
        S.op("dve", lambda e: e.memset(cx.epsc[:], EPS), writes=["epsc"])
        S.op("dve", lambda e: e.tensor_copy(out=cx.ident_b[:], in_=cx.ident_f[:]), reads=["ident_f"], writes=["ident_b"])
        S.op("act", lambda e: e.activation(out=sc[:], in_=cT[:], func=AF.Silu), reads=["cT"], writes=["sc"])

        wv = I["w_ada"].rearrange("(k p) n -> p k n", p=128)
        NP = 6 * D // 512
        for pn in range(NP):
            pb = pan[pn % 2]
            for j in range(4):
                q = "sp" if j % 2 == 0 else "act"
                S.dma(q, pslot[pn % 2], pb[:, 4 * j:4 * j + 4, :], wv[:, 4 * j:4 * j + 4, pn * 512:(pn + 1) * 512],
                      writes=[("pan", pn % 2)])
            ps = bk[pn % 2]
            for k in range(KC):
                S.op("pe", lambda e, ps=ps, pb=pb, k=k: e.matmul(ps[0:1, :], sc[:, k:k + 1], pb[:, k, :],
                                                                 start=(k == 0), stop=(k == KC - 1)),
                     reads=["sc", ("pan", pn % 2)], writes=[("bank", pn % 2)])
            S.op("dve", lambda e, ps=ps, pn=pn: e.tensor_tensor(out=modrow[0:1, pn * 512:(pn + 1) * 512], in0=ps[0:1, :],
                                                                 in1=brow[0:1, pn * 512:(pn + 1) * 512], op=ALU.add),
                 reads=[("bank", pn % 2), "brow"], writes=["modrow"])
        mr = lambda m: modrow[0:1, m * D:(m + 1) * D]
        S.op("dve", lambda e: e.scalar_tensor_tensor(out=grow[0:1, 0:D], in0=mr(1), scalar=1.0, in1=nrow[0:1, 0:D],
                                                     op0=ALU.add, op1=ALU.mult),
             reads=["modrow", "nrow"], writes=["grow"])
        S.op("dve", lambda e: e.scalar_tensor_tensor(out=grow[0:1, D:2 * D], in0=mr(4), scalar=1.0, in1=nrow[0:1, D:2 * D],
                                                     op0=ALU.add, op1=ALU.mult),
             reads=["modrow", "nrow"], writes=["grow"])
        psc = bk[2]
        for k in range(KC):
            S.op("pe", lambda e, k=k: e.matmul(psc[:, k:k + 1], grow[0:1, k * 128:(k + 1) * 128], cx.ones_f[0:1, 0:1],
                                               start=True, stop=True),
                 reads=["grow", "ones_f"], writes=[("bank", 2)])
        for k in range(KC):
            S.op("pe", lambda e, k=k: e.matmul(psc[:, KC + k:KC + k + 1], modrow[0:1, k * 128:(k + 1) * 128],
                                               cx.ones_f[0:1, 0:1], start=True, stop=True),
                 reads=["modrow", "ones_f"], writes=[("bank", 2)])
        S.op("dve", lambda e: e.tensor_copy(out=cx.colT[:], in_=psc[:, 0:32]), reads=[("bank", 2)], writes=["colT"])
        rs = cx.slot_const
        S.dma("sp", rs, cx.W["rows"][0:1, :], mr(2), reads=["modrow"], writes=["rows_d"])
        S.dma("sp", rs, cx.W["rows"][1:2, :], grow[0:1, D:2 * D], reads=["grow"], writes=["rows_d"])
        S.dma("sp", rs, cx.W["rows"][2:3, :], mr(3), reads=["modrow"], writes=["rows_d"])
        S.dma("sp", rs, cx.W["rows"][3:4, :], mr(5), reads=["modrow"], writes=["rows_d"])
        S.op("dve", lambda e: e.tensor_tensor(out=prod[:, 0:1], in0=lamT[:, 0:1], in1=lamT[:, 1:2], op=ALU.mult),
             reads=["lamT"], writes=["prod"])
        S.op("dve", lambda e: e.tensor_tensor(out=prod[:, 1:2], in0=lamT[:, 2:3], in1=lamT[:, 3:4], op=ALU.mult),
             reads=["lamT", "prod"], writes=["prod"])
        S.op("pe", lambda e: e.matmul(bk[3][:, 0:2], cx.ones_f[:], prod[:], start=True, stop=True),
             reads=["ones_f", "prod"], writes=[("bank", 3)])
        S.op("act", lambda e: e.activation(out=ex[:], in_=bk[3][:, 0:2], func=AF.Exp), reads=[("bank", 3)], writes=["ex"])
        S.op("dve", lambda e: e.tensor_tensor(out=cx.neglam[:], in0=ex[:, 1:2], in1=ex[:, 0:1], op=ALU.subtract),
             reads=["ex"], writes=["neglam"])
        S.op("dve", lambda e: e.tensor_scalar(out=cx.neglam[:], in0=cx.neglam[:], scalar1=-LAMBDA_INIT, scalar2=None,
                                              op0=ALU.add),
             reads=["neglam"], writes=["neglam"])
        S.op("dve", lambda e: e.tensor_scalar(out=cx.sg[:], in0=sublnT[:], scalar1=(1.0 - LAMBDA_INIT), scalar2=None,
                                              op0=ALU.mult),
             reads=["sublnT"], writes=["sg"])
        if "d_mod" in cx.debug:
            S.dma("sp", cx.dbg_slot, cx.O["d_mod"][:, :], modrow[:], reads=["modrow"])
        if "d_colT" in cx.debug:
            S.dma("sp", cx.dbg_slot, cx.O["d_colT"][:, :], cx.colT[:], reads=["colT"])
            S.dma("sp", cx.dbg_slot, cx.O["d_misc"][:, 0:1], cx.neglam[:], reads=["neglam"], allow_slow_non_contiguous=True)
            S.dma("sp", cx.dbg_slot, cx.O["d_misc"][:, 1:3], cx.sg[:], reads=["sg"], allow_slow_non_contiguous=True)
        S.barrier()


def evac_copy(S, i, out, in_, reads, writes, func=None):
    if func is not None or i % 2 == 0:
        f = func if func is not None else AF.Copy
        return S.op("act", lambda e: e.activation(out=out, in_=in_, func=f), reads=reads, writes=writes)
    return S.op("dve", lambda e: e.tensor_copy(out=out, in_=in_), reads=reads, writes=writes)


def phaseA(cx):
    nc, S, I, W = cx.nc, cx.S, cx.I, cx.W
    bk = cx.banks
    with contextlib.ExitStack() as st:
        sb = lambda name, shape, dt: st.enter_context(nc.sbuf_tensor(name, list(shape), dt))
        uT = sb("A_uT", [128, KC, SEQ], BF16)
        with contextlib.ExitStack() as st1:
            sb1 = lambda name, shape, dt: st1.enter_context(nc.sbuf_tensor(name, list(shape), dt))
            xt = [sb1(f"A_x{i}", [128, D], F32) for i in range(3)]
            xn = [sb1(f"A_xn{i}", [128, D], BF16) for i in range(2)]
            junk = sb1("A_junk", [128, D], BF16)
            ss = sb1("A_ss", [128, 4], F32)
            rt = sb1("A_rt", [128, 4], F32)
            rs = sb1("A_rs", [128, 4], F32)
            xslot = [S.slot("x") for _ in range(3)]
            xv = I["xr"].rearrange("(t p) d -> t p d", p=128)
            NTT = SEQ // 128
            for tt in range(NTT):
                xb = xt[tt % 3]
                S.dma("sp", xslot[tt % 3], xb[:, 0:1024], xv[tt, :, 0:1024], writes=[("x", tt % 3)])
                S.dma("act", xslot[tt % 3], xb[:, 1024:2048], xv[tt, :, 1024:2048], writes=[("x", tt % 3)])
                j = tt % 4
                S.op("act", lambda e, xb=xb, j=j: e.activation(out=junk[:], in_=xb[:], func=AF.Square,
                                                              accum_out=ss[:, j:j + 1]),
                     reads=[("x", tt % 3)], writes=["junk", ("ss", j)])
                S.op("act", lambda e, j=j: e.activation(out=rt[:, j:j + 1], in_=ss[:, j:j + 1], func=AF.Sqrt,
                                                        scale=1.0 / D, bias=cx.epsc[:, 0:1]),
                     reads=[("ss", j)], writes=[("rt", j)])
                S.op("dve", lambda e, j=j: e.reciprocal(out=rs[:, j:j + 1], in_=rt[:, j:j + 1]),
                     reads=[("rt", j)], writes=[("rs", j)])
                xnb = xn[tt % 2]
                S.op("act", lambda e, xb=xb, xnb=xnb, j=j: e.activation(out=xnb[:], in_=xb[:], func=AF.Copy,
                                                                       scale=rs[:, j:j + 1]),
                     reads=[("x", tt % 3), ("rs", j)], writes=[("xn", tt % 2)])
                for half in range(2):
                    bi = (2 * tt + half) % 4
                    pst = bk[bi][:].bitcast(BF16)
                    for q in range(8):
                        k = half * 8 + q
                        S.op("pe", lambda e, pst=pst, q=q, k=k, xnb=xnb: e.transpose(
                            pst[:, q * 128:(q + 1) * 128], xnb[:, k * 128:(k + 1) * 128], cx.ident_b[:]),
                            reads=[("xn", tt % 2), "ident_b"], writes=[("bank", bi)])
                    for q in range(8):
                        k = half * 8 + q
                        S.op("dve", lambda e, pst=pst, q=q, k=k, tt=tt: e.tensor_scalar(
                            out=uT[:, k, tt * 128:(tt + 1) * 128], in0=pst[:, q * 128:(q + 1) * 128],
                            scalar1=cx.colT[:, k:k + 1], scalar2=cx.colT[:, KC + k:KC + k + 1],
                            op0=ALU.mult, op1=ALU.add),
                            reads=[("bank", bi), "colT"], writes=[("uT", tt // 4)])
        if "d_uT" in cx.debug:
            for k in range(KC):
                S.dma("sp", cx.dbg_slot, cx.O["d_uT"][:, k * 512:(k + 1) * 512], uT[:, k, 1024:1536],
                      reads=[("uT", c) for c in range(8)])
        S.barrier()
        wch = [sb(f"A_w{i}", [128, KC, 512], BF16) for i in range(2)]
        stg = [sb(f"A_stg{i}", [128, 512], BF16) for i in range(4)]
        wslot = [S.slot("w") for _ in range(2)]
        sslot = [S.slot("stg") for _ in range(4)]
        wv = I["w_in"].rearrange("(k p) n -> p k n", p=128)
        groups = []
        for g in range(2):
            groups.append(("qa", "f", [2, 3], W["QaT"], g, 1024))
        for g in range(2):
            groups.append(("ka", "f", list(range(6)), W["KaT"], g, 0))
        for g in range(2):
            groups.append(("va", "t", list(range(6)), W["Va"], g, 0))
        for g in range(2):
            groups.append(("qb", "f", [2, 3], W["QbT"], g, 1024))
        for g in range(2):
            groups.append(("kb", "f", list(range(8)), W["KbT"], g, 0))
        for g in range(2):
            groups.append(("vb", "t", list(range(8)), W["Vb"], g, 0))
        for g in range(8):
            groups.append(("gt", "f", [2, 3], W["GT"], g, 1024))
        ev = 0
        for cg, (name, kind, tcs, dst, g, tok0) in enumerate(groups):
            wb = wch[cg % 2]
            for j in range(4):
                S.dma("pool", wslot[cg % 2], wb[:, 4 * j:4 * j + 4, :], wv[:, 4 * j:4 * j + 4, cg * 512:(cg + 1) * 512],
                      writes=[("w", cg % 2)])
            func = AF.Sigmoid if name == "gt" else None
            if kind == "f":
                for s4 in range(4):
                    for tc in tcs:
                        bi = ev % 4
                        ps = bk[bi]
                        for k in range(KC):
                            S.op("pe", lambda e, ps=ps, wb=wb, k=k, s4=s4, tc=tc: e.matmul(
                                ps[:, :], wb[:, k, s4 * 128:(s4 + 1) * 128], uT[:, k, tc * 512:(tc + 1) * 512],
                                start=(k == 0), stop=(k == KC - 1)),
                                reads=[("w", cg % 2), ("uT", tc)], writes=[("bank", bi)])
                        sg = stg[ev % 4]
                        evac_copy(S, ev, sg[:], ps[:, :], [("bank", bi)], [("stg", ev % 4)], func=func)
                        row0 = g * 512 + s4 * 128
                        col0 = tc * 512 - tok0
                        S.dma("sp", sslot[ev % 4], dst[row0:row0 + 128, col0:col0 + 512], sg[:],
                              reads=[("stg", ev % 4)])
                        ev += 1
            else:
                for tc in tcs:
                    for t4 in range(4):
                        tt = tc * 4 + t4
                        bi = ev % 4
                        ps = bk[bi]
                        for k in range(KC):
                            S.op("pe", lambda e, ps=ps, wb=wb, k=k, tt=tt: e.matmul(
                                ps[:, :], uT[:, k, tt * 128:(tt + 1) * 128], wb[:, k, :],
                                start=(k == 0), stop=(k == KC - 1)),
                                reads=[("w", cg % 2), ("uT", tc)], writes=[("bank", bi)])
                        sg = stg[ev % 4]
                        evac_copy(S, ev, sg[:], ps[:, :], [("bank", bi)], [("stg", ev % 4)])
                        S.dma("sp", sslot[ev % 4], dst[tt * 128:(tt + 1) * 128, g * 512:(g + 1) * 512], sg[:],
                              reads=[("stg", ev % 4)])
                        ev += 1


def attention(cx, mixer):
    nc, S, I, W = cx.nc, cx.S, cx.I, cx.W
    bk = cx.banks
    isB = mixer == "B"
    nheads = 4 if isB else 8
    nkt = 32 if isB else 24
    ncomp = 2 if isB else 1
    nv = 2 if isB else 1
    vw = 1024
    with contextlib.ExitStack() as st:
        sb = lambda name, shape, dt: st.enter_context(nc.sbuf_tensor(name, list(shape), dt))
        if isB:
            vbuf = [sb(f"B_v{i}", [128, nkt, 256], BF16) for i in range(2)]
        else:
            v_all = sb("A_v", [128, nkt, vw], BF16)
        yT = sb(mixer + "_yT", [128, 8, OWN], BF16)
        ktb = [sb(f"{mixer}_kt{i}", [128, ncomp, nkt * 128], BF16) for i in range(2)]
        qtb = [sb(f"{mixer}_qt{i}", [128, ncomp, OWN], BF16) for i in range(2)]
        if isB:
            strip = sb("B_strip", [128, 4 * 1920], F32)
            stripb = [strip, strip]
        else:
            stripb = [sb(f"A_strip{i}", [128, 3968], F32) for i in range(2)]
            kbias = sb("A_kbias", [128, 24], F32)
        NS = 5 if isB else 6
        LA = 3
        NB = 6
        tmpb = [sb(f"{mixer}_tmp{i}", [128, 512], F32) for i in range(NB)]
        eb = [sb(f"{mixer}_e{i}", [128, 512], BF16) for i in range(NB)]
        rcp = [sb(f"{mixer}_rcp{i}", [128, 512], F32) for i in range(2)]
        if isB:
            onb = [sb(f"B_on{i}", [128, 2, 2, 512], F32) for i in range(2)]
            diff = sb("B_diff", [128, 2, 512], F32)
            sq = sb("B_sq", [128, 2, 512], BF16)
            rt = sb("B_rt", [128, 512], F32)
            rr = sb("B_rr", [128, 512], F32)
        vslot = S.slot("v")
        hslot = [S.slot("hd") for _ in range(2)]
        vsrc = (W["Vb"] if isB else W["Va"])[0:nkt * 128, :].rearrange("(t p) c -> p t c", p=128)
        if not isB:
            for j in range(0, nkt, 4):
                S.dma("sp" if (j // 4) % 2 == 0 else "act", vslot, v_all[:, j:j + 4, :], vsrc[:, j:j + 4, :], writes=["v_all"])
        if isB:
            for j in range(4):
                S.dma("sp", vslot, strip[:, j * 1920:(j + 1) * 1920], I["distB"][:, j * 1920:(j + 1) * 1920], writes=["strip"])
        else:
            S.dma("sp", vslot, kbias[:], I["kbiasA"][:, :], writes=["kbias"])
        KT = W["KbT"] if isB else W["KaT"]
        QT = W["QbT"] if isB else W["QaT"]
        it = 0
        accset = 0
        for h in range(nheads):
            par = h % 2
            for c in range(ncomp):
                row = (h * ncomp + c) * 128
                for j in range(0, nkt * 128, 1024):
                    S.dma("sp" if c == 0 else "act", hslot[par], ktb[par][:, c, j:j + 1024], KT[row:row + 128, j:j + 1024],
                          writes=[("hd", par)])
                S.dma("act", hslot[par], qtb[par][:, c, :], QT[row:row + 128, :], writes=[("hd", par)])
            if not isB:
                S.dma("sp", hslot[par], stripb[par][:], I["stripAh"][h], writes=[("hd", par)])
            else:
                for j in range(0, nkt, 8):
                    S.dma("sp", hslot[par], vbuf[par][:, j:j + 8, :], vsrc[:, j:j + 8, h * 256:(h + 1) * 256],
                          writes=[("hd", par)])
            hk = [("hd", par), "v_all", "strip", "kbias"]
            slope = SLOPES_B[h] if isB else SLOPES_A[h]
            for qc in range(2):
                for c in range(ncomp):
                    abase = NS
                    O = [bk[abase + v] for v in range(nv)]
                    Dn = bk[abase + nv]
                    akeys = [("bank", abase + v) for v in range(nv)] + [("bank", abase + nv)]
                    if isB:
                        kts = list(range(nkt))
                    else:
                        kts = [kt for kt in range(nkt)
                               if not (128 * kt - 1024 - 512 * qc - 511 > 1024 or 128 * kt + 127 - 1024 - 512 * qc < -1024)]
                    pend = []

                    def emit_pv(first, last, kt, ebb, ei, O=O, Dn=Dn, akeys=akeys, par=par, h=h):
                        for v in range(nv):
                            col = (v * 128) if isB else (h * 128)
                            vt = vbuf[par] if isB else v_all
                            S.op("pe", lambda e, v=v, kt=kt, col=col, ebb=ebb, first=first, last=last, vt=vt: e.matmul(
                                O[v][:, :], vt[:, kt, col:col + 128], ebb[:], start=first, stop=last),
                                reads=[("e", ei), ("hd", par), "v_all"], writes=[akeys[v]])
                        S.op("pe", lambda e, ebb=ebb, first=first, last=last: e.matmul(
                            Dn[:, :], cx.ones_b[:], ebb[:], start=first, stop=last),
                            reads=[("e", ei), "ones_b"], writes=[akeys[-1]])

                    for ki, kt in enumerate(kts):
                        sbk = it % NS
                        ps = bk[sbk]
                        S.op("pe", lambda e, ps=ps, par=par, c=c, kt=kt, qc=qc: e.matmul(
                            ps[:, :], ktb[par][:, c, kt * 128:(kt + 1) * 128], qtb[par][:, c, qc * 512:(qc + 1) * 512],
                            start=True, stop=True), reads=hk, writes=[("bank", sbk)])
                        tb = tmpb[it % NB]
                        if isB:
                            g, ktp = kt // 8, kt % 8
                            x0 = g * 1920 + 512 * qc - 128 * ktp + 896
                            c1 = -SCALE / slope
                        else:
                            x0 = 512 * qc - 128 * kt + 2944
                            c1 = SCALE
                        sp_ = stripb[par]
                        S.op("dve", lambda e, tb=tb, ps=ps, sp_=sp_, x0=x0, c1=c1: e.scalar_tensor_tensor(
                            out=tb[:], in0=ps[:, :], scalar=c1, in1=sp_[:, x0:x0 + 512], op0=ALU.mult, op1=ALU.add),
                            reads=[("bank", sbk)] + hk, writes=[("tmp", it % NB)])
                        ebb = eb[it % NB]
                        if isB:
                            S.op("act", lambda e, ebb=ebb, tb=tb, slope=slope: e.activation(
                                out=ebb[:], in_=tb[:], func=AF.Exp, scale=-slope),
                                reads=[("tmp", it % NB)], writes=[("e", it % NB)])
                        else:
                            S.op("act", lambda e, ebb=ebb, tb=tb, kt=kt: e.activation(
                                out=ebb[:], in_=tb[:], func=AF.Exp, bias=kbias[:, kt:kt + 1]),
                                reads=[("tmp", it % NB)] + hk, writes=[("e", it % NB)])
                        first, last = (ki == 0), (ki == len(kts) - 1)
                        pend.append((first, last, kt, ebb, it % NB))
                        it += 1
                        if len(pend) > LA:
                            emit_pv(*pend.pop(0))
                    while pend:
                        emit_pv(*pend.pop(0))
                    rc = rcp[accset]
                    S.op("dve", lambda e, rc=rc, Dn=Dn: e.reciprocal(out=rc[:], in_=Dn[:, :]),
                         reads=[akeys[-1]], writes=[("rcp", accset)])
                    for v in range(nv):
                        if isB:
                            dst = onb[qc % 2][:, c, v, :]
                            wk = ("on", qc % 2, c)
                        else:
                            dst = yT[:, h, qc * 512:(qc + 1) * 512]
                            wk = ("yT", mixer)
                        S.op("dve", lambda e, dst=dst, O=O, v=v, rc=rc: e.tensor_tensor(
                            out=dst, in0=O[v][:, :], in1=rc[:], op=ALU.mult),
                            reads=[akeys[v], ("rcp", accset)], writes=[wk])
                    accset ^= 1
                if isB:
                    on = onb[qc % 2]
                    ok = [("on", qc % 2, 0), ("on", qc % 2, 1)]
                    for v in range(2):
                        S.op("dve", lambda e, on=on, v=v: e.scalar_tensor_tensor(
                            out=diff[:, v, :], in0=on[:, 1, v, :], scalar=cx.neglam[:, 0:1], in1=on[:, 0, v, :],
                            op0=ALU.mult, op1=ALU.add), reads=ok + ["neglam"], writes=["diff"])
                    S.op("act", lambda e: e.activation(out=sq[:], in_=diff[:], func=AF.Square), reads=["diff"], writes=["sq"])
                    sbk = it % NS
                    it += 1
                    ps = bk[sbk]
                    for v in range(2):
                        S.op("pe", lambda e, ps=ps, v=v: e.matmul(ps[:, :], cx.ones_b[:], sq[:, v, :], start=(v == 0), stop=(v == 1)),
                             reads=["sq", "ones_b"], writes=[("bank", sbk)])
                    S.op("act", lambda e, ps=ps: e.activation(out=rt[:], in_=ps[:, :], func=AF.Sqrt, scale=1.0 / 256,
                                                             bias=cx.epsc[:, 0:1]),
                         reads=[("bank", sbk), "epsc"], writes=["rt"])
                    S.op("dve", lambda e: e.reciprocal(out=rr[:], in_=rt[:]), reads=["rt"], writes=["rr"])
                    for v in range(2):
                        S.op("dve", lambda e, v=v, h=h, qc=qc: e.scalar_tensor_tensor(
                            out=yT[:, 2 * h + v, qc * 512:(qc + 1) * 512], in0=diff[:, v, :], scalar=cx.sg[:, v:v + 1],
                            in1=rr[:], op0=ALU.mult, op1=ALU.mult), reads=["diff", "rr", "sg"], writes=[("yT", mixer)])
        ydst = W["YbT"] if isB else W["YaT"]
        for j in range(8):
            S.dma("sp", vslot, ydst[j * 128:(j + 1) * 128, :], yT[:, j, :], reads=[("yT", mixer)])
        if ("d_y" + mixer) in cx.debug:
            for j in range(8):
                S.dma("sp", cx.dbg_slot, cx.O["d_y" + mixer][j * 128:(j + 1) * 128, :], yT[:, j, :], reads=[("yT", mixer)])
    S.barrier()


def phaseD(cx):
    nc, S, I, W = cx.nc, cx.S, cx.I, cx.W
    bk = cx.banks
    with contextlib.ExitStack() as st:
        sb = lambda name, shape, dt: st.enter_context(nc.sbuf_tensor(name, list(shape), dt))
        with contextlib.ExitStack() as st1:
            sb1 = lambda name, shape, dt: st1.enter_context(nc.sbuf_tensor(name, list(shape), dt))
            mT = sb1("D_mT", [128, KC, OWN], BF16)
            ya = sb1("D_ya", [128, 8, OWN], BF16)
            yb = sb1("D_yb", [128, 8, OWN], BF16)
            woa = sb1("D_woa", [128, 8, D], BF16)
            wob = sb1("D_wob", [128, 8, D], BF16)
            gat = [sb1(f"D_g{i}", [128, 2, 512], BF16) for i in range(2)]
            t1 = [sb1(f"D_t1{i}", [128, 512], F32) for i in range(2)]
            t2 = [sb1(f"D_t2{i}", [128, 512], F32) for i in range(2)]
            ls = S.slot("D1")
            gs = [S.slot("D1g") for _ in range(2)]
            for j in range(8):
                S.dma("sp", ls, ya[:, j, :], W["YaT"][j * 128:(j + 1) * 128, :], writes=["ya"])
                S.dma("act", ls, yb[:, j, :], W["YbT"][j * 128:(j + 1) * 128, :], writes=["yb"])
            wav = I["w_out_a"].rearrange("(k p) n -> p k n", p=128)
            wbv = I["w_out_b"].rearrange("(k p) n -> p k n", p=128)
            for j in range(0, 8, 2):
                S.dma("pool", ls, woa[:, j:j + 2, :], wav[:, j:j + 2, :], writes=["woa"])
                S.dma("pool", ls, wob[:, j:j + 2, :], wbv[:, j:j + 2, :], writes=["wob"])
            if "d_ya" in cx.debug:
                for j in range(8):
                    S.dma("sp", cx.dbg_slot, cx.O["d_ya"][j * 128:(j + 1) * 128, :], ya[:, j, :], reads=["ya"])
                    S.dma("sp", cx.dbg_slot, cx.O["d_yb"][j * 128:(j + 1) * 128, :], yb[:, j, :], reads=["yb"])
            it = 0
            for dc in range(KC):
                for qc in range(2):
                    gb_ = gat[it % 2]
                    S.dma("sp", gs[it % 2], gb_[:, 0, :], W["GT"][dc * 128:(dc + 1) * 128, qc * 512:(qc + 1) * 512],
                          writes=[("gat", it % 2)])
                    S.dma("sp", gs[it % 2], gb_[:, 1, :], W["GT"][2048 + dc * 128:2048 + (dc + 1) * 128, qc * 512:(qc + 1) * 512],
                          writes=[("gat", it % 2)])
                    pa, pb = bk[(2 * it) % 4], bk[(2 * it + 1) % 4]
                    for f in range(8):
                        S.op("pe", lambda e, pa=pa, f=f, dc=dc, qc=qc: e.matmul(
                            pa[:, :], woa[:, f, dc * 128:(dc + 1) * 128], ya[:, f, qc * 512:(qc + 1) * 512],
                            start=(f == 0), stop=(f == 7)), reads=["woa", "ya"], writes=[("bank", (2 * it) % 4)])
                    for f in range(8):
                        S.op("pe", lambda e, pb=pb, f=f, dc=dc, qc=qc: e.matmul(
                            pb[:, :], wob[:, f, dc * 128:(dc + 1) * 128], yb[:, f, qc * 512:(qc + 1) * 512],
                            start=(f == 0), stop=(f == 7)), reads=["wob", "yb"], writes=[("bank", (2 * it + 1) % 4)])
                    a1, a2 = t1[it % 2], t2[it % 2]
                    S.op("dve", lambda e, a1=a1, pa=pa, gb_=gb_: e.tensor_tensor(out=a1[:], in0=pa[:, :], in1=gb_[:, 0, :], op=ALU.mult),
                         reads=[("bank", (2 * it) % 4), ("gat", it % 2)], writes=[("t1", it % 2)])
                    S.op("dve", lambda e, a2=a2, pb=pb, gb_=gb_: e.tensor_tensor(out=a2[:], in0=pb[:, :], in1=gb_[:, 1, :], op=ALU.mult),
                         reads=[("bank", (2 * it + 1) % 4), ("gat", it % 2)], writes=[("t2", it % 2)])
                    if "d_gat" in cx.debug and it == 0:
                        S.dma("sp", cx.dbg_slot, cx.O["d_gat"][:, :], gb_[:].rearrange("p a b -> p (a b)"), reads=[("gat", 0)])
                        S.dma("sp", cx.dbg_slot, cx.O["d_a1"][:, 0:512], a1[:], reads=[("t1", 0)])
                        S.dma("sp", cx.dbg_slot, cx.O["d_a1"][:, 512:1024], a2[:], reads=[("t2", 0)])
                    S.op("dve", lambda e, a1=a1, a2=a2, dc=dc, qc=qc: e.tensor_tensor(
                        out=mT[:, dc, qc * 512:(qc + 1) * 512], in0=a1[:], in1=a2[:], op=ALU.add),
                        reads=[("t1", it % 2), ("t2", it % 2)], writes=["mT"])
                    it += 1
            for k in range(KC):
                S.dma("sp", ls, W["MT"][k * 128:(k + 1) * 128, :], mT[:, k, :], reads=["mT"])
                if "d_mT" in cx.debug:
                    S.dma("sp", cx.dbg_slot, cx.O["d_mT"][k * 128:(k + 1) * 128, :], mT[:, k, :], reads=["mT"])
        S.barrier()
        h = sb("D_h", [128, 8, D], F32)
        rowB = sb("D_rowB", [128, D], F32)
        rowC = sb("D_rowC", [128, D], F32)
        st2 = contextlib.ExitStack()
        sb2 = lambda name, shape, dt: st2.enter_context(nc.sbuf_tensor(name, list(shape), dt))
        mT2 = sb2("D_mT2", [128, KC, OWN], BF16)
        wo = sb2("D_wo", [128, KC, D], BF16)
        tq = [sb2(f"D_tq{i}", [128, 512], F32) for i in range(2)]
        ls = S.slot("D2")
        for k in range(KC):
            S.dma("act", ls, mT2[:, k, :], W["MT"][k * 128:(k + 1) * 128, :], writes=["mT2"])
        wov = I["w_o"].rearrange("(k p) n -> p k n", p=128)
        for j in range(0, KC, 2):
            S.dma("pool", ls, wo[:, j:j + 2, :], wov[:, j:j + 2, :], writes=["wo"])
        xo = I["xr"][1024:2048, :].rearrange("(t p) d -> p t d", p=128)
        for tt in range(8):
            S.dma("sp" if tt % 2 == 0 else "act", ls, h[:, tt, :], xo[:, tt, :], writes=[("h", tt)])
        S.dma("sp", ls, rowB[:], W["rows"][0:1, :].partition_broadcast(128), writes=["rowB"])
        S.dma("sp", ls, rowC[:], W["rows"][2:3, :].partition_broadcast(128), writes=["rowC"])
        it = 0
        for tt in range(8):
            for ch in range(4):
                ps = bk[it % 4]
                for k in range(KC):
                    S.op("pe", lambda e, ps=ps, k=k, tt=tt, ch=ch: e.matmul(
                        ps[:, :], mT2[:, k, tt * 128:(tt + 1) * 128], wo[:, k, ch * 512:(ch + 1) * 512],
                        start=(k == 0), stop=(k == KC - 1)), reads=["wo", "mT2"], writes=[("bank", it % 4)])
                tb = tq[it % 2]
                S.op("dve", lambda e, tb=tb, ps=ps, ch=ch: e.tensor_tensor(
                    out=tb[:], in0=ps[:, :], in1=rowB[:, ch * 512:(ch + 1) * 512], op=ALU.mult),
                    reads=[("bank", it % 4), "rowB"], writes=[("tq", it % 2)])
                S.op("pool", lambda e, tb=tb, tt=tt, ch=ch: e.tensor_tensor(
                    out=h[:, tt, ch * 512:(ch + 1) * 512], in0=h[:, tt, ch * 512:(ch + 1) * 512], in1=tb[:], op=ALU.add),
                    reads=[("tq", it % 2), ("h", tt)], writes=[("h", tt)])
                it += 1
        if "d_rowB" in cx.debug:
            S.dma("sp", cx.dbg_slot, cx.O["d_rowB"][:, :], rowB[:], reads=["rowB"])
        hs = S.slot("hs")
        for tt in range(8):
            S.dma("sp", hs, W["hs"][tt * 128:(tt + 1) * 128, :], h[:, tt, :], reads=[("h", tt)])
            if "d_h1" in cx.debug:
                S.dma("sp", cx.dbg_slot, cx.O["d_h1"][tt * 128:(tt + 1) * 128, :], h[:, tt, :], reads=[("h", tt)])
        S.barrier()
        st2.close()
        S.dma("sp", ls, rowB[:], W["rows"][1:2, :].partition_broadcast(128), reads=[], writes=["rowB"])
        wr = sb("D_wr", [128, KC, N_EXP], F32)
        brB = sb("D_brB", [128, N_EXP], F32)
        triu = sb("D_triu", [128, 128], BF16)
        triu_f = sb("D_triuf", [128, 128], F32)
        S.dma("sp", ls, wr[:], I["w_router"].rearrange("(k p) n -> p k n", p=128), writes=["wr"])
        S.dma("sp", ls, brB[:], I["b_routerB"][:, :], writes=["brB"])
        S.dma("sp", ls, triu_f[:], I["triu"][:, :], writes=["triu_f"])
        S.op("dve", lambda e: e.tensor_copy(out=triu[:], in_=triu_f[:]), reads=["triu_f"], writes=["triu"])
        u2f = [sb(f"D_u2f{i}", [128, D], F32) for i in range(2)]
        u2b = [sb(f"D_u2b{i}", [128, D], BF16) for i in range(2)]
        u2T = [sb(f"D_u2T{i}", [128, KC, 128], F32) for i in range(2)]
        ss = sb("D_ss", [128, 8], F32)
        rt = sb("D_rt", [128, 8], F32)
        rs = sb("D_rs", [128, 8], F32)
        lg = sb("D_lg", [128, 8, N_EXP], F32)
        mx8 = sb("D_mx8", [128, 8, 8], F32)
        negm = sb("D_negm", [128, 8], F32)
        mask = sb("D_mask", [128, 8, N_EXP], F32)
        mask_b = sb("D_maskb", [128, 8, N_EXP], BF16)
        ex = sb("D_ex", [128, 8, N_EXP], F32)
        sm = sb("D_sm", [128, 8], F32)
        rsm = sb("D_rsm", [128, 8], F32)
        us = [S.slot("u2") for _ in range(2)]
        pit = 0
        for tt in range(8):
            uf, ub, uT_ = u2f[tt % 2], u2b[tt % 2], u2T[tt % 2]
            S.op("act", lambda e, ub=ub, tt=tt: e.activation(out=ub[:], in_=h[:, tt, :], func=AF.Square, accum_out=ss[:, tt:tt + 1]),
                 reads=[("h", tt)], writes=[("u2b", tt % 2), ("ss", tt)])
            S.op("act", lambda e, tt=tt: e.activation(out=rt[:, tt:tt + 1], in_=ss[:, tt:tt + 1], func=AF.Sqrt, scale=1.0 / D,
                                                      bias=cx.epsc[:, 0:1]), reads=[("ss", tt), "epsc"], writes=[("rt", tt)])
            S.op("dve", lambda e, tt=tt: e.reciprocal(out=rs[:, tt:tt + 1], in_=rt[:, tt:tt + 1]), reads=[("rt", tt)], writes=[("rs", tt)])
            S.op("dve", lambda e, uf=uf, tt=tt: e.scalar_tensor_tensor(out=uf[:], in0=h[:, tt, :], scalar=rs[:, tt:tt + 1], in1=rowB[:],
                                                                       op0=ALU.mult, op1=ALU.mult),
                 reads=[("h", tt), ("rs", tt), "rowB"], writes=[("u2f", tt % 2)])
            S.op("pool", lambda e, uf=uf: e.tensor_tensor(out=uf[:], in0=uf[:], in1=rowC[:], op=ALU.add),
                 reads=[("u2f", tt % 2), "rowC"], writes=[("u2f", tt % 2)])
            S.op("act", lambda e, ub=ub, uf=uf: e.activation(out=ub[:], in_=uf[:], func=AF.Copy),
                 reads=[("u2f", tt % 2)], writes=[("u2b", tt % 2)])
            S.dma("sp", us[tt % 2], W["U2"][tt * 128:(tt + 1) * 128, :], ub[:], reads=[("u2b", tt % 2)])
            for q4 in range(4):
                bi = pit % 4
                pit += 1
                ps = bk[bi]
                for q in range(4):
                    k = q4 * 4 + q
                    S.op("pe", lambda e, ps=ps, q=q, k=k, uf=uf: e.transpose(ps[:, q * 128:(q + 1) * 128], uf[:, k * 128:(k + 1) * 128],
                                                                              cx.ident_f[:]),
                         reads=[("u2f", tt % 2), "ident_f"], writes=[("bank", bi)])
                evac_copy(S, q4, uT_[:, q4 * 4:(q4 + 1) * 4, :], ps[:, :].rearrange("p (a b) -> p a b", a=4), [("bank", bi)], [("u2T", tt % 2)])
            psl = bk[4 + tt % 2]
            for k in range(KC):
                S.op("pe", lambda e, psl=psl, k=k, uT_=uT_: e.matmul(psl[:, 0:N_EXP], uT_[:, k, :], wr[:, k, :],
                                                                     start=(k == 0), stop=(k == KC - 1)),
                     reads=[("u2T", tt % 2), "wr"], writes=[("bank", 4 + tt % 2)])
            S.op("dve", lambda e, psl=psl, tt=tt: e.tensor_tensor(out=lg[:, tt, :], in0=psl[:, 0:N_EXP], in1=brB[:], op=ALU.add),
                 reads=[("bank", 4 + tt % 2), "brB"], writes=[("lg", tt)])
            S.op("dve", lambda e, tt=tt: e.max(out=mx8[:, tt, :], in_=lg[:, tt, :]), reads=[("lg", tt)], writes=[("mx8", tt)])
            S.op("dve", lambda e, tt=tt: e.tensor_scalar(out=mask[:, tt, :], in0=lg[:, tt, :], scalar1=mx8[:, tt, 3:4], scalar2=None,
                                                         op0=ALU.is_ge), reads=[("lg", tt), ("mx8", tt)], writes=[("mask", tt)])
            S.op("dve", lambda e, tt=tt: e.tensor_scalar(out=negm[:, tt:tt + 1], in0=mx8[:, tt, 0:1], scalar1=-1.0, scalar2=None,
                                                         op0=ALU.mult), reads=[("mx8", tt)], writes=[("negm", tt)])
            S.op("act", lambda e, tt=tt: e.activation(out=ex[:, tt, :], in_=lg[:, tt, :], func=AF.Exp, bias=negm[:, tt:tt + 1]),
                 reads=[("lg", tt), ("negm", tt)], writes=[("ex", tt)])
            S.op("dve", lambda e, tt=tt: e.tensor_tensor(out=ex[:, tt, :], in0=ex[:, tt, :], in1=mask[:, tt, :], op=ALU.mult),
                 reads=[("ex", tt), ("mask", tt)], writes=[("ex", tt)])
            S.op("dve", lambda e, tt=tt: e.reduce_sum(out=sm[:, tt:tt + 1], in_=ex[:, tt, :], axis=AX.X),
                 reads=[("ex", tt)], writes=[("sm", tt)])
            S.op("dve", lambda e, tt=tt: e.reciprocal(out=rsm[:, tt:tt + 1], in_=sm[:, tt:tt + 1]), reads=[("sm", tt)], writes=[("rsm", tt)])
            S.op("dve", lambda e, tt=tt: e.tensor_scalar(out=cx.prob[:, tt, :], in0=ex[:, tt, :], scalar1=rsm[:, tt:tt + 1], scalar2=None,
                                                         op0=ALU.mult), reads=[("ex", tt), ("rsm", tt)], writes=[("prob", tt)])
            S.op("dve", lambda e, tt=tt: e.tensor_copy(out=cx.prob_hi[:, tt, :], in_=cx.prob[:, tt, :]), reads=[("prob", tt)], writes=[("phi", tt)])
            S.op("dve", lambda e, tt=tt: e.tensor_tensor(out=cx.prob_lo[:, tt, :], in0=cx.prob[:, tt, :], in1=cx.prob_hi[:, tt, :],
                                                         op=ALU.subtract), reads=[("prob", tt), ("phi", tt)], writes=[("plo", tt)])
            S.op("dve", lambda e, tt=tt: e.tensor_copy(out=mask_b[:, tt, :], in_=mask[:, tt, :]), reads=[("mask", tt)], writes=[("maskb", tt)])
            pp = bk[6 + tt % 2]
            S.op("pe", lambda e, pp=pp, tt=tt: e.matmul(pp[:, 0:N_EXP], triu[:], mask_b[:, tt, :], start=True, stop=(tt == 0)),
                 reads=["triu", ("maskb", tt)], writes=[("bank", 6 + tt % 2)])
            for t2_ in range(tt):
                S.op("pe", lambda e, pp=pp, t2_=t2_, tt=tt: e.matmul(pp[:, 0:N_EXP], cx.ones_b[:], mask_b[:, t2_, :], start=False,
                                                                    stop=(t2_ == tt - 1)),
                     reads=["ones_b", ("maskb", t2_)], writes=[("bank", 6 + tt % 2)])
            S.op("dve", lambda e, pp=pp, tt=tt: e.tensor_tensor(out=cx.posm[:, tt, :], in0=pp[:, 0:N_EXP], in1=mask[:, tt, :], op=ALU.mult),
                 reads=[("bank", 6 + tt % 2), ("mask", tt)], writes=[("posm", tt)])
        if "d_lg" in cx.debug:
            S.dma("sp", cx.dbg_slot, cx.O["d_lg"][:, :], lg[:].rearrange("p a b -> p (a b)"), reads=[("lg", t) for t in range(8)])
            S.dma("sp", cx.dbg_slot, cx.O["d_posm"][:, :], cx.posm[:].rearrange("p a b -> p (a b)"), reads=[("posm", t) for t in range(8)])
            S.dma("sp", cx.dbg_slot, cx.O["d_prob"][:, :], cx.prob[:].rearrange("p a b -> p (a b)"), reads=[("prob", t) for t in range(8)])
    S.barrier()


def phaseE1(cx):
    nc, S, I, W = cx.nc, cx.S, cx.I, cx.W
    bk = cx.banks
    NR = 5
    NST = CAP // 128
    with contextlib.ExitStack() as st:
        sb = lambda name, shape, dt: st.enter_context(nc.sbuf_tensor(name, list(shape), dt))
        u2b = sb("E_u2b", [128, 8, D], BF16)
        ring = [sb(f"E_w{i}", [128, KC, 512], BF16) for i in range(NR)]
        rslot = [S.slot("ew") for _ in range(NR)]
        sl = sb("E_sel", [128, 8, CAP], BF16)
        x_ = sb("E_xe", [128, KC, CAP], BF16)
        a_ = sb("E_act", [128, KC, CAP], BF16)
        ye = sb("E_ye", [128, NST, D], BF16)
        yslot = S.slot("ye")
        p_ = sb("E_psl", [128, NST], F32)
        bgT = sb("E_bgT", [128, N_EXP * KC], F32)
        buT = sb("E_buT", [128, N_EXP * KC], F32)
        gsb = [sb(f"E_g{i}", [128, CAP], F32) for i in range(2)]
        sig = [sb(f"E_s{i}", [128, CAP], F32) for i in range(2)]
        usb = [sb(f"E_u{i}", [128, CAP], F32) for i in range(2)]
        ls = S.slot("E1")
        S.op("dve", lambda en: en.memset(ye[:, 0, :], 0.0), writes=["ye"])
        S.dma("sp", ls, W["Y"][N_EXP * CAP:N_EXP * CAP + 128, :], ye[:, 0, :], reads=["ye"])
        for tt in range(8):
            S.dma("sp" if tt % 2 == 0 else "act", ls, u2b[:, tt, :], W["U2"][tt * 128:(tt + 1) * 128, :], writes=["u2b"])
        S.dma("sp", ls, bgT[:], I["b_gateT"][:, :], writes=["bgT"])
        S.dma("sp", ls, buT[:], I["b_upT"][:, :], writes=["buT"])
        wg = I["w_gate"].rearrange("e (k p) n -> e p k n", p=128)
        wu = I["w_up"].rearrange("e (k p) n -> e p k n", p=128)
        wd = I["w_down"].rearrange("e (k p) n -> e p k n", p=128)
        cnt = {"piece": 0}

        def load_piece(src, e, g):
            ri = cnt["piece"] % NR
            cnt["piece"] += 1
            for j in range(4):
                S.dma("pool", rslot[ri], ring[ri][:, 4 * j:4 * j + 4, :], src[e, :, 4 * j:4 * j + 4, g * 512:(g + 1) * 512],
                      writes=[("ring", ri)])
            return ri

        gi = 0
        fi = 0
        di = 0
        yi = 0
        for e in range(N_EXP):
            for tt in range(8):
                S.op("dve", lambda en, tt=tt, e=e: en.tensor_scalar(
                    out=sl[:, tt, :], in0=cx.iota[:], scalar1=cx.posm[:, tt, e:e + 1], scalar2=None, op0=ALU.is_equal),
                    reads=["iota", ("posm", tt)], writes=["sel"])
            for k in range(KC):
                bi = gi % 2
                gi += 1
                ps = bk[bi]
                for tt in range(8):
                    S.op("pe", lambda en, ps=ps, k=k, tt=tt: en.matmul(
                        ps[:, 0:CAP], u2b[:, tt, k * 128:(k + 1) * 128], sl[:, tt, :],
                        start=(tt == 0), stop=(tt == 7)), reads=["u2b", "sel"], writes=[("bank", bi)])
                evac_copy(S, k, x_[:, k, :], ps[:, 0:CAP], [("bank", bi)], ["xe"])
            bi = gi % 2
            gi += 1
            ps = bk[bi]
            for st_ in range(NST):
                n = 0
                for tt in range(8):
                    for pr in (cx.prob_hi, cx.prob_lo):
                        S.op("pe", lambda en, ps=ps, st_=st_, tt=tt, pr=pr, e=e, n=n: en.matmul(
                            ps[:, st_:st_ + 1], sl[:, tt, st_ * 128:(st_ + 1) * 128], pr[:, tt, e:e + 1],
                            start=(n == 0), stop=(n == 15)),
                            reads=["sel", ("phi", tt), ("plo", tt)], writes=[("bank", bi)])
                        n += 1
            S.op("dve", lambda en, ps=ps: en.tensor_copy(out=p_[:], in_=ps[:, 0:NST]), reads=[("bank", bi)], writes=["psl"])
            for g in range(4):
                rg = load_piece(wg, e, g)
                ru = load_piece(wu, e, g)
                for f4 in range(4):
                    fc = 4 * g + f4
                    pg, pu = bk[2 + fi % 2], bk[4 + fi % 2]
                    kg, ku = ("bank", 2 + fi % 2), ("bank", 4 + fi % 2)
                    for k in range(KC):
                        S.op("pe", lambda en, pg=pg, rg=rg, k=k, f4=f4: en.matmul(
                            pg[:, 0:CAP], ring[rg][:, k, f4 * 128:(f4 + 1) * 128], x_[:, k, :], start=(k == 0), stop=(k == KC - 1)),
                            reads=[("ring", rg), "xe"], writes=[kg])
                    for k in range(KC):
                        S.op("pe", lambda en, pu=pu, ru=ru, k=k, f4=f4: en.matmul(
                            pu[:, 0:CAP], ring[ru][:, k, f4 * 128:(f4 + 1) * 128], x_[:, k, :], start=(k == 0), stop=(k == KC - 1)),
                            reads=[("ring", ru), "xe"], writes=[ku])
                    w = fi % 2
                    fi += 1
                    gb_, sg_, ub_ = gsb[w], sig[w], usb[w]
                    col = e * KC + fc
                    S.op("dve", lambda en, gb_=gb_, pg=pg, col=col: en.tensor_scalar(
                        out=gb_[:], in0=pg[:, 0:CAP], scalar1=bgT[:, col:col + 1], scalar2=7.0, op0=ALU.add, op1=ALU.min),
                        reads=[kg, "bgT"], writes=[("gsb", w)])
                    S.op("act", lambda en, sg_=sg_, gb_=gb_: en.activation(out=sg_[:], in_=gb_[:], func=AF.Sigmoid, scale=1.702),
                         reads=[("gsb", w)], writes=[("sig", w)])
                    S.op("act", lambda en, ub_=ub_, pu=pu, col=col: en.activation(
                        out=ub_[:], in_=pu[:, 0:CAP], func=AF.Identity, bias=buT[:, col:col + 1]),
                        reads=[ku, "buT"], writes=[("usb", w)])
                    S.op("dve", lambda en, ub_=ub_: en.tensor_scalar(
                        out=ub_[:], in0=ub_[:], scalar1=7.0, scalar2=-7.0, op0=ALU.min, op1=ALU.max),
                        reads=[("usb", w)], writes=[("usb", w)])
                    S.op("dve", lambda en, gb_=gb_, sg_=sg_: en.tensor_tensor(out=gb_[:], in0=gb_[:], in1=sg_[:], op=ALU.mult),
                         reads=[("gsb", w), ("sig", w)], writes=[("gsb", w)])
                    S.op("dve", lambda en, fc=fc, gb_=gb_, ub_=ub_: en.scalar_tensor_tensor(
                        out=a_[:, fc, :], in0=ub_[:], scalar=1.0, in1=gb_[:], op0=ALU.add, op1=ALU.mult),
                        reads=[("gsb", w), ("usb", w)], writes=["act"])
            for ch in range(4):
                rd = load_piece(wd, e, ch)
                for st_ in range(NST):
                    pd = bk[6 + di % 2]
                    kd = ("bank", 6 + di % 2)
                    for f in range(KC):
                        S.op("pe", lambda en, pd=pd, rd=rd, f=f, st_=st_: en.matmul(
                            pd[:, :], a_[:, f, st_ * 128:(st_ + 1) * 128], ring[rd][:, f, :], start=(f == 0), stop=(f == KC - 1)),
                            reads=[("ring", rd), "act"], writes=[kd])
                    if di % 2 == 0:
                        S.op("act", lambda en, st_=st_, ch=ch, pd=pd: en.activation(
                            out=ye[:, st_, ch * 512:(ch + 1) * 512], in_=pd[:, :], func=AF.Copy, scale=p_[:, st_:st_ + 1]),
                            reads=[kd, "psl"], writes=["ye"])
                    else:
                        S.op("dve", lambda en, st_=st_, ch=ch, pd=pd: en.tensor_scalar(
                            out=ye[:, st_, ch * 512:(ch + 1) * 512], in0=pd[:, :], scalar1=p_[:, st_:st_ + 1], scalar2=None, op0=ALU.mult),
                            reads=[kd, "psl"], writes=["ye"])
                    di += 1
            for st_ in range(NST):
                S.dma("sp", yslot, W["Y"][e * CAP + st_ * 128:e * CAP + (st_ + 1) * 128, :], ye[:, st_, :], reads=["ye"])
    S.barrier()


def cx_rd(cnt, rds, ch):
    return rds[ch]


def phaseE2(cx):
    nc, S, I, W = cx.nc, cx.S, cx.I, cx.W
    bk = cx.banks
    I32 = mybir.dt.int32
    ZR = N_EXP * CAP
    with contextlib.ExitStack() as st:
        sb = lambda name, shape, dt: st.enter_context(nc.sbuf_tensor(name, list(shape), dt))
        ecapB = sb("F_ecap", [128, 8, N_EXP], F32)
        vv = sb("F_vv", [128, 8, N_EXP], F32)
        vt = sb("F_vt", [128, 8, N_EXP], F32)
        m8 = sb("F_m8", [128, 8, 8], F32)
        eq0 = sb("F_eq0", [128, 8, 4], F32)
        idf = sb("F_idf", [128, 8, 4], F32)
        idi = sb("F_idi", [128, 8, 4], I32)
        gb = [[sb(f"F_g{i}_{k}", [128, D], BF16) for k in range(4)] for i in range(2)]
        gslot = [S.slot("gg") for _ in range(2)]
        acc = [sb(f"F_acc{i}", [128, D], F32) for i in range(2)]
        ht = [sb(f"F_h{i}", [128, D], F32) for i in range(2)]
        hslot = [S.slot("hh") for _ in range(2)]
        g2B = sb("F_g2B", [128, D], F32)
        gfB = sb("F_gfB", [128, D], F32)
        ot = [sb(f"F_ot{i}", [128, D], F32) for i in range(2)]
        bdf = sb("F_bdf", [N_EXP, D], F32)
        bdb = sb("F_bdb", [N_EXP, D], BF16)
        pT = sb("F_pT", [N_EXP, 8, 2, 128], BF16)
        ss = sb("F_ss", [128, 8], F32)
        rt = sb("F_rt", [128, 8], F32)
        rs = sb("F_rs", [128, 8], F32)
        ls = S.slot("E2")
        S.dma("sp", ls, g2B[:], W["rows"][3:4, :].partition_broadcast(128), writes=["g2B"])
        S.dma("sp", ls, gfB[:], I["nf"][0:1, :].partition_broadcast(128), writes=["gfB"])
        S.dma("sp", ls, bdf[:], I["b_down"][:, :], writes=["bdf"])
        S.dma("sp", ls, ecapB[:].rearrange("p a b -> p (a b)"), I["ecapB"][:, :], writes=["ecapB"])
        S.op("dve", lambda e: e.tensor_copy(out=bdb[:], in_=bdf[:]), reads=["bdf"], writes=["bdb"])
        pk = [("posm", t) for t in range(8)]
        S.op("dve", lambda e: e.tensor_scalar(out=vt[:], in0=cx.posm[:], scalar1=0.0, scalar2=None, op0=ALU.is_gt),
             reads=pk, writes=["vt"])
        S.op("dve", lambda e: e.tensor_scalar(out=vv[:], in0=cx.posm[:], scalar1=float(CAP), scalar2=None, op0=ALU.is_le),
             reads=pk, writes=["vv"])
        S.op("dve", lambda e: e.tensor_tensor(out=vt[:], in0=vt[:], in1=vv[:], op=ALU.mult), reads=["vt", "vv"], writes=["vt"])
        S.op("dve", lambda e: e.tensor_tensor(out=vv[:], in0=cx.posm[:], in1=ecapB[:], op=ALU.add), reads=pk + ["ecapB", "vv"], writes=["vv"])
        S.op("dve", lambda e: e.tensor_tensor(out=vv[:], in0=vv[:], in1=vt[:], op=ALU.mult), reads=["vv", "vt"], writes=["vv"])
        for tt in range(8):
            S.op("dve", lambda e, tt=tt: e.max(out=m8[:, tt, :], in_=vv[:, tt, :]), reads=["vv"], writes=["m8"])
        S.op("dve", lambda e: e.tensor_scalar(out=eq0[:], in0=m8[:, :, 0:4], scalar1=0.0, scalar2=float(ZR + 1), op0=ALU.is_equal, op1=ALU.mult),
             reads=["m8"], writes=["eq0"])
        S.op("dve", lambda e: e.scalar_tensor_tensor(out=idf[:], in0=m8[:, :, 0:4], scalar=-1.0, in1=eq0[:], op0=ALU.add, op1=ALU.add),
             reads=["m8", "eq0"], writes=["idf"])
        S.op("dve", lambda e: e.tensor_copy(out=idi[:], in_=idf[:]), reads=["idf"], writes=["idi"])
        pTb = bk[6][:].bitcast(BF16)
        for tt in range(8):
            for hl, pr in enumerate((cx.prob_hi, cx.prob_lo)):
                j = (tt * 2 + hl) % 8
                S.op("pe", lambda e, j=j, pr=pr, tt=tt: e.transpose(pTb[0:N_EXP, j * 128:(j + 1) * 128], pr[:, tt, :], cx.ident_b[:]),
                     reads=[("phi", tt), ("plo", tt), "ident_b"], writes=[("bank", 6)])
                S.op("dve", lambda e, j=j, tt=tt, hl=hl: e.tensor_copy(out=pT[:, tt, hl, :], in_=pTb[0:N_EXP, j * 128:(j + 1) * 128]),
                     reads=[("bank", 6)], writes=["pT"])
        for tt in range(8):
            par = tt % 2
            hb = ht[par]
            S.dma("act", hslot[par], hb[:], W["hs"][tt * 128:(tt + 1) * 128, :], writes=[("ht", par)])
            for k in range(4):
                def fn(e, k=k, tt=tt, par=par):
                    return e.indirect_dma_start(out=gb[par][k][:, :], out_offset=None, in_=W["Y"][:, :],
                                                in_offset=bass.IndirectOffsetOnAxis(ap=idi[:, tt, k:k + 1], axis=0))
                o = Op("pool", fn)
                o.dma = True
                o.slot = gslot[par]
                o.slot_total = gslot[par].total + 16
                S._add(o, ["idi"], [("gb", par, k)])
                gslot[par].total += 16
            ac = acc[par]
            g_ = gb[par]
            S.op("dve", lambda e, ac=ac, g_=g_: e.tensor_tensor(out=ac[:], in0=g_[0][:], in1=g_[1][:], op=ALU.add),
                 reads=[("gb", par, 0), ("gb", par, 1)], writes=[("acc", par)])
            S.op("pool", lambda e, ac=ac, g_=g_: e.tensor_tensor(out=g_[2][:], in0=g_[2][:], in1=g_[3][:], op=ALU.add),
                 reads=[("gb", par, 2), ("gb", par, 3)], writes=[("gb", par, 2)])
            S.op("dve", lambda e, ac=ac, g_=g_: e.tensor_tensor(out=ac[:], in0=ac[:], in1=g_[2][:], op=ALU.add),
                 reads=[("gb", par, 2), ("acc", par)], writes=[("acc", par)])
            for ch in range(4):
                for hl in range(2):
                    S.op("pe", lambda en, tt=tt, hl=hl, ch=ch: en.matmul(
                        bk[ch][:, :], pT[:, tt, hl, :], bdb[:, ch * 512:(ch + 1) * 512], start=(hl == 0), stop=(hl == 1)),
                        reads=["pT", "bdb"], writes=[("bank", ch)])
                S.op("dve", lambda en, ac=ac, ch=ch: en.tensor_tensor(out=ac[:, ch * 512:(ch + 1) * 512], in0=bk[ch][:, :],
                                                                     in1=ac[:, ch * 512:(ch + 1) * 512], op=ALU.add),
                     reads=[("bank", ch), ("acc", par)], writes=[("acc", par)])
            S.op("pool", lambda en, ac=ac: en.tensor_tensor(out=ac[:], in0=ac[:], in1=g2B[:], op=ALU.mult),
                 reads=[("acc", par), "g2B"], writes=[("acc", par)])
            S.op("pool", lambda en, ac=ac, hb=hb: en.tensor_tensor(out=hb[:], in0=hb[:], in1=ac[:], op=ALU.add),
                 reads=[("acc", par), ("ht", par)], writes=[("ht", par)])
            o_ = ot[par]
            S.op("act", lambda en, o_=o_, hb=hb, tt=tt: en.activation(out=o_[:], in_=hb[:], func=AF.Square, accum_out=ss[:, tt:tt + 1]),
                 reads=[("ht", par)], writes=[("ot", par), ("ss", tt)])
            S.op("act", lambda en, tt=tt: en.activation(out=rt[:, tt:tt + 1], in_=ss[:, tt:tt + 1], func=AF.Sqrt, scale=1.0 / D,
                                                        bias=cx.epsc[:, 0:1]), reads=[("ss", tt), "epsc"], writes=[("rt", tt)])
            S.op("dve", lambda en, tt=tt: en.reciprocal(out=rs[:, tt:tt + 1], in_=rt[:, tt:tt + 1]), reads=[("rt", tt)], writes=[("rs", tt)])
            S.op("dve", lambda en, o_=o_, hb=hb, tt=tt: en.scalar_tensor_tensor(out=o_[:], in0=hb[:], scalar=rs[:, tt:tt + 1], in1=gfB[:],
                                                                                op0=ALU.mult, op1=ALU.mult),
                 reads=[("ht", par), ("rs", tt), "gfB", ("ot", par)], writes=[("ot", par)])
            S.dma("sp", cx.slot_out, cx.O["out"][tt * 128:(tt + 1) * 128, :], o_[:], reads=[("ot", par)])
    S.barrier()


def _colT(v, nchunk):
    return np.ascontiguousarray(v.reshape(nchunk, 128).T)


def prep_inputs(inp):
    f32 = np.float32
    x = np.asarray(inp["x"], f32)
    c = np.asarray(inp["c"], f32)
    shared = {
        "w_ada": np.ascontiguousarray(inp["w_ada"][0], f32),
        "b_ada": np.ascontiguousarray(inp["b_ada"][0][None, :], f32),
        "n1": np.ascontiguousarray(inp["norm1_g"][0][None, :], f32),
        "n2": np.ascontiguousarray(inp["norm2_g"][0][None, :], f32),
        "nf": np.ascontiguousarray(np.asarray(inp["final_g"])[None, :], f32),
        "w_in": np.ascontiguousarray(inp["w_in"][0], f32),
        "lamT": np.ascontiguousarray(np.stack([inp["lam_q1"][0], inp["lam_k1"][0], inp["lam_q2"][0], inp["lam_k2"][0]], axis=1), f32),
        "sublnT": _colT(np.asarray(inp["subln_g"][0], f32), 2),
        "w_out_a": np.ascontiguousarray(inp["w_out_a"][0], f32),
        "w_out_b": np.ascontiguousarray(inp["w_out_b"][0], f32),
        "w_o": np.ascontiguousarray(inp["w_o"][0], f32),
        "w_router": np.ascontiguousarray(inp["w_router"][0], f32),
        "b_routerB": np.ascontiguousarray(np.broadcast_to(np.asarray(inp["b_router"][0], f32)[None, :], (128, N_EXP))),
        "w_gate": np.ascontiguousarray(inp["w_gate"][0], f32),
        "w_up": np.ascontiguousarray(inp["w_up"][0], f32),
        "w_down": np.ascontiguousarray(inp["w_down"][0], f32),
        "b_gateT": np.ascontiguousarray(np.asarray(inp["b_gate"][0], f32).reshape(N_EXP, KC, 128).transpose(2, 0, 1).reshape(128, N_EXP * KC)),
        "b_upT": np.ascontiguousarray(np.asarray(inp["b_up"][0], f32).reshape(N_EXP, KC, 128).transpose(2, 0, 1).reshape(128, N_EXP * KC)),
        "b_down": np.ascontiguousarray(inp["b_down"][0], f32),
        "ident": np.eye(128, dtype=f32),
        "iota_row": np.ascontiguousarray(np.broadcast_to(np.arange(1, CAP + 1, dtype=f32)[None, :], (128, CAP))),
        "triu": np.triu(np.ones((128, 128), f32)),
        "ecapB": np.ascontiguousarray(np.broadcast_to((np.arange(N_EXP, dtype=f32) * CAP)[None, None, :], (128, 8, N_EXP)).reshape(128, 8 * N_EXP)),
    }
    p = np.arange(128)[:, None]
    xx = np.arange(3968)[None, :]
    dl = p + 1920 - xx
    ad = np.abs(dl)
    n_ = ((ad <= 64).astype(np.float64) + ((dl % 4 == 0) & (ad <= 256)) + ((dl % 16 == 0) & (ad <= 1024)))
    lnn = np.where(n_ > 0, np.log(np.maximum(n_, 1.0)), 0.0)
    shared["stripAh"] = np.ascontiguousarray(np.stack(
        [np.where(n_ > 0, -SLOPES_A[h] * ad + lnn, -BIG) for h in range(8)], axis=0).astype(f32))
    maps = []
    for core in range(8):
        b, r = core // 4, core % 4
        idx = (np.arange(SEQ) - 1024 + 1024 * r) % SEQ
        m = dict(shared)
        m["xr"] = np.ascontiguousarray(x[b][idx])
        m["cT"] = _colT(c[b], KC)
        dist = np.zeros((128, 4, 1920), f32)
        for g in range(4):
            s_g = ((1024 * g - 1024 + 1024 * r) % SEQ) - 1024 * g
            q_off = -1024 + 1024 * r
            xv = np.arange(1920)[None, :]
            dist[:, g, :] = np.abs(1024 * (1 - g) + (xv - 896) - p + q_off - s_g)
        m["distB"] = np.ascontiguousarray(dist.reshape(128, 4 * 1920))
        kb = np.zeros((128, 24), f32)
        for kt in range(24):
            tok = 128 * kt + np.arange(128) - 1024 + 1024 * r
            kb[:, kt] = np.where((tok >= 0) & (tok < SEQ), 0.0, -BIG)
        m["kbiasA"] = kb
        maps.append(m)
    return maps


_NC_CACHE = {}
_LAST_DECLARED = []


def kernel(**inputs):
    maps = prep_inputs(inputs)
    if "nc" not in _NC_CACHE:
        _NC_CACHE["nc"] = build_program()
    nc = _NC_CACHE["nc"]
    names = set(_LAST_DECLARED)
    maps = [{k: v for k, v in m.items() if k in names} for m in maps]
    res = run_bass_kernel_spmd(nc, maps, core_ids=list(range(8)))
    out = np.zeros((2, SEQ, D), np.float32)
    for core in range(8):
        b, r = core // 4, core % 4
        out[b, 1024 * r:1024 * (r + 1)] = np.asarray(res.results[core]["out"], np.float32)
    return out
```

```python
import contextlib
import math
import numpy as np
import concourse.bass as bass
import concourse.mybir as mybir
from concourse.bass_utils import run_bass_kernel_spmd

F32 = mybir.dt.float32
BF16 = mybir.dt.bfloat16
AF = mybir.ActivationFunctionType
ALU = mybir.AluOpType
AX = mybir.AxisListType

ENGS = ("pe", "act", "dve", "pool", "sp")

D = 2048
SEQ = 4096
OWN = 1024
KC = 16
N_EXP = 32
TOPK = 4
CAP = 512
EPS = 1e-5
SCALE = 128 ** -0.5
LAMBDA_INIT = 0.8 - 0.6 * math.exp(-0.3 * 0)
SLOPES = [2.0 ** (-8.0 * (i + 1) / 12) for i in range(12)]
SLOPES_A = SLOPES[:8]
SLOPES_B = SLOPES[8:]
BIG = 1.0e9
PATTERNS = (1, 4, 16)


class Op:
    __slots__ = ("eng", "fn", "deps", "dma", "idx", "count", "milestone", "slot", "slot_total", "name")

    def __init__(self, eng, fn, name=""):
        self.eng = eng
        self.fn = fn
        self.deps = []
        self.dma = False
        self.milestone = False
        self.count = None
        self.slot = None
        self.slot_total = None
        self.name = name


class Slot:
    def __init__(self, name):
        self.name = name
        self.total = 0
        self.sem = None


class Sched:
    def __init__(self, nc):
        self.nc = nc
        self.ops = []
        self.writers = {}
        self.readers = {}
        self.old_readers = {}
        self.slots = []
        self.nbar = 0

    def slot(self, name):
        s = Slot(name + str(len(self.slots)))
        self.slots.append(s)
        return s

    def _add(self, op, reads, writes):
        deps = {}

        def add_dep(o):
            if o is op:
                return
            if o.dma:
                if op.dma and op.slot is o.slot:
                    return
                deps[("slot", id(o.slot))] = ("slot", o.slot, o.slot.total)
            else:
                k = ("eng", o.eng)
                prev = deps.get(k)
                if prev is None or o.idx > prev[1].idx:
                    deps[k] = ("eng", o, None)

        for k in reads:
            for o in self.writers.get(k, {}).values():
                add_dep(o)
        for k in writes:
            for o in self.writers.get(k, {}).values():
                if (not o.dma) and (not op.dma) and o.eng == op.eng:
                    continue
                add_dep(o)
            for o in list(self.readers.get(k, {}).values()) + list(self.old_readers.get(k, {}).values()):
                if (not o.dma) and (not op.dma) and o.eng == op.eng:
                    continue
                add_dep(o)
        op.deps = list(deps.values())
        for d in op.deps:
            if d[0] == "eng":
                d[1].milestone = True
        op.idx = len(self.ops)
        self.ops.append(op)
        wkey = ("slot", id(op.slot)) if op.dma else op.eng
        for k in writes:
            self.writers[k] = {wkey: op}
            if self.readers.get(k):
                self.old_readers[k] = self.readers[k]
            self.readers[k] = {}
        for k in reads:
            self.readers.setdefault(k, {})[wkey] = op
        return op

    def op(self, eng, fn, reads=(), writes=(), name=""):
        o = Op(eng, fn, name)
        return self._add(o, list(reads), list(writes))

    def dma(self, eng, slot, out, in_, reads=(), writes=(), name="", **kw):
        def fn(e, out=out, in_=in_, kw=kw):
            return e.dma_start(out=out, in_=in_, **kw)
        o = Op(eng, fn, name)
        o.dma = True
        o.slot = slot
        o.slot_total = slot.total + 16
        r = self._add(o, list(reads), list(writes))
        slot.total += 16
        return r

    def barrier(self):
        n = self.nbar
        self.nbar += 1
        sc = self.bar_scratch
        comp = ("pe", "act", "dve", "pool")
        for e in comp:
            if e == "pe":
                self.op("pe", lambda en: en.matmul(self.bar_ps[0:1, 0:1], sc["b"][0:1, 0:1], sc["b"][0:1, 0:1],
                                                    start=True, stop=True),
                        writes=[("bar", n, e), ("bank", 7)])
            elif e == "act":
                self.op("act", lambda en: en.activation(out=sc["act"][0:1, 0:1], in_=sc["one"][0:1, 0:1], func=AF.Copy),
                        writes=[("bar", n, e)])
            elif e == "dve":
                self.op("dve", lambda en: en.tensor_copy(out=sc["dve"][0:1, 0:1], in_=sc["one"][0:1, 0:1]),
                        writes=[("bar", n, e)])
            else:
                self.op("pool", lambda en: en.tensor_copy(out=sc["pool"][0:1, 0:1], in_=sc["one"][0:1, 0:1]),
                        writes=[("bar", n, e)])
        rk = [("bar", n, e) for e in comp]
        for e in ENGS:
            o = Op(e, None, "barwait")
            self._add(o, rk, [])
            for s in self.slots:
                if s.total > 0:
                    o.deps.append(("slot", s, s.total))
        self.writers = {}
        self.readers = {}
        self.old_readers = {}

    def emit(self, final_waits=()):
        nc = self.nc
        by_eng = {e: [] for e in ENGS}
        for o in self.ops:
            by_eng[o.eng].append(o)
        for e in ENGS:
            c = 0
            for o in by_eng[e]:
                if o.milestone and not o.dma:
                    c += 1
                    o.count = c
        with contextlib.ExitStack() as st:
            esem = {e: st.enter_context(nc.semaphore("s_" + e)) for e in ENGS}
            for s in self.slots:
                s.sem = st.enter_context(nc.semaphore("d_" + s.name))
            block = st.enter_context(nc.Block())

            def run(e, eng):
                waited = {}
                for o in by_eng[e]:
                    for d in o.deps:
                        if d[0] == "slot":
                            sem, val, key = d[1].sem, d[2], ("slot", id(d[1]))
                        else:
                            dop = d[1]
                            if dop.eng == e and e == "pe":
                                continue
                            sem, val, key = esem[dop.eng], dop.count, ("eng", dop.eng)
                        if waited.get(key, 0) >= val:
                            continue
                        waited[key] = val
                        eng.wait_ge(sem, val)
                    if o.fn is None:
                        continue
                    ins = o.fn(eng)
                    if o.dma:
                        ins.then_inc(o.slot.sem, 16)
                    elif o.milestone:
                        ins.then_inc(esem[e], 1)
                if e == "sp":
                    for s in final_waits:
                        eng.wait_ge(s.sem, s.total)

            @block.tensor
            def _(eng):
                run("pe", eng)

            @block.scalar
            def _(eng):
                run("act", eng)

            @block.vector
            def _(eng):
                run("dve", eng)

            @block.gpsimd
            def _(eng):
                run("pool", eng)

            @block.sync
            def _(eng):
                run("sp", eng)


class Ctx:
    pass


def a_tiles():
    out = []
    for d in PATTERNS:
        nt = {1: 9, 4: 3, 16: 2}[d]
        for c in range(d):
            for m in range(nt):
                out.append((d, c, m))
    return out


A_TILES = a_tiles()
A_TILE_IDX = {t: i for i, t in enumerate(A_TILES)}


def build_program(debug=(), stages="ABCDE"):
    nc = bass.Bass("TRN2", target_bir_lowering=False)
    cx = Ctx()
    cx.stages = stages
    cx.nc = nc
    cx.debug = set(debug)
    S = Sched(nc)
    cx.S = S

    def din(name, shape, dt=F32):
        return nc.dram_tensor(name, list(shape), dt, kind="ExternalInput").ap()

    def dscr(name, shape, dt=BF16):
        return nc.dram_tensor(name, list(shape), dt, kind="Internal").ap()

    def dout(name, shape, dt=F32):
        return nc.dram_tensor(name, list(shape), dt, kind="ExternalOutput").ap()

    class Lazy(dict):
        def __init__(self):
            super().__init__()
            self.specs = {}

        def __setitem__(self, k, v):
            self.specs[k] = v

        def __missing__(self, k):
            ap = self.specs[k]()
            dict.__setitem__(self, k, ap)
            return ap
    _din = din
    din = lambda name, shape, dt=F32: (lambda: _din(name, shape, dt))
    I = Lazy()
    I["xr"] = din("xr", [SEQ, D])
    I["cT"] = din("cT", [128, KC])
    I["w_ada"] = din("w_ada", [D, 6 * D])
    I["b_ada"] = din("b_ada", [1, 6 * D])
    I["n1"] = din("n1", [1, D])
    I["n2"] = din("n2", [1, D])
    I["nf"] = din("nf", [1, D])
    I["w_in"] = din("w_in", [D, 10240])
    I["lamT"] = din("lamT", [128, 4])
    I["sublnT"] = din("sublnT", [128, 2])
    I["w_out_a"] = din("w_out_a", [1024, D])
    I["w_out_b"] = din("w_out_b", [1024, D])
    I["w_o"] = din("w_o", [D, D])
    I["w_router"] = din("w_router", [D, N_EXP])
    I["b_routerB"] = din("b_routerB", [128, N_EXP])
    I["w_gate"] = din("w_gate", [N_EXP, D, D])
    I["w_up"] = din("w_up", [N_EXP, D, D])
    I["w_down"] = din("w_down", [N_EXP, D, D])
    I["b_gateT"] = din("b_gateT", [128, N_EXP * KC])
    I["b_upT"] = din("b_upT", [128, N_EXP * KC])
    I["b_down"] = din("b_down", [N_EXP, D])
    I["ident"] = din("ident", [128, 128])
    I["distB"] = din("distB", [128, 4 * 1920])
    I["stripAh"] = din("stripAh", [8, 128, 3968])
    I["kbiasA"] = din("kbiasA", [128, 24])
    I["iota_row"] = din("iota_row", [128, CAP])
    I["triu"] = din("triu", [128, 128])
    I["ecapB"] = din("ecapB", [128, 8 * N_EXP])
    I["tokab"] = din("tokab", [128, 16])
    cx.I = I
    O = {}
    O["out"] = dout("out", [OWN, D])
    for name, shape, dt in DEBUG_SHAPES:
        if name in cx.debug:
            O[name] = dout(name, shape, dt)
    cx.O = O
    W = {}
    W["QaT"] = dscr("QaT", [1024, OWN])
    W["KaT"] = dscr("KaT", [1024, 3072])
    W["Va"] = dscr("Va", [SEQ, 1024])
    W["QbT"] = dscr("QbT", [1024, OWN])
    W["KbT"] = dscr("KbT", [1024, SEQ])
    W["Vb"] = dscr("Vb", [SEQ, 1024])
    W["GT"] = dscr("GT", [4096, OWN])
    W["rows"] = dscr("rows", [4, D], F32)
    W["YaT"] = dscr("YaT", [1024, OWN])
    W["YbT"] = dscr("YbT", [1024, OWN])
    W["U2"] = dscr("U2", [OWN, D])
    W["MT"] = dscr("MT", [D, OWN])
    W["hs"] = dscr("hs", [OWN, D], F32)
    W["Y"] = dscr("Y", [N_EXP * CAP + 128, D])
    cx.W = W

    with contextlib.ExitStack() as st:
        cx.st = st
        sb = lambda name, shape, dt: st.enter_context(nc.sbuf_tensor(name, list(shape), dt))
        cx.banks = [st.enter_context(nc.psum_tensor(f"bank{i}", [128, 512], F32)) for i in range(8)]
        cx.ident_f = sb("ident_f", [128, 128], F32)
        cx.ident_b = sb("ident_b", [128, 128], BF16)
        cx.ones_f = sb("ones_f", [128, 128], F32)
        cx.ones_b = sb("ones_b", [128, 128], BF16)
        cx.colT = sb("colT", [128, 32], F32)
        cx.neglam = sb("neglam", [128, 1], F32)
        cx.sg = sb("sg", [128, 2], F32)
        cx.barsc = sb("barsc", [128, 8], F32)
        cx.barb = sb("barb", [128, 2], BF16)
        cx.epsc = sb("epsc", [128, 2], F32)
        S.bar_scratch = {"b": cx.barb, "one": cx.barsc[:, 0:1], "act": cx.barsc[:, 1:2],
                         "dve": cx.barsc[:, 2:3], "pool": cx.barsc[:, 3:4]}
        S.bar_ps = cx.banks[7]
        cx.slot_const = S.slot("const")
        cx.slot_out = S.slot("out")
        cx.dbg_slot = S.slot("dbg")

        cx.posm = sb("posm", [128, 8, N_EXP], F32)
        cx.prob = sb("prob", [128, 8, N_EXP], F32)
        cx.prob_hi = sb("prob_hi", [128, 8, N_EXP], BF16)
        cx.prob_lo = sb("prob_lo", [128, 8, N_EXP], BF16)
        cx.iota = sb("iota", [128, CAP], F32)
        S.dma("sp", cx.slot_const, cx.iota[:], I["iota_row"][:, :], writes=["iota"])
        stages = cx.stages
        phase0(cx)
        if "A" in stages:
            phaseA(cx)
            S.barrier()
        if "B" in stages:
            attention(cx, "B")
        if "C" in stages:
            attention(cx, "A")
        if "D" in stages:
            phaseD(cx)
        if "E" in stages:
            phaseE1(cx)
            phaseE2(cx)
        S.emit(final_waits=[cx.slot_out, cx.dbg_slot])
    global _LAST_DECLARED
    _LAST_DECLARED = list(dict.keys(I))
    return nc


DEBUG_SHAPES = [
    ("d_yA", [1024, OWN], BF16),
    ("d_mT", [D, OWN], BF16),
    ("d_ya", [1024, OWN], BF16),
    ("d_yb", [1024, OWN], BF16),
    ("d_gat", [128, 1024], BF16),
    ("d_a1", [128, 1024], F32),
    ("d_rowB", [128, D], F32),
    ("d_yB", [1024, OWN], BF16),
    ("d_h1", [OWN, D], F32),
    ("d_lg", [128, 8 * N_EXP], F32),
    ("d_posm", [128, 8 * N_EXP], F32),
    ("d_prob", [128, 8 * N_EXP], F32),
    ("d_colT", [128, 32], F32),
    ("d_mod", [1, 6 * D], F32),
    ("d_rows", [4, D], F32),
    ("d_misc", [128, 4], F32),
    ("d_uT", [128, KC * 512], BF16),
]


def phase0(cx):
    nc, S, I = cx.nc, cx.S, cx.I
    with contextlib.ExitStack() as st:
        sb = lambda name, shape, dt: st.enter_context(nc.sbuf_tensor(name, list(shape), dt))
        cT = sb("p0_cT", [128, KC], F32)
        sc = sb("p0_sc", [128, KC], F32)
        brow = sb("p0_brow", [1, 6 * D], F32)
        modrow = sb("p0_modrow", [1, 6 * D], F32)
        nrow = sb("p0_nrow", [1, 2 * D], F32)
        grow = sb("p0_grow", [1, 2 * D], F32)
        lamT = sb("p0_lamT", [128, 4], F32)
        sublnT = sb("p0_subln", [128, 2], F32)
        prod = sb("p0_prod", [128, 2], F32)
        ex = sb("p0_ex", [128, 2], F32)
        pan = [sb(f"p0_pan{i}", [128, KC, 512], F32) for i in range(2)]
        pslot = [S.slot("pan") for _ in range(2)]
        sc_slot = cx.slot_const
        bk = cx.banks

        S.dma("sp", sc_slot, cx.ident_f[:], I["ident"][:, :], writes=["ident_f"])
        S.dma("sp", sc_slot, cT[:], I["cT"][:, :], writes=["cT"])
        S.dma("sp", sc_slot, brow[:], I["b_ada"][:, :], writes=["brow"])
        S.dma("sp", sc_slot, nrow[0:1, 0:D], I["n1"][:, :], writes=["nrow"])
        S.dma("sp", sc_slot, nrow[0:1, D:2 * D], I["n2"][:, :], writes=["nrow"])
        S.dma("sp", sc_slot, lamT[:], I["lamT"][:, :], writes=["lamT"])
        S.dma("sp", sc_slot, sublnT[:], I["sublnT"][:, :], writes=["sublnT"])

        S.op("dve", lambda e: e.memset(cx.ones_f[:], 1.0), writes=["ones_f"])
        S.op("dve", lambda e: e.memset(cx.ones_b[:], 1.0), writes=["ones_b"])
        S.op("dve", lambda e: e.memset(cx.barsc[:], 1.0), writes=["barsc"])
        S.op("dve", lambda e: e.memset(cx.barb[:], 1.0), writes=["barb"])
        S.op("dve", lambda e: e.memset(cx.epsc[:], EPS), writes=["epsc"])
        S.op("dve", lambda e: e.tensor_copy(out=cx.ident_b[:], in_=cx.ident_f[:]), reads=["ident_f"], writes=["ident_b"])
        S.op("act", lambda e: e.activation(out=sc[:], in_=cT[:], func=AF.Silu), reads=["cT"], writes=["sc"])

        wv = I["w_ada"].rearrange("(k p) n -> p k n", p=128)
        NP = 6 * D // 512
        for pn in range(NP):
            pb = pan[pn % 2]
            for j in range(4):
                q = "sp" if j % 2 == 0 else "act"
                S.dma(q, pslot[pn % 2], pb[:, 4 * j:4 * j + 4, :], wv[:, 4 * j:4 * j + 4, pn * 512:(pn + 1) * 512],
                      writes=[("pan", pn % 2)])
            ps = bk[pn % 2]
            for k in range(KC):
                S.op("pe", lambda e, ps=ps, pb=pb, k=k: e.matmul(ps[0:1, :], sc[:, k:k + 1], pb[:, k, :],
                                                                 start=(k == 0), stop=(k == KC - 1)),
                     reads=["sc", ("pan", pn % 2)], writes=[("bank", pn % 2)])
            S.op("dve", lambda e, ps=ps, pn=pn: e.tensor_tensor(out=modrow[0:1, pn * 512:(pn + 1) * 512], in0=ps[0:1, :],
                                                                 in1=brow[0:1, pn * 512:(pn + 1) * 512], op=ALU.add),
                 reads=[("bank", pn % 2), "brow"], writes=["modrow"])
        mr = lambda m: modrow[0:1, m * D:(m + 1) * D]
        S.op("dve", lambda e: e.scalar_tensor_tensor(out=grow[0:1, 0:D], in0=mr(1), scalar=1.0, in1=nrow[0:1, 0:D],
                                                     op0=ALU.add, op1=ALU.mult),
             reads=["modrow", "nrow"], writes=["grow"])
        S.op("dve", lambda e: e.scalar_tensor_tensor(out=grow[0:1, D:2 * D], in0=mr(4), scalar=1.0, in1=nrow[0:1, D:2 * D],
                                                     op0=ALU.add, op1=ALU.mult),
             reads=["modrow", "nrow"], writes=["grow"])
        psc = bk[2]
        for k in range(KC):
            S.op("pe", lambda e, k=k: e.matmul(psc[:, k:k + 1], grow[0:1, k * 128:(k + 1) * 128], cx.ones_f[0:1, 0:1],
                                               start=True, stop=True),
                 reads=["grow", "ones_f"], writes=[("bank", 2)])
        for k in range(KC):
            S.op("pe", lambda e, k=k: e.matmul(psc[:, KC + k:KC + k + 1], modrow[0:1, k * 128:(k + 1) * 128],
                                               cx.ones_f[0:1, 0:1], start=True, stop=True),
                 reads=["modrow", "ones_f"], writes=[("bank", 2)])
        S.op("dve", lambda e: e.tensor_copy(out=cx.colT[:], in_=psc[:, 0:32]), reads=[("bank", 2)], writes=["colT"])
        rs = cx.slot_const
        S.dma("sp", rs, cx.W["rows"][0:1, :], mr(2), reads=["modrow"], writes=["rows_d"])
        S.dma("sp", rs, cx.W["rows"][1:2, :], grow[0:1, D:2 * D], reads=["grow"], writes=["rows_d"])
        S.dma("sp", rs, cx.W["rows"][2:3, :], mr(3), reads=["modrow"], writes=["rows_d"])
        S.dma("sp", rs, cx.W["rows"][3:4, :], mr(5), reads=["modrow"], writes=["rows_d"])
        S.op("dve", lambda e: e.tensor_tensor(out=prod[:, 0:1], in0=lamT[:, 0:1], in1=lamT[:, 1:2], op=ALU.mult),
             reads=["lamT"], writes=["prod"])
        S.op("dve", lambda e: e.tensor_tensor(out=prod[:, 1:2], in0=lamT[:, 2:3], in1=lamT[:, 3:4], op=ALU.mult),
             reads=["lamT", "prod"], writes=["prod"])
        S.op("pe", lambda e: e.matmul(bk[3][:, 0:2], cx.ones_f[:], prod[:], start=True, stop=True),
             reads=["ones_f", "prod"], writes=[("bank", 3)])
        S.op("act", lambda e: e.activation(out=ex[:], in_=bk[3][:, 0:2], func=AF.Exp), reads=[("bank", 3)], writes=["ex"])
        S.op("dve", lambda e: e.tensor_tensor(out=cx.neglam[:], in0=ex[:, 1:2], in1=ex[:, 0:1], op=ALU.subtract),
             reads=["ex"], writes=["neglam"])
        S.op("dve", lambda e: e.tensor_scalar(out=cx.neglam[:], in0=cx.neglam[:], scalar1=-LAMBDA_INIT, scalar2=None,
                                              op0=ALU.add),
             reads=["neglam"], writes=["neglam"])
        S.op("dve", lambda e: e.tensor_scalar(out=cx.sg[:], in0=sublnT[:], scalar1=(1.0 - LAMBDA_INIT), scalar2=None,
                                              op0=ALU.mult),
             reads=["sublnT"], writes=["sg"])
        if "d_mod" in cx.debug:
            S.dma("sp", cx.dbg_slot, cx.O["d_mod"][:, :], modrow[:], reads=["modrow"])
        if "d_colT" in cx.debug:
            S.dma("sp", cx.dbg_slot, cx.O["d_colT"][:, :], cx.colT[:], reads=["colT"])
            S.dma("sp", cx.dbg_slot, cx.O["d_misc"][:, 0:1], cx.neglam[:], reads=["neglam"], allow_slow_non_contiguous=True)
            S.dma("sp", cx.dbg_slot, cx.O["d_misc"][:, 1:3], cx.sg[:], reads=["sg"], allow_slow_non_contiguous=True)
        S.barrier()


def evac_copy(S, i, out, in_, reads, writes, func=None):
    if func is not None or i % 2 == 0:
        f = func if func is not None else AF.Copy
        return S.op("act", lambda e: e.activation(out=out, in_=in_, func=f), reads=reads, writes=writes)
    return S.op("dve", lambda e: e.tensor_copy(out=out, in_=in_), reads=reads, writes=writes)


def phaseA(cx):
    nc, S, I, W = cx.nc, cx.S, cx.I, cx.W
    bk = cx.banks
    with contextlib.ExitStack() as st:
        sb = lambda name, shape, dt: st.enter_context(nc.sbuf_tensor(name, list(shape), dt))
        uT = sb("A_uT", [128, KC, SEQ], BF16)
        with contextlib.ExitStack() as st1:
            sb1 = lambda name, shape, dt: st1.enter_context(nc.sbuf_tensor(name, list(shape), dt))
            xt = [sb1(f"A_x{i}", [128, D], F32) for i in range(3)]
            xn = [sb1(f"A_xn{i}", [128, D], BF16) for i in range(2)]
            junk = sb1("A_junk", [128, D], BF16)
            ss = sb1("A_ss", [128, 4], F32)
            rt = sb1("A_rt", [128, 4], F32)
            rs = sb1("A_rs", [128, 4], F32)
            xslot = [S.slot("x") for _ in range(3)]
            xv = I["xr"].rearrange("(t p) d -> t p d", p=128)
            NTT = SEQ // 128
            for tt in range(NTT):
                xb = xt[tt % 3]
                S.dma("sp", xslot[tt % 3], xb[:, 0:1024], xv[tt, :, 0:1024], writes=[("x", tt % 3)])
                S.dma("act", xslot[tt % 3], xb[:, 1024:2048], xv[tt, :, 1024:2048], writes=[("x", tt % 3)])
                j = tt % 4
                S.op("act", lambda e, xb=xb, j=j: e.activation(out=junk[:], in_=xb[:], func=AF.Square,
                                                              accum_out=ss[:, j:j + 1]),
                     reads=[("x", tt % 3)], writes=["junk", ("ss", j)])
                S.op("act", lambda e, j=j: e.activation(out=rt[:, j:j + 1], in_=ss[:, j:j + 1], func=AF.Sqrt,
                                                        scale=1.0 / D, bias=cx.epsc[:, 0:1]),
                     reads=[("ss", j)], writes=[("rt", j)])
                S.op("dve", lambda e, j=j: e.reciprocal(out=rs[:, j:j + 1], in_=rt[:, j:j + 1]),
                     reads=[("rt", j)], writes=[("rs", j)])
                xnb = xn[tt % 2]
                S.op("act", lambda e, xb=xb, xnb=xnb, j=j: e.activation(out=xnb[:], in_=xb[:], func=AF.Copy,
                                                                       scale=rs[:, j:j + 1]),
                     reads=[("x", tt % 3), ("rs", j)], writes=[("xn", tt % 2)])
                for half in range(2):
                    bi = (2 * tt + half) % 4
                    pst = bk[bi][:].bitcast(BF16)
                    for q in range(8):
                        k = half * 8 + q
                        S.op("pe", lambda e, pst=pst, q=q, k=k, xnb=xnb: e.transpose(
                            pst[:, q * 128:(q + 1) * 128], xnb[:, k * 128:(k + 1) * 128], cx.ident_b[:]),
                            reads=[("xn", tt % 2), "ident_b"], writes=[("bank", bi)])
                    for q in range(8):
                        k = half * 8 + q
                        S.op("dve", lambda e, pst=pst, q=q, k=k, tt=tt: e.tensor_scalar(
                            out=uT[:, k, tt * 128:(tt + 1) * 128], in0=pst[:, q * 128:(q + 1) * 128],
                            scalar1=cx.colT[:, k:k + 1], scalar2=cx.colT[:, KC + k:KC + k + 1],
                            op0=ALU.mult, op1=ALU.add),
                            reads=[("bank", bi), "colT"], writes=[("uT", tt // 4)])
        if "d_uT" in cx.debug:
            for k in range(KC):
                S.dma("sp", cx.dbg_slot, cx.O["d_uT"][:, k * 512:(k + 1) * 512], uT[:, k, 1024:1536],
                      reads=[("uT", c) for c in range(8)])
        S.barrier()
        wch = [sb(f"A_w{i}", [128, KC, 512], BF16) for i in range(2)]
        stg = [sb(f"A_stg{i}", [128, 512], BF16) for i in range(4)]
        wslot = [S.slot("w") for _ in range(2)]
        sslot = [S.slot("stg") for _ in range(4)]
        wv = I["w_in"].rearrange("(k p) n -> p k n", p=128)
        groups = []
        for g in range(2):
            groups.append(("qa", "f", [2, 3], W["QaT"], g, 1024))
        for g in range(2):
            groups.append(("ka", "f", list(range(6)), W["KaT"], g, 0))
        for g in range(2):
            groups.append(("va", "t", list(range(6)), W["Va"], g, 0))
        for g in range(2):
            groups.append(("qb", "f", [2, 3], W["QbT"], g, 1024))
        for g in range(2):
            groups.append(("kb", "f", list(range(8)), W["KbT"], g, 0))
        for g in range(2):
            groups.append(("vb", "t", list(range(8)), W["Vb"], g, 0))
        for g in range(8):
            groups.append(("gt", "f", [2, 3], W["GT"], g, 1024))
        ev = 0
        for cg, (name, kind, tcs, dst, g, tok0) in enumerate(groups):
            wb = wch[cg % 2]
            for j in range(4):
                S.dma("pool", wslot[cg % 2], wb[:, 4 * j:4 * j + 4, :], wv[:, 4 * j:4 * j + 4, cg * 512:(cg + 1) * 512],
                      writes=[("w", cg % 2)])
            func = AF.Sigmoid if name == "gt" else None
            if kind == "f":
                for s4 in range(4):
                    for tc in tcs:
                        bi = ev % 4
                        ps = bk[bi]
                        for k in range(KC):
                            S.op("pe", lambda e, ps=ps, wb=wb, k=k, s4=s4, tc=tc: e.matmul(
                                ps[:, :], wb[:, k, s4 * 128:(s4 + 1) * 128], uT[:, k, tc * 512:(tc + 1) * 512],
                                start=(k == 0), stop=(k == KC - 1)),
                                reads=[("w", cg % 2), ("uT", tc)], writes=[("bank", bi)])
                        sg = stg[ev % 4]
                        evac_copy(S, ev, sg[:], ps[:, :], [("bank", bi)], [("stg", ev % 4)], func=func)
                        row0 = g * 512 + s4 * 128
                        col0 = tc * 512 - tok0
                        S.dma("sp", sslot[ev % 4], dst[row0:row0 + 128, col0:col0 + 512], sg[:],
                              reads=[("stg", ev % 4)])
                        ev += 1
            else:
                for tc in tcs:
                    for t4 in range(4):
                        tt = tc * 4 + t4
                        bi = ev % 4
                        ps = bk[bi]
                        for k in range(KC):
                            S.op("pe", lambda e, ps=ps, wb=wb, k=k, tt=tt: e.matmul(
                                ps[:, :], uT[:, k, tt * 128:(tt + 1) * 128], wb[:, k, :],
                                start=(k == 0), stop=(k == KC - 1)),
                                reads=[("w", cg % 2), ("uT", tc)], writes=[("bank", bi)])
                        sg = stg[ev % 4]
                        evac_copy(S, ev, sg[:], ps[:, :], [("bank", bi)], [("stg", ev % 4)])
                        S.dma("sp", sslot[ev % 4], dst[tt * 128:(tt + 1) * 128, g * 512:(g + 1) * 512], sg[:],
                              reads=[("stg", ev % 4)])
                        ev += 1


def attention(cx, mixer):
    nc, S, I, W = cx.nc, cx.S, cx.I, cx.W
    bk = cx.banks
    isB = mixer == "B"
    nheads = 4 if isB else 8
    nkt = 32 if isB else 24
    ncomp = 2 if isB else 1
    nv = 2 if isB else 1
    vw = 1024
    with contextlib.ExitStack() as st:
        sb = lambda name, shape, dt: st.enter_context(nc.sbuf_tensor(name, list(shape), dt))
        if isB:
            vbuf = [sb(f"B_v{i}", [128, nkt, 256], BF16) for i in range(2)]
        else:
            v_all = sb("A_v", [128, nkt, vw], BF16)
        yT = sb(mixer + "_yT", [128, 8, OWN], BF16)
        ktb = [sb(f"{mixer}_kt{i}", [128, ncomp, nkt * 128], BF16) for i in range(2)]
        qtb = [sb(f"{mixer}_qt{i}", [128, ncomp, OWN], BF16) for i in range(2)]
        if isB:
            strip = sb("B_strip", [128, 4 * 1920], F32)
            stripb = [strip, strip]
        else:
            stripb = [sb(f"A_strip{i}", [128, 3968], F32) for i in range(2)]
            kbias = sb("A_kbias", [128, 24], F32)
        NS = 5 if isB else 6
        LA = 3
        NB = 6
        tmpb = [sb(f"{mixer}_tmp{i}", [128, 512], F32) for i in range(NB)]
        eb = [sb(f"{mixer}_e{i}", [128, 512], BF16) for i in range(NB)]
        rcp = [sb(f"{mixer}_rcp{i}", [128, 512], F32) for i in range(2)]
        if isB:
            onb = [sb(f"B_on{i}", [128, 2, 2, 512], F32) for i in range(2)]
            diff = sb("B_diff", [128, 2, 512], F32)
            sq = sb("B_sq", [128, 2, 512], BF16)
            rt = sb("B_rt", [128, 512], F32)
            rr = sb("B_rr", [128, 512], F32)
        vslot = S.slot("v")
        hslot = [S.slot("hd") for _ in range(2)]
        vsrc = (W["Vb"] if isB else W["Va"])[0:nkt * 128, :].rearrange("(t p) c -> p t c", p=128)
        if not isB:
            for j in range(0, nkt, 4):
                S.dma("sp" if (j // 4) % 2 == 0 else "act", vslot, v_all[:, j:j + 4, :], vsrc[:, j:j + 4, :], writes=["v_all"])
        if isB:
            for j in range(4):
                S.dma("sp", vslot, strip[:, j * 1920:(j + 1) * 1920], I["distB"][:, j * 1920:(j + 1) * 1920], writes=["strip"])
        else:
            S.dma("sp", vslot, kbias[:], I["kbiasA"][:, :], writes=["kbias"])
        KT = W["KbT"] if isB else W["KaT"]
        QT = W["QbT"] if isB else W["QaT"]
        it = 0
        accset = 0
        for h in range(nheads):
            par = h % 2
            for c in range(ncomp):
                row = (h * ncomp + c) * 128
                for j in range(0, nkt * 128, 1024):
                    S.dma("sp" if c == 0 else "act", hslot[par], ktb[par][:, c, j:j + 1024], KT[row:row + 128, j:j + 1024],
                          writes=[("hd", par)])
                S.dma("act", hslot[par], qtb[par][:, c, :], QT[row:row + 128, :], writes=[("hd", par)])
            if not isB:
                S.dma("sp", hslot[par], stripb[par][:], I["stripAh"][h], writes=[("hd", par)])
            else:
                for j in range(0, nkt, 8):
                    S.dma("sp", hslot[par], vbuf[par][:, j:j + 8, :], vsrc[:, j:j + 8, h * 256:(h + 1) * 256],
                          writes=[("hd", par)])
            hk = [("hd", par), "v_all", "strip", "kbias"]
            slope = SLOPES_B[h] if isB else SLOPES_A[h]
            for qc in range(2):
                for c in range(ncomp):
                    abase = NS
                    O = [bk[abase + v] for v in range(nv)]
                    Dn = bk[abase + nv]
                    akeys = [("bank", abase + v) for v in range(nv)] + [("bank", abase + nv)]
                    if isB:
                        kts = list(range(nkt))
                    else:
                        kts = [kt for kt in range(nkt)
                               if not (128 * kt - 1024 - 512 * qc - 511 > 1024 or 128 * kt + 127 - 1024 - 512 * qc < -1024)]
                    pend = []

                    def emit_pv(first, last, kt, ebb, ei, O=O, Dn=Dn, akeys=akeys, par=par, h=h):
                        for v in range(nv):
                            col = (v * 128) if isB else (h * 128)
                            vt = vbuf[par] if isB else v_all
                            S.op("pe", lambda e, v=v, kt=kt, col=col, ebb=ebb, first=first, last=last, vt=vt: e.matmul(
                                O[v][:, :], vt[:, kt, col:col + 128], ebb[:], start=first, stop=last),
                                reads=[("e", ei), ("hd", par), "v_all"], writes=[akeys[v]])
                        S.op("pe", lambda e, ebb=ebb, first=first, last=last: e.matmul(
                            Dn[:, :], cx.ones_b[:], ebb[:], start=first, stop=last),
                            reads=[("e", ei), "ones_b"], writes=[akeys[-1]])

                    for ki, kt in enumerate(kts):
                        sbk = it % NS
                        ps = bk[sbk]
                        S.op("pe", lambda e, ps=ps, par=par, c=c, kt=kt, qc=qc: e.matmul(
                            ps[:, :], ktb[par][:, c, kt * 128:(kt + 1) * 128], qtb[par][:, c, qc * 512:(qc + 1) * 512],
                            start=True, stop=True), reads=hk, writes=[("bank", sbk)])
                        tb = tmpb[it % NB]
                        if isB:
                            g, ktp = kt // 8, kt % 8
                            x0 = g * 1920 + 512 * qc - 128 * ktp + 896
                            c1 = -SCALE / slope
                        else:
                            x0 = 512 * qc - 128 * kt + 2944
                            c1 = SCALE
                        sp_ = stripb[par]
                        S.op("dve", lambda e, tb=tb, ps=ps, sp_=sp_, x0=x0, c1=c1: e.scalar_tensor_tensor(
                            out=tb[:], in0=ps[:, :], scalar=c1, in1=sp_[:, x0:x0 + 512], op0=ALU.mult, op1=ALU.add),
                            reads=[("bank", sbk)] + hk, writes=[("tmp", it % NB)])
                        ebb = eb[it % NB]
                        if isB:
                            S.op("act", lambda e, ebb=ebb, tb=tb, slope=slope: e.activation(
                                out=ebb[:], in_=tb[:], func=AF.Exp, scale=-slope),
                                reads=[("tmp", it % NB)], writes=[("e", it % NB)])
                        else:
                            S.op("act", lambda e, ebb=ebb, tb=tb, kt=kt: e.activation(
                                out=ebb[:], in_=tb[:], func=AF.Exp, bias=kbias[:, kt:kt + 1]),
                                reads=[("tmp", it % NB)] + hk, writes=[("e", it % NB)])
                        first, last = (ki == 0), (ki == len(kts) - 1)
                        pend.append((first, last, kt, ebb, it % NB))
                        it += 1
                        if len(pend) > LA:
                            emit_pv(*pend.pop(0))
                    while pend:
                        emit_pv(*pend.pop(0))
                    rc = rcp[accset]
                    S.op("dve", lambda e, rc=rc, Dn=Dn: e.reciprocal(out=rc[:], in_=Dn[:, :]),
                         reads=[akeys[-1]], writes=[("rcp", accset)])
                    for v in range(nv):
                        if isB:
                            dst = onb[qc % 2][:, c, v, :]
                            wk = ("on", qc % 2, c)
                        else:
                            dst = yT[:, h, qc * 512:(qc + 1) * 512]
                            wk = ("yT", mixer)
                        S.op("dve", lambda e, dst=dst, O=O, v=v, rc=rc: e.tensor_tensor(
                            out=dst, in0=O[v][:, :], in1=rc[:], op=ALU.mult),
                            reads=[akeys[v], ("rcp", accset)], writes=[wk])
                    accset ^= 1
                if isB:
                    on = onb[qc % 2]
                    ok = [("on", qc % 2, 0), ("on", qc % 2, 1)]
                    for v in range(2):
                        S.op("dve", lambda e, on=on, v=v: e.scalar_tensor_tensor(
                            out=diff[:, v, :], in0=on[:, 1, v, :], scalar=cx.neglam[:, 0:1], in1=on[:, 0, v, :],
                            op0=ALU.mult, op1=ALU.add), reads=ok + ["neglam"], writes=["diff"])
                    S.op("act", lambda e: e.activation(out=sq[:], in_=diff[:], func=AF.Square), reads=["diff"], writes=["sq"])
                    sbk = it % NS
                    it += 1
                    ps = bk[sbk]
                    for v in range(2):
                        S.op("pe", lambda e, ps=ps, v=v: e.matmul(ps[:, :], cx.ones_b[:], sq[:, v, :], start=(v == 0), stop=(v == 1)),
                             reads=["sq", "ones_b"], writes=[("bank", sbk)])
                    S.op("act", lambda e, ps=ps: e.activation(out=rt[:], in_=ps[:, :], func=AF.Sqrt, scale=1.0 / 256,
                                                             bias=cx.epsc[:, 0:1]),
                         reads=[("bank", sbk), "epsc"], writes=["rt"])
                    S.op("dve", lambda e: e.reciprocal(out=rr[:], in_=rt[:]), reads=["rt"], writes=["rr"])
                    for v in range(2):
                        S.op("dve", lambda e, v=v, h=h, qc=qc: e.scalar_tensor_tensor(
                            out=yT[:, 2 * h + v, qc * 512:(qc + 1) * 512], in0=diff[:, v, :], scalar=cx.sg[:, v:v + 1],
                            in1=rr[:], op0=ALU.mult, op1=ALU.mult), reads=["diff", "rr", "sg"], writes=[("yT", mixer)])
        ydst = W["YbT"] if isB else W["YaT"]
        for j in range(8):
            S.dma("sp", vslot, ydst[j * 128:(j + 1) * 128, :], yT[:, j, :], reads=[("yT", mixer)])
        if ("d_y" + mixer) in cx.debug:
            for j in range(8):
                S.dma("sp", cx.dbg_slot, cx.O["d_y" + mixer][j * 128:(j + 1) * 128, :], yT[:, j, :], reads=[("yT", mixer)])
    S.barrier()


def phaseD(cx):
    nc, S, I, W = cx.nc, cx.S, cx.I, cx.W
    bk = cx.banks
    with contextlib.ExitStack() as st:
        sb = lambda name, shape, dt: st.enter_context(nc.sbuf_tensor(name, list(shape), dt))
        with contextlib.ExitStack() as st1:
            sb1 = lambda name, shape, dt: st1.enter_context(nc.sbuf_tensor(name, list(shape), dt))
            mT = sb1("D_mT", [128, KC, OWN], BF16)
            ya = sb1("D_ya", [128, 8, OWN], BF16)
            yb = sb1("D_yb", [128, 8, OWN], BF16)
            woa = sb1("D_woa", [128, 8, D], BF16)
            wob = sb1("D_wob", [128, 8, D], BF16)
            gat = [sb1(f"D_g{i}", [128, 2, 512], BF16) for i in range(2)]
            t1 = [sb1(f"D_t1{i}", [128, 512], F32) for i in range(2)]
            t2 = [sb1(f"D_t2{i}", [128, 512], F32) for i in range(2)]
            ls = S.slot("D1")
            gs = [S.slot("D1g") for _ in range(2)]
            for j in range(8):
                S.dma("sp", ls, ya[:, j, :], W["YaT"][j * 128:(j + 1) * 128, :], writes=["ya"])
                S.dma("act", ls, yb[:, j, :], W["YbT"][j * 128:(j + 1) * 128, :], writes=["yb"])
            wav = I["w_out_a"].rearrange("(k p) n -> p k n", p=128)
            wbv = I["w_out_b"].rearrange("(k p) n -> p k n", p=128)
            for j in range(0, 8, 2):
                S.dma("pool", ls, woa[:, j:j + 2, :], wav[:, j:j + 2, :], writes=["woa"])
                S.dma("pool", ls, wob[:, j:j + 2, :], wbv[:, j:j + 2, :], writes=["wob"])
            if "d_ya" in cx.debug:
                for j in range(8):
                    S.dma("sp", cx.dbg_slot, cx.O["d_ya"][j * 128:(j + 1) * 128, :], ya[:, j, :], reads=["ya"])
                    S.dma("sp", cx.dbg_slot, cx.O["d_yb"][j * 128:(j + 1) * 128, :], yb[:, j, :], reads=["yb"])
            it = 0
            for dc in range(KC):
                for qc in range(2):
                    gb_ = gat[it % 2]
                    S.dma("sp", gs[it % 2], gb_[:, 0, :], W["GT"][dc * 128:(dc + 1) * 128, qc * 512:(qc + 1) * 512],
                          writes=[("gat", it % 2)])
                    S.dma("sp", gs[it % 2], gb_[:, 1, :], W["GT"][2048 + dc * 128:2048 + (dc + 1) * 128, qc * 512:(qc + 1) * 512],
                          writes=[("gat", it % 2)])
                    pa, pb = bk[(2 * it) % 4], bk[(2 * it + 1) % 4]
                    for f in range(8):
                        S.op("pe", lambda e, pa=pa, f=f, dc=dc, qc=qc: e.matmul(
                            pa[:, :], woa[:, f, dc * 128:(dc + 1) * 128], ya[:, f, qc * 512:(qc + 1) * 512],
                            start=(f == 0), stop=(f == 7)), reads=["woa", "ya"], writes=[("bank", (2 * it) % 4)])
                    for f in range(8):
                        S.op("pe", lambda e, pb=pb, f=f, dc=dc, qc=qc: e.matmul(
                            pb[:, :], wob[:, f, dc * 128:(dc + 1) * 128], yb[:, f, qc * 512:(qc + 1) * 512],
                            start=(f == 0), stop=(f == 7)), reads=["wob", "yb"], writes=[("bank", (2 * it + 1) % 4)])
                    a1, a2 = t1[it % 2], t2[it % 2]
                    S.op("dve", lambda e, a1=a1, pa=pa, gb_=gb_: e.tensor_tensor(out=a1[:], in0=pa[:, :], in1=gb_[:, 0, :], op=ALU.mult),
                         reads=[("bank", (2 * it) % 4), ("gat", it % 2)], writes=[("t1", it % 2)])
                    S.op("dve", lambda e, a2=a2, pb=pb, gb_=gb_: e.tensor_tensor(out=a2[:], in0=pb[:, :], in1=gb_[:, 1, :], op=ALU.mult),
                         reads=[("bank", (2 * it + 1) % 4), ("gat", it % 2)], writes=[("t2", it % 2)])
                    if "d_gat" in cx.debug and it == 0:
                        S.dma("sp", cx.dbg_slot, cx.O["d_gat"][:, :], gb_[:].rearrange("p a b -> p (a b)"), reads=[("gat", 0)])
                        S.dma("sp", cx.dbg_slot, cx.O["d_a1"][:, 0:512], a1[:], reads=[("t1", 0)])
                        S.dma("sp", cx.dbg_slot, cx.O["d_a1"][:, 512:1024], a2[:], reads=[("t2", 0)])
                    S.op("dve", lambda e, a1=a1, a2=a2, dc=dc, qc=qc: e.tensor_tensor(
                        out=mT[:, dc, qc * 512:(qc + 1) * 512], in0=a1[:], in1=a2[:], op=ALU.add),
                        reads=[("t1", it % 2), ("t2", it % 2)], writes=["mT"])
                    it += 1
            for k in range(KC):
                S.dma("sp", ls, W["MT"][k * 128:(k + 1) * 128, :], mT[:, k, :], reads=["mT"])
                if "d_mT" in cx.debug:
                    S.dma("sp", cx.dbg_slot, cx.O["d_mT"][k * 128:(k + 1) * 128, :], mT[:, k, :], reads=["mT"])
        S.barrier()
        h = sb("D_h", [128, 8, D], F32)
        rowB = sb("D_rowB", [128, D], F32)
        rowC = sb("D_rowC", [128, D], F32)
        st2 = contextlib.ExitStack()
        sb2 = lambda name, shape, dt: st2.enter_context(nc.sbuf_tensor(name, list(shape), dt))
        mT2 = sb2("D_mT2", [128, KC, OWN], BF16)
        wo = sb2("D_wo", [128, KC, D], BF16)
        tq = [sb2(f"D_tq{i}", [128, 512], F32) for i in range(2)]
        ls = S.slot("D2")
        for k in range(KC):
            S.dma("act", ls, mT2[:, k, :], W["MT"][k * 128:(k + 1) * 128, :], writes=["mT2"])
        wov = I["w_o"].rearrange("(k p) n -> p k n", p=128)
        for j in range(0, KC, 2):
            S.dma("pool", ls, wo[:, j:j + 2, :], wov[:, j:j + 2, :], writes=["wo"])
        xo = I["xr"][1024:2048, :].rearrange("(t p) d -> p t d", p=128)
        for tt in range(8):
            S.dma("sp" if tt % 2 == 0 else "act", ls, h[:, tt, :], xo[:, tt, :], writes=[("h", tt)])
        S.dma("sp", ls, rowB[:], W["rows"][0:1, :].partition_broadcast(128), writes=["rowB"])
        S.dma("sp", ls, rowC[:], W["rows"][2:3, :].partition_broadcast(128), writes=["rowC"])
        it = 0
        for tt in range(8):
            for ch in range(4):
                ps = bk[it % 4]
                for k in range(KC):
                    S.op("pe", lambda e, ps=ps, k=k, tt=tt, ch=ch: e.matmul(
                        ps[:, :], mT2[:, k, tt * 128:(tt + 1) * 128], wo[:, k, ch * 512:(ch + 1) * 512],
                        start=(k == 0), stop=(k == KC - 1)), reads=["wo", "mT2"], writes=[("bank", it % 4)])
                tb = tq[it % 2]
                S.op("dve", lambda e, tb=tb, ps=ps, ch=ch: e.tensor_tensor(
                    out=tb[:], in0=ps[:, :], in1=rowB[:, ch * 512:(ch + 1) * 512], op=ALU.mult),
                    reads=[("bank", it % 4), "rowB"], writes=[("tq", it % 2)])
                S.op("pool", lambda e, tb=tb, tt=tt, ch=ch: e.tensor_tensor(
                    out=h[:, tt, ch * 512:(ch + 1) * 512], in0=h[:, tt, ch * 512:(ch + 1) * 512], in1=tb[:], op=ALU.add),
                    reads=[("tq", it % 2), ("h", tt)], writes=[("h", tt)])
                it += 1
        if "d_rowB" in cx.debug:
            S.dma("sp", cx.dbg_slot, cx.O["d_rowB"][:, :], rowB[:], reads=["rowB"])
        hs = S.slot("hs")
        for tt in range(8):
            S.dma("sp", hs, W["hs"][tt * 128:(tt + 1) * 128, :], h[:, tt, :], reads=[("h", tt)])
            if "d_h1" in cx.debug:
                S.dma("sp", cx.dbg_slot, cx.O["d_h1"][tt * 128:(tt + 1) * 128, :], h[:, tt, :], reads=[("h", tt)])
        S.barrier()
        st2.close()
        S.dma("sp", ls, rowB[:], W["rows"][1:2, :].partition_broadcast(128), reads=[], writes=["rowB"])
        wr = sb("D_wr", [128, KC, N_EXP], F32)
        brB = sb("D_brB", [128, N_EXP], F32)
        triu = sb("D_triu", [128, 128], BF16)
        triu_f = sb("D_triuf", [128, 128], F32)
        S.dma("sp", ls, wr[:], I["w_router"].rearrange("(k p) n -> p k n", p=128), writes=["wr"])
        S.dma("sp", ls, brB[:], I["b_routerB"][:, :], writes=["brB"])
        S.dma("sp", ls, triu_f[:], I["triu"][:, :], writes=["triu_f"])
        S.op("dve", lambda e: e.tensor_copy(out=triu[:], in_=triu_f[:]), reads=["triu_f"], writes=["triu"])
        u2f = [sb(f"D_u2f{i}", [128, D], F32) for i in range(2)]
        u2b = [sb(f"D_u2b{i}", [128, D], BF16) for i in range(2)]
        u2T = [sb(f"D_u2T{i}", [128, KC, 128], F32) for i in range(2)]
        ss = sb("D_ss", [128, 8], F32)
        rt = sb("D_rt", [128, 8], F32)
        rs = sb("D_rs", [128, 8], F32)
        lg = sb("D_lg", [128, 8, N_EXP], F32)
        mx8 = sb("D_mx8", [128, 8, 8], F32)
        negm = sb("D_negm", [128, 8], F32)
        mask = sb("D_mask", [128, 8, N_EXP], F32)
        mask_b = sb("D_maskb", [128, 8, N_EXP], BF16)
        ex = sb("D_ex", [128, 8, N_EXP], F32)
        sm = sb("D_sm", [128, 8], F32)
        rsm = sb("D_rsm", [128, 8], F32)
        us = [S.slot("u2") for _ in range(2)]
        pit = 0
        for tt in range(8):
            uf, ub, uT_ = u2f[tt % 2], u2b[tt % 2], u2T[tt % 2]
            S.op("act", lambda e, ub=ub, tt=tt: e.activation(out=ub[:], in_=h[:, tt, :], func=AF.Square, accum_out=ss[:, tt:tt + 1]),
                 reads=[("h", tt)], writes=[("u2b", tt % 2), ("ss", tt)])
            S.op("act", lambda e, tt=tt: e.activation(out=rt[:, tt:tt + 1], in_=ss[:, tt:tt + 1], func=AF.Sqrt, scale=1.0 / D,
                                                      bias=cx.epsc[:, 0:1]), reads=[("ss", tt), "epsc"], writes=[("rt", tt)])
            S.op("dve", lambda e, tt=tt: e.reciprocal(out=rs[:, tt:tt + 1], in_=rt[:, tt:tt + 1]), reads=[("rt", tt)], writes=[("rs", tt)])
            S.op("dve", lambda e, uf=uf, tt=tt: e.scalar_tensor_tensor(out=uf[:], in0=h[:, tt, :], scalar=rs[:, tt:tt + 1], in1=rowB[:],
                                                                       op0=ALU.mult, op1=ALU.mult),
                 reads=[("h", tt), ("rs", tt), "rowB"], writes=[("u2f", tt % 2)])
            S.op("pool", lambda e, uf=uf: e.tensor_tensor(out=uf[:], in0=uf[:], in1=rowC[:], op=ALU.add),
                 reads=[("u2f", tt % 2), "rowC"], writes=[("u2f", tt % 2)])
            S.op("act", lambda e, ub=ub, uf=uf: e.activation(out=ub[:], in_=uf[:], func=AF.Copy),
                 reads=[("u2f", tt % 2)], writes=[("u2b", tt % 2)])
            S.dma("sp", us[tt % 2], W["U2"][tt * 128:(tt + 1) * 128, :], ub[:], reads=[("u2b", tt % 2)])
            for q4 in range(4):
                bi = pit % 4
                pit += 1
                ps = bk[bi]
                for q in range(4):
                    k = q4 * 4 + q
                    S.op("pe", lambda e, ps=ps, q=q, k=k, uf=uf: e.transpose(ps[:, q * 128:(q + 1) * 128], uf[:, k * 128:(k + 1) * 128],
                                                                              cx.ident_f[:]),
                         reads=[("u2f", tt % 2), "ident_f"], writes=[("bank", bi)])
                evac_copy(S, q4, uT_[:, q4 * 4:(q4 + 1) * 4, :], ps[:, :].rearrange("p (a b) -> p a b", a=4), [("bank", bi)], [("u2T", tt % 2)])
            psl = bk[4 + tt % 2]
            for k in range(KC):
                S.op("pe", lambda e, psl=psl, k=k, uT_=uT_: e.matmul(psl[:, 0:N_EXP], uT_[:, k, :], wr[:, k, :],
                                                                     start=(k == 0), stop=(k == KC - 1)),
                     reads=[("u2T", tt % 2), "wr"], writes=[("bank", 4 + tt % 2)])
            S.op("dve", lambda e, psl=psl, tt=tt: e.tensor_tensor(out=lg[:, tt, :], in0=psl[:, 0:N_EXP], in1=brB[:], op=ALU.add),
                 reads=[("bank", 4 + tt % 2), "brB"], writes=[("lg", tt)])
            S.op("dve", lambda e, tt=tt: e.max(out=mx8[:, tt, :], in_=lg[:, tt, :]), reads=[("lg", tt)], writes=[("mx8", tt)])
            S.op("dve", lambda e, tt=tt: e.tensor_scalar(out=mask[:, tt, :], in0=lg[:, tt, :], scalar1=mx8[:, tt, 3:4], scalar2=None,
                                                         op0=ALU.is_ge), reads=[("lg", tt), ("mx8", tt)], writes=[("mask", tt)])
            S.op("dve", lambda e, tt=tt: e.tensor_scalar(out=negm[:, tt:tt + 1], in0=mx8[:, tt, 0:1], scalar1=-1.0, scalar2=None,
                                                         op0=ALU.mult), reads=[("mx8", tt)], writes=[("negm", tt)])
            S.op("act", lambda e, tt=tt: e.activation(out=ex[:, tt, :], in_=lg[:, tt, :], func=AF.Exp, bias=negm[:, tt:tt + 1]),
                 reads=[("lg", tt), ("negm", tt)], writes=[("ex", tt)])
            S.op("dve", lambda e, tt=tt: e.tensor_tensor(out=ex[:, tt, :], in0=ex[:, tt, :], in1=mask[:, tt, :], op=ALU.mult),
                 reads=[("ex", tt), ("mask", tt)], writes=[("ex", tt)])
            S.op("dve", lambda e, tt=tt: e.reduce_sum(out=sm[:, tt:tt + 1], in_=ex[:, tt, :], axis=AX.X),
                 reads=[("ex", tt)], writes=[("sm", tt)])
            S.op("dve", lambda e, tt=tt: e.reciprocal(out=rsm[:, tt:tt + 1], in_=sm[:, tt:tt + 1]), reads=[("sm", tt)], writes=[("rsm", tt)])
            S.op("dve", lambda e, tt=tt: e.tensor_scalar(out=cx.prob[:, tt, :], in0=ex[:, tt, :], scalar1=rsm[:, tt:tt + 1], scalar2=None,
                                                         op0=ALU.mult), reads=[("ex", tt), ("rsm", tt)], writes=[("prob", tt)])
            S.op("dve", lambda e, tt=tt: e.tensor_copy(out=cx.prob_hi[:, tt, :], in_=cx.prob[:, tt, :]), reads=[("prob", tt)], writes=[("phi", tt)])
            S.op("dve", lambda e, tt=tt: e.tensor_tensor(out=cx.prob_lo[:, tt, :], in0=cx.prob[:, tt, :], in1=cx.prob_hi[:, tt, :],
                                                         op=ALU.subtract), reads=[("prob", tt), ("phi", tt)], writes=[("plo", tt)])
            S.op("dve", lambda e, tt=tt: e.tensor_copy(out=mask_b[:, tt, :], in_=mask[:, tt, :]), reads=[("mask", tt)], writes=[("maskb", tt)])
            pp = bk[6 + tt % 2]
            S.op("pe", lambda e, pp=pp, tt=tt: e.matmul(pp[:, 0:N_EXP], triu[:], mask_b[:, tt, :], start=True, stop=(tt == 0)),
                 reads=["triu", ("maskb", tt)], writes=[("bank", 6 + tt % 2)])
            for t2_ in range(tt):
                S.op("pe", lambda e, pp=pp, t2_=t2_, tt=tt: e.matmul(pp[:, 0:N_EXP], cx.ones_b[:], mask_b[:, t2_, :], start=False,
                                                                    stop=(t2_ == tt - 1)),
                     reads=["ones_b", ("maskb", t2_)], writes=[("bank", 6 + tt % 2)])
            S.op("dve", lambda e, pp=pp, tt=tt: e.tensor_tensor(out=cx.posm[:, tt, :], in0=pp[:, 0:N_EXP], in1=mask[:, tt, :], op=ALU.mult),
                 reads=[("bank", 6 + tt % 2), ("mask", tt)], writes=[("posm", tt)])
        if "d_lg" in cx.debug:
            S.dma("sp", cx.dbg_slot, cx.O["d_lg"][:, :], lg[:].rearrange("p a b -> p (a b)"), reads=[("lg", t) for t in range(8)])
            S.dma("sp", cx.dbg_slot, cx.O["d_posm"][:, :], cx.posm[:].rearrange("p a b -> p (a b)"), reads=[("posm", t) for t in range(8)])
            S.dma("sp", cx.dbg_slot, cx.O["d_prob"][:, :], cx.prob[:].rearrange("p a b -> p (a b)"), reads=[("prob", t) for t in range(8)])
    S.barrier()


def phaseE1(cx):
    nc, S, I, W = cx.nc, cx.S, cx.I, cx.W
    bk = cx.banks
    I32 = mybir.dt.int32
    NR = 5
    NST = CAP // 128
    with contextlib.ExitStack() as st:
        sb = lambda name, shape, dt: st.enter_context(nc.sbuf_tensor(name, list(shape), dt))
        ring = [sb(f"E_w{i}", [128, KC, 512], BF16) for i in range(NR)]
        rslot = [S.slot("ew") for _ in range(NR)]
        slb = [sb(f"E_sel{i}", [128, 8, CAP], BF16) for i in range(2)]
        xg = [[sb(f"E_xg{i}_{j}", [128, D], BF16) for j in range(NST)] for i in range(2)]
        xslot = [S.slot("xg") for _ in range(2)]
        pz = [sb(f"E_pz{i}", [128, 16], F32) for i in range(2)]
        idf = [sb(f"E_idf{i}", [128, NST], F32) for i in range(2)]
        idi = [sb(f"E_idi{i}", [128, NST], I32) for i in range(2)]
        tokab = sb("E_tokab", [128, 8, 2], BF16)
        tokab_f = sb("E_tokabf", [128, 8, 2], F32)
        x_ = sb("E_xe", [128, KC, CAP], BF16)
        a_ = sb("E_act", [128, KC, CAP], BF16)
        ye = sb("E_ye", [128, NST, D], BF16)
        yslot = S.slot("ye")
        bgT = sb("E_bgT", [128, N_EXP * KC], F32)
        buT = sb("E_buT", [128, N_EXP * KC], F32)
        gsb = [sb(f"E_g{i}", [128, CAP], F32) for i in range(2)]
        sig = [sb(f"E_s{i}", [128, CAP], F32) for i in range(2)]
        usb = [sb(f"E_u{i}", [128, CAP], F32) for i in range(2)]
        ls = S.slot("E1")
        S.op("dve", lambda en: en.memset(ye[:, 0, :], 0.0), writes=["ye"])
        S.dma("sp", ls, W["Y"][N_EXP * CAP:N_EXP * CAP + 128, :], ye[:, 0, :], reads=["ye"])
        S.dma("sp", ls, bgT[:], I["b_gateT"][:, :], writes=["bgT"])
        S.dma("sp", ls, buT[:], I["b_upT"][:, :], writes=["buT"])
        S.dma("sp", ls, tokab_f[:].rearrange("p a b -> p (a b)"), I["tokab"][:, :], writes=["tokab_f"])
        S.op("dve", lambda en: en.tensor_copy(out=tokab[:], in_=tokab_f[:]), reads=["tokab_f"], writes=["tokab"])
        wg = I["w_gate"].rearrange("e (k p) n -> e p k n", p=128)
        wu = I["w_up"].rearrange("e (k p) n -> e p k n", p=128)
        wd = I["w_down"].rearrange("e (k p) n -> e p k n", p=128)
        cnt = {"piece": 0}

        def load_piece(src, e, g):
            ri = cnt["piece"] % NR
            cnt["piece"] += 1
            for j in range(4):
                S.dma("pool", rslot[ri], ring[ri][:, 4 * j:4 * j + 4, :], src[e, :, 4 * j:4 * j + 4, g * 512:(g + 1) * 512],
                      writes=[("ring", ri)])
            return ri

        def prep(e):
            par = e % 2
            sl = slb[par]
            for tt in range(8):
                S.op("dve", lambda en, tt=tt, e=e, sl=sl: en.tensor_scalar(
                    out=sl[:, tt, :], in0=cx.iota[:], scalar1=cx.posm[:, tt, e:e + 1], scalar2=None, op0=ALU.is_equal),
                    reads=["iota", ("posm", tt)], writes=[("sel", par)])
            ps = bk[1]
            for st_ in range(NST):
                n = 0
                for tt in range(8):
                    for pr in (cx.prob_hi, cx.prob_lo):
                        S.op("pe", lambda en, st_=st_, tt=tt, pr=pr, e=e, n=n, sl=sl: en.matmul(
                            ps[:, st_:st_ + 1], sl[:, tt, st_ * 128:(st_ + 1) * 128], pr[:, tt, e:e + 1],
                            start=(n == 0), stop=(n == 15)),
                            reads=[("sel", par), ("phi", tt), ("plo", tt)], writes=[("bank", 1)])
                        n += 1
                for tt in range(8):
                    S.op("pe", lambda en, st_=st_, tt=tt, sl=sl: en.matmul(
                        ps[:, 8 + 2 * st_:10 + 2 * st_], sl[:, tt, st_ * 128:(st_ + 1) * 128], tokab[:, tt, :],
                        start=(tt == 0), stop=(tt == 7)),
                        reads=[("sel", par), "tokab"], writes=[("bank", 1)])
            pzz = pz[par]
            S.op("dve", lambda en, pzz=pzz: en.tensor_copy(out=pzz[:], in_=ps[:, 0:16]), reads=[("bank", 1)], writes=[("pz", par)])
            idf_, idi_ = idf[par], idi[par]
            S.op("dve", lambda en, pzz=pzz, idf_=idf_: en.scalar_tensor_tensor(
                out=idf_[:], in0=pzz[:, 8:16:2], scalar=32.0, in1=pzz[:, 9:16:2], op0=ALU.mult, op1=ALU.add),
                reads=[("pz", par)], writes=[("idf", par)])
            S.op("dve", lambda en, idf_=idf_, idi_=idi_: en.tensor_copy(out=idi_[:], in_=idf_[:]), reads=[("idf", par)], writes=[("idi", par)])
            for st_ in range(NST):
                def fn(en, st_=st_, par=par, idi_=idi_):
                    return en.indirect_dma_start(out=xg[par][st_][:, :], out_offset=None, in_=W["U2"][:, :],
                                                 in_offset=bass.IndirectOffsetOnAxis(ap=idi_[:, st_:st_ + 1], axis=0))
                o = Op("pool", fn)
                o.dma = True
                o.slot = xslot[par]
                o.slot_total = xslot[par].total + 16
                S._add(o, [("idi", par)], [("xg", par)])
                xslot[par].total += 16

        fi = 0
        di = 0
        ti = 0
        prep(0)
        for e in range(N_EXP):
            par = e % 2
            p_ = pz[par]
            for k2 in range(KC // 2):
                bi = ti % 2
                ti += 1
                pst = bk[bi][:].bitcast(BF16)
                for q in range(2):
                    k = 2 * k2 + q
                    for st_ in range(NST):
                        S.op("pe", lambda en, pst=pst, q=q, k=k, st_=st_, par=par: en.transpose(
                            pst[:, q * CAP + st_ * 128:q * CAP + (st_ + 1) * 128], xg[par][st_][:, k * 128:(k + 1) * 128], cx.ident_b[:]),
                            reads=[("xg", par), "ident_b"], writes=[("bank", bi)])
                evac_copy(S, k2, x_[:, 2 * k2:2 * k2 + 2, :], pst[:, :].rearrange("p (a b) -> p a b", a=2),
                          [("bank", bi)], ["xe"])
            for g in range(4):
                rg = load_piece(wg, e, g)
                ru = load_piece(wu, e, g)
                if g == 1 and e + 1 < N_EXP:
                    prep(e + 1)
                for f4 in range(4):
                    fc = 4 * g + f4
                    pg, pu = bk[2 + fi % 2], bk[4 + fi % 2]
                    kg, ku = ("bank", 2 + fi % 2), ("bank", 4 + fi % 2)
                    for k in range(KC):
                        S.op("pe", lambda en, pg=pg, rg=rg, k=k, f4=f4: en.matmul(
                            pg[:, 0:CAP], ring[rg][:, k, f4 * 128:(f4 + 1) * 128], x_[:, k, :], start=(k == 0), stop=(k == KC - 1)),
                            reads=[("ring", rg), "xe"], writes=[kg])
                    for k in range(KC):
                        S.op("pe", lambda en, pu=pu, ru=ru, k=k, f4=f4: en.matmul(
                            pu[:, 0:CAP], ring[ru][:, k, f4 * 128:(f4 + 1) * 128], x_[:, k, :], start=(k == 0), stop=(k == KC - 1)),
                            reads=[("ring", ru), "xe"], writes=[ku])
                    w = fi % 2
                    fi += 1
                    gb_, sg_, ub_ = gsb[w], sig[w], usb[w]
                    col = e * KC + fc
                    S.op("dve", lambda en, gb_=gb_, pg=pg, col=col: en.tensor_scalar(
                        out=gb_[:], in0=pg[:, 0:CAP], scalar1=bgT[:, col:col + 1], scalar2=7.0, op0=ALU.add, op1=ALU.min),
                        reads=[kg, "bgT"], writes=[("gsb", w)])
                    S.op("act", lambda en, sg_=sg_, gb_=gb_: en.activation(out=sg_[:], in_=gb_[:], func=AF.Sigmoid, scale=1.702),
                         reads=[("gsb", w)], writes=[("sig", w)])
                    S.op("act", lambda en, ub_=ub_, pu=pu, col=col: en.activation(
                        out=ub_[:], in_=pu[:, 0:CAP], func=AF.Identity, bias=buT[:, col:col + 1]),
                        reads=[ku, "buT"], writes=[("usb", w)])
                    S.op("dve", lambda en, ub_=ub_: en.tensor_scalar(
                        out=ub_[:], in0=ub_[:], scalar1=7.0, scalar2=-7.0, op0=ALU.min, op1=ALU.max),
                        reads=[("usb", w)], writes=[("usb", w)])
                    S.op("dve", lambda en, gb_=gb_, sg_=sg_: en.tensor_tensor(out=gb_[:], in0=gb_[:], in1=sg_[:], op=ALU.mult),
                         reads=[("gsb", w), ("sig", w)], writes=[("gsb", w)])
                    S.op("dve", lambda en, fc=fc, gb_=gb_, ub_=ub_: en.scalar_tensor_tensor(
                        out=a_[:, fc, :], in0=ub_[:], scalar=1.0, in1=gb_[:], op0=ALU.add, op1=ALU.mult),
                        reads=[("gsb", w), ("usb", w)], writes=["act"])
            for ch in range(4):
                rd = load_piece(wd, e, ch)
                for st_ in range(NST):
                    pd = bk[6 + di % 2]
                    kd = ("bank", 6 + di % 2)
                    for f in range(KC):
                        S.op("pe", lambda en, pd=pd, rd=rd, f=f, st_=st_: en.matmul(
                            pd[:, :], a_[:, f, st_ * 128:(st_ + 1) * 128], ring[rd][:, f, :], start=(f == 0), stop=(f == KC - 1)),
                            reads=[("ring", rd), "act"], writes=[kd])
                    if di % 2 == 0:
                        S.op("act", lambda en, st_=st_, ch=ch, pd=pd, p_=p_: en.activation(
                            out=ye[:, st_, ch * 512:(ch + 1) * 512], in_=pd[:, :], func=AF.Copy, scale=p_[:, st_:st_ + 1]),
                            reads=[kd, ("pz", par)], writes=["ye"])
                    else:
                        S.op("dve", lambda en, st_=st_, ch=ch, pd=pd, p_=p_: en.tensor_scalar(
                            out=ye[:, st_, ch * 512:(ch + 1) * 512], in0=pd[:, :], scalar1=p_[:, st_:st_ + 1], scalar2=None, op0=ALU.mult),
                            reads=[kd, ("pz", par)], writes=["ye"])
                    di += 1
            for st_ in range(NST):
                S.dma("sp", yslot, W["Y"][e * CAP + st_ * 128:e * CAP + (st_ + 1) * 128, :], ye[:, st_, :], reads=["ye"])
    S.barrier()


def cx_rd(cnt, rds, ch):
    return rds[ch]


def phaseE2(cx):
    nc, S, I, W = cx.nc, cx.S, cx.I, cx.W
    bk = cx.banks
    I32 = mybir.dt.int32
    ZR = N_EXP * CAP
    with contextlib.ExitStack() as st:
        sb = lambda name, shape, dt: st.enter_context(nc.sbuf_tensor(name, list(shape), dt))
        ecapB = sb("F_ecap", [128, 8, N_EXP], F32)
        vv = sb("F_vv", [128, 8, N_EXP], F32)
        vt = sb("F_vt", [128, 8, N_EXP], F32)
        m8 = sb("F_m8", [128, 8, 8], F32)
        eq0 = sb("F_eq0", [128, 8, 4], F32)
        idf = sb("F_idf", [128, 8, 4], F32)
        idi = sb("F_idi", [128, 8, 4], I32)
        gb = [[sb(f"F_g{i}_{k}", [128, D], BF16) for k in range(4)] for i in range(2)]
        gslot = [S.slot("gg") for _ in range(2)]
        acc = [sb(f"F_acc{i}", [128, D], F32) for i in range(2)]
        ht = [sb(f"F_h{i}", [128, D], F32) for i in range(2)]
        hslot = [S.slot("hh") for _ in range(2)]
        g2B = sb("F_g2B", [128, D], F32)
        gfB = sb("F_gfB", [128, D], F32)
        ot = [sb(f"F_ot{i}", [128, D], F32) for i in range(2)]
        bdf = sb("F_bdf", [N_EXP, D], F32)
        bdb = sb("F_bdb", [N_EXP, D], BF16)
        pT = sb("F_pT", [N_EXP, 8, 2, 128], BF16)
        ss = sb("F_ss", [128, 8], F32)
        rt = sb("F_rt", [128, 8], F32)
        rs = sb("F_rs", [128, 8], F32)
        ls = S.slot("E2")
        S.dma("sp", ls, g2B[:], W["rows"][3:4, :].partition_broadcast(128), writes=["g2B"])
        S.dma("sp", ls, gfB[:], I["nf"][0:1, :].partition_broadcast(128), writes=["gfB"])
        S.dma("sp", ls, bdf[:], I["b_down"][:, :], writes=["bdf"])
        S.dma("sp", ls, ecapB[:].rearrange("p a b -> p (a b)"), I["ecapB"][:, :], writes=["ecapB"])
        S.op("dve", lambda e: e.tensor_copy(out=bdb[:], in_=bdf[:]), reads=["bdf"], writes=["bdb"])
        pk = [("posm", t) for t in range(8)]
        S.op("dve", lambda e: e.tensor_scalar(out=vt[:], in0=cx.posm[:], scalar1=0.0, scalar2=None, op0=ALU.is_gt),
             reads=pk, writes=["vt"])
        S.op("dve", lambda e: e.tensor_scalar(out=vv[:], in0=cx.posm[:], scalar1=float(CAP), scalar2=None, op0=ALU.is_le),
             reads=pk, writes=["vv"])
        S.op("dve", lambda e: e.tensor_tensor(out=vt[:], in0=vt[:], in1=vv[:], op=ALU.mult), reads=["vt", "vv"], writes=["vt"])
        S.op("dve", lambda e: e.tensor_tensor(out=vv[:], in0=cx.posm[:], in1=ecapB[:], op=ALU.add), reads=pk + ["ecapB", "vv"], writes=["vv"])
        S.op("dve", lambda e: e.tensor_tensor(out=vv[:], in0=vv[:], in1=vt[:], op=ALU.mult), reads=["vv", "vt"], writes=["vv"])
        for tt in range(8):
            S.op("dve", lambda e, tt=tt: e.max(out=m8[:, tt, :], in_=vv[:, tt, :]), reads=["vv"], writes=["m8"])
        S.op("dve", lambda e: e.tensor_scalar(out=eq0[:], in0=m8[:, :, 0:4], scalar1=0.0, scalar2=float(ZR + 1), op0=ALU.is_equal, op1=ALU.mult),
             reads=["m8"], writes=["eq0"])
        S.op("dve", lambda e: e.scalar_tensor_tensor(out=idf[:], in0=m8[:, :, 0:4], scalar=-1.0, in1=eq0[:], op0=ALU.add, op1=ALU.add),
             reads=["m8", "eq0"], writes=["idf"])
        S.op("dve", lambda e: e.tensor_copy(out=idi[:], in_=idf[:]), reads=["idf"], writes=["idi"])
        pTb = bk[6][:].bitcast(BF16)
        for tt in range(8):
            for hl, pr in enumerate((cx.prob_hi, cx.prob_lo)):
                j = (tt * 2 + hl) % 8
                S.op("pe", lambda e, j=j, pr=pr, tt=tt: e.transpose(pTb[0:N_EXP, j * 128:(j + 1) * 128], pr[:, tt, :], cx.ident_b[:]),
                     reads=[("phi", tt), ("plo", tt), "ident_b"], writes=[("bank", 6)])
                S.op("dve", lambda e, j=j, tt=tt, hl=hl: e.tensor_copy(out=pT[:, tt, hl, :], in_=pTb[0:N_EXP, j * 128:(j + 1) * 128]),
                     reads=[("bank", 6)], writes=["pT"])
        for tt in range(8):
            par = tt % 2
            hb = ht[par]
            S.dma("act", hslot[par], hb[:], W["hs"][tt * 128:(tt + 1) * 128, :], writes=[("ht", par)])
            for k in range(4):
                def fn(e, k=k, tt=tt, par=par):
                    return e.indirect_dma_start(out=gb[par][k][:, :], out_offset=None, in_=W["Y"][:, :],
                                                in_offset=bass.IndirectOffsetOnAxis(ap=idi[:, tt, k:k + 1], axis=0))
                o = Op("pool", fn)
                o.dma = True
                o.slot = gslot[par]
                o.slot_total = gslot[par].total + 16
                S._add(o, ["idi"], [("gb", par, k)])
                gslot[par].total += 16
            ac = acc[par]
            g_ = gb[par]
            S.op("dve", lambda e, ac=ac, g_=g_: e.tensor_tensor(out=ac[:], in0=g_[0][:], in1=g_[1][:], op=ALU.add),
                 reads=[("gb", par, 0), ("gb", par, 1)], writes=[("acc", par)])
            S.op("pool", lambda e, ac=ac, g_=g_: e.tensor_tensor(out=g_[2][:], in0=g_[2][:], in1=g_[3][:], op=ALU.add),
                 reads=[("gb", par, 2), ("gb", par, 3)], writes=[("gb", par, 2)])
            S.op("dve", lambda e, ac=ac, g_=g_: e.tensor_tensor(out=ac[:], in0=ac[:], in1=g_[2][:], op=ALU.add),
                 reads=[("gb", par, 2), ("acc", par)], writes=[("acc", par)])
            for ch in range(4):
                for hl in range(2):
                    S.op("pe", lambda en, tt=tt, hl=hl, ch=ch: en.matmul(
                        bk[ch][:, :], pT[:, tt, hl, :], bdb[:, ch * 512:(ch + 1) * 512], start=(hl == 0), stop=(hl == 1)),
                        reads=["pT", "bdb"], writes=[("bank", ch)])
                S.op("dve", lambda en, ac=ac, ch=ch: en.tensor_tensor(out=ac[:, ch * 512:(ch + 1) * 512], in0=bk[ch][:, :],
                                                                     in1=ac[:, ch * 512:(ch + 1) * 512], op=ALU.add),
                     reads=[("bank", ch), ("acc", par)], writes=[("acc", par)])
            S.op("pool", lambda en, ac=ac: en.tensor_tensor(out=ac[:], in0=ac[:], in1=g2B[:], op=ALU.mult),
                 reads=[("acc", par), "g2B"], writes=[("acc", par)])
            S.op("pool", lambda en, ac=ac, hb=hb: en.tensor_tensor(out=hb[:], in0=hb[:], in1=ac[:], op=ALU.add),
                 reads=[("acc", par), ("ht", par)], writes=[("ht", par)])
            o_ = ot[par]
            S.op("act", lambda en, o_=o_, hb=hb, tt=tt: en.activation(out=o_[:], in_=hb[:], func=AF.Square, accum_out=ss[:, tt:tt + 1]),
                 reads=[("ht", par)], writes=[("ot", par), ("ss", tt)])
            S.op("act", lambda en, tt=tt: en.activation(out=rt[:, tt:tt + 1], in_=ss[:, tt:tt + 1], func=AF.Sqrt, scale=1.0 / D,
                                                        bias=cx.epsc[:, 0:1]), reads=[("ss", tt), "epsc"], writes=[("rt", tt)])
            S.op("dve", lambda en, tt=tt: en.reciprocal(out=rs[:, tt:tt + 1], in_=rt[:, tt:tt + 1]), reads=[("rt", tt)], writes=[("rs", tt)])
            S.op("dve", lambda en, o_=o_, hb=hb, tt=tt: en.scalar_tensor_tensor(out=o_[:], in0=hb[:], scalar=rs[:, tt:tt + 1], in1=gfB[:],
                                                                                op0=ALU.mult, op1=ALU.mult),
                 reads=[("ht", par), ("rs", tt), "gfB", ("ot", par)], writes=[("ot", par)])
            S.dma("sp", cx.slot_out, cx.O["out"][tt * 128:(tt + 1) * 128, :], o_[:], reads=[("ot", par)])
    S.barrier()


def _colT(v, nchunk):
    return np.ascontiguousarray(v.reshape(nchunk, 128).T)


def prep_inputs(inp):
    f32 = np.float32
    x = np.asarray(inp["x"], f32)
    c = np.asarray(inp["c"], f32)
    shared = {
        "w_ada": np.ascontiguousarray(inp["w_ada"][0], f32),
        "b_ada": np.ascontiguousarray(inp["b_ada"][0][None, :], f32),
        "n1": np.ascontiguousarray(inp["norm1_g"][0][None, :], f32),
        "n2": np.ascontiguousarray(inp["norm2_g"][0][None, :], f32),
        "nf": np.ascontiguousarray(np.asarray(inp["final_g"])[None, :], f32),
        "w_in": np.ascontiguousarray(inp["w_in"][0], f32),
        "lamT": np.ascontiguousarray(np.stack([inp["lam_q1"][0], inp["lam_k1"][0], inp["lam_q2"][0], inp["lam_k2"][0]], axis=1), f32),
        "sublnT": _colT(np.asarray(inp["subln_g"][0], f32), 2),
        "w_out_a": np.ascontiguousarray(inp["w_out_a"][0], f32),
        "w_out_b": np.ascontiguousarray(inp["w_out_b"][0], f32),
        "w_o": np.ascontiguousarray(inp["w_o"][0], f32),
        "w_router": np.ascontiguousarray(inp["w_router"][0], f32),
        "b_routerB": np.ascontiguousarray(np.broadcast_to(np.asarray(inp["b_router"][0], f32)[None, :], (128, N_EXP))),
        "w_gate": np.ascontiguousarray(inp["w_gate"][0], f32),
        "w_up": np.ascontiguousarray(inp["w_up"][0], f32),
        "w_down": np.ascontiguousarray(inp["w_down"][0], f32),
        "b_gateT": np.ascontiguousarray(np.asarray(inp["b_gate"][0], f32).reshape(N_EXP, KC, 128).transpose(2, 0, 1).reshape(128, N_EXP * KC)),
        "b_upT": np.ascontiguousarray(np.asarray(inp["b_up"][0], f32).reshape(N_EXP, KC, 128).transpose(2, 0, 1).reshape(128, N_EXP * KC)),
        "b_down": np.ascontiguousarray(inp["b_down"][0], f32),
        "ident": np.eye(128, dtype=f32),
        "iota_row": np.ascontiguousarray(np.broadcast_to(np.arange(1, CAP + 1, dtype=f32)[None, :], (128, CAP))),
        "triu": np.triu(np.ones((128, 128), f32)),
        "tokab": np.ascontiguousarray(np.stack([(np.arange(8)[None, :] * 128 + np.arange(128)[:, None]) // 32,
                                                (np.arange(8)[None, :] * 128 + np.arange(128)[:, None]) % 32], axis=2).reshape(128, 16).astype(f32)),
        "ecapB": np.ascontiguousarray(np.broadcast_to((np.arange(N_EXP, dtype=f32) * CAP)[None, None, :], (128, 8, N_EXP)).reshape(128, 8 * N_EXP)),
    }
    p = np.arange(128)[:, None]
    xx = np.arange(3968)[None, :]
    dl = p + 1920 - xx
    ad = np.abs(dl)
    n_ = ((ad <= 64).astype(np.float64) + ((dl % 4 == 0) & (ad <= 256)) + ((dl % 16 == 0) & (ad <= 1024)))
    lnn = np.where(n_ > 0, np.log(np.maximum(n_, 1.0)), 0.0)
    shared["stripAh"] = np.ascontiguousarray(np.stack(
        [np.where(n_ > 0, -SLOPES_A[h] * ad + lnn, -BIG) for h in range(8)], axis=0).astype(f32))
    maps = []
    for core in range(8):
        b, r = core // 4, core % 4
        idx = (np.arange(SEQ) - 1024 + 1024 * r) % SEQ
        m = dict(shared)
        m["xr"] = np.ascontiguousarray(x[b][idx])
        m["cT"] = _colT(c[b], KC)
        dist = np.zeros((128, 4, 1920), f32)
        for g in range(4):
            s_g = ((1024 * g - 1024 + 1024 * r) % SEQ) - 1024 * g
            q_off = -1024 + 1024 * r
            xv = np.arange(1920)[None, :]
            dist[:, g, :] = np.abs(1024 * (1 - g) + (xv - 896) - p + q_off - s_g)
        m["distB"] = np.ascontiguousarray(dist.reshape(128, 4 * 1920))
        kb = np.zeros((128, 24), f32)
        for kt in range(24):
            tok = 128 * kt + np.arange(128) - 1024 + 1024 * r
            kb[:, kt] = np.where((tok >= 0) & (tok < SEQ), 0.0, -BIG)
        m["kbiasA"] = kb
        maps.append(m)
    return maps


_NC_CACHE = {}
_LAST_DECLARED = []


def kernel(**inputs):
    maps = prep_inputs(inputs)
    if "nc" not in _NC_CACHE:
        _NC_CACHE["nc"] = build_program()
    nc = _NC_CACHE["nc"]
    names = set(_LAST_DECLARED)
    maps = [{k: v for k, v in m.items() if k in names} for m in maps]
    res = run_bass_kernel_spmd(nc, maps, core_ids=list(range(8)))
    out = np.zeros((2, SEQ, D), np.float32)
    for core in range(8):
        b, r = core // 4, core % 4
        out[b, 1024 * r:1024 * (r + 1)] = np.asarray(res.results[core]["out"], np.float32)
    return out
```

```python
import contextlib
import math
import numpy as np
import concourse.bass as bass
import concourse.mybir as mybir
from concourse.bass_utils import run_bass_kernel_spmd

F32 = mybir.dt.float32
BF16 = mybir.dt.bfloat16
AF = mybir.ActivationFunctionType
ALU = mybir.AluOpType
AX = mybir.AxisListType

ENGS = ("pe", "act", "dve", "pool", "sp")

D = 2048
SEQ = 4096
OWN = 1024
KC = 16
N_EXP = 32
TOPK = 4
CAP = 512
EPS = 1e-5
SCALE = 128 ** -0.5
LAMBDA_INIT = 0.8 - 0.6 * math.exp(-0.3 * 0)
SLOPES = [2.0 ** (-8.0 * (i + 1) / 12) for i in range(12)]
SLOPES_A = SLOPES[:8]
SLOPES_B = SLOPES[8:]
BIG = 1.0e9
PATTERNS = (1, 4, 16)


class Op:
    __slots__ = ("eng", "fn", "deps", "dma", "idx", "count", "milestone", "slot", "slot_total", "name")

    def __init__(self, eng, fn, name=""):
        self.eng = eng
        self.fn = fn
        self.deps = []
        self.dma = False
        self.milestone = False
        self.count = None
        self.slot = None
        self.slot_total = None
        self.name = name


class Slot:
    def __init__(self, name):
        self.name = name
        self.total = 0
        self.sem = None


class Sched:
    def __init__(self, nc):
        self.nc = nc
        self.ops = []
        self.writers = {}
        self.readers = {}
        self.old_readers = {}
        self.slots = []
        self.nbar = 0

    def slot(self, name):
        s = Slot(name + str(len(self.slots)))
        self.slots.append(s)
        return s

    def _add(self, op, reads, writes):
        deps = {}

        def add_dep(o):
            if o is op:
                return
            if o.dma:
                if op.dma and op.slot is o.slot:
                    return
                deps[("slot", id(o.slot))] = ("slot", o.slot, o.slot.total)
            else:
                k = ("eng", o.eng)
                prev = deps.get(k)
                if prev is None or o.idx > prev[1].idx:
                    deps[k] = ("eng", o, None)

        for k in reads:
            for o in self.writers.get(k, {}).values():
                add_dep(o)
        for k in writes:
            for o in self.writers.get(k, {}).values():
                if (not o.dma) and (not op.dma) and o.eng == op.eng:
                    continue
                add_dep(o)
            for o in list(self.readers.get(k, {}).values()) + list(self.old_readers.get(k, {}).values()):
                if (not o.dma) and (not op.dma) and o.eng == op.eng:
                    continue
                add_dep(o)
        op.deps = list(deps.values())
        for d in op.deps:
            if d[0] == "eng":
                d[1].milestone = True
        op.idx = len(self.ops)
        self.ops.append(op)
        wkey = ("slot", id(op.slot)) if op.dma else op.eng
        for k in writes:
            self.writers[k] = {wkey: op}
            if self.readers.get(k):
                self.old_readers[k] = self.readers[k]
            self.readers[k] = {}
        for k in reads:
            self.readers.setdefault(k, {})[wkey] = op
        return op

    def op(self, eng, fn, reads=(), writes=(), name=""):
        o = Op(eng, fn, name)
        return self._add(o, list(reads), list(writes))

    def dma(self, eng, slot, out, in_, reads=(), writes=(), name="", **kw):
        def fn(e, out=out, in_=in_, kw=kw):
            return e.dma_start(out=out, in_=in_, **kw)
        o = Op(eng, fn, name)
        o.dma = True
        o.slot = slot
        o.slot_total = slot.total + 16
        r = self._add(o, list(reads), list(writes))
        slot.total += 16
        return r

    def barrier(self):
        n = self.nbar
        self.nbar += 1
        sc = self.bar_scratch
        comp = ("pe", "act", "dve", "pool")
        for e in comp:
            if e == "pe":
                self.op("pe", lambda en: en.matmul(self.bar_ps[0:1, 0:1], sc["b"][0:1, 0:1], sc["b"][0:1, 0:1],
                                                    start=True, stop=True),
                        writes=[("bar", n, e), ("bank", 7)])
            elif e == "act":
                self.op("act", lambda en: en.activation(out=sc["act"][0:1, 0:1], in_=sc["one"][0:1, 0:1], func=AF.Copy),
                        writes=[("bar", n, e)])
            elif e == "dve":
                self.op("dve", lambda en: en.tensor_copy(out=sc["dve"][0:1, 0:1], in_=sc["one"][0:1, 0:1]),
                        writes=[("bar", n, e)])
            else:
                self.op("pool", lambda en: en.tensor_copy(out=sc["pool"][0:1, 0:1], in_=sc["one"][0:1, 0:1]),
                        writes=[("bar", n, e)])
        rk = [("bar", n, e) for e in comp]
        for e in ENGS:
            o = Op(e, None, "barwait")
            self._add(o, rk, [])
            for s in self.slots:
                if s.total > 0:
                    o.deps.append(("slot", s, s.total))
        self.writers = {}
        self.readers = {}
        self.old_readers = {}

    def emit(self, final_waits=()):
        nc = self.nc
        by_eng = {e: [] for e in ENGS}
        for o in self.ops:
            by_eng[o.eng].append(o)
        for e in ENGS:
            c = 0
            for o in by_eng[e]:
                if o.milestone and not o.dma:
                    c += 1
                    o.count = c
        with contextlib.ExitStack() as st:
            esem = {e: st.enter_context(nc.semaphore("s_" + e)) for e in ENGS}
            for s in self.slots:
                s.sem = st.enter_context(nc.semaphore("d_" + s.name))
            block = st.enter_context(nc.Block())

            def run(e, eng):
                waited = {}
                for o in by_eng[e]:
                    for d in o.deps:
                        if d[0] == "slot":
                            sem, val, key = d[1].sem, d[2], ("slot", id(d[1]))
                        else:
                            dop = d[1]
                            if dop.eng == e and e == "pe":
                                continue
                            sem, val, key = esem[dop.eng], dop.count, ("eng", dop.eng)
                        if waited.get(key, 0) >= val:
                            continue
                        waited[key] = val
                        eng.wait_ge(sem, val)
                    if o.fn is None:
                        continue
                    ins = o.fn(eng)
                    if o.dma:
                        ins.then_inc(o.slot.sem, 16)
                    elif o.milestone:
                        ins.then_inc(esem[e], 1)
                if e == "sp":
                    for s in final_waits:
                        eng.wait_ge(s.sem, s.total)

            @block.tensor
            def _(eng):
                run("pe", eng)

            @block.scalar
            def _(eng):
                run("act", eng)

            @block.vector
            def _(eng):
                run("dve", eng)

            @block.gpsimd
            def _(eng):
                run("pool", eng)

            @block.sync
            def _(eng):
                run("sp", eng)


class Ctx:
    pass


def a_tiles():
    out = []
    for d in PATTERNS:
        nt = {1: 9, 4: 3, 16: 2}[d]
        for c in range(d):
            for m in range(nt):
                out.append((d, c, m))
    return out


A_TILES = a_tiles()
A_TILE_IDX = {t: i for i, t in enumerate(A_TILES)}


def build_program(debug=(), stages="ABCDE"):
    nc = bass.Bass("TRN2", target_bir_lowering=False)
    cx = Ctx()
    cx.stages = stages
    cx.nc = nc
    cx.debug = set(debug)
    S = Sched(nc)
    cx.S = S

    def din(name, shape, dt=F32):
        return nc.dram_tensor(name, list(shape), dt, kind="ExternalInput").ap()

    def dscr(name, shape, dt=BF16):
        return nc.dram_tensor(name, list(shape), dt, kind="Internal").ap()

    def dout(name, shape, dt=F32):
        return nc.dram_tensor(name, list(shape), dt, kind="ExternalOutput").ap()

    class Lazy(dict):
        def __init__(self):
            super().__init__()
            self.specs = {}

        def __setitem__(self, k, v):
            self.specs[k] = v

        def __missing__(self, k):
            ap = self.specs[k]()
            dict.__setitem__(self, k, ap)
            return ap
    _din = din
    din = lambda name, shape, dt=F32: (lambda: _din(name, shape, dt))
    I = Lazy()
    I["xr"] = din("xr", [SEQ, D])
    I["cT"] = din("cT", [128, KC])
    I["w_ada"] = din("w_ada", [D, 6 * D])
    I["b_ada"] = din("b_ada", [1, 6 * D])
    I["n1"] = din("n1", [1, D])
    I["n2"] = din("n2", [1, D])
    I["nf"] = din("nf", [1, D])
    I["w_in"] = din("w_in", [D, 10240])
    I["lamT"] = din("lamT", [128, 4])
    I["sublnT"] = din("sublnT", [128, 2])
    I["w_out_a"] = din("w_out_a", [1024, D])
    I["w_out_b"] = din("w_out_b", [1024, D])
    I["w_o"] = din("w_o", [D, D])
    I["w_router"] = din("w_router", [D, N_EXP])
    I["b_routerB"] = din("b_routerB", [128, N_EXP])
    I["w_gate"] = din("w_gate", [N_EXP, D, D])
    I["w_up"] = din("w_up", [N_EXP, D, D])
    I["w_down"] = din("w_down", [N_EXP, D, D])
    I["b_gateT"] = din("b_gateT", [128, N_EXP * KC])
    I["b_upT"] = din("b_upT", [128, N_EXP * KC])
    I["b_down"] = din("b_down", [N_EXP, D])
    I["ident"] = din("ident", [128, 128])
    I["distB"] = din("distB", [128, 4 * 1920])
    I["stripAh"] = din("stripAh", [8, 128, 3968])
    I["kbiasA"] = din("kbiasA", [128, 24])
    I["iota_row"] = din("iota_row", [128, CAP])
    I["triu"] = din("triu", [128, 128])
    I["ecapB"] = din("ecapB", [128, 8 * N_EXP])
    I["tokab"] = din("tokab", [128, 16])
    cx.I = I
    O = {}
    O["out"] = dout("out", [OWN, D])
    for name, shape, dt in DEBUG_SHAPES:
        if name in cx.debug:
            O[name] = dout(name, shape, dt)
    cx.O = O
    W = {}
    W["QaT"] = dscr("QaT", [1024, OWN])
    W["KaT"] = dscr("KaT", [1024, 3072])
    W["Va"] = dscr("Va", [SEQ, 1024])
    W["QbT"] = dscr("QbT", [1024, OWN])
    W["KbT"] = dscr("KbT", [1024, SEQ])
    W["Vb"] = dscr("Vb", [SEQ, 1024])
    W["GT"] = dscr("GT", [4096, OWN])
    W["rows"] = dscr("rows", [4, D], F32)
    W["YaT"] = dscr("YaT", [1024, OWN])
    W["YbT"] = dscr("YbT", [1024, OWN])
    W["U2"] = dscr("U2", [OWN, D])
    W["MT"] = dscr("MT", [D, OWN])
    W["hs"] = dscr("hs", [OWN, D], F32)
    W["Y"] = dscr("Y", [N_EXP * CAP + 128, D])
    cx.W = W

    with contextlib.ExitStack() as st:
        cx.st = st
        sb = lambda name, shape, dt: st.enter_context(nc.sbuf_tensor(name, list(shape), dt))
        cx.banks = [st.enter_context(nc.psum_tensor(f"bank{i}", [128, 512], F32)) for i in range(8)]
        cx.ident_f = sb("ident_f", [128, 128], F32)
        cx.ident_b = sb("ident_b", [128, 128], BF16)
        cx.ones_f = sb("ones_f", [128, 128], F32)
        cx.ones_b = sb("ones_b", [128, 128], BF16)
        cx.colT = sb("colT", [128, 32], F32)
        cx.neglam = sb("neglam", [128, 1], F32)
        cx.sg = sb("sg", [128, 2], F32)
        cx.barsc = sb("barsc", [128, 8], F32)
        cx.barb = sb("barb", [128, 2], BF16)
        cx.epsc = sb("epsc", [128, 2], F32)
        S.bar_scratch = {"b": cx.barb, "one": cx.barsc[:, 0:1], "act": cx.barsc[:, 1:2],
                         "dve": cx.barsc[:, 2:3], "pool": cx.barsc[:, 3:4]}
        S.bar_ps = cx.banks[7]
        cx.slot_const = S.slot("const")
        cx.slot_out = S.slot("out")
        cx.dbg_slot = S.slot("dbg")

        cx.posm = sb("posm", [128, 8, N_EXP], F32)
        cx.prob = sb("prob", [128, 8, N_EXP], F32)
        cx.prob_hi = sb("prob_hi", [128, 8, N_EXP], BF16)
        cx.prob_lo = sb("prob_lo", [128, 8, N_EXP], BF16)
        cx.iota = sb("iota", [128, CAP], F32)
        S.dma("sp", cx.slot_const, cx.iota[:], I["iota_row"][:, :], writes=["iota"])
        stages = cx.stages
        phase0(cx)
        if "A" in stages:
            phaseA(cx)
            S.barrier()
        if "B" in stages:
            attention(cx, "B")
        if "C" in stages:
            attention(cx, "A")
        if "D" in stages:
            phaseD(cx)
        if "E" in stages:
            phaseE1(cx)
            phaseE2(cx)
        S.emit(final_waits=[cx.slot_out, cx.dbg_slot])
    global _LAST_DECLARED
    _LAST_DECLARED = list(dict.keys(I))
    return nc


DEBUG_SHAPES = [
    ("d_yA", [1024, OWN], BF16),
    ("d_mT", [D, OWN], BF16),
    ("d_ya", [1024, OWN], BF16),
    ("d_yb", [1024, OWN], BF16),
    ("d_gat", [128, 1024], BF16),
    ("d_a1", [128, 1024], F32),
    ("d_rowB", [128, D], F32),
    ("d_yB", [1024, OWN], BF16),
    ("d_h1", [OWN, D], F32),
    ("d_lg", [128, 8 * N_EXP], F32),
    ("d_posm", [128, 8 * N_EXP], F32),
    ("d_prob", [128, 8 * N_EXP], F32),
    ("d_colT", [128, 32], F32),
    ("d_mod", [1, 6 * D], F32),
    ("d_rows", [4, D], F32),
    ("d_misc", [128, 4], F32),
    ("d_uT", [128, KC * 512], BF16),
]


def phase0(cx):
    nc, S, I = cx.nc, cx.S, cx.I
    with contextlib.ExitStack() as st:
        sb = lambda name, shape, dt: st.enter_context(nc.sbuf_tensor(name, list(shape), dt))
        cT = sb("p0_cT", [128, KC], F32)
        sc = sb("p0_sc", [128, KC], F32)
        brow = sb("p0_brow", [1, 6 * D], F32)
        modrow = sb("p0_modrow", [1, 6 * D], F32)
        nrow = sb("p0_nrow", [1, 2 * D], F32)
        grow = sb("p0_grow", [1, 2 * D], F32)
        lamT = sb("p0_lamT", [128, 4], F32)
        sublnT = sb("p0_subln", [128, 2], F32)
        prod = sb("p0_prod", [128, 2], F32)
        ex = sb("p0_ex", [128, 2], F32)
        pan = [sb(f"p0_pan{i}", [128, KC, 512], F32) for i in range(2)]
        pslot = [S.slot("pan") for _ in range(2)]
        sc_slot = cx.slot_const
        bk = cx.banks

        S.dma("sp", sc_slot, cx.ident_f[:], I["ident"][:, :], writes=["ident_f"])
        S.dma("sp", sc_slot, cT[:], I["cT"][:, :], writes=["cT"])
        S.dma("sp", sc_slot, brow[:], I["b_ada"][:, :], writes=["brow"])
        S.dma("sp", sc_slot, nrow[0:1, 0:D], I["n1"][:, :], writes=["nrow"])
        S.dma("sp", sc_slot, nrow[0:1, D:2 * D], I["n2"][:, :], writes=["nrow"])
        S.dma("sp", sc_slot, lamT[:], I["lamT"][:, :], writes=["lamT"])
        S.dma("sp", sc_slot, sublnT[:], I["sublnT"][:, :], writes=["sublnT"])

        S.op("dve", lambda e: e.memset(cx.ones_f[:], 1.0), writes=["ones_f"])
        S.op("dve", lambda e: e.memset(cx.ones_b[:], 1.0), writes=["ones_b"])
        S.op("dve", lambda e: e.memset(cx.barsc[:], 1.0), writes=["barsc"])
        S.op("dve", lambda e: e.memset(cx.barb[:], 1.0), writes=["barb"])
        S.op("dve", lambda e: e.memset(cx.epsc[:], EPS), writes=["epsc"])
        S.op("dve", lambda e: e.tensor_copy(out=cx.ident_b[:], in_=cx.ident_f[:]), reads=["ident_f"], writes=["ident_b"])
        S.op("act", lambda e: e.activation(out=sc[:], in_=cT[:], func=AF.Silu), reads=["cT"], writes=["sc"])

        wv = I["w_ada"].rearrange("(k p) n -> p k n", p=128)
        NP = 6 * D // 512
        for pn in range(NP):
            pb = pan[pn % 2]
            for j in range(4):
                q = "sp" if j % 2 == 0 else "act"
                S.dma(q, pslot[pn % 2], pb[:, 4 * j:4 * j + 4, :], wv[:, 4 * j:4 * j + 4, pn * 512:(pn + 1) * 512],
                      writes=[("pan", pn % 2)])
            ps = bk[pn % 2]
            for k in range(KC):
                S.op("pe", lambda e, ps=ps, pb=pb, k=k: e.matmul(ps[0:1, :], sc[:, k:k + 1], pb[:, k, :],
                                                                 start=(k == 0), stop=(k == KC - 1)),
                     reads=["sc", ("pan", pn % 2)], writes=[("bank", pn % 2)])
            S.op("dve", lambda e, ps=ps, pn=pn: e.tensor_tensor(out=modrow[0:1, pn * 512:(pn + 1) * 512], in0=ps[0:1, :],
                                                                 in1=brow[0:1, pn * 512:(pn + 1) * 512], op=ALU.add),
                 reads=[("bank", pn % 2), "brow"], writes=["modrow"])
        mr = lambda m: modrow[0:1, m * D:(m + 1) * D]
        S.op("dve", lambda e: e.scalar_tensor_tensor(out=grow[0:1, 0:D], in0=mr(1), scalar=1.0, in1=nrow[0:1, 0:D],
                                                     op0=ALU.add, op1=ALU.mult),
             reads=["modrow", "nrow"], writes=["grow"])
        S.op("dve", lambda e: e.scalar_tensor_tensor(out=grow[0:1, D:2 * D], in0=mr(4), scalar=1.0, in1=nrow[0:1, D:2 * D],
                                                     op0=ALU.add, op1=ALU.mult),
             reads=["modrow", "nrow"], writes=["grow"])
        psc = bk[2]
        for k in range(KC):
            S.op("pe", lambda e, k=k: e.matmul(psc[:, k:k + 1], grow[0:1, k * 128:(k + 1) * 128], cx.ones_f[0:1, 0:1],
                                               start=True, stop=True),
                 reads=["grow", "ones_f"], writes=[("bank", 2)])
        for k in range(KC):
            S.op("pe", lambda e, k=k: e.matmul(psc[:, KC + k:KC + k + 1], modrow[0:1, k * 128:(k + 1) * 128],
                                               cx.ones_f[0:1, 0:1], start=True, stop=True),
                 reads=["modrow", "ones_f"], writes=[("bank", 2)])
        S.op("dve", lambda e: e.tensor_copy(out=cx.colT[:], in_=psc[:, 0:32]), reads=[("bank", 2)], writes=["colT"])
        rs = cx.slot_const
        S.dma("sp", rs, cx.W["rows"][0:1, :], mr(2), reads=["modrow"], writes=["rows_d"])
        S.dma("sp", rs, cx.W["rows"][1:2, :], grow[0:1, D:2 * D], reads=["grow"], writes=["rows_d"])
        S.dma("sp", rs, cx.W["rows"][2:3, :], mr(3), reads=["modrow"], writes=["rows_d"])
        S.dma("sp", rs, cx.W["rows"][3:4, :], mr(5), reads=["modrow"], writes=["rows_d"])
        S.op("dve", lambda e: e.tensor_tensor(out=prod[:, 0:1], in0=lamT[:, 0:1], in1=lamT[:, 1:2], op=ALU.mult),
             reads=["lamT"], writes=["prod"])
        S.op("dve", lambda e: e.tensor_tensor(out=prod[:, 1:2], in0=lamT[:, 2:3], in1=lamT[:, 3:4], op=ALU.mult),
             reads=["lamT", "prod"], writes=["prod"])
        S.op("pe", lambda e: e.matmul(bk[3][:, 0:2], cx.ones_f[:], prod[:], start=True, stop=True),
             reads=["ones_f", "prod"], writes=[("bank", 3)])
        S.op("act", lambda e: e.activation(out=ex[:], in_=bk[3][:, 0:2], func=AF.Exp), reads=[("bank", 3)], writes=["ex"])
        S.op("dve", lambda e: e.tensor_tensor(out=cx.neglam[:], in0=ex[:, 1:2], in1=ex[:, 0:1], op=ALU.subtract),
             reads=["ex"], writes=["neglam"])
        S.op("dve", lambda e: e.tensor_scalar(out=cx.neglam[:], in0=cx.neglam[:], scalar1=-LAMBDA_INIT, scalar2=None,
                                              op0=ALU.add),
             reads=["neglam"], writes=["neglam"])
        S.op("dve", lambda e: e.tensor_scalar(out=cx.sg[:], in0=sublnT[:], scalar1=(1.0 - LAMBDA_INIT), scalar2=None,
                                              op0=ALU.mult),
             reads=["sublnT"], writes=["sg"])
        if "d_mod" in cx.debug:
            S.dma("sp", cx.dbg_slot, cx.O["d_mod"][:, :], modrow[:], reads=["modrow"])
        if "d_colT" in cx.debug:
            S.dma("sp", cx.dbg_slot, cx.O["d_colT"][:, :], cx.colT[:], reads=["colT"])
            S.dma("sp", cx.dbg_slot, cx.O["d_misc"][:, 0:1], cx.neglam[:], reads=["neglam"], allow_slow_non_contiguous=True)
            S.dma("sp", cx.dbg_slot, cx.O["d_misc"][:, 1:3], cx.sg[:], reads=["sg"], allow_slow_non_contiguous=True)
        S.barrier()


def evac_copy(S, i, out, in_, reads, writes, func=None):
    if func is not None or i % 2 == 0:
        f = func if func is not None else AF.Copy
        return S.op("act", lambda e: e.activation(out=out, in_=in_, func=f), reads=reads, writes=writes)
    return S.op("dve", lambda e: e.tensor_copy(out=out, in_=in_), reads=reads, writes=writes)


def phaseA(cx):
    nc, S, I, W = cx.nc, cx.S, cx.I, cx.W
    bk = cx.banks
    with contextlib.ExitStack() as st:
        sb = lambda name, shape, dt: st.enter_context(nc.sbuf_tensor(name, list(shape), dt))
        uT = sb("A_uT", [128, KC, SEQ], BF16)
        with contextlib.ExitStack() as st1:
            sb1 = lambda name, shape, dt: st1.enter_context(nc.sbuf_tensor(name, list(shape), dt))
            xt = [sb1(f"A_x{i}", [128, D], F32) for i in range(3)]
            xn = [sb1(f"A_xn{i}", [128, D], BF16) for i in range(2)]
            junk = sb1("A_junk", [128, D], BF16)
            ss = sb1("A_ss", [128, 4], F32)
            rt = sb1("A_rt", [128, 4], F32)
            rs = sb1("A_rs", [128, 4], F32)
            xslot = [S.slot("x") for _ in range(3)]
            xv = I["xr"].rearrange("(t p) d -> t p d", p=128)
            NTT = SEQ // 128
            def stage_a(tt):
                xb = xt[tt % 3]
                S.dma("sp", xslot[tt % 3], xb[:, 0:1024], xv[tt, :, 0:1024], writes=[("x", tt % 3)])
                S.dma("sp", xslot[tt % 3], xb[:, 1024:2048], xv[tt, :, 1024:2048], writes=[("x", tt % 3)])
                j = tt % 4
                S.op("act", lambda e, xb=xb, j=j: e.activation(out=junk[:], in_=xb[:], func=AF.Square,
                                                              accum_out=ss[:, j:j + 1]),
                     reads=[("x", tt % 3)], writes=["junk", ("ss", j)])
                S.op("act", lambda e, j=j: e.activation(out=rt[:, j:j + 1], in_=ss[:, j:j + 1], func=AF.Sqrt,
                                                        scale=1.0 / D, bias=cx.epsc[:, 0:1]),
                     reads=[("ss", j)], writes=[("rt", j)])
                S.op("dve", lambda e, j=j: e.reciprocal(out=rs[:, j:j + 1], in_=rt[:, j:j + 1]),
                     reads=[("rt", j)], writes=[("rs", j)])

            def stage_b(tt):
                xb = xt[tt % 3]
                j = tt % 4
                xnb = xn[tt % 2]
                S.op("act", lambda e, xb=xb, xnb=xnb, j=j: e.activation(out=xnb[:], in_=xb[:], func=AF.Copy,
                                                                       scale=rs[:, j:j + 1]),
                     reads=[("x", tt % 3), ("rs", j)], writes=[("xn", tt % 2)])
                for half in range(2):
                    bi = (2 * tt + half) % 4
                    pst = bk[bi][:].bitcast(BF16)
                    for q in range(8):
                        k = half * 8 + q
                        S.op("pe", lambda e, pst=pst, q=q, k=k, xnb=xnb: e.transpose(
                            pst[:, q * 128:(q + 1) * 128], xnb[:, k * 128:(k + 1) * 128], cx.ident_b[:]),
                            reads=[("xn", tt % 2), "ident_b"], writes=[("bank", bi)])
                    for q in range(8):
                        k = half * 8 + q
                        S.op("dve", lambda e, pst=pst, q=q, k=k, tt=tt: e.tensor_scalar(
                            out=uT[:, k, tt * 128:(tt + 1) * 128], in0=pst[:, q * 128:(q + 1) * 128],
                            scalar1=cx.colT[:, k:k + 1], scalar2=cx.colT[:, KC + k:KC + k + 1],
                            op0=ALU.mult, op1=ALU.add),
                            reads=[("bank", bi), "colT"], writes=[("uT", tt // 4)])

            for tt in range(NTT):
                stage_a(tt)
                if tt >= 1:
                    stage_b(tt - 1)
            stage_b(NTT - 1)
        if "d_uT" in cx.debug:
            for k in range(KC):
                S.dma("sp", cx.dbg_slot, cx.O["d_uT"][:, k * 512:(k + 1) * 512], uT[:, k, 1024:1536],
                      reads=[("uT", c) for c in range(8)])
        S.barrier()
        wch = [sb(f"A_w{i}", [128, KC, 512], BF16) for i in range(2)]
        stg = [sb(f"A_stg{i}", [128, 512], BF16) for i in range(4)]
        wslot = [S.slot("w") for _ in range(2)]
        sslot = [S.slot("stg") for _ in range(4)]
        wv = I["w_in"].rearrange("(k p) n -> p k n", p=128)
        groups = []
        for g in range(2):
            groups.append(("qa", "f", [2, 3], W["QaT"], g, 1024))
        for g in range(2):
            groups.append(("ka", "f", list(range(6)), W["KaT"], g, 0))
        for g in range(2):
            groups.append(("va", "t", list(range(6)), W["Va"], g, 0))
        for g in range(2):
            groups.append(("qb", "f", [2, 3], W["QbT"], g, 1024))
        for g in range(2):
            groups.append(("kb", "f", list(range(8)), W["KbT"], g, 0))
        for g in range(2):
            groups.append(("vb", "t", list(range(8)), W["Vb"], g, 0))
        for g in range(8):
            groups.append(("gt", "f", [2, 3], W["GT"], g, 1024))
        ev = 0
        for cg, (name, kind, tcs, dst, g, tok0) in enumerate(groups):
            wb = wch[cg % 2]
            for j in range(4):
                S.dma("pool", wslot[cg % 2], wb[:, 4 * j:4 * j + 4, :], wv[:, 4 * j:4 * j + 4, cg * 512:(cg + 1) * 512],
                      writes=[("w", cg % 2)])
            func = AF.Sigmoid if name == "gt" else None
            if kind == "f":
                for s4 in range(4):
                    for tc in tcs:
                        bi = ev % 4
                        ps = bk[bi]
                        for k in range(KC):
                            S.op("pe", lambda e, ps=ps, wb=wb, k=k, s4=s4, tc=tc: e.matmul(
                                ps[:, :], wb[:, k, s4 * 128:(s4 + 1) * 128], uT[:, k, tc * 512:(tc + 1) * 512],
                                start=(k == 0), stop=(k == KC - 1)),
                                reads=[("w", cg % 2), ("uT", tc)], writes=[("bank", bi)])
                        sg = stg[ev % 4]
                        evac_copy(S, ev, sg[:], ps[:, :], [("bank", bi)], [("stg", ev % 4)], func=func)
                        row0 = g * 512 + s4 * 128
                        col0 = tc * 512 - tok0
                        S.dma("sp", sslot[ev % 4], dst[row0:row0 + 128, col0:col0 + 512], sg[:],
                              reads=[("stg", ev % 4)])
                        ev += 1
            else:
                for tc in tcs:
                    for t4 in range(4):
                        tt = tc * 4 + t4
                        bi = ev % 4
                        ps = bk[bi]
                        for k in range(KC):
                            S.op("pe", lambda e, ps=ps, wb=wb, k=k, tt=tt: e.matmul(
                                ps[:, :], uT[:, k, tt * 128:(tt + 1) * 128], wb[:, k, :],
                                start=(k == 0), stop=(k == KC - 1)),
                                reads=[("w", cg % 2), ("uT", tc)], writes=[("bank", bi)])
                        sg = stg[ev % 4]
                        evac_copy(S, ev, sg[:], ps[:, :], [("bank", bi)], [("stg", ev % 4)])
                        S.dma("sp", sslot[ev % 4], dst[tt * 128:(tt + 1) * 128, g * 512:(g + 1) * 512], sg[:],
                              reads=[("stg", ev % 4)])
                        ev += 1


def attention(cx, mixer):
    nc, S, I, W = cx.nc, cx.S, cx.I, cx.W
    bk = cx.banks
    isB = mixer == "B"
    nheads = 4 if isB else 8
    nkt = 32 if isB else 24
    ncomp = 2 if isB else 1
    nv = 2 if isB else 1
    vw = 1024
    with contextlib.ExitStack() as st:
        sb = lambda name, shape, dt: st.enter_context(nc.sbuf_tensor(name, list(shape), dt))
        if isB:
            vbuf = [sb(f"B_v{i}", [128, nkt, 256], BF16) for i in range(2)]
        else:
            v_all = sb("A_v", [128, nkt, vw], BF16)
        yT = sb(mixer + "_yT", [128, 8, OWN], BF16)
        ktb = [sb(f"{mixer}_kt{i}", [128, ncomp, nkt * 128], BF16) for i in range(2)]
        qtb = [sb(f"{mixer}_qt{i}", [128, ncomp, OWN], BF16) for i in range(2)]
        if isB:
            strip = sb("B_strip", [128, 4 * 1920], F32)
            stripb = [strip, strip]
        else:
            stripb = [sb(f"A_strip{i}", [128, 3968], F32) for i in range(2)]
            kbias = sb("A_kbias", [128, 24], F32)
        NS = 5 if isB else 6
        LA = 3
        NB = 6
        tmpb = [sb(f"{mixer}_tmp{i}", [128, 512], F32) for i in range(NB)]
        eb = [sb(f"{mixer}_e{i}", [128, 512], BF16) for i in range(NB)]
        rcp = [sb(f"{mixer}_rcp{i}", [128, 512], F32) for i in range(2)]
        if isB:
            onb = [sb(f"B_on{i}", [128, 2, 2, 512], F32) for i in range(2)]
            diff = sb("B_diff", [128, 2, 512], F32)
            sq = sb("B_sq", [128, 2, 512], BF16)
            rt = sb("B_rt", [128, 512], F32)
            rr = sb("B_rr", [128, 512], F32)
        vslot = S.slot("v")
        hslot = [S.slot("hd") for _ in range(2)]
        vsrc = (W["Vb"] if isB else W["Va"])[0:nkt * 128, :].rearrange("(t p) c -> p t c", p=128)
        if not isB:
            for j in range(0, nkt, 4):
                S.dma("sp" if (j // 4) % 2 == 0 else "act", vslot, v_all[:, j:j + 4, :], vsrc[:, j:j + 4, :], writes=["v_all"])
        if isB:
            for j in range(4):
                S.dma("sp", vslot, strip[:, j * 1920:(j + 1) * 1920], I["distB"][:, j * 1920:(j + 1) * 1920], writes=["strip"])
        else:
            S.dma("sp", vslot, kbias[:], I["kbiasA"][:, :], writes=["kbias"])
        KT = W["KbT"] if isB else W["KaT"]
        QT = W["QbT"] if isB else W["QaT"]
        it = 0
        accset = 0
        for h in range(nheads):
            par = h % 2
            for c in range(ncomp):
                row = (h * ncomp + c) * 128
                for j in range(0, nkt * 128, 1024):
                    S.dma("sp" if c == 0 else "act", hslot[par], ktb[par][:, c, j:j + 1024], KT[row:row + 128, j:j + 1024],
                          writes=[("hd", par)])
                S.dma("act", hslot[par], qtb[par][:, c, :], QT[row:row + 128, :], writes=[("hd", par)])
            if not isB:
                S.dma("sp", hslot[par], stripb[par][:], I["stripAh"][h], writes=[("hd", par)])
            else:
                for j in range(0, nkt, 8):
                    S.dma("sp", hslot[par], vbuf[par][:, j:j + 8, :], vsrc[:, j:j + 8, h * 256:(h + 1) * 256],
                          writes=[("hd", par)])
            hk = [("hd", par), "v_all", "strip", "kbias"]
            slope = SLOPES_B[h] if isB else SLOPES_A[h]
            for qc in range(2):
                for c in range(ncomp):
                    abase = NS
                    O = [bk[abase + v] for v in range(nv)]
                    Dn = bk[abase + nv]
                    akeys = [("bank", abase + v) for v in range(nv)] + [("bank", abase + nv)]
                    if isB:
                        kts = list(range(nkt))
                    else:
                        kts = [kt for kt in range(nkt)
                               if not (128 * kt - 1024 - 512 * qc - 511 > 1024 or 128 * kt + 127 - 1024 - 512 * qc < -1024)]
                    pend = []

                    def emit_pv(first, last, kt, ebb, ei, O=O, Dn=Dn, akeys=akeys, par=par, h=h):
                        for v in range(nv):
                            col = (v * 128) if isB else (h * 128)
                            vt = vbuf[par] if isB else v_all
                            S.op("pe", lambda e, v=v, kt=kt, col=col, ebb=ebb, first=first, last=last, vt=vt: e.matmul(
                                O[v][:, :], vt[:, kt, col:col + 128], ebb[:], start=first, stop=last),
                                reads=[("e", ei), ("hd", par), "v_all"], writes=[akeys[v]])
                        S.op("pe", lambda e, ebb=ebb, first=first, last=last: e.matmul(
                            Dn[:, :], cx.ones_b[:], ebb[:], start=first, stop=last),
                            reads=[("e", ei), "ones_b"], writes=[akeys[-1]])

                    for ki, kt in enumerate(kts):
                        sbk = it % NS
                        ps = bk[sbk]
                        S.op("pe", lambda e, ps=ps, par=par, c=c, kt=kt, qc=qc: e.matmul(
                            ps[:, :], ktb[par][:, c, kt * 128:(kt + 1) * 128], qtb[par][:, c, qc * 512:(qc + 1) * 512],
                            start=True, stop=True), reads=hk, writes=[("bank", sbk)])
                        tb = tmpb[it % NB]
                        if isB:
                            g, ktp = kt // 8, kt % 8
                            x0 = g * 1920 + 512 * qc - 128 * ktp + 896
                            c1 = -SCALE / slope
                        else:
                            x0 = 512 * qc - 128 * kt + 2944
                            c1 = SCALE
                        sp_ = stripb[par]
                        S.op("dve", lambda e, tb=tb, ps=ps, sp_=sp_, x0=x0, c1=c1: e.scalar_tensor_tensor(
                            out=tb[:], in0=ps[:, :], scalar=c1, in1=sp_[:, x0:x0 + 512], op0=ALU.mult, op1=ALU.add),
                            reads=[("bank", sbk)] + hk, writes=[("tmp", it % NB)])
                        ebb = eb[it % NB]
                        if isB:
                            S.op("act", lambda e, ebb=ebb, tb=tb, slope=slope: e.activation(
                                out=ebb[:], in_=tb[:], func=AF.Exp, scale=-slope),
                                reads=[("tmp", it % NB)], writes=[("e", it % NB)])
                        else:
                            S.op("act", lambda e, ebb=ebb, tb=tb, kt=kt: e.activation(
                                out=ebb[:], in_=tb[:], func=AF.Exp, bias=kbias[:, kt:kt + 1]),
                                reads=[("tmp", it % NB)] + hk, writes=[("e", it % NB)])
                        first, last = (ki == 0), (ki == len(kts) - 1)
                        pend.append((first, last, kt, ebb, it % NB))
                        it += 1
                        if len(pend) > LA:
                            emit_pv(*pend.pop(0))
                    while pend:
                        emit_pv(*pend.pop(0))
                    rc = rcp[accset]
                    S.op("dve", lambda e, rc=rc, Dn=Dn: e.reciprocal(out=rc[:], in_=Dn[:, :]),
                         reads=[akeys[-1]], writes=[("rcp", accset)])
                    for v in range(nv):
                        if isB:
                            dst = onb[qc % 2][:, c, v, :]
                            wk = ("on", qc % 2, c)
                        else:
                            dst = yT[:, h, qc * 512:(qc + 1) * 512]
                            wk = ("yT", mixer)
                        S.op("dve", lambda e, dst=dst, O=O, v=v, rc=rc: e.tensor_tensor(
                            out=dst, in0=O[v][:, :], in1=rc[:], op=ALU.mult),
                            reads=[akeys[v], ("rcp", accset)], writes=[wk])
                    accset ^= 1
                if isB:
                    on = onb[qc % 2]
                    ok = [("on", qc % 2, 0), ("on", qc % 2, 1)]
                    for v in range(2):
                        S.op("dve", lambda e, on=on, v=v: e.scalar_tensor_tensor(
                            out=diff[:, v, :], in0=on[:, 1, v, :], scalar=cx.neglam[:, 0:1], in1=on[:, 0, v, :],
                            op0=ALU.mult, op1=ALU.add), reads=ok + ["neglam"], writes=["diff"])
                    S.op("act", lambda e: e.activation(out=sq[:], in_=diff[:], func=AF.Square), reads=["diff"], writes=["sq"])
                    sbk = it % NS
                    it += 1
                    ps = bk[sbk]
                    for v in range(2):
                        S.op("pe", lambda e, ps=ps, v=v: e.matmul(ps[:, :], cx.ones_b[:], sq[:, v, :], start=(v == 0), stop=(v == 1)),
                             reads=["sq", "ones_b"], writes=[("bank", sbk)])
                    S.op("act", lambda e, ps=ps: e.activation(out=rt[:], in_=ps[:, :], func=AF.Sqrt, scale=1.0 / 256,
                                                             bias=cx.epsc[:, 0:1]),
                         reads=[("bank", sbk), "epsc"], writes=["rt"])
                    S.op("dve", lambda e: e.reciprocal(out=rr[:], in_=rt[:]), reads=["rt"], writes=["rr"])
                    for v in range(2):
                        S.op("dve", lambda e, v=v, h=h, qc=qc: e.scalar_tensor_tensor(
                            out=yT[:, 2 * h + v, qc * 512:(qc + 1) * 512], in0=diff[:, v, :], scalar=cx.sg[:, v:v + 1],
                            in1=rr[:], op0=ALU.mult, op1=ALU.mult), reads=["diff", "rr", "sg"], writes=[("yT", mixer)])
        ydst = W["YbT"] if isB else W["YaT"]
        for j in range(8):
            S.dma("sp", vslot, ydst[j * 128:(j + 1) * 128, :], yT[:, j, :], reads=[("yT", mixer)])
        if ("d_y" + mixer) in cx.debug:
            for j in range(8):
                S.dma("sp", cx.dbg_slot, cx.O["d_y" + mixer][j * 128:(j + 1) * 128, :], yT[:, j, :], reads=[("yT", mixer)])
    S.barrier()


def phaseD(cx):
    nc, S, I, W = cx.nc, cx.S, cx.I, cx.W
    bk = cx.banks
    with contextlib.ExitStack() as st:
        sb = lambda name, shape, dt: st.enter_context(nc.sbuf_tensor(name, list(shape), dt))
        with contextlib.ExitStack() as st1:
            sb1 = lambda name, shape, dt: st1.enter_context(nc.sbuf_tensor(name, list(shape), dt))
            mT = sb1("D_mT", [128, KC, OWN], BF16)
            ya = sb1("D_ya", [128, 8, OWN], BF16)
            yb = sb1("D_yb", [128, 8, OWN], BF16)
            woa = sb1("D_woa", [128, 8, D], BF16)
            wob = sb1("D_wob", [128, 8, D], BF16)
            gat = [sb1(f"D_g{i}", [128, 2, 512], BF16) for i in range(2)]
            t1 = [sb1(f"D_t1{i}", [128, 512], F32) for i in range(2)]
            t2 = [sb1(f"D_t2{i}", [128, 512], F32) for i in range(2)]
            ls = S.slot("D1")
            gs = [S.slot("D1g") for _ in range(2)]
            for j in range(8):
                S.dma("sp", ls, ya[:, j, :], W["YaT"][j * 128:(j + 1) * 128, :], writes=["ya"])
                S.dma("act", ls, yb[:, j, :], W["YbT"][j * 128:(j + 1) * 128, :], writes=["yb"])
            wav = I["w_out_a"].rearrange("(k p) n -> p k n", p=128)
            wbv = I["w_out_b"].rearrange("(k p) n -> p k n", p=128)
            for j in range(0, 8, 2):
                S.dma("pool", ls, woa[:, j:j + 2, :], wav[:, j:j + 2, :], writes=["woa"])
                S.dma("pool", ls, wob[:, j:j + 2, :], wbv[:, j:j + 2, :], writes=["wob"])
            if "d_ya" in cx.debug:
                for j in range(8):
                    S.dma("sp", cx.dbg_slot, cx.O["d_ya"][j * 128:(j + 1) * 128, :], ya[:, j, :], reads=["ya"])
                    S.dma("sp", cx.dbg_slot, cx.O["d_yb"][j * 128:(j + 1) * 128, :], yb[:, j, :], reads=["yb"])
            it = 0
            for dc in range(KC):
                for qc in range(2):
                    gb_ = gat[it % 2]
                    S.dma("sp", gs[it % 2], gb_[:, 0, :], W["GT"][dc * 128:(dc + 1) * 128, qc * 512:(qc + 1) * 512],
                          writes=[("gat", it % 2)])
                    S.dma("sp", gs[it % 2], gb_[:, 1, :], W["GT"][2048 + dc * 128:2048 + (dc + 1) * 128, qc * 512:(qc + 1) * 512],
                          writes=[("gat", it % 2)])
                    pa, pb = bk[(2 * it) % 4], bk[(2 * it + 1) % 4]
                    for f in range(8):
                        S.op("pe", lambda e, pa=pa, f=f, dc=dc, qc=qc: e.matmul(
                            pa[:, :], woa[:, f, dc * 128:(dc + 1) * 128], ya[:, f, qc * 512:(qc + 1) * 512],
                            start=(f == 0), stop=(f == 7)), reads=["woa", "ya"], writes=[("bank", (2 * it) % 4)])
                    for f in range(8):
                        S.op("pe", lambda e, pb=pb, f=f, dc=dc, qc=qc: e.matmul(
                            pb[:, :], wob[:, f, dc * 128:(dc + 1) * 128], yb[:, f, qc * 512:(qc + 1) * 512],
                            start=(f == 0), stop=(f == 7)), reads=["wob", "yb"], writes=[("bank", (2 * it + 1) % 4)])
                    a1, a2 = t1[it % 2], t2[it % 2]
                    S.op("dve", lambda e, a1=a1, pa=pa, gb_=gb_: e.tensor_tensor(out=a1[:], in0=pa[:, :], in1=gb_[:, 0, :], op=ALU.mult),
                         reads=[("bank", (2 * it) % 4), ("gat", it % 2)], writes=[("t1", it % 2)])
                    S.op("dve", lambda e, a2=a2, pb=pb, gb_=gb_: e.tensor_tensor(out=a2[:], in0=pb[:, :], in1=gb_[:, 1, :], op=ALU.mult),
                         reads=[("bank", (2 * it + 1) % 4), ("gat", it % 2)], writes=[("t2", it % 2)])
                    if "d_gat" in cx.debug and it == 0:
                        S.dma("sp", cx.dbg_slot, cx.O["d_gat"][:, :], gb_[:].rearrange("p a b -> p (a b)"), reads=[("gat", 0)])
                        S.dma("sp", cx.dbg_slot, cx.O["d_a1"][:, 0:512], a1[:], reads=[("t1", 0)])
                        S.dma("sp", cx.dbg_slot, cx.O["d_a1"][:, 512:1024], a2[:], reads=[("t2", 0)])
                    S.op("dve", lambda e, a1=a1, a2=a2, dc=dc, qc=qc: e.tensor_tensor(
                        out=mT[:, dc, qc * 512:(qc + 1) * 512], in0=a1[:], in1=a2[:], op=ALU.add),
                        reads=[("t1", it % 2), ("t2", it % 2)], writes=["mT"])
                    it += 1
            for k in range(KC):
                S.dma("sp", ls, W["MT"][k * 128:(k + 1) * 128, :], mT[:, k, :], reads=["mT"])
                if "d_mT" in cx.debug:
                    S.dma("sp", cx.dbg_slot, cx.O["d_mT"][k * 128:(k + 1) * 128, :], mT[:, k, :], reads=["mT"])
        S.barrier()
        h = sb("D_h", [128, 8, D], F32)
        rowB = sb("D_rowB", [128, D], F32)
        rowC = sb("D_rowC", [128, D], F32)
        st2 = contextlib.ExitStack()
        sb2 = lambda name, shape, dt: st2.enter_context(nc.sbuf_tensor(name, list(shape), dt))
        mT2 = sb2("D_mT2", [128, KC, OWN], BF16)
        wo = sb2("D_wo", [128, KC, D], BF16)
        tq = [sb2(f"D_tq{i}", [128, 512], F32) for i in range(2)]
        ls = S.slot("D2")
        for k in range(KC):
            S.dma("act", ls, mT2[:, k, :], W["MT"][k * 128:(k + 1) * 128, :], writes=["mT2"])
        wov = I["w_o"].rearrange("(k p) n -> p k n", p=128)
        for j in range(0, KC, 2):
            S.dma("pool", ls, wo[:, j:j + 2, :], wov[:, j:j + 2, :], writes=["wo"])
        xo = I["xr"][1024:2048, :].rearrange("(t p) d -> p t d", p=128)
        for tt in range(8):
            S.dma("sp" if tt % 2 == 0 else "act", ls, h[:, tt, :], xo[:, tt, :], writes=[("h", tt)])
        S.dma("sp", ls, rowB[:], W["rows"][0:1, :].partition_broadcast(128), writes=["rowB"])
        S.dma("sp", ls, rowC[:], W["rows"][2:3, :].partition_broadcast(128), writes=["rowC"])
        it = 0
        for tt in range(8):
            for ch in range(4):
                ps = bk[it % 4]
                for k in range(KC):
                    S.op("pe", lambda e, ps=ps, k=k, tt=tt, ch=ch: e.matmul(
                        ps[:, :], mT2[:, k, tt * 128:(tt + 1) * 128], wo[:, k, ch * 512:(ch + 1) * 512],
                        start=(k == 0), stop=(k == KC - 1)), reads=["wo", "mT2"], writes=[("bank", it % 4)])
                tb = tq[it % 2]
                S.op("dve", lambda e, tb=tb, ps=ps, ch=ch: e.tensor_tensor(
                    out=tb[:], in0=ps[:, :], in1=rowB[:, ch * 512:(ch + 1) * 512], op=ALU.mult),
                    reads=[("bank", it % 4), "rowB"], writes=[("tq", it % 2)])
                S.op("pool", lambda e, tb=tb, tt=tt, ch=ch: e.tensor_tensor(
                    out=h[:, tt, ch * 512:(ch + 1) * 512], in0=h[:, tt, ch * 512:(ch + 1) * 512], in1=tb[:], op=ALU.add),
                    reads=[("tq", it % 2), ("h", tt)], writes=[("h", tt)])
                it += 1
        if "d_rowB" in cx.debug:
            S.dma("sp", cx.dbg_slot, cx.O["d_rowB"][:, :], rowB[:], reads=["rowB"])
        hs = S.slot("hs")
        for tt in range(8):
            S.dma("sp", hs, W["hs"][tt * 128:(tt + 1) * 128, :], h[:, tt, :], reads=[("h", tt)])
            if "d_h1" in cx.debug:
                S.dma("sp", cx.dbg_slot, cx.O["d_h1"][tt * 128:(tt + 1) * 128, :], h[:, tt, :], reads=[("h", tt)])
        S.barrier()
        st2.close()
        S.dma("sp", ls, rowB[:], W["rows"][1:2, :].partition_broadcast(128), reads=[], writes=["rowB"])
        wr = sb("D_wr", [128, KC, N_EXP], F32)
        brB = sb("D_brB", [128, N_EXP], F32)
        triu = sb("D_triu", [128, 128], BF16)
        triu_f = sb("D_triuf", [128, 128], F32)
        S.dma("sp", ls, wr[:], I["w_router"].rearrange("(k p) n -> p k n", p=128), writes=["wr"])
        S.dma("sp", ls, brB[:], I["b_routerB"][:, :], writes=["brB"])
        S.dma("sp", ls, triu_f[:], I["triu"][:, :], writes=["triu_f"])
        S.op("dve", lambda e: e.tensor_copy(out=triu[:], in_=triu_f[:]), reads=["triu_f"], writes=["triu"])
        u2f = [sb(f"D_u2f{i}", [128, D], F32) for i in range(2)]
        u2b = [sb(f"D_u2b{i}", [128, D], BF16) for i in range(2)]
        u2T = [sb(f"D_u2T{i}", [128, KC, 128], F32) for i in range(2)]
        ss = sb("D_ss", [128, 8], F32)
        rt = sb("D_rt", [128, 8], F32)
        rs = sb("D_rs", [128, 8], F32)
        lg = sb("D_lg", [128, 8, N_EXP], F32)
        mx8 = sb("D_mx8", [128, 8, 8], F32)
        negm = sb("D_negm", [128, 8], F32)
        mask = sb("D_mask", [128, 8, N_EXP], F32)
        mask_b = sb("D_maskb", [128, 8, N_EXP], BF16)
        ex = sb("D_ex", [128, 8, N_EXP], F32)
        sm = sb("D_sm", [128, 8], F32)
        rsm = sb("D_rsm", [128, 8], F32)
        us = [S.slot("u2") for _ in range(2)]
        pit = 0
        for tt in range(8):
            uf, ub, uT_ = u2f[tt % 2], u2b[tt % 2], u2T[tt % 2]
            S.op("act", lambda e, ub=ub, tt=tt: e.activation(out=ub[:], in_=h[:, tt, :], func=AF.Square, accum_out=ss[:, tt:tt + 1]),
                 reads=[("h", tt)], writes=[("u2b", tt % 2), ("ss", tt)])
            S.op("act", lambda e, tt=tt: e.activation(out=rt[:, tt:tt + 1], in_=ss[:, tt:tt + 1], func=AF.Sqrt, scale=1.0 / D,
                                                      bias=cx.epsc[:, 0:1]), reads=[("ss", tt), "epsc"], writes=[("rt", tt)])
            S.op("dve", lambda e, tt=tt: e.reciprocal(out=rs[:, tt:tt + 1], in_=rt[:, tt:tt + 1]), reads=[("rt", tt)], writes=[("rs", tt)])
            S.op("dve", lambda e, uf=uf, tt=tt: e.scalar_tensor_tensor(out=uf[:], in0=h[:, tt, :], scalar=rs[:, tt:tt + 1], in1=rowB[:],
                                                                       op0=ALU.mult, op1=ALU.mult),
                 reads=[("h", tt), ("rs", tt), "rowB"], writes=[("u2f", tt % 2)])
            S.op("pool", lambda e, uf=uf: e.tensor_tensor(out=uf[:], in0=uf[:], in1=rowC[:], op=ALU.add),
                 reads=[("u2f", tt % 2), "rowC"], writes=[("u2f", tt % 2)])
            S.op("act", lambda e, ub=ub, uf=uf: e.activation(out=ub[:], in_=uf[:], func=AF.Copy),
                 reads=[("u2f", tt % 2)], writes=[("u2b", tt % 2)])
            S.dma("sp", us[tt % 2], W["U2"][tt * 128:(tt + 1) * 128, :], ub[:], reads=[("u2b", tt % 2)])
            for q4 in range(4):
                bi = pit % 4
                pit += 1
                ps = bk[bi]
                for q in range(4):
                    k = q4 * 4 + q
                    S.op("pe", lambda e, ps=ps, q=q, k=k, uf=uf: e.transpose(ps[:, q * 128:(q + 1) * 128], uf[:, k * 128:(k + 1) * 128],
                                                                              cx.ident_f[:]),
                         reads=[("u2f", tt % 2), "ident_f"], writes=[("bank", bi)])
                evac_copy(S, q4, uT_[:, q4 * 4:(q4 + 1) * 4, :], ps[:, :].rearrange("p (a b) -> p a b", a=4), [("bank", bi)], [("u2T", tt % 2)])
            psl = bk[4 + tt % 2]
            for k in range(KC):
                S.op("pe", lambda e, psl=psl, k=k, uT_=uT_: e.matmul(psl[:, 0:N_EXP], uT_[:, k, :], wr[:, k, :],
                                                                     start=(k == 0), stop=(k == KC - 1)),
                     reads=[("u2T", tt % 2), "wr"], writes=[("bank", 4 + tt % 2)])
            S.op("dve", lambda e, psl=psl, tt=tt: e.tensor_tensor(out=lg[:, tt, :], in0=psl[:, 0:N_EXP], in1=brB[:], op=ALU.add),
                 reads=[("bank", 4 + tt % 2), "brB"], writes=[("lg", tt)])
            S.op("dve", lambda e, tt=tt: e.max(out=mx8[:, tt, :], in_=lg[:, tt, :]), reads=[("lg", tt)], writes=[("mx8", tt)])
            S.op("dve", lambda e, tt=tt: e.tensor_scalar(out=mask[:, tt, :], in0=lg[:, tt, :], scalar1=mx8[:, tt, 3:4], scalar2=None,
                                                         op0=ALU.is_ge), reads=[("lg", tt), ("mx8", tt)], writes=[("mask", tt)])
            S.op("dve", lambda e, tt=tt: e.tensor_scalar(out=negm[:, tt:tt + 1], in0=mx8[:, tt, 0:1], scalar1=-1.0, scalar2=None,
                                                         op0=ALU.mult), reads=[("mx8", tt)], writes=[("negm", tt)])
            S.op("act", lambda e, tt=tt: e.activation(out=ex[:, tt, :], in_=lg[:, tt, :], func=AF.Exp, bias=negm[:, tt:tt + 1]),
                 reads=[("lg", tt), ("negm", tt)], writes=[("ex", tt)])
            S.op("dve", lambda e, tt=tt: e.tensor_tensor(out=ex[:, tt, :], in0=ex[:, tt, :], in1=mask[:, tt, :], op=ALU.mult),
                 reads=[("ex", tt), ("mask", tt)], writes=[("ex", tt)])
            S.op("dve", lambda e, tt=tt: e.reduce_sum(out=sm[:, tt:tt + 1], in_=ex[:, tt, :], axis=AX.X),
                 reads=[("ex", tt)], writes=[("sm", tt)])
            S.op("dve", lambda e, tt=tt: e.reciprocal(out=rsm[:, tt:tt + 1], in_=sm[:, tt:tt + 1]), reads=[("sm", tt)], writes=[("rsm", tt)])
            S.op("dve", lambda e, tt=tt: e.tensor_scalar(out=cx.prob[:, tt, :], in0=ex[:, tt, :], scalar1=rsm[:, tt:tt + 1], scalar2=None,
                                                         op0=ALU.mult), reads=[("ex", tt), ("rsm", tt)], writes=[("prob", tt)])
            S.op("dve", lambda e, tt=tt: e.tensor_copy(out=cx.prob_hi[:, tt, :], in_=cx.prob[:, tt, :]), reads=[("prob", tt)], writes=[("phi", tt)])
            S.op("dve", lambda e, tt=tt: e.tensor_tensor(out=cx.prob_lo[:, tt, :], in0=cx.prob[:, tt, :], in1=cx.prob_hi[:, tt, :],
                                                         op=ALU.subtract), reads=[("prob", tt), ("phi", tt)], writes=[("plo", tt)])
            S.op("dve", lambda e, tt=tt: e.tensor_copy(out=mask_b[:, tt, :], in_=mask[:, tt, :]), reads=[("mask", tt)], writes=[("maskb", tt)])
            pp = bk[6 + tt % 2]
            S.op("pe", lambda e, pp=pp, tt=tt: e.matmul(pp[:, 0:N_EXP], triu[:], mask_b[:, tt, :], start=True, stop=(tt == 0)),
                 reads=["triu", ("maskb", tt)], writes=[("bank", 6 + tt % 2)])
            for t2_ in range(tt):
                S.op("pe", lambda e, pp=pp, t2_=t2_, tt=tt: e.matmul(pp[:, 0:N_EXP], cx.ones_b[:], mask_b[:, t2_, :], start=False,
                                                                    stop=(t2_ == tt - 1)),
                     reads=["ones_b", ("maskb", t2_)], writes=[("bank", 6 + tt % 2)])
            S.op("dve", lambda e, pp=pp, tt=tt: e.tensor_tensor(out=cx.posm[:, tt, :], in0=pp[:, 0:N_EXP], in1=mask[:, tt, :], op=ALU.mult),
                 reads=[("bank", 6 + tt % 2), ("mask", tt)], writes=[("posm", tt)])
        if "d_lg" in cx.debug:
            S.dma("sp", cx.dbg_slot, cx.O["d_lg"][:, :], lg[:].rearrange("p a b -> p (a b)"), reads=[("lg", t) for t in range(8)])
            S.dma("sp", cx.dbg_slot, cx.O["d_posm"][:, :], cx.posm[:].rearrange("p a b -> p (a b)"), reads=[("posm", t) for t in range(8)])
            S.dma("sp", cx.dbg_slot, cx.O["d_prob"][:, :], cx.prob[:].rearrange("p a b -> p (a b)"), reads=[("prob", t) for t in range(8)])
    S.barrier()


def phaseE1(cx):
    nc, S, I, W = cx.nc, cx.S, cx.I, cx.W
    bk = cx.banks
    I32 = mybir.dt.int32
    NR = 5
    NST = CAP // 128
    with contextlib.ExitStack() as st:
        sb = lambda name, shape, dt: st.enter_context(nc.sbuf_tensor(name, list(shape), dt))
        ring = [sb(f"E_w{i}", [128, KC, 512], BF16) for i in range(NR)]
        rslot = [S.slot("ew") for _ in range(NR)]
        slb = [sb(f"E_sel{i}", [128, 8, CAP], BF16) for i in range(2)]
        xg = [[sb(f"E_xg{i}_{j}", [128, D], BF16) for j in range(NST)] for i in range(2)]
        xslot = [S.slot("xg") for _ in range(2)]
        pz = [sb(f"E_pz{i}", [128, 16], F32) for i in range(2)]
        idf = [sb(f"E_idf{i}", [128, NST], F32) for i in range(2)]
        idi = [sb(f"E_idi{i}", [128, NST], I32) for i in range(2)]
        tokab = sb("E_tokab", [128, 8, 2], BF16)
        tokab_f = sb("E_tokabf", [128, 8, 2], F32)
        x_ = sb("E_xe", [128, KC, CAP], BF16)
        a_ = sb("E_act", [128, KC, CAP], BF16)
        ye = sb("E_ye", [128, NST, D], BF16)
        yslot = S.slot("ye")
        bgT = sb("E_bgT", [128, N_EXP * KC], F32)
        buT = sb("E_buT", [128, N_EXP * KC], F32)
        gsb = [sb(f"E_g{i}", [128, CAP], F32) for i in range(2)]
        sig = [sb(f"E_s{i}", [128, CAP], F32) for i in range(2)]
        usb = [sb(f"E_u{i}", [128, CAP], F32) for i in range(2)]
        ls = S.slot("E1")
        S.op("dve", lambda en: en.memset(ye[:, 0, :], 0.0), writes=["ye"])
        S.dma("sp", ls, W["Y"][N_EXP * CAP:N_EXP * CAP + 128, :], ye[:, 0, :], reads=["ye"])
        S.dma("sp", ls, bgT[:], I["b_gateT"][:, :], writes=["bgT"])
        S.dma("sp", ls, buT[:], I["b_upT"][:, :], writes=["buT"])
        S.dma("sp", ls, tokab_f[:].rearrange("p a b -> p (a b)"), I["tokab"][:, :], writes=["tokab_f"])
        S.op("dve", lambda en: en.tensor_copy(out=tokab[:], in_=tokab_f[:]), reads=["tokab_f"], writes=["tokab"])
        wg = I["w_gate"].rearrange("e (k p) n -> e p k n", p=128)
        wu = I["w_up"].rearrange("e (k p) n -> e p k n", p=128)
        wd = I["w_down"].rearrange("e (k p) n -> e p k n", p=128)
        cnt = {"piece": 0}

        def load_piece(src, e, g):
            ri = cnt["piece"] % NR
            cnt["piece"] += 1
            for j in range(4):
                S.dma("pool", rslot[ri], ring[ri][:, 4 * j:4 * j + 4, :], src[e, :, 4 * j:4 * j + 4, g * 512:(g + 1) * 512],
                      writes=[("ring", ri)])
            return ri

        def prep(e):
            par = e % 2
            sl = slb[par]
            for tt in range(8):
                S.op("dve", lambda en, tt=tt, e=e, sl=sl: en.tensor_scalar(
                    out=sl[:, tt, :], in0=cx.iota[:], scalar1=cx.posm[:, tt, e:e + 1], scalar2=None, op0=ALU.is_equal),
                    reads=["iota", ("posm", tt)], writes=[("sel", par)])
            ps = bk[1]
            for st_ in range(NST):
                n = 0
                for tt in range(8):
                    for pr in (cx.prob_hi, cx.prob_lo):
                        S.op("pe", lambda en, st_=st_, tt=tt, pr=pr, e=e, n=n, sl=sl: en.matmul(
                            ps[:, st_:st_ + 1], sl[:, tt, st_ * 128:(st_ + 1) * 128], pr[:, tt, e:e + 1],
                            start=(n == 0), stop=(n == 15)),
                            reads=[("sel", par), ("phi", tt), ("plo", tt)], writes=[("bank", 1)])
                        n += 1
                for tt in range(8):
                    S.op("pe", lambda en, st_=st_, tt=tt, sl=sl: en.matmul(
                        ps[:, 8 + 2 * st_:10 + 2 * st_], sl[:, tt, st_ * 128:(st_ + 1) * 128], tokab[:, tt, :],
                        start=(tt == 0), stop=(tt == 7)),
                        reads=[("sel", par), "tokab"], writes=[("bank", 1)])
            pzz = pz[par]
            S.op("dve", lambda en, pzz=pzz: en.tensor_copy(out=pzz[:], in_=ps[:, 0:16]), reads=[("bank", 1)], writes=[("pz", par)])
            idf_, idi_ = idf[par], idi[par]
            S.op("dve", lambda en, pzz=pzz, idf_=idf_: en.scalar_tensor_tensor(
                out=idf_[:], in0=pzz[:, 8:16:2], scalar=32.0, in1=pzz[:, 9:16:2], op0=ALU.mult, op1=ALU.add),
                reads=[("pz", par)], writes=[("idf", par)])
            S.op("dve", lambda en, idf_=idf_, idi_=idi_: en.tensor_copy(out=idi_[:], in_=idf_[:]), reads=[("idf", par)], writes=[("idi", par)])
            for st_ in range(NST):
                def fn(en, st_=st_, par=par, idi_=idi_):
                    return en.indirect_dma_start(out=xg[par][st_][:, :], out_offset=None, in_=W["U2"][:, :],
                                                 in_offset=bass.IndirectOffsetOnAxis(ap=idi_[:, st_:st_ + 1], axis=0))
                o = Op("pool", fn)
                o.dma = True
                o.slot = xslot[par]
                o.slot_total = xslot[par].total + 16
                S._add(o, [("idi", par)], [("xg", par)])
                xslot[par].total += 16

        fi = 0
        di = 0
        ti = 0
        prep(0)
        for e in range(N_EXP):
            par = e % 2
            p_ = pz[par]
            for k2 in range(KC // 2):
                bi = ti % 2
                ti += 1
                pst = bk[bi][:].bitcast(BF16)
                for q in range(2):
                    k = 2 * k2 + q
                    for st_ in range(NST):
                        S.op("pe", lambda en, pst=pst, q=q, k=k, st_=st_, par=par: en.transpose(
                            pst[:, q * CAP + st_ * 128:q * CAP + (st_ + 1) * 128], xg[par][st_][:, k * 128:(k + 1) * 128], cx.ident_b[:]),
                            reads=[("xg", par), "ident_b"], writes=[("bank", bi)])
                evac_copy(S, k2, x_[:, 2 * k2:2 * k2 + 2, :], pst[:, :].rearrange("p (a b) -> p a b", a=2),
                          [("bank", bi)], ["xe"])
            for g in range(4):
                rg = load_piece(wg, e, g)
                ru = load_piece(wu, e, g)
                if g == 1 and e + 1 < N_EXP:
                    prep(e + 1)
                for f4 in range(4):
                    fc = 4 * g + f4
                    pg, pu = bk[2 + fi % 2], bk[4 + fi % 2]
                    kg, ku = ("bank", 2 + fi % 2), ("bank", 4 + fi % 2)
                    for k in range(KC):
                        S.op("pe", lambda en, pg=pg, rg=rg, k=k, f4=f4: en.matmul(
                            pg[:, 0:CAP], ring[rg][:, k, f4 * 128:(f4 + 1) * 128], x_[:, k, :], start=(k == 0), stop=(k == KC - 1)),
                            reads=[("ring", rg), "xe"], writes=[kg])
                    for k in range(KC):
                        S.op("pe", lambda en, pu=pu, ru=ru, k=k, f4=f4: en.matmul(
                            pu[:, 0:CAP], ring[ru][:, k, f4 * 128:(f4 + 1) * 128], x_[:, k, :], start=(k == 0), stop=(k == KC - 1)),
                            reads=[("ring", ru), "xe"], writes=[ku])
                    w = fi % 2
                    fi += 1
                    gb_, sg_, ub_ = gsb[w], sig[w], usb[w]
                    col = e * KC + fc
                    S.op("dve", lambda en, gb_=gb_, pg=pg, col=col: en.tensor_scalar(
                        out=gb_[:], in0=pg[:, 0:CAP], scalar1=bgT[:, col:col + 1], scalar2=7.0, op0=ALU.add, op1=ALU.min),
                        reads=[kg, "bgT"], writes=[("gsb", w)])
                    S.op("act", lambda en, sg_=sg_, gb_=gb_: en.activation(out=sg_[:], in_=gb_[:], func=AF.Sigmoid, scale=1.702),
                         reads=[("gsb", w)], writes=[("sig", w)])
                    S.op("act", lambda en, ub_=ub_, pu=pu, col=col: en.activation(
                        out=ub_[:], in_=pu[:, 0:CAP], func=AF.Identity, bias=buT[:, col:col + 1]),
                        reads=[ku, "buT"], writes=[("usb", w)])
                    S.op("dve", lambda en, ub_=ub_: en.tensor_scalar(
                        out=ub_[:], in0=ub_[:], scalar1=7.0, scalar2=-7.0, op0=ALU.min, op1=ALU.max),
                        reads=[("usb", w)], writes=[("usb", w)])
                    S.op("dve", lambda en, gb_=gb_, sg_=sg_: en.tensor_tensor(out=gb_[:], in0=gb_[:], in1=sg_[:], op=ALU.mult),
                         reads=[("gsb", w), ("sig", w)], writes=[("gsb", w)])
                    S.op("dve", lambda en, fc=fc, gb_=gb_, ub_=ub_: en.scalar_tensor_tensor(
                        out=a_[:, fc, :], in0=ub_[:], scalar=1.0, in1=gb_[:], op0=ALU.add, op1=ALU.mult),
                        reads=[("gsb", w), ("usb", w)], writes=["act"])
            for ch in range(4):
                rd = load_piece(wd, e, ch)
                for st_ in range(NST):
                    pd = bk[6 + di % 2]
                    kd = ("bank", 6 + di % 2)
                    for f in range(KC):
                        S.op("pe", lambda en, pd=pd, rd=rd, f=f, st_=st_: en.matmul(
                            pd[:, :], a_[:, f, st_ * 128:(st_ + 1) * 128], ring[rd][:, f, :], start=(f == 0), stop=(f == KC - 1)),
                            reads=[("ring", rd), "act"], writes=[kd])
                    if di % 2 == 0:
                        S.op("act", lambda en, st_=st_, ch=ch, pd=pd, p_=p_: en.activation(
                            out=ye[:, st_, ch * 512:(ch + 1) * 512], in_=pd[:, :], func=AF.Copy, scale=p_[:, st_:st_ + 1]),
                            reads=[kd, ("pz", par)], writes=["ye"])
                    else:
                        S.op("dve", lambda en, st_=st_, ch=ch, pd=pd, p_=p_: en.tensor_scalar(
                            out=ye[:, st_, ch * 512:(ch + 1) * 512], in0=pd[:, :], scalar1=p_[:, st_:st_ + 1], scalar2=None, op0=ALU.mult),
                            reads=[kd, ("pz", par)], writes=["ye"])
                    di += 1
            for st_ in range(NST):
                S.dma("sp", yslot, W["Y"][e * CAP + st_ * 128:e * CAP + (st_ + 1) * 128, :], ye[:, st_, :], reads=["ye"])
    S.barrier()


def cx_rd(cnt, rds, ch):
    return rds[ch]


def phaseE2(cx):
    nc, S, I, W = cx.nc, cx.S, cx.I, cx.W
    bk = cx.banks
    I32 = mybir.dt.int32
    ZR = N_EXP * CAP
    with contextlib.ExitStack() as st:
        sb = lambda name, shape, dt: st.enter_context(nc.sbuf_tensor(name, list(shape), dt))
        ecapB = sb("F_ecap", [128, 8, N_EXP], F32)
        vv = sb("F_vv", [128, 8, N_EXP], F32)
        vt = sb("F_vt", [128, 8, N_EXP], F32)
        m8 = sb("F_m8", [128, 8, 8], F32)
        eq0 = sb("F_eq0", [128, 8, 4], F32)
        idf = sb("F_idf", [128, 8, 4], F32)
        idi = sb("F_idi", [128, 8, 4], I32)
        gb = [[sb(f"F_g{i}_{k}", [128, D], BF16) for k in range(4)] for i in range(2)]
        gslot = [S.slot("gg") for _ in range(2)]
        acc = [sb(f"F_acc{i}", [128, D], F32) for i in range(2)]
        ht = [sb(f"F_h{i}", [128, D], F32) for i in range(2)]
        hslot = [S.slot("hh") for _ in range(2)]
        g2B = sb("F_g2B", [128, D], F32)
        gfB = sb("F_gfB", [128, D], F32)
        ot = [sb(f"F_ot{i}", [128, D], F32) for i in range(2)]
        bdf = sb("F_bdf", [N_EXP, D], F32)
        bdb = sb("F_bdb", [N_EXP, D], BF16)
        pT = sb("F_pT", [N_EXP, 8, 2, 128], BF16)
        ss = sb("F_ss", [128, 8], F32)
        rt = sb("F_rt", [128, 8], F32)
        rs = sb("F_rs", [128, 8], F32)
        ls = S.slot("E2")
        S.dma("sp", ls, g2B[:], W["rows"][3:4, :].partition_broadcast(128), writes=["g2B"])
        S.dma("sp", ls, gfB[:], I["nf"][0:1, :].partition_broadcast(128), writes=["gfB"])
        S.dma("sp", ls, bdf[:], I["b_down"][:, :], writes=["bdf"])
        S.dma("sp", ls, ecapB[:].rearrange("p a b -> p (a b)"), I["ecapB"][:, :], writes=["ecapB"])
        S.op("dve", lambda e: e.tensor_copy(out=bdb[:], in_=bdf[:]), reads=["bdf"], writes=["bdb"])
        pk = [("posm", t) for t in range(8)]
        S.op("dve", lambda e: e.tensor_scalar(out=vt[:], in0=cx.posm[:], scalar1=0.0, scalar2=None, op0=ALU.is_gt),
             reads=pk, writes=["vt"])
        S.op("dve", lambda e: e.tensor_scalar(out=vv[:], in0=cx.posm[:], scalar1=float(CAP), scalar2=None, op0=ALU.is_le),
             reads=pk, writes=["vv"])
        S.op("dve", lambda e: e.tensor_tensor(out=vt[:], in0=vt[:], in1=vv[:], op=ALU.mult), reads=["vt", "vv"], writes=["vt"])
        S.op("dve", lambda e: e.tensor_tensor(out=vv[:], in0=cx.posm[:], in1=ecapB[:], op=ALU.add), reads=pk + ["ecapB", "vv"], writes=["vv"])
        S.op("dve", lambda e: e.tensor_tensor(out=vv[:], in0=vv[:], in1=vt[:], op=ALU.mult), reads=["vv", "vt"], writes=["vv"])
        for tt in range(8):
            S.op("dve", lambda e, tt=tt: e.max(out=m8[:, tt, :], in_=vv[:, tt, :]), reads=["vv"], writes=["m8"])
        S.op("dve", lambda e: e.tensor_scalar(out=eq0[:], in0=m8[:, :, 0:4], scalar1=0.0, scalar2=float(ZR + 1), op0=ALU.is_equal, op1=ALU.mult),
             reads=["m8"], writes=["eq0"])
        S.op("dve", lambda e: e.scalar_tensor_tensor(out=idf[:], in0=m8[:, :, 0:4], scalar=-1.0, in1=eq0[:], op0=ALU.add, op1=ALU.add),
             reads=["m8", "eq0"], writes=["idf"])
        S.op("dve", lambda e: e.tensor_copy(out=idi[:], in_=idf[:]), reads=["idf"], writes=["idi"])
        pTb = bk[6][:].bitcast(BF16)
        for tt in range(8):
            for hl, pr in enumerate((cx.prob_hi, cx.prob_lo)):
                j = (tt * 2 + hl) % 8
                S.op("pe", lambda e, j=j, pr=pr, tt=tt: e.transpose(pTb[0:N_EXP, j * 128:(j + 1) * 128], pr[:, tt, :], cx.ident_b[:]),
                     reads=[("phi", tt), ("plo", tt), "ident_b"], writes=[("bank", 6)])
                S.op("dve", lambda e, j=j, tt=tt, hl=hl: e.tensor_copy(out=pT[:, tt, hl, :], in_=pTb[0:N_EXP, j * 128:(j + 1) * 128]),
                     reads=[("bank", 6)], writes=["pT"])
        for tt in range(8):
            par = tt % 2
            hb = ht[par]
            S.dma("act", hslot[par], hb[:], W["hs"][tt * 128:(tt + 1) * 128, :], writes=[("ht", par)])
            for k in range(4):
                def fn(e, k=k, tt=tt, par=par):
                    return e.indirect_dma_start(out=gb[par][k][:, :], out_offset=None, in_=W["Y"][:, :],
                                                in_offset=bass.IndirectOffsetOnAxis(ap=idi[:, tt, k:k + 1], axis=0))
                o = Op("pool", fn)
                o.dma = True
                o.slot = gslot[par]
                o.slot_total = gslot[par].total + 16
                S._add(o, ["idi"], [("gb", par, k)])
                gslot[par].total += 16
            ac = acc[par]
            g_ = gb[par]
            S.op("dve", lambda e, ac=ac, g_=g_: e.tensor_tensor(out=ac[:], in0=g_[0][:], in1=g_[1][:], op=ALU.add),
                 reads=[("gb", par, 0), ("gb", par, 1)], writes=[("acc", par)])
            S.op("pool", lambda e, ac=ac, g_=g_: e.tensor_tensor(out=g_[2][:], in0=g_[2][:], in1=g_[3][:], op=ALU.add),
                 reads=[("gb", par, 2), ("gb", par, 3)], writes=[("gb", par, 2)])
            S.op("dve", lambda e, ac=ac, g_=g_: e.tensor_tensor(out=ac[:], in0=ac[:], in1=g_[2][:], op=ALU.add),
                 reads=[("gb", par, 2), ("acc", par)], writes=[("acc", par)])
            for ch in range(4):
                for hl in range(2):
                    S.op("pe", lambda en, tt=tt, hl=hl, ch=ch: en.matmul(
                        bk[ch][:, :], pT[:, tt, hl, :], bdb[:, ch * 512:(ch + 1) * 512], start=(hl == 0), stop=(hl == 1)),
                        reads=["pT", "bdb"], writes=[("bank", ch)])
                S.op("dve", lambda en, ac=ac, ch=ch: en.tensor_tensor(out=ac[:, ch * 512:(ch + 1) * 512], in0=bk[ch][:, :],
                                                                     in1=ac[:, ch * 512:(ch + 1) * 512], op=ALU.add),
                     reads=[("bank", ch), ("acc", par)], writes=[("acc", par)])
            S.op("pool", lambda en, ac=ac: en.tensor_tensor(out=ac[:], in0=ac[:], in1=g2B[:], op=ALU.mult),
                 reads=[("acc", par), "g2B"], writes=[("acc", par)])
            S.op("pool", lambda en, ac=ac, hb=hb: en.tensor_tensor(out=hb[:], in0=hb[:], in1=ac[:], op=ALU.add),
                 reads=[("acc", par), ("ht", par)], writes=[("ht", par)])
            o_ = ot[par]
            S.op("act", lambda en, o_=o_, hb=hb, tt=tt: en.activation(out=o_[:], in_=hb[:], func=AF.Square, accum_out=ss[:, tt:tt + 1]),
                 reads=[("ht", par)], writes=[("ot", par), ("ss", tt)])
            S.op("act", lambda en, tt=tt: en.activation(out=rt[:, tt:tt + 1], in_=ss[:, tt:tt + 1], func=AF.Sqrt, scale=1.0 / D,
                                                        bias=cx.epsc[:, 0:1]), reads=[("ss", tt), "epsc"], writes=[("rt", tt)])
            S.op("dve", lambda en, tt=tt: en.reciprocal(out=rs[:, tt:tt + 1], in_=rt[:, tt:tt + 1]), reads=[("rt", tt)], writes=[("rs", tt)])
            S.op("dve", lambda en, o_=o_, hb=hb, tt=tt: en.scalar_tensor_tensor(out=o_[:], in0=hb[:], scalar=rs[:, tt:tt + 1], in1=gfB[:],
                                                                                op0=ALU.mult, op1=ALU.mult),
                 reads=[("ht", par), ("rs", tt), "gfB", ("ot", par)], writes=[("ot", par)])
            S.dma("sp", cx.slot_out, cx.O["out"][tt * 128:(tt + 1) * 128, :], o_[:], reads=[("ot", par)])
    S.barrier()


def _colT(v, nchunk):
    return np.ascontiguousarray(v.reshape(nchunk, 128).T)


def prep_inputs(inp):
    f32 = np.float32
    x = np.asarray(inp["x"], f32)
    c = np.asarray(inp["c"], f32)
    shared = {
        "w_ada": np.ascontiguousarray(inp["w_ada"][0], f32),
        "b_ada": np.ascontiguousarray(inp["b_ada"][0][None, :], f32),
        "n1": np.ascontiguousarray(inp["norm1_g"][0][None, :], f32),
        "n2": np.ascontiguousarray(inp["norm2_g"][0][None, :], f32),
        "nf": np.ascontiguousarray(np.asarray(inp["final_g"])[None, :], f32),
        "w_in": np.ascontiguousarray(inp["w_in"][0], f32),
        "lamT": np.ascontiguousarray(np.stack([inp["lam_q1"][0], inp["lam_k1"][0], inp["lam_q2"][0], inp["lam_k2"][0]], axis=1), f32),
        "sublnT": _colT(np.asarray(inp["subln_g"][0], f32), 2),
        "w_out_a": np.ascontiguousarray(inp["w_out_a"][0], f32),
        "w_out_b": np.ascontiguousarray(inp["w_out_b"][0], f32),
        "w_o": np.ascontiguousarray(inp["w_o"][0], f32),
        "w_router": np.ascontiguousarray(inp["w_router"][0], f32),
        "b_routerB": np.ascontiguousarray(np.broadcast_to(np.asarray(inp["b_router"][0], f32)[None, :], (128, N_EXP))),
        "w_gate": np.ascontiguousarray(inp["w_gate"][0], f32),
        "w_up": np.ascontiguousarray(inp["w_up"][0], f32),
        "w_down": np.ascontiguousarray(inp["w_down"][0], f32),
        "b_gateT": np.ascontiguousarray(np.asarray(inp["b_gate"][0], f32).reshape(N_EXP, KC, 128).transpose(2, 0, 1).reshape(128, N_EXP * KC)),
        "b_upT": np.ascontiguousarray(np.asarray(inp["b_up"][0], f32).reshape(N_EXP, KC, 128).transpose(2, 0, 1).reshape(128, N_EXP * KC)),
        "b_down": np.ascontiguousarray(inp["b_down"][0], f32),
        "ident": np.eye(128, dtype=f32),
        "iota_row": np.ascontiguousarray(np.broadcast_to(np.arange(1, CAP + 1, dtype=f32)[None, :], (128, CAP))),
        "triu": np.triu(np.ones((128, 128), f32)),
        "tokab": np.ascontiguousarray(np.stack([(np.arange(8)[None, :] * 128 + np.arange(128)[:, None]) // 32,
                                                (np.arange(8)[None, :] * 128 + np.arange(128)[:, None]) % 32], axis=2).reshape(128, 16).astype(f32)),
        "ecapB": np.ascontiguousarray(np.broadcast_to((np.arange(N_EXP, dtype=f32) * CAP)[None, None, :], (128, 8, N_EXP)).reshape(128, 8 * N_EXP)),
    }
    p = np.arange(128)[:, None]
    xx = np.arange(3968)[None, :]
    dl = p + 1920 - xx
    ad = np.abs(dl)
    n_ = ((ad <= 64).astype(np.float64) + ((dl % 4 == 0) & (ad <= 256)) + ((dl % 16 == 0) & (ad <= 1024)))
    lnn = np.where(n_ > 0, np.log(np.maximum(n_, 1.0)), 0.0)
    shared["stripAh"] = np.ascontiguousarray(np.stack(
        [np.where(n_ > 0, -SLOPES_A[h] * ad + lnn, -BIG) for h in range(8)], axis=0).astype(f32))
    maps = []
    for core in range(8):
        b, r = core // 4, core % 4
        idx = (np.arange(SEQ) - 1024 + 1024 * r) % SEQ
        m = dict(shared)
        m["xr"] = np.ascontiguousarray(x[b][idx])
        m["cT"] = _colT(c[b], KC)
        dist = np.zeros((128, 4, 1920), f32)
        for g in range(4):
            s_g = ((1024 * g - 1024 + 1024 * r) % SEQ) - 1024 * g
            q_off = -1024 + 1024 * r
            xv = np.arange(1920)[None, :]
            dist[:, g, :] = np.abs(1024 * (1 - g) + (xv - 896) - p + q_off - s_g)
        m["distB"] = np.ascontiguousarray(dist.reshape(128, 4 * 1920))
        kb = np.zeros((128, 24), f32)
        for kt in range(24):
            tok = 128 * kt + np.arange(128) - 1024 + 1024 * r
            kb[:, kt] = np.where((tok >= 0) & (tok < SEQ), 0.0, -BIG)
        m["kbiasA"] = kb
        maps.append(m)
    return maps


_NC_CACHE = {}
_LAST_DECLARED = []


def kernel(**inputs):
    maps = prep_inputs(inputs)
    if "nc" not in _NC_CACHE:
        _NC_CACHE["nc"] = build_program()
    nc = _NC_CACHE["nc"]
    names = set(_LAST_DECLARED)
    maps = [{k: v for k, v in m.items() if k in names} for m in maps]
    res = run_bass_kernel_spmd(nc, maps, core_ids=list(range(8)))
    out = np.zeros((2, SEQ, D), np.float32)
    for core in range(8):
        b, r = core // 4, core % 4
        out[b, 1024 * r:1024 * (r + 1)] = np.asarray(res.results[core]["out"], np.float32)
    return out
```

```python
import contextlib
import math
import numpy as np
import concourse.bass as bass
import concourse.mybir as mybir
from concourse.bass_utils import run_bass_kernel_spmd

F32 = mybir.dt.float32
BF16 = mybir.dt.bfloat16
AF = mybir.ActivationFunctionType
ALU = mybir.AluOpType
AX = mybir.AxisListType

ENGS = ("pe", "act", "dve", "pool", "sp")

D = 2048
SEQ = 4096
OWN = 1024
KC = 16
N_EXP = 32
TOPK = 4
CAP = 512
EPS = 1e-5
SCALE = 128 ** -0.5
LAMBDA_INIT = 0.8 - 0.6 * math.exp(-0.3 * 0)
SLOPES = [2.0 ** (-8.0 * (i + 1) / 12) for i in range(12)]
SLOPES_A = SLOPES[:8]
SLOPES_B = SLOPES[8:]
BIG = 1.0e9
PATTERNS = (1, 4, 16)


class Op:
    __slots__ = ("eng", "fn", "deps", "dma", "idx", "count", "milestone", "slot", "slot_total", "name")

    def __init__(self, eng, fn, name=""):
        self.eng = eng
        self.fn = fn
        self.deps = []
        self.dma = False
        self.milestone = False
        self.count = None
        self.slot = None
        self.slot_total = None
        self.name = name


class Slot:
    def __init__(self, name):
        self.name = name
        self.total = 0
        self.sem = None


class Sched:
    def __init__(self, nc):
        self.nc = nc
        self.ops = []
        self.writers = {}
        self.readers = {}
        self.old_readers = {}
        self.slots = []
        self.nbar = 0

    def slot(self, name):
        s = Slot(name + str(len(self.slots)))
        self.slots.append(s)
        return s

    def _add(self, op, reads, writes):
        deps = {}

        def add_dep(o):
            if o is op:
                return
            if o.dma:
                if op.dma and op.slot is o.slot:
                    return
                deps[("slot", id(o.slot))] = ("slot", o.slot, o.slot.total)
            else:
                k = ("eng", o.eng)
                prev = deps.get(k)
                if prev is None or o.idx > prev[1].idx:
                    deps[k] = ("eng", o, None)

        for k in reads:
            for o in self.writers.get(k, {}).values():
                add_dep(o)
        for k in writes:
            for o in self.writers.get(k, {}).values():
                if (not o.dma) and (not op.dma) and o.eng == op.eng:
                    continue
                add_dep(o)
            for o in list(self.readers.get(k, {}).values()) + list(self.old_readers.get(k, {}).values()):
                if (not o.dma) and (not op.dma) and o.eng == op.eng:
                    continue
                add_dep(o)
        op.deps = list(deps.values())
        for d in op.deps:
            if d[0] == "eng":
                d[1].milestone = True
        op.idx = len(self.ops)
        self.ops.append(op)
        wkey = ("slot", id(op.slot)) if op.dma else op.eng
        for k in writes:
            self.writers[k] = {wkey: op}
            if self.readers.get(k):
                self.old_readers[k] = self.readers[k]
            self.readers[k] = {}
        for k in reads:
            self.readers.setdefault(k, {})[wkey] = op
        return op

    def op(self, eng, fn, reads=(), writes=(), name=""):
        o = Op(eng, fn, name)
        return self._add(o, list(reads), list(writes))

    def dma(self, eng, slot, out, in_, reads=(), writes=(), name="", **kw):
        def fn(e, out=out, in_=in_, kw=kw):
            return e.dma_start(out=out, in_=in_, **kw)
        o = Op(eng, fn, name)
        o.dma = True
        o.slot = slot
        o.slot_total = slot.total + 16
        r = self._add(o, list(reads), list(writes))
        slot.total += 16
        return r

    def barrier(self):
        n = self.nbar
        self.nbar += 1
        sc = self.bar_scratch
        comp = ("pe", "act", "dve", "pool")
        for e in comp:
            if e == "pe":
                self.op("pe", lambda en: en.matmul(self.bar_ps[0:1, 0:1], sc["b"][0:1, 0:1], sc["b"][0:1, 0:1],
                                                    start=True, stop=True),
                        writes=[("bar", n, e), ("bank", 7)])
            elif e == "act":
                self.op("act", lambda en: en.activation(out=sc["act"][0:1, 0:1], in_=sc["one"][0:1, 0:1], func=AF.Copy),
                        writes=[("bar", n, e)])
            elif e == "dve":
                self.op("dve", lambda en: en.tensor_copy(out=sc["dve"][0:1, 0:1], in_=sc["one"][0:1, 0:1]),
                        writes=[("bar", n, e)])
            else:
                self.op("pool", lambda en: en.tensor_copy(out=sc["pool"][0:1, 0:1], in_=sc["one"][0:1, 0:1]),
                        writes=[("bar", n, e)])
        rk = [("bar", n, e) for e in comp]
        for e in ENGS:
            o = Op(e, None, "barwait")
            self._add(o, rk, [])
            for s in self.slots:
                if s.total > 0:
                    o.deps.append(("slot", s, s.total))
        self.writers = {}
        self.readers = {}
        self.old_readers = {}

    def emit(self, final_waits=()):
        nc = self.nc
        by_eng = {e: [] for e in ENGS}
        for o in self.ops:
            by_eng[o.eng].append(o)
        for e in ENGS:
            c = 0
            for o in by_eng[e]:
                if o.milestone and not o.dma:
                    c += 1
                    o.count = c
        with contextlib.ExitStack() as st:
            esem = {e: st.enter_context(nc.semaphore("s_" + e)) for e in ENGS}
            for s in self.slots:
                s.sem = st.enter_context(nc.semaphore("d_" + s.name))
            block = st.enter_context(nc.Block())

            def run(e, eng):
                waited = {}
                for o in by_eng[e]:
                    for d in o.deps:
                        if d[0] == "slot":
                            sem, val, key = d[1].sem, d[2], ("slot", id(d[1]))
                        else:
                            dop = d[1]
                            if dop.eng == e and e == "pe":
                                continue
                            sem, val, key = esem[dop.eng], dop.count, ("eng", dop.eng)
                        if waited.get(key, 0) >= val:
                            continue
                        waited[key] = val
                        eng.wait_ge(sem, val)
                    if o.fn is None:
                        continue
                    ins = o.fn(eng)
                    if o.dma:
                        ins.then_inc(o.slot.sem, 16)
                    elif o.milestone:
                        ins.then_inc(esem[e], 1)
                if e == "sp":
                    for s in final_waits:
                        eng.wait_ge(s.sem, s.total)

            @block.tensor
            def _(eng):
                run("pe", eng)

            @block.scalar
            def _(eng):
                run("act", eng)

            @block.vector
            def _(eng):
                run("dve", eng)

            @block.gpsimd
            def _(eng):
                run("pool", eng)

            @block.sync
            def _(eng):
                run("sp", eng)


class Ctx:
    pass


def a_tiles():
    out = []
    for d in PATTERNS:
        nt = {1: 9, 4: 3, 16: 2}[d]
        for c in range(d):
            for m in range(nt):
                out.append((d, c, m))
    return out


A_TILES = a_tiles()
A_TILE_IDX = {t: i for i, t in enumerate(A_TILES)}


def build_program(debug=(), stages="ABCDE"):
    nc = bass.Bass("TRN2", target_bir_lowering=False)
    cx = Ctx()
    cx.stages = stages
    cx.nc = nc
    cx.debug = set(debug)
    S = Sched(nc)
    cx.S = S

    def din(name, shape, dt=F32):
        return nc.dram_tensor(name, list(shape), dt, kind="ExternalInput").ap()

    def dscr(name, shape, dt=BF16):
        return nc.dram_tensor(name, list(shape), dt, kind="Internal").ap()

    def dout(name, shape, dt=F32):
        return nc.dram_tensor(name, list(shape), dt, kind="ExternalOutput").ap()

    class Lazy(dict):
        def __init__(self):
            super().__init__()
            self.specs = {}

        def __setitem__(self, k, v):
            self.specs[k] = v

        def __missing__(self, k):
            ap = self.specs[k]()
            dict.__setitem__(self, k, ap)
            return ap
    _din = din
    din = lambda name, shape, dt=F32: (lambda: _din(name, shape, dt))
    I = Lazy()
    I["xr"] = din("xr", [SEQ, D])
    I["cT"] = din("cT", [128, KC])
    I["w_ada"] = din("w_ada", [D, 6 * D])
    I["b_ada"] = din("b_ada", [1, 6 * D])
    I["n1"] = din("n1", [1, D])
    I["n2"] = din("n2", [1, D])
    I["nf"] = din("nf", [1, D])
    I["w_in"] = din("w_in", [D, 10240])
    I["lamT"] = din("lamT", [128, 4])
    I["sublnT"] = din("sublnT", [128, 2])
    I["w_out_a"] = din("w_out_a", [1024, D])
    I["w_out_b"] = din("w_out_b", [1024, D])
    I["w_o"] = din("w_o", [D, D])
    I["w_router"] = din("w_router", [D, N_EXP])
    I["b_routerB"] = din("b_routerB", [128, N_EXP])
    I["w_gate"] = din("w_gate", [N_EXP, D, D])
    I["w_up"] = din("w_up", [N_EXP, D, D])
    I["w_down"] = din("w_down", [N_EXP, D, D])
    I["b_gateT"] = din("b_gateT", [128, N_EXP * KC])
    I["b_upT"] = din("b_upT", [128, N_EXP * KC])
    I["b_down"] = din("b_down", [N_EXP, D])
    I["ident"] = din("ident", [128, 128])
    I["distB"] = din("distB", [128, 4 * 1920])
    I["stripAh"] = din("stripAh", [8, 128, 3968])
    I["kbiasA"] = din("kbiasA", [128, 24])
    I["iota_row"] = din("iota_row", [128, CAP])
    I["triu"] = din("triu", [128, 128])
    I["ecapB"] = din("ecapB", [128, 8 * N_EXP])
    I["tokab"] = din("tokab", [128, 16])
    cx.I = I
    O = {}
    O["out"] = dout("out", [OWN, D])
    for name, shape, dt in DEBUG_SHAPES:
        if name in cx.debug:
            O[name] = dout(name, shape, dt)
    cx.O = O
    W = {}
    W["QaT"] = dscr("QaT", [1024, OWN])
    W["KaT"] = dscr("KaT", [1024, 3072])
    W["Va"] = dscr("Va", [SEQ, 1024])
    W["QbT"] = dscr("QbT", [1024, OWN])
    W["KbT"] = dscr("KbT", [1024, SEQ])
    W["Vb"] = dscr("Vb", [SEQ, 1024])
    W["GT"] = dscr("GT", [4096, OWN])
    W["rows"] = dscr("rows", [4, D], F32)
    W["YaT"] = dscr("YaT", [1024, OWN])
    W["YbT"] = dscr("YbT", [1024, OWN])
    W["U2"] = dscr("U2", [OWN, D])
    W["MT"] = dscr("MT", [D, OWN])
    W["hs"] = dscr("hs", [OWN, D], F32)
    W["Y"] = dscr("Y", [N_EXP * CAP + 128, D])
    cx.W = W

    with contextlib.ExitStack() as st:
        cx.st = st
        sb = lambda name, shape, dt: st.enter_context(nc.sbuf_tensor(name, list(shape), dt))
        cx.banks = [st.enter_context(nc.psum_tensor(f"bank{i}", [128, 512], F32)) for i in range(8)]
        cx.ident_f = sb("ident_f", [128, 128], F32)
        cx.ident_b = sb("ident_b", [128, 128], BF16)
        cx.ones_f = sb("ones_f", [128, 128], F32)
        cx.ones_b = sb("ones_b", [128, 128], BF16)
        cx.colT = sb("colT", [128, 32], F32)
        cx.neglam = sb("neglam", [128, 1], F32)
        cx.sg = sb("sg", [128, 2], F32)
        cx.barsc = sb("barsc", [128, 8], F32)
        cx.barb = sb("barb", [128, 2], BF16)
        cx.epsc = sb("epsc", [128, 2], F32)
        S.bar_scratch = {"b": cx.barb, "one": cx.barsc[:, 0:1], "act": cx.barsc[:, 1:2],
                         "dve": cx.barsc[:, 2:3], "pool": cx.barsc[:, 3:4]}
        S.bar_ps = cx.banks[7]
        cx.slot_const = S.slot("const")
        cx.slot_out = S.slot("out")
        cx.dbg_slot = S.slot("dbg")

        cx.posm = sb("posm", [128, 8, N_EXP], F32)
        cx.prob = sb("prob", [128, 8, N_EXP], F32)
        cx.prob_hi = sb("prob_hi", [128, 8, N_EXP], BF16)
        cx.prob_lo = sb("prob_lo", [128, 8, N_EXP], BF16)
        cx.iota = sb("iota", [128, CAP], F32)
        S.dma("sp", cx.slot_const, cx.iota[:], I["iota_row"][:, :], writes=["iota"])
        stages = cx.stages
        phase0(cx)
        if "A" in stages:
            phaseA(cx)
            S.barrier()
        if "B" in stages:
            attention(cx, "B")
        if "C" in stages:
            attention(cx, "A")
        if "D" in stages:
            phaseD(cx)
        if "E" in stages:
            phaseE1(cx)
            phaseE2(cx)
        S.emit(final_waits=[cx.slot_out, cx.dbg_slot])
    global _LAST_DECLARED
    _LAST_DECLARED = list(dict.keys(I))
    return nc


DEBUG_SHAPES = [
    ("d_yA", [1024, OWN], BF16),
    ("d_mT", [D, OWN], BF16),
    ("d_ya", [1024, OWN], BF16),
    ("d_yb", [1024, OWN], BF16),
    ("d_gat", [128, 1024], BF16),
    ("d_a1", [128, 1024], F32),
    ("d_rowB", [128, D], F32),
    ("d_yB", [1024, OWN], BF16),
    ("d_h1", [OWN, D], F32),
    ("d_lg", [128, 8 * N_EXP], F32),
    ("d_posm", [128, 8 * N_EXP], F32),
    ("d_prob", [128, 8 * N_EXP], F32),
    ("d_colT", [128, 32], F32),
    ("d_mod", [1, 6 * D], F32),
    ("d_rows", [4, D], F32),
    ("d_misc", [128, 4], F32),
    ("d_uT", [128, KC * 512], BF16),
]


def phase0(cx):
    nc, S, I = cx.nc, cx.S, cx.I
    with contextlib.ExitStack() as st:
        sb = lambda name, shape, dt: st.enter_context(nc.sbuf_tensor(name, list(shape), dt))
        cT = sb("p0_cT", [128, KC], F32)
        sc = sb("p0_sc", [128, KC], F32)
        brow = sb("p0_brow", [1, 6 * D], F32)
        modrow = sb("p0_modrow", [1, 6 * D], F32)
        nrow = sb("p0_nrow", [1, 2 * D], F32)
        grow = sb("p0_grow", [1, 2 * D], F32)
        lamT = sb("p0_lamT", [128, 4], F32)
        sublnT = sb("p0_subln", [128, 2], F32)
        prod = sb("p0_prod", [128, 2], F32)
        ex = sb("p0_ex", [128, 2], F32)
        pan = [sb(f"p0_pan{i}", [128, KC, 512], F32) for i in range(2)]
        pslot = [S.slot("pan") for _ in range(2)]
        sc_slot = cx.slot_const
        bk = cx.banks

        S.dma("sp", sc_slot, cx.ident_f[:], I["ident"][:, :], writes=["ident_f"])
        S.dma("sp", sc_slot, cT[:], I["cT"][:, :], writes=["cT"])
        S.dma("sp", sc_slot, brow[:], I["b_ada"][:, :], writes=["brow"])
        S.dma("sp", sc_slot, nrow[0:1, 0:D], I["n1"][:, :], writes=["nrow"])
        S.dma("sp", sc_slot, nrow[0:1, D:2 * D], I["n2"][:, :], writes=["nrow"])
        S.dma("sp", sc_slot, lamT[:], I["lamT"][:, :], writes=["lamT"])
        S.dma("sp", sc_slot, sublnT[:], I["sublnT"][:, :], writes=["sublnT"])

        S.op("dve", lambda e: e.memset(cx.ones_f[:], 1.0), writes=["ones_f"])
        S.op("dve", lambda e: e.memset(cx.ones_b[:], 1.0), writes=["ones_b"])
        S.op("dve", lambda e: e.memset(cx.barsc[:], 1.0), writes=["barsc"])
        S.op("dve", lambda e: e.memset(cx.barb[:], 1.0), writes=["barb"])
        S.op("dve", lambda e: e.memset(cx.epsc[:], EPS), writes=["epsc"])
        S.op("dve", lambda e: e.tensor_copy(out=cx.ident_b[:], in_=cx.ident_f[:]), reads=["ident_f"], writes=["ident_b"])
        S.op("act", lambda e: e.activation(out=sc[:], in_=cT[:], func=AF.Silu), reads=["cT"], writes=["sc"])

        wv = I["w_ada"].rearrange("(k p) n -> p k n", p=128)
        NP = 6 * D // 512
        for pn in range(NP):
            pb = pan[pn % 2]
            for j in range(4):
                q = "sp" if j % 2 == 0 else "act"
                S.dma(q, pslot[pn % 2], pb[:, 4 * j:4 * j + 4, :], wv[:, 4 * j:4 * j + 4, pn * 512:(pn + 1) * 512],
                      writes=[("pan", pn % 2)])
            ps = bk[pn % 2]
            for k in range(KC):
                S.op("pe", lambda e, ps=ps, pb=pb, k=k: e.matmul(ps[0:1, :], sc[:, k:k + 1], pb[:, k, :],
                                                                 start=(k == 0), stop=(k == KC - 1)),
                     reads=["sc", ("pan", pn % 2)], writes=[("bank", pn % 2)])
            S.op("dve", lambda e, ps=ps, pn=pn: e.tensor_tensor(out=modrow[0:1, pn * 512:(pn + 1) * 512], in0=ps[0:1, :],
                                                                 in1=brow[0:1, pn * 512:(pn + 1) * 512], op=ALU.add),
                 reads=[("bank", pn % 2), "brow"], writes=["modrow"])
        mr = lambda m: modrow[0:1, m * D:(m + 1) * D]
        S.op("dve", lambda e: e.scalar_tensor_tensor(out=grow[0:1, 0:D], in0=mr(1), scalar=1.0, in1=nrow[0:1, 0:D],
                                                     op0=ALU.add, op1=ALU.mult),
             reads=["modrow", "nrow"], writes=["grow"])
        S.op("dve", lambda e: e.scalar_tensor_tensor(out=grow[0:1, D:2 * D], in0=mr(4), scalar=1.0, in1=nrow[0:1, D:2 * D],
                                                     op0=ALU.add, op1=ALU.mult),
             reads=["modrow", "nrow"], writes=["grow"])
        psc = bk[2]
        for k in range(KC):
            S.op("pe", lambda e, k=k: e.matmul(psc[:, k:k + 1], grow[0:1, k * 128:(k + 1) * 128], cx.ones_f[0:1, 0:1],
                                               start=True, stop=True),
                 reads=["grow", "ones_f"], writes=[("bank", 2)])
        for k in range(KC):
            S.op("pe", lambda e, k=k: e.matmul(psc[:, KC + k:KC + k + 1], modrow[0:1, k * 128:(k + 1) * 128],
                                               cx.ones_f[0:1, 0:1], start=True, stop=True),
                 reads=["modrow", "ones_f"], writes=[("bank", 2)])
        S.op("dve", lambda e: e.tensor_copy(out=cx.colT[:], in_=psc[:, 0:32]), reads=[("bank", 2)], writes=["colT"])
        rs = cx.slot_const
        S.dma("sp", rs, cx.W["rows"][0:1, :], mr(2), reads=["modrow"], writes=["rows_d"])
        S.dma("sp", rs, cx.W["rows"][1:2, :], grow[0:1, D:2 * D], reads=["grow"], writes=["rows_d"])
        S.dma("sp", rs, cx.W["rows"][2:3, :], mr(3), reads=["modrow"], writes=["rows_d"])
        S.dma("sp", rs, cx.W["rows"][3:4, :], mr(5), reads=["modrow"], writes=["rows_d"])
        S.op("dve", lambda e: e.tensor_tensor(out=prod[:, 0:1], in0=lamT[:, 0:1], in1=lamT[:, 1:2], op=ALU.mult),
             reads=["lamT"], writes=["prod"])
        S.op("dve", lambda e: e.tensor_tensor(out=prod[:, 1:2], in0=lamT[:, 2:3], in1=lamT[:, 3:4], op=ALU.mult),
             reads=["lamT", "prod"], writes=["prod"])
        S.op("pe", lambda e: e.matmul(bk[3][:, 0:2], cx.ones_f[:], prod[:], start=True, stop=True),
             reads=["ones_f", "prod"], writes=[("bank", 3)])
        S.op("act", lambda e: e.activation(out=ex[:], in_=bk[3][:, 0:2], func=AF.Exp), reads=[("bank", 3)], writes=["ex"])
        S.op("dve", lambda e: e.tensor_tensor(out=cx.neglam[:], in0=ex[:, 1:2], in1=ex[:, 0:1], op=ALU.subtract),
             reads=["ex"], writes=["neglam"])
        S.op("dve", lambda e: e.tensor_scalar(out=cx.neglam[:], in0=cx.neglam[:], scalar1=-LAMBDA_INIT, scalar2=None,
                                              op0=ALU.add),
             reads=["neglam"], writes=["neglam"])
        S.op("dve", lambda e: e.tensor_scalar(out=cx.sg[:], in0=sublnT[:], scalar1=(1.0 - LAMBDA_INIT), scalar2=None,
                                              op0=ALU.mult),
             reads=["sublnT"], writes=["sg"])
        if "d_mod" in cx.debug:
            S.dma("sp", cx.dbg_slot, cx.O["d_mod"][:, :], modrow[:], reads=["modrow"])
        if "d_colT" in cx.debug:
            S.dma("sp", cx.dbg_slot, cx.O["d_colT"][:, :], cx.colT[:], reads=["colT"])
            S.dma("sp", cx.dbg_slot, cx.O["d_misc"][:, 0:1], cx.neglam[:], reads=["neglam"], allow_slow_non_contiguous=True)
            S.dma("sp", cx.dbg_slot, cx.O["d_misc"][:, 1:3], cx.sg[:], reads=["sg"], allow_slow_non_contiguous=True)
        S.barrier()


def evac_copy(S, i, out, in_, reads, writes, func=None):
    if func is not None or i % 2 == 0:
        f = func if func is not None else AF.Copy
        return S.op("act", lambda e: e.activation(out=out, in_=in_, func=f), reads=reads, writes=writes)
    return S.op("dve", lambda e: e.tensor_copy(out=out, in_=in_), reads=reads, writes=writes)


def phaseA(cx):
    nc, S, I, W = cx.nc, cx.S, cx.I, cx.W
    bk = cx.banks
    with contextlib.ExitStack() as st:
        sb = lambda name, shape, dt: st.enter_context(nc.sbuf_tensor(name, list(shape), dt))
        uT = sb("A_uT", [128, KC, SEQ], BF16)
        with contextlib.ExitStack() as st1:
            sb1 = lambda name, shape, dt: st1.enter_context(nc.sbuf_tensor(name, list(shape), dt))
            xt = [sb1(f"A_x{i}", [128, D], F32) for i in range(3)]
            xn = [sb1(f"A_xn{i}", [128, D], BF16) for i in range(2)]
            junk = sb1("A_junk", [128, D], BF16)
            ss = sb1("A_ss", [128, 4], F32)
            rt = sb1("A_rt", [128, 4], F32)
            rs = sb1("A_rs", [128, 4], F32)
            xslot = [S.slot("x") for _ in range(3)]
            xv = I["xr"].rearrange("(t p) d -> t p d", p=128)
            NTT = SEQ // 128
            def stage_a(tt):
                xb = xt[tt % 3]
                S.dma("sp", xslot[tt % 3], xb[:, 0:1024], xv[tt, :, 0:1024], writes=[("x", tt % 3)])
                S.dma("sp", xslot[tt % 3], xb[:, 1024:2048], xv[tt, :, 1024:2048], writes=[("x", tt % 3)])
                j = tt % 4
                S.op("act", lambda e, xb=xb, j=j: e.activation(out=junk[:], in_=xb[:], func=AF.Square,
                                                              accum_out=ss[:, j:j + 1]),
                     reads=[("x", tt % 3)], writes=["junk", ("ss", j)])
                S.op("act", lambda e, j=j: e.activation(out=rt[:, j:j + 1], in_=ss[:, j:j + 1], func=AF.Sqrt,
                                                        scale=1.0 / D, bias=cx.epsc[:, 0:1]),
                     reads=[("ss", j)], writes=[("rt", j)])
                S.op("dve", lambda e, j=j: e.reciprocal(out=rs[:, j:j + 1], in_=rt[:, j:j + 1]),
                     reads=[("rt", j)], writes=[("rs", j)])

            def stage_b(tt):
                xb = xt[tt % 3]
                j = tt % 4
                xnb = xn[tt % 2]
                S.op("act", lambda e, xb=xb, xnb=xnb, j=j: e.activation(out=xnb[:], in_=xb[:], func=AF.Copy,
                                                                       scale=rs[:, j:j + 1]),
                     reads=[("x", tt % 3), ("rs", j)], writes=[("xn", tt % 2)])
                for half in range(2):
                    bi = (2 * tt + half) % 4
                    pst = bk[bi][:].bitcast(BF16)
                    for q in range(8):
                        k = half * 8 + q
                        S.op("pe", lambda e, pst=pst, q=q, k=k, xnb=xnb: e.transpose(
                            pst[:, q * 128:(q + 1) * 128], xnb[:, k * 128:(k + 1) * 128], cx.ident_b[:]),
                            reads=[("xn", tt % 2), "ident_b"], writes=[("bank", bi)])
                    for q in range(8):
                        k = half * 8 + q
                        S.op("dve", lambda e, pst=pst, q=q, k=k, tt=tt: e.tensor_scalar(
                            out=uT[:, k, tt * 128:(tt + 1) * 128], in0=pst[:, q * 128:(q + 1) * 128],
                            scalar1=cx.colT[:, k:k + 1], scalar2=cx.colT[:, KC + k:KC + k + 1],
                            op0=ALU.mult, op1=ALU.add),
                            reads=[("bank", bi), "colT"], writes=[("uT", tt // 4)])

            for tt in range(NTT):
                stage_a(tt)
                if tt >= 1:
                    stage_b(tt - 1)
            stage_b(NTT - 1)
        if "d_uT" in cx.debug:
            for k in range(KC):
                S.dma("sp", cx.dbg_slot, cx.O["d_uT"][:, k * 512:(k + 1) * 512], uT[:, k, 1024:1536],
                      reads=[("uT", c) for c in range(8)])
        S.barrier()
        wch = [sb(f"A_w{i}", [128, KC, 512], BF16) for i in range(2)]
        stg = [sb(f"A_stg{i}", [128, 512], BF16) for i in range(4)]
        wslot = [S.slot("w") for _ in range(2)]
        sslot = [S.slot("stg") for _ in range(4)]
        wv = I["w_in"].rearrange("(k p) n -> p k n", p=128)
        groups = []
        for g in range(2):
            groups.append(("qa", "f", [2, 3], W["QaT"], g, 1024))
        for g in range(2):
            groups.append(("ka", "f", list(range(6)), W["KaT"], g, 0))
        for g in range(2):
            groups.append(("va", "t", list(range(6)), W["Va"], g, 0))
        for g in range(2):
            groups.append(("qb", "f", [2, 3], W["QbT"], g, 1024))
        for g in range(2):
            groups.append(("kb", "f", list(range(8)), W["KbT"], g, 0))
        for g in range(2):
            groups.append(("vb", "t", list(range(8)), W["Vb"], g, 0))
        for g in range(8):
            groups.append(("gt", "f", [2, 3], W["GT"], g, 1024))
        ev = 0
        for cg, (name, kind, tcs, dst, g, tok0) in enumerate(groups):
            wb = wch[cg % 2]
            for j in range(4):
                S.dma("pool", wslot[cg % 2], wb[:, 4 * j:4 * j + 4, :], wv[:, 4 * j:4 * j + 4, cg * 512:(cg + 1) * 512],
                      writes=[("w", cg % 2)])
            func = AF.Sigmoid if name == "gt" else None
            if kind == "f":
                for s4 in range(4):
                    for tc in tcs:
                        bi = ev % 4
                        ps = bk[bi]
                        for k in range(KC):
                            S.op("pe", lambda e, ps=ps, wb=wb, k=k, s4=s4, tc=tc: e.matmul(
                                ps[:, :], wb[:, k, s4 * 128:(s4 + 1) * 128], uT[:, k, tc * 512:(tc + 1) * 512],
                                start=(k == 0), stop=(k == KC - 1)),
                                reads=[("w", cg % 2), ("uT", tc)], writes=[("bank", bi)])
                        sg = stg[ev % 4]
                        evac_copy(S, ev, sg[:], ps[:, :], [("bank", bi)], [("stg", ev % 4)], func=func)
                        row0 = g * 512 + s4 * 128
                        col0 = tc * 512 - tok0
                        S.dma("sp", sslot[ev % 4], dst[row0:row0 + 128, col0:col0 + 512], sg[:],
                              reads=[("stg", ev % 4)])
                        ev += 1
            else:
                for tc in tcs:
                    for t4 in range(4):
                        tt = tc * 4 + t4
                        bi = ev % 4
                        ps = bk[bi]
                        for k in range(KC):
                            S.op("pe", lambda e, ps=ps, wb=wb, k=k, tt=tt: e.matmul(
                                ps[:, :], uT[:, k, tt * 128:(tt + 1) * 128], wb[:, k, :],
                                start=(k == 0), stop=(k == KC - 1)),
                                reads=[("w", cg % 2), ("uT", tc)], writes=[("bank", bi)])
                        sg = stg[ev % 4]
                        evac_copy(S, ev, sg[:], ps[:, :], [("bank", bi)], [("stg", ev % 4)])
                        S.dma("sp", sslot[ev % 4], dst[tt * 128:(tt + 1) * 128, g * 512:(g + 1) * 512], sg[:],
                              reads=[("stg", ev % 4)])
                        ev += 1


def attention(cx, mixer):
    nc, S, I, W = cx.nc, cx.S, cx.I, cx.W
    bk = cx.banks
    isB = mixer == "B"
    nheads = 4 if isB else 8
    nkt = 32 if isB else 24
    ncomp = 2 if isB else 1
    nv = 2 if isB else 1
    vw = 1024
    with contextlib.ExitStack() as st:
        sb = lambda name, shape, dt: st.enter_context(nc.sbuf_tensor(name, list(shape), dt))
        if isB:
            vbuf = [sb(f"B_v{i}", [128, nkt, 256], BF16) for i in range(2)]
        else:
            v_all = sb("A_v", [128, nkt, vw], BF16)
        yT = sb(mixer + "_yT", [128, 8, OWN], BF16)
        ktb = [sb(f"{mixer}_kt{i}", [128, ncomp, nkt * 128], BF16) for i in range(2)]
        qtb = [sb(f"{mixer}_qt{i}", [128, ncomp, OWN], BF16) for i in range(2)]
        if isB:
            strip = sb("B_strip", [128, 4 * 1920], F32)
            stripb = [strip, strip]
        else:
            stripb = [sb(f"A_strip{i}", [128, 3968], F32) for i in range(2)]
            kbias = sb("A_kbias", [128, 24], F32)
        NS = 5 if isB else 6
        LA = 3
        NB = 6
        tmpb = [sb(f"{mixer}_tmp{i}", [128, 512], F32) for i in range(NB)]
        eb = [sb(f"{mixer}_e{i}", [128, 512], BF16) for i in range(NB)]
        rcp = [sb(f"{mixer}_rcp{i}", [128, 512], F32) for i in range(2)]
        if isB:
            onb = [sb(f"B_on{i}", [128, 2, 2, 512], F32) for i in range(2)]
            diff = sb("B_diff", [128, 2, 512], F32)
            sq = sb("B_sq", [128, 2, 512], BF16)
            rt = sb("B_rt", [128, 512], F32)
            rr = sb("B_rr", [128, 512], F32)
        vslot = S.slot("v")
        hslot = [S.slot("hd") for _ in range(2)]
        vsrc = (W["Vb"] if isB else W["Va"])[0:nkt * 128, :].rearrange("(t p) c -> p t c", p=128)
        if not isB:
            for j in range(0, nkt, 4):
                S.dma("sp" if (j // 4) % 2 == 0 else "act", vslot, v_all[:, j:j + 4, :], vsrc[:, j:j + 4, :], writes=["v_all"])
        if isB:
            for j in range(4):
                S.dma("sp", vslot, strip[:, j * 1920:(j + 1) * 1920], I["distB"][:, j * 1920:(j + 1) * 1920], writes=["strip"])
        else:
            S.dma("sp", vslot, kbias[:], I["kbiasA"][:, :], writes=["kbias"])
        KT = W["KbT"] if isB else W["KaT"]
        QT = W["QbT"] if isB else W["QaT"]
        it = 0
        accset = 0
        for h in range(nheads):
            par = h % 2
            for c in range(ncomp):
                row = (h * ncomp + c) * 128
                for j in range(0, nkt * 128, 1024):
                    S.dma("sp" if c == 0 else "act", hslot[par], ktb[par][:, c, j:j + 1024], KT[row:row + 128, j:j + 1024],
                          writes=[("hd", par)])
                S.dma("act", hslot[par], qtb[par][:, c, :], QT[row:row + 128, :], writes=[("hd", par)])
            if not isB:
                S.dma("sp", hslot[par], stripb[par][:], I["stripAh"][h], writes=[("hd", par)])
            else:
                for j in range(0, nkt, 8):
                    S.dma("sp", hslot[par], vbuf[par][:, j:j + 8, :], vsrc[:, j:j + 8, h * 256:(h + 1) * 256],
                          writes=[("hd", par)])
            hk = [("hd", par), "v_all", "strip", "kbias"]
            slope = SLOPES_B[h] if isB else SLOPES_A[h]
            for qc in range(2):
                for c in range(ncomp):
                    abase = NS
                    O = [bk[abase + v] for v in range(nv)]
                    Dn = bk[abase + nv]
                    akeys = [("bank", abase + v) for v in range(nv)] + [("bank", abase + nv)]
                    if isB:
                        kts = list(range(nkt))
                    else:
                        kts = [kt for kt in range(nkt)
                               if not (128 * kt - 1024 - 512 * qc - 511 > 1024 or 128 * kt + 127 - 1024 - 512 * qc < -1024)]
                    pend = []

                    def emit_pv(first, last, kt, ebb, ei, O=O, Dn=Dn, akeys=akeys, par=par, h=h):
                        for v in range(nv):
                            col = (v * 128) if isB else (h * 128)
                            vt = vbuf[par] if isB else v_all
                            S.op("pe", lambda e, v=v, kt=kt, col=col, ebb=ebb, first=first, last=last, vt=vt: e.matmul(
                                O[v][:, :], vt[:, kt, col:col + 128], ebb[:], start=first, stop=last),
                                reads=[("e", ei), ("hd", par), "v_all"], writes=[akeys[v]])
                        S.op("pe", lambda e, ebb=ebb, first=first, last=last: e.matmul(
                            Dn[:, :], cx.ones_b[:], ebb[:], start=first, stop=last),
                            reads=[("e", ei), "ones_b"], writes=[akeys[-1]])

                    for ki, kt in enumerate(kts):
                        sbk = it % NS
                        ps = bk[sbk]
                        S.op("pe", lambda e, ps=ps, par=par, c=c, kt=kt, qc=qc: e.matmul(
                            ps[:, :], ktb[par][:, c, kt * 128:(kt + 1) * 128], qtb[par][:, c, qc * 512:(qc + 1) * 512],
                            start=True, stop=True), reads=hk, writes=[("bank", sbk)])
                        tb = tmpb[it % NB]
                        if isB:
                            g, ktp = kt // 8, kt % 8
                            x0 = g * 1920 + 512 * qc - 128 * ktp + 896
                            c1 = -SCALE / slope
                        else:
                            x0 = 512 * qc - 128 * kt + 2944
                            c1 = SCALE
                        sp_ = stripb[par]
                        S.op("dve", lambda e, tb=tb, ps=ps, sp_=sp_, x0=x0, c1=c1: e.scalar_tensor_tensor(
                            out=tb[:], in0=ps[:, :], scalar=c1, in1=sp_[:, x0:x0 + 512], op0=ALU.mult, op1=ALU.add),
                            reads=[("bank", sbk)] + hk, writes=[("tmp", it % NB)])
                        ebb = eb[it % NB]
                        if isB:
                            S.op("act", lambda e, ebb=ebb, tb=tb, slope=slope: e.activation(
                                out=ebb[:], in_=tb[:], func=AF.Exp, scale=-slope),
                                reads=[("tmp", it % NB)], writes=[("e", it % NB)])
                        else:
                            S.op("act", lambda e, ebb=ebb, tb=tb, kt=kt: e.activation(
                                out=ebb[:], in_=tb[:], func=AF.Exp, bias=kbias[:, kt:kt + 1]),
                                reads=[("tmp", it % NB)] + hk, writes=[("e", it % NB)])
                        first, last = (ki == 0), (ki == len(kts) - 1)
                        pend.append((first, last, kt, ebb, it % NB))
                        it += 1
                        if len(pend) > LA:
                            emit_pv(*pend.pop(0))
                    while pend:
                        emit_pv(*pend.pop(0))
                    rc = rcp[accset]
                    S.op("dve", lambda e, rc=rc, Dn=Dn: e.reciprocal(out=rc[:], in_=Dn[:, :]),
                         reads=[akeys[-1]], writes=[("rcp", accset)])
                    for v in range(nv):
                        if isB:
                            dst = onb[qc % 2][:, c, v, :]
                            wk = ("on", qc % 2, c)
                        else:
                            dst = yT[:, h, qc * 512:(qc + 1) * 512]
                            wk = ("yT", mixer)
                        S.op("dve", lambda e, dst=dst, O=O, v=v, rc=rc: e.tensor_tensor(
                            out=dst, in0=O[v][:, :], in1=rc[:], op=ALU.mult),
                            reads=[akeys[v], ("rcp", accset)], writes=[wk])
                    accset ^= 1
                if isB:
                    on = onb[qc % 2]
                    ok = [("on", qc % 2, 0), ("on", qc % 2, 1)]
                    for v in range(2):
                        S.op("dve", lambda e, on=on, v=v: e.scalar_tensor_tensor(
                            out=diff[:, v, :], in0=on[:, 1, v, :], scalar=cx.neglam[:, 0:1], in1=on[:, 0, v, :],
                            op0=ALU.mult, op1=ALU.add), reads=ok + ["neglam"], writes=["diff"])
                    S.op("act", lambda e: e.activation(out=sq[:], in_=diff[:], func=AF.Square), reads=["diff"], writes=["sq"])
                    sbk = it % NS
                    it += 1
                    ps = bk[sbk]
                    for v in range(2):
                        S.op("pe", lambda e, ps=ps, v=v: e.matmul(ps[:, :], cx.ones_b[:], sq[:, v, :], start=(v == 0), stop=(v == 1)),
                             reads=["sq", "ones_b"], writes=[("bank", sbk)])
                    S.op("act", lambda e, ps=ps: e.activation(out=rt[:], in_=ps[:, :], func=AF.Sqrt, scale=1.0 / 256,
                                                             bias=cx.epsc[:, 0:1]),
                         reads=[("bank", sbk), "epsc"], writes=["rt"])
                    S.op("dve", lambda e: e.reciprocal(out=rr[:], in_=rt[:]), reads=["rt"], writes=["rr"])
                    for v in range(2):
                        S.op("dve", lambda e, v=v, h=h, qc=qc: e.scalar_tensor_tensor(
                            out=yT[:, 2 * h + v, qc * 512:(qc + 1) * 512], in0=diff[:, v, :], scalar=cx.sg[:, v:v + 1],
                            in1=rr[:], op0=ALU.mult, op1=ALU.mult), reads=["diff", "rr", "sg"], writes=[("yT", mixer)])
        ydst = W["YbT"] if isB else W["YaT"]
        for j in range(8):
            S.dma("sp", vslot, ydst[j * 128:(j + 1) * 128, :], yT[:, j, :], reads=[("yT", mixer)])
        if ("d_y" + mixer) in cx.debug:
            for j in range(8):
                S.dma("sp", cx.dbg_slot, cx.O["d_y" + mixer][j * 128:(j + 1) * 128, :], yT[:, j, :], reads=[("yT", mixer)])
    S.barrier()


def phaseD(cx):
    nc, S, I, W = cx.nc, cx.S, cx.I, cx.W
    bk = cx.banks
    with contextlib.ExitStack() as st:
        sb = lambda name, shape, dt: st.enter_context(nc.sbuf_tensor(name, list(shape), dt))
        with contextlib.ExitStack() as st1:
            sb1 = lambda name, shape, dt: st1.enter_context(nc.sbuf_tensor(name, list(shape), dt))
            mT = sb1("D_mT", [128, KC, OWN], BF16)
            ya = sb1("D_ya", [128, 8, OWN], BF16)
            yb = sb1("D_yb", [128, 8, OWN], BF16)
            woa = sb1("D_woa", [128, 8, D], BF16)
            wob = sb1("D_wob", [128, 8, D], BF16)
            gat = [sb1(f"D_g{i}", [128, 2, 512], BF16) for i in range(2)]
            t1 = [sb1(f"D_t1{i}", [128, 512], F32) for i in range(2)]
            t2 = [sb1(f"D_t2{i}", [128, 512], F32) for i in range(2)]
            ls = S.slot("D1")
            gs = [S.slot("D1g") for _ in range(2)]
            for j in range(8):
                S.dma("sp", ls, ya[:, j, :], W["YaT"][j * 128:(j + 1) * 128, :], writes=["ya"])
                S.dma("act", ls, yb[:, j, :], W["YbT"][j * 128:(j + 1) * 128, :], writes=["yb"])
            wav = I["w_out_a"].rearrange("(k p) n -> p k n", p=128)
            wbv = I["w_out_b"].rearrange("(k p) n -> p k n", p=128)
            for j in range(0, 8, 2):
                S.dma("pool", ls, woa[:, j:j + 2, :], wav[:, j:j + 2, :], writes=["woa"])
                S.dma("pool", ls, wob[:, j:j + 2, :], wbv[:, j:j + 2, :], writes=["wob"])
            if "d_ya" in cx.debug:
                for j in range(8):
                    S.dma("sp", cx.dbg_slot, cx.O["d_ya"][j * 128:(j + 1) * 128, :], ya[:, j, :], reads=["ya"])
                    S.dma("sp", cx.dbg_slot, cx.O["d_yb"][j * 128:(j + 1) * 128, :], yb[:, j, :], reads=["yb"])
            it = 0
            for dc in range(KC):
                for qc in range(2):
                    gb_ = gat[it % 2]
                    S.dma("sp", gs[it % 2], gb_[:, 0, :], W["GT"][dc * 128:(dc + 1) * 128, qc * 512:(qc + 1) * 512],
                          writes=[("gat", it % 2)])
                    S.dma("sp", gs[it % 2], gb_[:, 1, :], W["GT"][2048 + dc * 128:2048 + (dc + 1) * 128, qc * 512:(qc + 1) * 512],
                          writes=[("gat", it % 2)])
                    pa, pb = bk[(2 * it) % 4], bk[(2 * it + 1) % 4]
                    for f in range(8):
                        S.op("pe", lambda e, pa=pa, f=f, dc=dc, qc=qc: e.matmul(
                            pa[:, :], woa[:, f, dc * 128:(dc + 1) * 128], ya[:, f, qc * 512:(qc + 1) * 512],
                            start=(f == 0), stop=(f == 7)), reads=["woa", "ya"], writes=[("bank", (2 * it) % 4)])
                    for f in range(8):
                        S.op("pe", lambda e, pb=pb, f=f, dc=dc, qc=qc: e.matmul(
                            pb[:, :], wob[:, f, dc * 128:(dc + 1) * 128], yb[:, f, qc * 512:(qc + 1) * 512],
                            start=(f == 0), stop=(f == 7)), reads=["wob", "yb"], writes=[("bank", (2 * it + 1) % 4)])
                    a1, a2 = t1[it % 2], t2[it % 2]
                    S.op("dve", lambda e, a1=a1, pa=pa, gb_=gb_: e.tensor_tensor(out=a1[:], in0=pa[:, :], in1=gb_[:, 0, :], op=ALU.mult),
                         reads=[("bank", (2 * it) % 4), ("gat", it % 2)], writes=[("t1", it % 2)])
                    S.op("dve", lambda e, a2=a2, pb=pb, gb_=gb_: e.tensor_tensor(out=a2[:], in0=pb[:, :], in1=gb_[:, 1, :], op=ALU.mult),
                         reads=[("bank", (2 * it + 1) % 4), ("gat", it % 2)], writes=[("t2", it % 2)])
                    if "d_gat" in cx.debug and it == 0:
                        S.dma("sp", cx.dbg_slot, cx.O["d_gat"][:, :], gb_[:].rearrange("p a b -> p (a b)"), reads=[("gat", 0)])
                        S.dma("sp", cx.dbg_slot, cx.O["d_a1"][:, 0:512], a1[:], reads=[("t1", 0)])
                        S.dma("sp", cx.dbg_slot, cx.O["d_a1"][:, 512:1024], a2[:], reads=[("t2", 0)])
                    S.op("dve", lambda e, a1=a1, a2=a2, dc=dc, qc=qc: e.tensor_tensor(
                        out=mT[:, dc, qc * 512:(qc + 1) * 512], in0=a1[:], in1=a2[:], op=ALU.add),
                        reads=[("t1", it % 2), ("t2", it % 2)], writes=["mT"])
                    it += 1
            for k in range(KC):
                S.dma("sp", ls, W["MT"][k * 128:(k + 1) * 128, :], mT[:, k, :], reads=["mT"])
                if "d_mT" in cx.debug:
                    S.dma("sp", cx.dbg_slot, cx.O["d_mT"][k * 128:(k + 1) * 128, :], mT[:, k, :], reads=["mT"])
        S.barrier()
        h = sb("D_h", [128, 8, D], F32)
        rowB = sb("D_rowB", [128, D], F32)
        rowC = sb("D_rowC", [128, D], F32)
        st2 = contextlib.ExitStack()
        sb2 = lambda name, shape, dt: st2.enter_context(nc.sbuf_tensor(name, list(shape), dt))
        mT2 = sb2("D_mT2", [128, KC, OWN], BF16)
        wo = sb2("D_wo", [128, KC, D], BF16)
        tq = [sb2(f"D_tq{i}", [128, 512], F32) for i in range(2)]
        ls = S.slot("D2")
        for k in range(KC):
            S.dma("act", ls, mT2[:, k, :], W["MT"][k * 128:(k + 1) * 128, :], writes=["mT2"])
        wov = I["w_o"].rearrange("(k p) n -> p k n", p=128)
        for j in range(0, KC, 2):
            S.dma("pool", ls, wo[:, j:j + 2, :], wov[:, j:j + 2, :], writes=["wo"])
        xo = I["xr"][1024:2048, :].rearrange("(t p) d -> p t d", p=128)
        for tt in range(8):
            S.dma("sp" if tt % 2 == 0 else "act", ls, h[:, tt, :], xo[:, tt, :], writes=[("h", tt)])
        S.dma("sp", ls, rowB[:], W["rows"][0:1, :].partition_broadcast(128), writes=["rowB"])
        S.dma("sp", ls, rowC[:], W["rows"][2:3, :].partition_broadcast(128), writes=["rowC"])
        it = 0
        for tt in range(8):
            for ch in range(4):
                ps = bk[it % 4]
                for k in range(KC):
                    S.op("pe", lambda e, ps=ps, k=k, tt=tt, ch=ch: e.matmul(
                        ps[:, :], mT2[:, k, tt * 128:(tt + 1) * 128], wo[:, k, ch * 512:(ch + 1) * 512],
                        start=(k == 0), stop=(k == KC - 1)), reads=["wo", "mT2"], writes=[("bank", it % 4)])
                tb = tq[it % 2]
                S.op("dve", lambda e, tb=tb, ps=ps, ch=ch: e.tensor_tensor(
                    out=tb[:], in0=ps[:, :], in1=rowB[:, ch * 512:(ch + 1) * 512], op=ALU.mult),
                    reads=[("bank", it % 4), "rowB"], writes=[("tq", it % 2)])
                S.op("pool", lambda e, tb=tb, tt=tt, ch=ch: e.tensor_tensor(
                    out=h[:, tt, ch * 512:(ch + 1) * 512], in0=h[:, tt, ch * 512:(ch + 1) * 512], in1=tb[:], op=ALU.add),
                    reads=[("tq", it % 2), ("h", tt)], writes=[("h", tt)])
                it += 1
        if "d_rowB" in cx.debug:
            S.dma("sp", cx.dbg_slot, cx.O["d_rowB"][:, :], rowB[:], reads=["rowB"])
        hs = S.slot("hs")
        for tt in range(8):
            S.dma("sp", hs, W["hs"][tt * 128:(tt + 1) * 128, :], h[:, tt, :], reads=[("h", tt)])
            if "d_h1" in cx.debug:
                S.dma("sp", cx.dbg_slot, cx.O["d_h1"][tt * 128:(tt + 1) * 128, :], h[:, tt, :], reads=[("h", tt)])
        S.barrier()
        st2.close()
        S.dma("sp", ls, rowB[:], W["rows"][1:2, :].partition_broadcast(128), reads=[], writes=["rowB"])
        wr = sb("D_wr", [128, KC, N_EXP], F32)
        brB = sb("D_brB", [128, N_EXP], F32)
        triu = sb("D_triu", [128, 128], BF16)
        triu_f = sb("D_triuf", [128, 128], F32)
        S.dma("sp", ls, wr[:], I["w_router"].rearrange("(k p) n -> p k n", p=128), writes=["wr"])
        S.dma("sp", ls, brB[:], I["b_routerB"][:, :], writes=["brB"])
        S.dma("sp", ls, triu_f[:], I["triu"][:, :], writes=["triu_f"])
        S.op("dve", lambda e: e.tensor_copy(out=triu[:], in_=triu_f[:]), reads=["triu_f"], writes=["triu"])
        u2f = [sb(f"D_u2f{i}", [128, D], F32) for i in range(2)]
        u2b = [sb(f"D_u2b{i}", [128, D], BF16) for i in range(2)]
        u2T = [sb(f"D_u2T{i}", [128, KC, 128], F32) for i in range(2)]
        ss = sb("D_ss", [128, 8], F32)
        rt = sb("D_rt", [128, 8], F32)
        rs = sb("D_rs", [128, 8], F32)
        lg = sb("D_lg", [128, 8, N_EXP], F32)
        mx8 = sb("D_mx8", [128, 8, 8], F32)
        negm = sb("D_negm", [128, 8], F32)
        mask = sb("D_mask", [128, 8, N_EXP], F32)
        mask_b = sb("D_maskb", [128, 8, N_EXP], BF16)
        ex = sb("D_ex", [128, 8, N_EXP], F32)
        sm = sb("D_sm", [128, 8], F32)
        rsm = sb("D_rsm", [128, 8], F32)
        us = [S.slot("u2") for _ in range(2)]
        pit = 0
        for tt in range(8):
            uf, ub, uT_ = u2f[tt % 2], u2b[tt % 2], u2T[tt % 2]
            S.op("act", lambda e, ub=ub, tt=tt: e.activation(out=ub[:], in_=h[:, tt, :], func=AF.Square, accum_out=ss[:, tt:tt + 1]),
                 reads=[("h", tt)], writes=[("u2b", tt % 2), ("ss", tt)])
            S.op("act", lambda e, tt=tt: e.activation(out=rt[:, tt:tt + 1], in_=ss[:, tt:tt + 1], func=AF.Sqrt, scale=1.0 / D,
                                                      bias=cx.epsc[:, 0:1]), reads=[("ss", tt), "epsc"], writes=[("rt", tt)])
            S.op("dve", lambda e, tt=tt: e.reciprocal(out=rs[:, tt:tt + 1], in_=rt[:, tt:tt + 1]), reads=[("rt", tt)], writes=[("rs", tt)])
            S.op("dve", lambda e, uf=uf, tt=tt: e.scalar_tensor_tensor(out=uf[:], in0=h[:, tt, :], scalar=rs[:, tt:tt + 1], in1=rowB[:],
                                                                       op0=ALU.mult, op1=ALU.mult),
                 reads=[("h", tt), ("rs", tt), "rowB"], writes=[("u2f", tt % 2)])
            S.op("pool", lambda e, uf=uf: e.tensor_tensor(out=uf[:], in0=uf[:], in1=rowC[:], op=ALU.add),
                 reads=[("u2f", tt % 2), "rowC"], writes=[("u2f", tt % 2)])
            S.op("act", lambda e, ub=ub, uf=uf: e.activation(out=ub[:], in_=uf[:], func=AF.Copy),
                 reads=[("u2f", tt % 2)], writes=[("u2b", tt % 2)])
            S.dma("sp", us[tt % 2], W["U2"][tt * 128:(tt + 1) * 128, :], ub[:], reads=[("u2b", tt % 2)])
            for q4 in range(4):
                bi = pit % 4
                pit += 1
                ps = bk[bi]
                for q in range(4):
                    k = q4 * 4 + q
                    S.op("pe", lambda e, ps=ps, q=q, k=k, uf=uf: e.transpose(ps[:, q * 128:(q + 1) * 128], uf[:, k * 128:(k + 1) * 128],
                                                                              cx.ident_f[:]),
                         reads=[("u2f", tt % 2), "ident_f"], writes=[("bank", bi)])
                evac_copy(S, q4, uT_[:, q4 * 4:(q4 + 1) * 4, :], ps[:, :].rearrange("p (a b) -> p a b", a=4), [("bank", bi)], [("u2T", tt % 2)])
            psl = bk[4 + tt % 2]
            for k in range(KC):
                S.op("pe", lambda e, psl=psl, k=k, uT_=uT_: e.matmul(psl[:, 0:N_EXP], uT_[:, k, :], wr[:, k, :],
                                                                     start=(k == 0), stop=(k == KC - 1)),
                     reads=[("u2T", tt % 2), "wr"], writes=[("bank", 4 + tt % 2)])
            S.op("dve", lambda e, psl=psl, tt=tt: e.tensor_tensor(out=lg[:, tt, :], in0=psl[:, 0:N_EXP], in1=brB[:], op=ALU.add),
                 reads=[("bank", 4 + tt % 2), "brB"], writes=[("lg", tt)])
            S.op("dve", lambda e, tt=tt: e.max(out=mx8[:, tt, :], in_=lg[:, tt, :]), reads=[("lg", tt)], writes=[("mx8", tt)])
            S.op("dve", lambda e, tt=tt: e.tensor_scalar(out=mask[:, tt, :], in0=lg[:, tt, :], scalar1=mx8[:, tt, 3:4], scalar2=None,
                                                         op0=ALU.is_ge), reads=[("lg", tt), ("mx8", tt)], writes=[("mask", tt)])
            S.op("dve", lambda e, tt=tt: e.tensor_scalar(out=negm[:, tt:tt + 1], in0=mx8[:, tt, 0:1], scalar1=-1.0, scalar2=None,
                                                         op0=ALU.mult), reads=[("mx8", tt)], writes=[("negm", tt)])
            S.op("act", lambda e, tt=tt: e.activation(out=ex[:, tt, :], in_=lg[:, tt, :], func=AF.Exp, bias=negm[:, tt:tt + 1]),
                 reads=[("lg", tt), ("negm", tt)], writes=[("ex", tt)])
            S.op("dve", lambda e, tt=tt: e.tensor_tensor(out=ex[:, tt, :], in0=ex[:, tt, :], in1=mask[:, tt, :], op=ALU.mult),
                 reads=[("ex", tt), ("mask", tt)], writes=[("ex", tt)])
            S.op("dve", lambda e, tt=tt: e.reduce_sum(out=sm[:, tt:tt + 1], in_=ex[:, tt, :], axis=AX.X),
                 reads=[("ex", tt)], writes=[("sm", tt)])
            S.op("dve", lambda e, tt=tt: e.reciprocal(out=rsm[:, tt:tt + 1], in_=sm[:, tt:tt + 1]), reads=[("sm", tt)], writes=[("rsm", tt)])
            S.op("dve", lambda e, tt=tt: e.tensor_scalar(out=cx.prob[:, tt, :], in0=ex[:, tt, :], scalar1=rsm[:, tt:tt + 1], scalar2=None,
                                                         op0=ALU.mult), reads=[("ex", tt), ("rsm", tt)], writes=[("prob", tt)])
            S.op("dve", lambda e, tt=tt: e.tensor_copy(out=cx.prob_hi[:, tt, :], in_=cx.prob[:, tt, :]), reads=[("prob", tt)], writes=[("phi", tt)])
            S.op("dve", lambda e, tt=tt: e.tensor_tensor(out=cx.prob_lo[:, tt, :], in0=cx.prob[:, tt, :], in1=cx.prob_hi[:, tt, :],
                                                         op=ALU.subtract), reads=[("prob", tt), ("phi", tt)], writes=[("plo", tt)])
            S.op("dve", lambda e, tt=tt: e.tensor_copy(out=mask_b[:, tt, :], in_=mask[:, tt, :]), reads=[("mask", tt)], writes=[("maskb", tt)])
            pp = bk[6 + tt % 2]
            S.op("pe", lambda e, pp=pp, tt=tt: e.matmul(pp[:, 0:N_EXP], triu[:], mask_b[:, tt, :], start=True, stop=(tt == 0)),
                 reads=["triu", ("maskb", tt)], writes=[("bank", 6 + tt % 2)])
            for t2_ in range(tt):
                S.op("pe", lambda e, pp=pp, t2_=t2_, tt=tt: e.matmul(pp[:, 0:N_EXP], cx.ones_b[:], mask_b[:, t2_, :], start=False,
                                                                    stop=(t2_ == tt - 1)),
                     reads=["ones_b", ("maskb", t2_)], writes=[("bank", 6 + tt % 2)])
            S.op("dve", lambda e, pp=pp, tt=tt: e.tensor_tensor(out=cx.posm[:, tt, :], in0=pp[:, 0:N_EXP], in1=mask[:, tt, :], op=ALU.mult),
                 reads=[("bank", 6 + tt % 2), ("mask", tt)], writes=[("posm", tt)])
        if "d_lg" in cx.debug:
            S.dma("sp", cx.dbg_slot, cx.O["d_lg"][:, :], lg[:].rearrange("p a b -> p (a b)"), reads=[("lg", t) for t in range(8)])
            S.dma("sp", cx.dbg_slot, cx.O["d_posm"][:, :], cx.posm[:].rearrange("p a b -> p (a b)"), reads=[("posm", t) for t in range(8)])
            S.dma("sp", cx.dbg_slot, cx.O["d_prob"][:, :], cx.prob[:].rearrange("p a b -> p (a b)"), reads=[("prob", t) for t in range(8)])
    S.barrier()


def phaseE1(cx):
    nc, S, I, W = cx.nc, cx.S, cx.I, cx.W
    bk = cx.banks
    I32 = mybir.dt.int32
    NR = 5
    NST = CAP // 128
    with contextlib.ExitStack() as st:
        sb = lambda name, shape, dt: st.enter_context(nc.sbuf_tensor(name, list(shape), dt))
        ring = [sb(f"E_w{i}", [128, KC, 512], BF16) for i in range(NR)]
        rslot = [S.slot("ew") for _ in range(NR)]
        slb = [sb(f"E_sel{i}", [128, 8, CAP], BF16) for i in range(2)]
        xg = [[sb(f"E_xg{i}_{j}", [128, D], BF16) for j in range(NST)] for i in range(2)]
        xslot = [S.slot("xg") for _ in range(2)]
        pz = [sb(f"E_pz{i}", [128, 16], F32) for i in range(2)]
        idf = [sb(f"E_idf{i}", [128, NST], F32) for i in range(2)]
        idi = [sb(f"E_idi{i}", [128, NST], I32) for i in range(2)]
        tokab = sb("E_tokab", [128, 8, 2], BF16)
        tokab_f = sb("E_tokabf", [128, 8, 2], F32)
        x_ = sb("E_xe", [128, KC, CAP], BF16)
        a_ = sb("E_act", [128, KC, CAP], BF16)
        ye = sb("E_ye", [128, NST, D], BF16)
        yslot = S.slot("ye")
        bgT = sb("E_bgT", [128, N_EXP * KC], F32)
        buT = sb("E_buT", [128, N_EXP * KC], F32)
        gsb = [sb(f"E_g{i}", [128, CAP], F32) for i in range(2)]
        sig = [sb(f"E_s{i}", [128, CAP], F32) for i in range(2)]
        usb = [sb(f"E_u{i}", [128, CAP], F32) for i in range(2)]
        ls = S.slot("E1")
        S.op("dve", lambda en: en.memset(ye[:, 0, :], 0.0), writes=["ye"])
        S.dma("sp", ls, W["Y"][N_EXP * CAP:N_EXP * CAP + 128, :], ye[:, 0, :], reads=["ye"])
        S.dma("sp", ls, bgT[:], I["b_gateT"][:, :], writes=["bgT"])
        S.dma("sp", ls, buT[:], I["b_upT"][:, :], writes=["buT"])
        S.dma("sp", ls, tokab_f[:].rearrange("p a b -> p (a b)"), I["tokab"][:, :], writes=["tokab_f"])
        S.op("dve", lambda en: en.tensor_copy(out=tokab[:], in_=tokab_f[:]), reads=["tokab_f"], writes=["tokab"])
        wg = I["w_gate"].rearrange("e (k p) n -> e p k n", p=128)
        wu = I["w_up"].rearrange("e (k p) n -> e p k n", p=128)
        wd = I["w_down"].rearrange("e (k p) n -> e p k n", p=128)
        cnt = {"piece": 0}

        def load_piece(src, e, g):
            ri = cnt["piece"] % NR
            cnt["piece"] += 1
            for j in range(2):
                S.dma("pool", rslot[ri], ring[ri][:, 8 * j:8 * j + 8, :], src[e, :, 8 * j:8 * j + 8, g * 512:(g + 1) * 512],
                      writes=[("ring", ri)])
            return ri

        def prep(e):
            par = e % 2
            sl = slb[par]
            for tt in range(8):
                S.op("dve", lambda en, tt=tt, e=e, sl=sl: en.tensor_scalar(
                    out=sl[:, tt, :], in0=cx.iota[:], scalar1=cx.posm[:, tt, e:e + 1], scalar2=None, op0=ALU.is_equal),
                    reads=["iota", ("posm", tt)], writes=[("sel", par)])
            ps = bk[1]
            for st_ in range(NST):
                n = 0
                for tt in range(8):
                    for pr in (cx.prob_hi, cx.prob_lo):
                        S.op("pe", lambda en, st_=st_, tt=tt, pr=pr, e=e, n=n, sl=sl: en.matmul(
                            ps[:, st_:st_ + 1], sl[:, tt, st_ * 128:(st_ + 1) * 128], pr[:, tt, e:e + 1],
                            start=(n == 0), stop=(n == 15)),
                            reads=[("sel", par), ("phi", tt), ("plo", tt)], writes=[("bank", 1)])
                        n += 1
                for tt in range(8):
                    S.op("pe", lambda en, st_=st_, tt=tt, sl=sl: en.matmul(
                        ps[:, 8 + 2 * st_:10 + 2 * st_], sl[:, tt, st_ * 128:(st_ + 1) * 128], tokab[:, tt, :],
                        start=(tt == 0), stop=(tt == 7)),
                        reads=[("sel", par), "tokab"], writes=[("bank", 1)])
            pzz = pz[par]
            S.op("dve", lambda en, pzz=pzz: en.tensor_copy(out=pzz[:], in_=ps[:, 0:16]), reads=[("bank", 1)], writes=[("pz", par)])
            idf_, idi_ = idf[par], idi[par]
            S.op("dve", lambda en, pzz=pzz, idf_=idf_: en.scalar_tensor_tensor(
                out=idf_[:], in0=pzz[:, 8:16:2], scalar=32.0, in1=pzz[:, 9:16:2], op0=ALU.mult, op1=ALU.add),
                reads=[("pz", par)], writes=[("idf", par)])
            S.op("dve", lambda en, idf_=idf_, idi_=idi_: en.tensor_copy(out=idi_[:], in_=idf_[:]), reads=[("idf", par)], writes=[("idi", par)])
            for st_ in range(NST):
                def fn(en, st_=st_, par=par, idi_=idi_):
                    return en.indirect_dma_start(out=xg[par][st_][:, :], out_offset=None, in_=W["U2"][:, :],
                                                 in_offset=bass.IndirectOffsetOnAxis(ap=idi_[:, st_:st_ + 1], axis=0))
                o = Op("pool", fn)
                o.dma = True
                o.slot = xslot[par]
                o.slot_total = xslot[par].total + 16
                S._add(o, [("idi", par)], [("xg", par)])
                xslot[par].total += 16

        fi = 0
        di = 0
        ti = 0
        prep(0)
        for e in range(N_EXP):
            par = e % 2
            p_ = pz[par]
            for k2 in range(KC // 2):
                bi = ti % 2
                ti += 1
                pst = bk[bi][:].bitcast(BF16)
                for q in range(2):
                    k = 2 * k2 + q
                    for st_ in range(NST):
                        S.op("pe", lambda en, pst=pst, q=q, k=k, st_=st_, par=par: en.transpose(
                            pst[:, q * CAP + st_ * 128:q * CAP + (st_ + 1) * 128], xg[par][st_][:, k * 128:(k + 1) * 128], cx.ident_b[:]),
                            reads=[("xg", par), "ident_b"], writes=[("bank", bi)])
                evac_copy(S, k2, x_[:, 2 * k2:2 * k2 + 2, :], pst[:, :].rearrange("p (a b) -> p a b", a=2),
                          [("bank", bi)], ["xe"])
            for g in range(4):
                rg = load_piece(wg, e, g)
                ru = load_piece(wu, e, g)
                if g == 1 and e + 1 < N_EXP:
                    prep(e + 1)
                for f4 in range(4):
                    fc = 4 * g + f4
                    pg, pu = bk[2 + fi % 2], bk[4 + fi % 2]
                    kg, ku = ("bank", 2 + fi % 2), ("bank", 4 + fi % 2)
                    for k in range(KC):
                        S.op("pe", lambda en, pg=pg, rg=rg, k=k, f4=f4: en.matmul(
                            pg[:, 0:CAP], ring[rg][:, k, f4 * 128:(f4 + 1) * 128], x_[:, k, :], start=(k == 0), stop=(k == KC - 1)),
                            reads=[("ring", rg), "xe"], writes=[kg])
                    for k in range(KC):
                        S.op("pe", lambda en, pu=pu, ru=ru, k=k, f4=f4: en.matmul(
                            pu[:, 0:CAP], ring[ru][:, k, f4 * 128:(f4 + 1) * 128], x_[:, k, :], start=(k == 0), stop=(k == KC - 1)),
                            reads=[("ring", ru), "xe"], writes=[ku])
                    w = fi % 2
                    fi += 1
                    gb_, sg_, ub_ = gsb[w], sig[w], usb[w]
                    col = e * KC + fc
                    S.op("dve", lambda en, gb_=gb_, pg=pg, col=col: en.tensor_scalar(
                        out=gb_[:], in0=pg[:, 0:CAP], scalar1=bgT[:, col:col + 1], scalar2=7.0, op0=ALU.add, op1=ALU.min),
                        reads=[kg, "bgT"], writes=[("gsb", w)])
                    S.op("act", lambda en, sg_=sg_, gb_=gb_: en.activation(out=sg_[:], in_=gb_[:], func=AF.Sigmoid, scale=1.702),
                         reads=[("gsb", w)], writes=[("sig", w)])
                    S.op("act", lambda en, ub_=ub_, pu=pu, col=col: en.activation(
                        out=ub_[:], in_=pu[:, 0:CAP], func=AF.Identity, bias=buT[:, col:col + 1]),
                        reads=[ku, "buT"], writes=[("usb", w)])
                    S.op("dve", lambda en, ub_=ub_: en.tensor_scalar(
                        out=ub_[:], in0=ub_[:], scalar1=7.0, scalar2=-7.0, op0=ALU.min, op1=ALU.max),
                        reads=[("usb", w)], writes=[("usb", w)])
                    S.op("dve", lambda en, gb_=gb_, sg_=sg_: en.tensor_tensor(out=gb_[:], in0=gb_[:], in1=sg_[:], op=ALU.mult),
                         reads=[("gsb", w), ("sig", w)], writes=[("gsb", w)])
                    S.op("dve", lambda en, fc=fc, gb_=gb_, ub_=ub_: en.scalar_tensor_tensor(
                        out=a_[:, fc, :], in0=ub_[:], scalar=1.0, in1=gb_[:], op0=ALU.add, op1=ALU.mult),
                        reads=[("gsb", w), ("usb", w)], writes=["act"])
            for ch in range(4):
                rd = load_piece(wd, e, ch)
                for st_ in range(NST):
                    pd = bk[6 + di % 2]
                    kd = ("bank", 6 + di % 2)
                    for f in range(KC):
                        S.op("pe", lambda en, pd=pd, rd=rd, f=f, st_=st_: en.matmul(
                            pd[:, :], a_[:, f, st_ * 128:(st_ + 1) * 128], ring[rd][:, f, :], start=(f == 0), stop=(f == KC - 1)),
                            reads=[("ring", rd), "act"], writes=[kd])
                    if di % 2 == 0:
                        S.op("act", lambda en, st_=st_, ch=ch, pd=pd, p_=p_: en.activation(
                            out=ye[:, st_, ch * 512:(ch + 1) * 512], in_=pd[:, :], func=AF.Copy, scale=p_[:, st_:st_ + 1]),
                            reads=[kd, ("pz", par)], writes=["ye"])
                    else:
                        S.op("dve", lambda en, st_=st_, ch=ch, pd=pd, p_=p_: en.tensor_scalar(
                            out=ye[:, st_, ch * 512:(ch + 1) * 512], in0=pd[:, :], scalar1=p_[:, st_:st_ + 1], scalar2=None, op0=ALU.mult),
                            reads=[kd, ("pz", par)], writes=["ye"])
                    di += 1
            for st_ in range(NST):
                S.dma("sp", yslot, W["Y"][e * CAP + st_ * 128:e * CAP + (st_ + 1) * 128, :], ye[:, st_, :], reads=["ye"])
    S.barrier()


def cx_rd(cnt, rds, ch):
    return rds[ch]


def phaseE2(cx):
    nc, S, I, W = cx.nc, cx.S, cx.I, cx.W
    bk = cx.banks
    I32 = mybir.dt.int32
    ZR = N_EXP * CAP
    with contextlib.ExitStack() as st:
        sb = lambda name, shape, dt: st.enter_context(nc.sbuf_tensor(name, list(shape), dt))
        ecapB = sb("F_ecap", [128, 8, N_EXP], F32)
        vv = sb("F_vv", [128, 8, N_EXP], F32)
        vt = sb("F_vt", [128, 8, N_EXP], F32)
        m8 = sb("F_m8", [128, 8, 8], F32)
        eq0 = sb("F_eq0", [128, 8, 4], F32)
        idf = sb("F_idf", [128, 8, 4], F32)
        idi = sb("F_idi", [128, 8, 4], I32)
        gb = [[sb(f"F_g{i}_{k}", [128, D], BF16) for k in range(4)] for i in range(2)]
        gslot = [S.slot("gg") for _ in range(2)]
        acc = [sb(f"F_acc{i}", [128, D], F32) for i in range(2)]
        ht = [sb(f"F_h{i}", [128, D], F32) for i in range(2)]
        hslot = [S.slot("hh") for _ in range(2)]
        g2B = sb("F_g2B", [128, D], F32)
        gfB = sb("F_gfB", [128, D], F32)
        ot = [sb(f"F_ot{i}", [128, D], F32) for i in range(2)]
        bdf = sb("F_bdf", [N_EXP, D], F32)
        bdb = sb("F_bdb", [N_EXP, D], BF16)
        pT = sb("F_pT", [N_EXP, 8, 2, 128], BF16)
        ss = sb("F_ss", [128, 8], F32)
        rt = sb("F_rt", [128, 8], F32)
        rs = sb("F_rs", [128, 8], F32)
        ls = S.slot("E2")
        S.dma("sp", ls, g2B[:], W["rows"][3:4, :].partition_broadcast(128), writes=["g2B"])
        S.dma("sp", ls, gfB[:], I["nf"][0:1, :].partition_broadcast(128), writes=["gfB"])
        S.dma("sp", ls, bdf[:], I["b_down"][:, :], writes=["bdf"])
        S.dma("sp", ls, ecapB[:].rearrange("p a b -> p (a b)"), I["ecapB"][:, :], writes=["ecapB"])
        S.op("dve", lambda e: e.tensor_copy(out=bdb[:], in_=bdf[:]), reads=["bdf"], writes=["bdb"])
        pk = [("posm", t) for t in range(8)]
        S.op("dve", lambda e: e.tensor_scalar(out=vt[:], in0=cx.posm[:], scalar1=0.0, scalar2=None, op0=ALU.is_gt),
             reads=pk, writes=["vt"])
        S.op("dve", lambda e: e.tensor_scalar(out=vv[:], in0=cx.posm[:], scalar1=float(CAP), scalar2=None, op0=ALU.is_le),
             reads=pk, writes=["vv"])
        S.op("dve", lambda e: e.tensor_tensor(out=vt[:], in0=vt[:], in1=vv[:], op=ALU.mult), reads=["vt", "vv"], writes=["vt"])
        S.op("dve", lambda e: e.tensor_tensor(out=vv[:], in0=cx.posm[:], in1=ecapB[:], op=ALU.add), reads=pk + ["ecapB", "vv"], writes=["vv"])
        S.op("dve", lambda e: e.tensor_tensor(out=vv[:], in0=vv[:], in1=vt[:], op=ALU.mult), reads=["vv", "vt"], writes=["vv"])
        for tt in range(8):
            S.op("dve", lambda e, tt=tt: e.max(out=m8[:, tt, :], in_=vv[:, tt, :]), reads=["vv"], writes=["m8"])
        S.op("dve", lambda e: e.tensor_scalar(out=eq0[:], in0=m8[:, :, 0:4], scalar1=0.0, scalar2=float(ZR + 1), op0=ALU.is_equal, op1=ALU.mult),
             reads=["m8"], writes=["eq0"])
        S.op("dve", lambda e: e.scalar_tensor_tensor(out=idf[:], in0=m8[:, :, 0:4], scalar=-1.0, in1=eq0[:], op0=ALU.add, op1=ALU.add),
             reads=["m8", "eq0"], writes=["idf"])
        S.op("dve", lambda e: e.tensor_copy(out=idi[:], in_=idf[:]), reads=["idf"], writes=["idi"])
        pTb = bk[6][:].bitcast(BF16)
        for tt in range(8):
            for hl, pr in enumerate((cx.prob_hi, cx.prob_lo)):
                j = (tt * 2 + hl) % 8
                S.op("pe", lambda e, j=j, pr=pr, tt=tt: e.transpose(pTb[0:N_EXP, j * 128:(j + 1) * 128], pr[:, tt, :], cx.ident_b[:]),
                     reads=[("phi", tt), ("plo", tt), "ident_b"], writes=[("bank", 6)])
                S.op("dve", lambda e, j=j, tt=tt, hl=hl: e.tensor_copy(out=pT[:, tt, hl, :], in_=pTb[0:N_EXP, j * 128:(j + 1) * 128]),
                     reads=[("bank", 6)], writes=["pT"])
        for tt in range(8):
            par = tt % 2
            hb = ht[par]
            S.dma("act", hslot[par], hb[:], W["hs"][tt * 128:(tt + 1) * 128, :], writes=[("ht", par)])
            for k in range(4):
                def fn(e, k=k, tt=tt, par=par):
                    return e.indirect_dma_start(out=gb[par][k][:, :], out_offset=None, in_=W["Y"][:, :],
                                                in_offset=bass.IndirectOffsetOnAxis(ap=idi[:, tt, k:k + 1], axis=0))
                o = Op("pool", fn)
                o.dma = True
                o.slot = gslot[par]
                o.slot_total = gslot[par].total + 16
                S._add(o, ["idi"], [("gb", par, k)])
                gslot[par].total += 16
            ac = acc[par]
            g_ = gb[par]
            S.op("dve", lambda e, ac=ac, g_=g_: e.tensor_tensor(out=ac[:], in0=g_[0][:], in1=g_[1][:], op=ALU.add),
                 reads=[("gb", par, 0), ("gb", par, 1)], writes=[("acc", par)])
            S.op("pool", lambda e, ac=ac, g_=g_: e.tensor_tensor(out=g_[2][:], in0=g_[2][:], in1=g_[3][:], op=ALU.add),
                 reads=[("gb", par, 2), ("gb", par, 3)], writes=[("gb", par, 2)])
            S.op("dve", lambda e, ac=ac, g_=g_: e.tensor_tensor(out=ac[:], in0=ac[:], in1=g_[2][:], op=ALU.add),
                 reads=[("gb", par, 2), ("acc", par)], writes=[("acc", par)])
            for ch in range(4):
                for hl in range(2):
                    S.op("pe", lambda en, tt=tt, hl=hl, ch=ch: en.matmul(
                        bk[ch][:, :], pT[:, tt, hl, :], bdb[:, ch * 512:(ch + 1) * 512], start=(hl == 0), stop=(hl == 1)),
                        reads=["pT", "bdb"], writes=[("bank", ch)])
                S.op("dve", lambda en, ac=ac, ch=ch: en.tensor_tensor(out=ac[:, ch * 512:(ch + 1) * 512], in0=bk[ch][:, :],
                                                                     in1=ac[:, ch * 512:(ch + 1) * 512], op=ALU.add),
                     reads=[("bank", ch), ("acc", par)], writes=[("acc", par)])
            S.op("pool", lambda en, ac=ac: en.tensor_tensor(out=ac[:], in0=ac[:], in1=g2B[:], op=ALU.mult),
                 reads=[("acc", par), "g2B"], writes=[("acc", par)])
            S.op("pool", lambda en, ac=ac, hb=hb: en.tensor_tensor(out=hb[:], in0=hb[:], in1=ac[:], op=ALU.add),
                 reads=[("acc", par), ("ht", par)], writes=[("ht", par)])
            o_ = ot[par]
            S.op("act", lambda en, o_=o_, hb=hb, tt=tt: en.activation(out=o_[:], in_=hb[:], func=AF.Square, accum_out=ss[:, tt:tt + 1]),
                 reads=[("ht", par)], writes=[("ot", par), ("ss", tt)])
            S.op("act", lambda en, tt=tt: en.activation(out=rt[:, tt:tt + 1], in_=ss[:, tt:tt + 1], func=AF.Sqrt, scale=1.0 / D,
                                                        bias=cx.epsc[:, 0:1]), reads=[("ss", tt), "epsc"], writes=[("rt", tt)])
            S.op("dve", lambda en, tt=tt: en.reciprocal(out=rs[:, tt:tt + 1], in_=rt[:, tt:tt + 1]), reads=[("rt", tt)], writes=[("rs", tt)])
            S.op("dve", lambda en, o_=o_, hb=hb, tt=tt: en.scalar_tensor_tensor(out=o_[:], in0=hb[:], scalar=rs[:, tt:tt + 1], in1=gfB[:],
                                                                                op0=ALU.mult, op1=ALU.mult),
                 reads=[("ht", par), ("rs", tt), "gfB", ("ot", par)], writes=[("ot", par)])
            S.dma("sp", cx.slot_out, cx.O["out"][tt * 128:(tt + 1) * 128, :], o_[:], reads=[("ot", par)])
    S.barrier()


def _colT(v, nchunk):
    return np.ascontiguousarray(v.reshape(nchunk, 128).T)


def prep_inputs(inp):
    f32 = np.float32
    x = np.asarray(inp["x"], f32)
    c = np.asarray(inp["c"], f32)
    shared = {
        "w_ada": np.ascontiguousarray(inp["w_ada"][0], f32),
        "b_ada": np.ascontiguousarray(inp["b_ada"][0][None, :], f32),
        "n1": np.ascontiguousarray(inp["norm1_g"][0][None, :], f32),
        "n2": np.ascontiguousarray(inp["norm2_g"][0][None, :], f32),
        "nf": np.ascontiguousarray(np.asarray(inp["final_g"])[None, :], f32),
        "w_in": np.ascontiguousarray(inp["w_in"][0], f32),
        "lamT": np.ascontiguousarray(np.stack([inp["lam_q1"][0], inp["lam_k1"][0], inp["lam_q2"][0], inp["lam_k2"][0]], axis=1), f32),
        "sublnT": _colT(np.asarray(inp["subln_g"][0], f32), 2),
        "w_out_a": np.ascontiguousarray(inp["w_out_a"][0], f32),
        "w_out_b": np.ascontiguousarray(inp["w_out_b"][0], f32),
        "w_o": np.ascontiguousarray(inp["w_o"][0], f32),
        "w_router": np.ascontiguousarray(inp["w_router"][0], f32),
        "b_routerB": np.ascontiguousarray(np.broadcast_to(np.asarray(inp["b_router"][0], f32)[None, :], (128, N_EXP))),
        "w_gate": np.ascontiguousarray(inp["w_gate"][0], f32),
        "w_up": np.ascontiguousarray(inp["w_up"][0], f32),
        "w_down": np.ascontiguousarray(inp["w_down"][0], f32),
        "b_gateT": np.ascontiguousarray(np.asarray(inp["b_gate"][0], f32).reshape(N_EXP, KC, 128).transpose(2, 0, 1).reshape(128, N_EXP * KC)),
        "b_upT": np.ascontiguousarray(np.asarray(inp["b_up"][0], f32).reshape(N_EXP, KC, 128).transpose(2, 0, 1).reshape(128, N_EXP * KC)),
        "b_down": np.ascontiguousarray(inp["b_down"][0], f32),
        "ident": np.eye(128, dtype=f32),
        "iota_row": np.ascontiguousarray(np.broadcast_to(np.arange(1, CAP + 1, dtype=f32)[None, :], (128, CAP))),
        "triu": np.triu(np.ones((128, 128), f32)),
        "tokab": np.ascontiguousarray(np.stack([(np.arange(8)[None, :] * 128 + np.arange(128)[:, None]) // 32,
                                                (np.arange(8)[None, :] * 128 + np.arange(128)[:, None]) % 32], axis=2).reshape(128, 16).astype(f32)),
        "ecapB": np.ascontiguousarray(np.broadcast_to((np.arange(N_EXP, dtype=f32) * CAP)[None, None, :], (128, 8, N_EXP)).reshape(128, 8 * N_EXP)),
    }
    p = np.arange(128)[:, None]
    xx = np.arange(3968)[None, :]
    dl = p + 1920 - xx
    ad = np.abs(dl)
    n_ = ((ad <= 64).astype(np.float64) + ((dl % 4 == 0) & (ad <= 256)) + ((dl % 16 == 0) & (ad <= 1024)))
    lnn = np.where(n_ > 0, np.log(np.maximum(n_, 1.0)), 0.0)
    shared["stripAh"] = np.ascontiguousarray(np.stack(
        [np.where(n_ > 0, -SLOPES_A[h] * ad + lnn, -BIG) for h in range(8)], axis=0).astype(f32))
    maps = []
    for core in range(8):
        b, r = core // 4, core % 4
        idx = (np.arange(SEQ) - 1024 + 1024 * r) % SEQ
        m = dict(shared)
        m["xr"] = np.ascontiguousarray(x[b][idx])
        m["cT"] = _colT(c[b], KC)
        dist = np.zeros((128, 4, 1920), f32)
        for g in range(4):
            s_g = ((1024 * g - 1024 + 1024 * r) % SEQ) - 1024 * g
            q_off = -1024 + 1024 * r
            xv = np.arange(1920)[None, :]
            dist[:, g, :] = np.abs(1024 * (1 - g) + (xv - 896) - p + q_off - s_g)
        m["distB"] = np.ascontiguousarray(dist.reshape(128, 4 * 1920))
        kb = np.zeros((128, 24), f32)
        for kt in range(24):
            tok = 128 * kt + np.arange(128) - 1024 + 1024 * r
            kb[:, kt] = np.where((tok >= 0) & (tok < SEQ), 0.0, -BIG)
        m["kbiasA"] = kb
        maps.append(m)
    return maps


_NC_CACHE = {}
_LAST_DECLARED = []


def kernel(**inputs):
    maps = prep_inputs(inputs)
    if "nc" not in _NC_CACHE:
        _NC_CACHE["nc"] = build_program()
    nc = _NC_CACHE["nc"]
    names = set(_LAST_DECLARED)
    maps = [{k: v for k, v in m.items() if k in names} for m in maps]
    res = run_bass_kernel_spmd(nc, maps, core_ids=list(range(8)))
    out = np.zeros((2, SEQ, D), np.float32)
    for core in range(8):
        b, r = core // 4, core % 4
        out[b, 1024 * r:1024 * (r + 1)] = np.asarray(res.results[core]["out"], np.float32)
    return out
```
